# Optimizing a Trainium2 kernel written in Bass

```python
import math
import jax
import jax.numpy as jnp
from jax import lax
import numpy as np

D_MODEL = 1024
BATCH = 4
SEQ = 4096
DEPTH = 4

GRID_W = 64
CTX_LEN = 256
N_MIXERS = 3
EPS = 1e-6

N_HEADS = 16
N_KV_HEADS = 4
HEAD_DIM = D_MODEL // N_HEADS
Q_PER_KV = N_HEADS // N_KV_HEADS
QKV_DIM = (N_HEADS + 2 * N_KV_HEADS) * HEAD_DIM
ROPE_THETA = 10000.0
Q_BLOCK = 128

POOL_WINDOWS = (2, 4, 8, 16)
POOL_GROUP = D_MODEL // len(POOL_WINDOWS)

SSM_D_INNER = 2 * D_MODEL
SSM_HEAD_DIM = 64
SSM_HEADS = SSM_D_INNER // SSM_HEAD_DIM
SSM_GROUPS = 4
SSM_HPG = SSM_HEADS // SSM_GROUPS
SSM_STATE = 128
SSM_CONV = 4
SSM_CHUNK = 128
SSM_GN = SSM_GROUPS * SSM_STATE
SSM_CONV_DIM = SSM_D_INNER + 2 * SSM_GN
SSM_IN_DIM = SSM_D_INNER + SSM_CONV_DIM + 2 * SSM_HEADS
DT_MIN = 0.001
DT_MAX = 0.1

MOE_GROUPS = 4
MOE_EXPERTS_PER_GROUP = 8
MOE_EXPERTS = MOE_GROUPS * MOE_EXPERTS_PER_GROUP
MOE_TOPK = 2
MOE_HIDDEN = 512
MOE_BLOCK = 256

N_ATTN_LAYERS = (DEPTH + 2) // N_MIXERS
N_POOL_LAYERS = (DEPTH + 1) // N_MIXERS
N_SSM_LAYERS = DEPTH // N_MIXERS

kernel_name = 'hybrid_attn_pool_ssd_hmoe_dit'


def rms_norm(x, g):
    xf = x.astype(jnp.float32)
    y = xf * lax.rsqrt(jnp.mean(xf * xf, axis=-1, keepdims=True) + EPS)
    return (y * g.astype(jnp.float32)).astype(x.dtype)


def modulate(h, shift, scale):
    return h * (1 + scale) + shift


def axial_rope_tables(n_tokens):
    rows = n_tokens // GRID_W
    row = jnp.broadcast_to(jnp.arange(rows)[:, None], (rows, GRID_W)).reshape(-1).astype(jnp.float32)
    col = jnp.broadcast_to(jnp.arange(GRID_W)[None, :], (rows, GRID_W)).reshape(-1).astype(jnp.float32)
    n_freq = HEAD_DIM // 4
    inv_freq = ROPE_THETA ** (-jnp.arange(n_freq, dtype=jnp.float32) / n_freq)
    ang = jnp.concatenate([row[:, None] * inv_freq, col[:, None] * inv_freq], axis=-1)
    return jnp.cos(ang), jnp.sin(ang)


def apply_rope(x, cos, sin):
    shape = (1, x.shape[1]) + (1,) * (x.ndim - 3) + (HEAD_DIM // 2,)
    cs = cos.reshape(shape)
    sn = sin.reshape(shape)
    xf = x.astype(jnp.float32).reshape(x.shape[:-1] + (HEAD_DIM // 2, 2))
    x0 = xf[..., 0]
    x1 = xf[..., 1]
    out = jnp.stack([x0 * cs - x1 * sn, x0 * sn + x1 * cs], axis=-1).reshape(x.shape)
    return out.astype(x.dtype)


def _attend(q, k, v):
    s = jnp.einsum('bqkgd,btkd->bkgqt', q, k, preferred_element_type=jnp.float32) * (HEAD_DIM ** -0.5)
    p = jax.nn.softmax(s, axis=-1).astype(v.dtype)
    return jnp.einsum('bkgqt,btkd->bqkgd', p, v)


def attention_mixer(h_lat, h_ctx, w_qkv, w_o, q_g, k_g, cos, sin, ctx_out):
    b, n, _ = h_lat.shape
    m = h_ctx.shape[1]
    hq = N_HEADS * HEAD_DIM
    hkv = N_KV_HEADS * HEAD_DIM
    qkv = h_lat @ w_qkv
    q_l = rms_norm(qkv[..., :hq].reshape(b, n, N_KV_HEADS, Q_PER_KV, HEAD_DIM), q_g)
    k_l = rms_norm(qkv[..., hq:hq + hkv].reshape(b, n, N_KV_HEADS, HEAD_DIM), k_g)
    v_l = qkv[..., hq + hkv:].reshape(b, n, N_KV_HEADS, HEAD_DIM)
    q_l = apply_rope(q_l, cos, sin)
    k_l = apply_rope(k_l, cos, sin)
    if ctx_out:
        qkv_c = h_ctx @ w_qkv
        q_c = rms_norm(qkv_c[..., :hq].reshape(b, m, N_KV_HEADS, Q_PER_KV, HEAD_DIM), q_g)
        kv_c = qkv_c[..., hq:]
    else:
        kv_c = h_ctx @ w_qkv[:, hq:]
    k_c = rms_norm(kv_c[..., :hkv].reshape(b, m, N_KV_HEADS, HEAD_DIM), k_g)
    v_c = kv_c[..., hkv:].reshape(b, m, N_KV_HEADS, HEAD_DIM)
    k_all = jnp.concatenate([k_c, k_l], axis=1)
    v_all = jnp.concatenate([v_c, v_l], axis=1)
    n_blk = n // Q_BLOCK
    qb = jnp.moveaxis(q_l.reshape(b, n_blk, Q_BLOCK, N_KV_HEADS, Q_PER_KV, HEAD_DIM), 1, 0)
    o_l = lax.map(lambda qblk: _attend(qblk, k_all, v_all), qb)
    o_l = jnp.moveaxis(o_l, 0, 1).reshape(b, n, hq) @ w_o
    o_c = (_attend(q_c, k_c, v_c).reshape(b, m, hq) @ w_o) if ctx_out else None
    return o_l, o_c


def pool_mixer(h, w_pool, scale):
    b, n, d = h.shape
    cs = jnp.concatenate([jnp.zeros((b, 1, d), jnp.float32), jnp.cumsum(h.astype(jnp.float32), axis=1)], axis=1)
    t = jnp.arange(n)
    outs = []
    for gi, win in enumerate(POOL_WINDOWS):
        lo = jnp.maximum(t - win // 2, 0)
        hi = jnp.minimum(t + win // 2, n)
        sl = slice(gi * POOL_GROUP, (gi + 1) * POOL_GROUP)
        cg = cs[..., sl]
        mean = (cg[:, hi] - cg[:, lo]) / (hi - lo).astype(jnp.float32)[None, :, None]
        diff = (mean - h[..., sl].astype(jnp.float32)).astype(h.dtype)
        outs.append(diff @ w_pool[gi])
    return jnp.concatenate(outs, axis=-1) * scale


def depthwise_conv(u, w, bias):
    out = lax.conv_general_dilated(u, w[:, None, :], window_strides=(1,),
                                   padding=[(SSM_CONV // 2, (SSM_CONV - 1) // 2)],
                                   dimension_numbers=('NWC', 'WIO', 'NWC'),
                                   feature_group_count=u.shape[-1])
    return out + bias


def ssd_chunked(x, dt, a, bm, cm, h0, with_y):
    b, n = x.shape[:2]
    nc = n // SSM_CHUNK
    Q = SSM_CHUNK
    xs = (x * dt[..., None]).reshape(b, nc, Q, SSM_GROUPS, SSM_HPG, SSM_HEAD_DIM)
    la = (dt * a).reshape(b, nc, Q, SSM_GROUPS, SSM_HPG)
    la_cs = jnp.cumsum(la, axis=2)
    bc = bm.reshape(b, nc, Q, SSM_GROUPS, SSM_STATE)
    cc = cm.reshape(b, nc, Q, SSM_GROUPS, SSM_STATE)
    decay_end = jnp.exp(la_cs[:, :, -1:] - la_cs)
    states = jnp.einsum('bcsgn,bcsgrp->bcgrpn', bc, xs * decay_end[..., None])
    chunk_decay = jnp.exp(la_cs[:, :, -1])

    def step(h, inp):
        st, dec = inp
        return h * dec[..., None, None] + st, h

    h_fin, h_in = lax.scan(step, h0.reshape(b, SSM_GROUPS, SSM_HPG, SSM_HEAD_DIM, SSM_STATE),
                           (jnp.moveaxis(states, 1, 0), jnp.moveaxis(chunk_decay, 1, 0)))
    h_fin = h_fin.reshape(b, SSM_HEADS, SSM_HEAD_DIM, SSM_STATE)
    if not with_y:
        return None, h_fin
    h_in = jnp.moveaxis(h_in, 0, 1)
    la_t = jnp.moveaxis(la_cs, 2, -1)
    seg = la_t[..., :, None] - la_t[..., None, :]
    scan_order = jnp.tril(jnp.ones((Q, Q), dtype=bool))
    decay = jnp.exp(jnp.where(scan_order, seg, -jnp.inf))
    scores = jnp.einsum('bclgn,bcsgn->bcgls', cc, bc)
    y_diag = jnp.einsum('bcgrls,bcsgrp->bclgrp', decay * scores[:, :, :, None], xs)
    y_off = jnp.einsum('bclgn,bcgrpn->bclgrp', cc, h_in) * jnp.exp(la_cs)[..., None]
    return (y_diag + y_off).reshape(b, n, SSM_HEADS, SSM_HEAD_DIM), h_fin


def _seq_flip(t, rev):
    return jnp.flip(t, axis=1) if rev else t


def ssd_mixer(h_lat, h_ctx, w_in, conv_w, conv_b, dt_bias, a_log, d_skip, norm_g, w_out, ctx_out):
    def project(h, with_gate):
        b, n = h.shape[:2]
        proj = h @ (w_in if with_gate else w_in[:, SSM_D_INNER:])
        z = proj[..., :SSM_D_INNER] if with_gate else None
        rest = proj[..., SSM_D_INNER:] if with_gate else proj
        xbc = jax.nn.silu(depthwise_conv(rest[..., :SSM_CONV_DIM], conv_w, conv_b)).astype(jnp.float32)
        xh = xbc[..., :SSM_D_INNER].reshape(b, n, SSM_HEADS, SSM_HEAD_DIM)
        bm = xbc[..., SSM_D_INNER:SSM_D_INNER + SSM_GN].reshape(b, n, SSM_GROUPS, SSM_STATE)
        cm = xbc[..., SSM_D_INNER + SSM_GN:].reshape(b, n, SSM_GROUPS, SSM_STATE)
        dt = jax.nn.softplus(rest[..., SSM_CONV_DIM:].astype(jnp.float32).reshape(b, n, 2, SSM_HEADS)
                             + dt_bias.astype(jnp.float32))
        return z, xh, bm, cm, dt

    z_l, x_l, b_l, c_l, dt_l = project(h_lat, True)
    z_c, x_c, b_c, c_c, dt_c = project(h_ctx, ctx_out)
    a = -jnp.exp(a_log.astype(jnp.float32))
    dsk = d_skip.astype(jnp.float32)
    h0 = jnp.zeros((h_ctx.shape[0], SSM_HEADS, SSM_HEAD_DIM, SSM_STATE), jnp.float32)
    y_l = jnp.zeros_like(x_l)
    y_c = jnp.zeros_like(x_c) if ctx_out else None
    for direction in range(2):
        rev = direction == 1
        yc_d, h_ctx_fin = ssd_chunked(_seq_flip(x_c, rev), _seq_flip(dt_c[:, :, direction], rev), a[direction],
                                      _seq_flip(b_c, rev), _seq_flip(c_c, rev), h0, ctx_out)
        yl_d, _ = ssd_chunked(_seq_flip(x_l, rev), _seq_flip(dt_l[:, :, direction], rev), a[direction],
                              _seq_flip(b_l, rev), _seq_flip(c_l, rev), h_ctx_fin, True)
        y_l = y_l + _seq_flip(yl_d, rev) + dsk[direction][:, None] * x_l
        if ctx_out:
            y_c = y_c + _seq_flip(yc_d, rev) + dsk[direction][:, None] * x_c

    def gated_out(y, z):
        b, n = y.shape[:2]
        g = y.reshape(b, n, SSM_D_INNER) * jax.nn.silu(z.astype(jnp.float32))
        g = g.reshape(b, n, SSM_GROUPS, SSM_D_INNER // SSM_GROUPS)
        g = g * lax.rsqrt(jnp.mean(g * g, axis=-1, keepdims=True) + EPS)
        g = g.reshape(b, n, SSM_D_INNER) * norm_g.astype(jnp.float32)
        return g.astype(h_lat.dtype) @ w_out

    return gated_out(y_l, z_l), (gated_out(y_c, z_c) if ctx_out else None)


def hierarchical_moe(h, w_rg, w_re, w_gu, w_down):
    T, d = h.shape
    hf = h.astype(jnp.float32)
    pg = jax.nn.softmax(hf @ w_rg.astype(jnp.float32), axis=-1)
    gate_g, grp = lax.top_k(pg, 1)
    le = (hf @ w_re.astype(jnp.float32)).reshape(T, MOE_GROUPS, MOE_EXPERTS_PER_GROUP)
    le = le[jnp.arange(T), grp[:, 0]]
    top_v, top_i = lax.top_k(le, MOE_TOPK)
    w_sel = gate_g * jax.nn.softmax(top_v, axis=-1)
    expert = grp * MOE_EXPERTS_PER_GROUP + top_i
    n_assign = T * MOE_TOPK
    e_flat = expert.reshape(-1)
    w_flat = w_sel.reshape(-1).astype(h.dtype)
    tok_flat = jnp.repeat(jnp.arange(T, dtype=jnp.int32), MOE_TOPK)
    order = jnp.argsort(e_flat)
    e_sorted = e_flat[order]
    counts = jnp.zeros((MOE_EXPERTS,), jnp.int32).at[e_flat].add(1)
    starts = jnp.cumsum(counts) - counts
    padded = (counts + MOE_BLOCK - 1) // MOE_BLOCK * MOE_BLOCK
    pends = jnp.cumsum(padded)
    pstarts = pends - padded
    dest = pstarts[e_sorted] + jnp.arange(n_assign, dtype=jnp.int32) - starts[e_sorted]
    n_blocks = -(-n_assign // MOE_BLOCK) + MOE_EXPERTS
    n_rows = n_blocks * MOE_BLOCK
    row_tok = jnp.full((n_rows,), T, jnp.int32).at[dest].set(tok_flat[order])
    row_w = jnp.zeros((n_rows,), h.dtype).at[dest].set(w_flat[order])
    block_expert = jnp.minimum(
        jnp.searchsorted(pends, jnp.arange(n_blocks, dtype=jnp.int32) * MOE_BLOCK, side='right'),
        MOE_EXPERTS - 1)
    h_pad = jnp.concatenate([h, jnp.zeros((1, d), h.dtype)], axis=0)

    def expert_block(args):
        rows, e = args
        gu = h_pad[rows] @ w_gu[e]
        return (jax.nn.silu(gu[:, :MOE_HIDDEN]) * gu[:, MOE_HIDDEN:]) @ w_down[e]

    y = lax.map(expert_block, (row_tok.reshape(n_blocks, MOE_BLOCK), block_expert))
    y = y.reshape(n_rows, d) * row_w[:, None]
    return jnp.zeros_like(h_pad).at[row_tok].add(y)[:T]


def setup_inputs(seed: int = 0) -> dict:
    key = jax.random.key(seed)
    ks = iter(jax.random.split(key, 40))
    D = D_MODEL

    def nrm(shape, scale):
        return jax.random.normal(next(ks), shape, jnp.float32) * scale

    u = jax.random.uniform(next(ks), (N_SSM_LAYERS, 2, SSM_HEADS), jnp.float32)
    dt0 = jnp.exp(u * (math.log(DT_MAX) - math.log(DT_MIN)) + math.log(DT_MIN))
    dt_bias = dt0 + jnp.log(-jnp.expm1(-dt0))
    a_log = jnp.log(jax.random.uniform(next(ks), (N_SSM_LAYERS, 2, SSM_HEADS), jnp.float32, 1.0, 16.0))
    return {
        'x': nrm((BATCH, SEQ, D), 1.0),
        'c': nrm((BATCH, D), 1.0),
        'ctx': nrm((BATCH, CTX_LEN, D), 1.0),
        'c_ctx': nrm((D,), 1.0),
        'w_ada': nrm((DEPTH, D, 6 * D), 0.5 * D ** -0.5),
        'b_ada': nrm((DEPTH, 6 * D), 0.02),
        'norm_mix_g': 1.0 + nrm((DEPTH, D), 0.05),
        'norm_ffn_g': 1.0 + nrm((DEPTH, D), 0.05),
        'final_norm_g': 1.0 + nrm((D,), 0.05),
        'attn_w_qkv': nrm((N_ATTN_LAYERS, D, QKV_DIM), D ** -0.5),
        'attn_w_o': nrm((N_ATTN_LAYERS, N_HEADS * HEAD_DIM, D), (N_HEADS * HEAD_DIM) ** -0.5),
        'attn_q_norm_g': 1.0 + nrm((N_ATTN_LAYERS, HEAD_DIM), 0.05),
        'attn_k_norm_g': 1.0 + nrm((N_ATTN_LAYERS, HEAD_DIM), 0.05),
        'pool_w': nrm((N_POOL_LAYERS, len(POOL_WINDOWS), POOL_GROUP, POOL_GROUP), POOL_GROUP ** -0.5),
        'pool_scale': 1.0 + nrm((N_POOL_LAYERS, D), 0.05),
        'ssm_w_in': nrm((N_SSM_LAYERS, D, SSM_IN_DIM), D ** -0.5),
        'ssm_conv_w': nrm((N_SSM_LAYERS, SSM_CONV, SSM_CONV_DIM), SSM_CONV ** -0.5),
        'ssm_conv_b': nrm((N_SSM_LAYERS, SSM_CONV_DIM), 0.02),
        'ssm_dt_bias': dt_bias,
        'ssm_a_log': a_log,
        'ssm_d': 1.0 + nrm((N_SSM_LAYERS, 2, SSM_HEADS), 0.05),
        'ssm_norm_g': 1.0 + nrm((N_SSM_LAYERS, SSM_D_INNER), 0.05),
        'ssm_w_out': nrm((N_SSM_LAYERS, SSM_D_INNER, D), SSM_D_INNER ** -0.5),
        'moe_w_router_group': nrm((DEPTH, D, MOE_GROUPS), D ** -0.5),
        'moe_w_router_expert': nrm((DEPTH, D, MOE_EXPERTS), D ** -0.5),
        'moe_w_gate_up': nrm((DEPTH, MOE_EXPERTS, D, 2 * MOE_HIDDEN), D ** -0.5),
        'moe_w_down': nrm((DEPTH, MOE_EXPERTS, MOE_HIDDEN, D), MOE_HIDDEN ** -0.5),
    }


def reference(x, c, ctx, c_ctx, w_ada, b_ada, norm_mix_g, norm_ffn_g, final_norm_g,
              attn_w_qkv, attn_w_o, attn_q_norm_g, attn_k_norm_g, pool_w, pool_scale,
              ssm_w_in, ssm_conv_w, ssm_conv_b, ssm_dt_bias, ssm_a_log, ssm_d, ssm_norm_g, ssm_w_out,
              moe_w_router_group, moe_w_router_expert, moe_w_gate_up, moe_w_down):
    b, n, d = x.shape
    m = ctx.shape[1]
    cos, sin = axial_rope_tables(n)
    cond_lat = jax.nn.silu(c)
    cond_ctx = jax.nn.silu(c_ctx)
    x_lat, x_ctx = x, ctx
    for i in range(DEPTH):
        kind, j = i % N_MIXERS, i // N_MIXERS
        ctx_out = i < DEPTH - 1
        ctx_in = ctx_out or kind != 1
        mod_l = (cond_lat @ w_ada[i] + b_ada[i])[:, None, :]
        sh_m, sc_m, g_m, sh_f, sc_f, g_f = jnp.split(mod_l, 6, axis=-1)
        h_l = modulate(rms_norm(x_lat, norm_mix_g[i]), sh_m, sc_m)
        if ctx_in:
            n_mod = 6 if ctx_out else 3
            mc = jnp.split(cond_ctx @ w_ada[i][:, :n_mod * d] + b_ada[i][:n_mod * d], n_mod)
            h_c = modulate(rms_norm(x_ctx, norm_mix_g[i]), mc[0], mc[1])
        if kind == 0:
            o_l, o_c = attention_mixer(h_l, h_c, attn_w_qkv[j], attn_w_o[j], attn_q_norm_g[j],
                                       attn_k_norm_g[j], cos, sin, ctx_out)
        elif kind == 1:
            o_l = pool_mixer(h_l, pool_w[j], pool_scale[j])
            o_c = pool_mixer(h_c, pool_w[j], pool_scale[j]) if ctx_out else None
        else:
            o_l, o_c = ssd_mixer(h_l, h_c, ssm_w_in[j], ssm_conv_w[j], ssm_conv_b[j], ssm_dt_bias[j],
                                 ssm_a_log[j], ssm_d[j], ssm_norm_g[j], ssm_w_out[j], ctx_out)
        x_lat = x_lat + g_m * o_l
        if ctx_out:
            x_ctx = x_ctx + mc[2] * o_c
        tokens = modulate(rms_norm(x_lat, norm_ffn_g[i]), sh_f, sc_f).reshape(b * n, d)
        if ctx_out:
            h_cf = modulate(rms_norm(x_ctx, norm_ffn_g[i]), mc[3], mc[4])
            tokens = jnp.concatenate([tokens, h_cf.reshape(b * m, d)], axis=0)
        f = hierarchical_moe(tokens, moe_w_router_group[i], moe_w_router_expert[i],
                             moe_w_gate_up[i], moe_w_down[i])
        x_lat = x_lat + g_f * f[:b * n].reshape(b, n, d)
        if ctx_out:
            x_ctx = x_ctx + mc[5] * f[b * n:].reshape(b, m, d)
    return rms_norm(x_lat, final_norm_g)
```

```python
import contextlib
import numpy as np
import concourse.bass as bass
import concourse.mybir as mybir
from concourse.bass_utils import run_bass_kernel_spmd

F32 = mybir.dt.float32
BF16 = mybir.dt.bfloat16
I32 = mybir.dt.int32
ALU = mybir.AluOpType
AF = mybir.ActivationFunctionType
AX = mybir.AxisListType

D = 1024
NB = 4
SEQ = 4096
CTX = 256
T = SEQ + CTX
NT = T // 128
DEPTH = 4
EPS = 1e-6
GRID_W = 64
NH, NKV, HD = 16, 4, 64
POOL_WINDOWS = (2, 4, 8, 16)
SSM_DI, SSM_H, SSM_P, SSM_G, SSM_N = 2048, 32, 64, 4, 128
SSM_CONV_DIM = SSM_DI + 2 * SSM_G * SSM_N
SSM_IN = SSM_DI + SSM_CONV_DIM + 2 * SSM_H
NE, EPG, FH = 32, 8, 512
BIG = 1.0e30
BLK = 512
NBLK = (2 * T + BLK - 1) // BLK + NE
NSLOT = NBLK * BLK

SAME_ENGINE_SYNC = ("act", "dve", "pool")


class Sched:
    def __init__(self, nc):
        self.nc = nc
        self.eng = {"pe": nc.tensor, "act": nc.scalar, "dve": nc.vector,
                    "pool": nc.gpsimd, "sp": nc.sync}
        self.esem = {}
        self.ecnt = {}
        for e in ("pe", "act", "dve", "pool"):
            self.esem[e] = nc.alloc_semaphore("s_" + e)
            self.ecnt[e] = 0
        self.seen = {e: {} for e in self.eng}
        self.wr = {}
        self.rd = {}
        self.dsem = {}
        self.dfree = []
        self.nsem = 0
        self.bregs = {}
        self.n_inst = 0

    def _wait(self, e, toks):
        eng = self.eng[e]
        for name, (sem, val, src) in toks.items():
            if src == e and e not in SAME_ENGINE_SYNC:
                continue
            if self.seen[e].get(name, 0) >= val:
                continue
            eng.wait_ge(sem, val)
            self.seen[e][name] = val

    def _deps(self, e, reads, writes):
        for k in reads:
            self._wait(e, self.wr.get(k, {}))
        for k in writes:
            self._wait(e, self.wr.get(k, {}))
            self._wait(e, self.rd.get(k, {}))

    def _commit(self, name, tok, reads, writes):
        for k in reads:
            self.rd.setdefault(k, {})[name] = tok
        for k in writes:
            self.wr[k] = {name: tok}
            self.rd[k] = {}

    def op(self, e, fn, reads=(), writes=()):
        self._deps(e, reads, writes)
        ins = fn(self.eng[e])
        self.ecnt[e] += 1
        ins.then_inc(self.esem[e], 1)
        self._commit("s_" + e, (self.esem[e], self.ecnt[e], e), reads, writes)
        self.n_inst += 1
        return ins

    def dma(self, q, out, in_, reads=(), writes=(), semkey=None, **kw):
        if semkey is None:
            semkey = tuple(writes) + tuple(reads)
        ent = self._dsem_get(semkey)
        self._deps(q, reads, writes)
        ins = self.eng[q].dma_start(out=out, in_=in_, **kw)
        ent[1] += 16
        ins.then_inc(ent[0], 16)
        self._commit(ent[2], (ent[0], ent[1], "dma"), reads, writes)
        self.n_inst += 1

    def idma(self, out, out_off, in_, in_off, bounds, reads=(), writes=(), semkey=None):
        q = "pool"
        ent = self._dsem_get(semkey)
        self._deps(q, reads, writes)
        if bounds not in self.bregs:
            self.bregs[bounds] = self.eng[q].to_reg(bounds)
        ins = self.eng[q].indirect_dma_start(out=out, out_offset=out_off, in_=in_, in_offset=in_off,
                                             bounds_check=self.bregs[bounds], oob_is_err=False)
        ent[1] += 16
        ins.then_inc(ent[0], 16)
        self._commit(ent[2], (ent[0], ent[1], "dma"), reads, writes)
        self.n_inst += 1

    def recycle(self):
        for key, ent in list(self.dsem.items()):
            for e in self.eng:
                if self.seen[e].get(ent[2], 0) < ent[1]:
                    self.eng[e].wait_ge(ent[0], ent[1])
                    self.seen[e][ent[2]] = ent[1]
            self.dfree.append(ent)
            del self.dsem[key]

    def _dsem_get(self, semkey):
        if semkey not in self.dsem:
            if self.dfree:
                self.dsem[semkey] = self.dfree.pop()
            else:
                nm = "d%d" % self.nsem
                self.nsem += 1
                self.dsem[semkey] = [self.nc.alloc_semaphore(nm), 0, nm]
        return self.dsem[semkey]

    def wait_all(self, e, keys):
        for k in keys:
            self._wait(e, self.wr.get(k, {}))
            self._wait(e, self.rd.get(k, {}))


class Prog:
    def __init__(self, cfg):
        self.cfg = cfg
        self.nc = nc = bass.Bass("TRN2", target_bir_lowering=False)
        self.S = Sched(nc)
        self.es = contextlib.ExitStack()
        self.inp = {}
        self.uid = 0

    def din(self, name, shape, dt=F32):
        t = self.nc.dram_tensor(name, list(shape), dt, kind="ExternalInput").ap()
        self.inp[name] = t
        return t

    def dscratch(self, name, shape, dt=F32):
        return self.nc.dram_tensor(name, list(shape), dt, kind="Internal").ap()

    def sb(self, stack, name, shape, dt=F32):
        self.uid += 1
        return stack.enter_context(self.nc.sbuf_tensor("%s_u%d" % (name, self.uid), list(shape), dt))

    def mm(self, out, lhsT, rhs, start, stop, reads, writes):
        self.S.op("pe", lambda e: e.matmul(out, lhsT=lhsT, rhs=rhs, start=start, stop=stop),
                  reads=reads, writes=writes)

    def tr(self, out, in_, ident, reads, writes):
        self.S.op("pe", lambda e: e.transpose(out, in_, ident), reads=reads, writes=writes)

    def act(self, out, in_, func, reads, writes, bias=None, scale=None, accum_out=None):
        kw = {}
        if bias is not None:
            kw["bias"] = bias
        if scale is not None:
            kw["scale"] = scale
        if accum_out is not None:
            kw["accum_out"] = accum_out
        self.S.op("act", lambda e: e.activation(out=out, in_=in_, func=func, **kw),
                  reads=reads, writes=writes)

    def tt(self, e, out, in0, in1, op, reads, writes):
        self.S.op(e, lambda g: g.tensor_tensor(out=out, in0=in0, in1=in1, op=op),
                  reads=reads, writes=writes)

    def ts(self, e, out, in0, s1, op0, reads, writes, s2=None, op1=None, accum_out=None):
        kw = {}
        if op1 is not None:
            kw["op1"] = op1
        if accum_out is not None:
            kw["accum_out"] = accum_out
        self.S.op(e, lambda g: g.tensor_scalar(out=out, in0=in0, scalar1=s1, scalar2=s2, op0=op0, **kw),
                  reads=reads, writes=writes)

    def stt(self, e, out, in0, scalar, in1, op0, op1, reads, writes):
        self.S.op(e, lambda g: g.scalar_tensor_tensor(out=out, in0=in0, scalar=scalar, in1=in1,
                                                      op0=op0, op1=op1),
                  reads=reads, writes=writes)

    def cp(self, e, out, in_, reads, writes):
        if e == "act":
            self.S.op("act", lambda g: g.copy(out=out, in_=in_), reads=reads, writes=writes)
        else:
            self.S.op(e, lambda g: g.tensor_copy(out=out, in_=in_), reads=reads, writes=writes)

    def red(self, e, out, in_, op, reads, writes, axis=AX.X):
        self.S.op(e, lambda g: g.tensor_reduce(out=out, in_=in_, axis=axis, op=op),
                  reads=reads, writes=writes)

    def recip(self, out, in_, reads, writes):
        self.S.op("dve", lambda g: g.reciprocal(out=out, in_=in_), reads=reads, writes=writes)

    def memset(self, e, ap, val, writes):
        self.S.op(e, lambda g: g.memset(ap, val), writes=writes)

    def dma(self, q, out, in_, reads=(), writes=(), semkey=None, **kw):
        self.S.dma(q, out, in_, reads=reads, writes=writes, semkey=semkey, **kw)


def build(cfg):
    P = Prog(cfg)
    nc, S = P.nc, P.S
    layers = cfg.get("layers", list(range(DEPTH)))
    mixers_on = cfg.get("mixers", True)
    moe_on = cfg.get("moe", True)

    x_in = P.din("x", [SEQ, D])
    ctx_in = P.din("ctx", [CTX, D])
    c_in = P.din("c", [1, D])
    cctx_in = P.din("c_ctx", [1, D])
    w_ada = {i: P.din("w_ada_%d" % i, [D, 6 * D]) for i in layers}
    b_ada = {i: P.din("b_ada_%d" % i, [1, 6 * D]) for i in layers}
    norm_mix_g = P.din("norm_mix_g", [DEPTH, D])
    norm_ffn_g = P.din("norm_ffn_g", [DEPTH, D])
    final_g = P.din("final_norm_g", [1, D])
    attn_w_qkv = {j: P.din("attn_w_qkv_%d" % j, [D, 1536]) for j in range(2) if 3 * j in layers and mixers_on}
    attn_w_o = {j: P.din("attn_w_o_%d" % j, [D, D]) for j in range(2) if 3 * j in layers and mixers_on}
    attn_qg = P.din("attn_q_norm_g", [2, HD])
    attn_kg = P.din("attn_k_norm_g", [2, HD])
    pool_w = P.din("pool_w", [1, 4, 256, 256])
    pool_scale = P.din("pool_scale", [1, D])
    ssm_on = 2 in layers and mixers_on
    ssm_w_in = P.din("ssm_w_in", [1, D, SSM_IN]) if ssm_on else None
    ssm_conv_w = P.din("ssm_conv_w", [1, 4, SSM_CONV_DIM])
    ssm_conv_b = P.din("ssm_conv_b", [1, SSM_CONV_DIM])
    ssm_dt_bias = P.din("ssm_dt_bias", [1, 2 * SSM_H])
    ssm_a_log = P.din("ssm_a_log", [1, 2 * SSM_H])
    ssm_d = P.din("ssm_d", [1, 2 * SSM_H])
    ssm_norm_g = P.din("ssm_norm_g", [1, SSM_DI])
    ssm_w_out = P.din("ssm_w_out", [1, SSM_DI, D]) if ssm_on else None
    moe_l = [i for i in layers if moe_on]
    moe_wr = {i: P.din("moe_wr_%d" % i, [D, 36]) for i in moe_l}
    moe_w_gu = {i: P.din("moe_w_gu_%d" % i, [NE * 128, 8 * 2 * FH]) for i in moe_l}
    moe_w_dn = {i: P.din("moe_w_dn_%d" % i, [NE * 128, 4 * D]) for i in moe_l}
    k_ident = P.din("k_ident", [128, 128])
    k_rope = P.din("k_rope", [T, 64])
    k_band = P.din("k_band", [4, 5, 128, 128])
    k_tri = P.din("k_tri", [4, 128, 128])
    k_moe = P.din("k_moe", [128, 128])
    out = nc.dram_tensor("out", [SEQ, D], F32, kind="ExternalOutput").ap()
    dbg = None
    if cfg.get("dbg"):
        dbg = nc.dram_tensor("dbg", list(cfg["dbg"]), F32, kind="ExternalOutput").ap()

    X = P.dscratch("X", [T, D])

    def xk(t):
        return ("X", t)

    top = P.es
    ident = P.sb(top, "ident", [128, 128])
    identb = P.sb(top, "identb", [128, 128], BF16)
    ones = P.sb(top, "ones", [128, 128])
    cT = P.sb(top, "cT", [128, 8, 2])
    modL = P.sb(top, "modL", [128, 48])
    modC = P.sb(top, "modC", [128, 48])
    vec = P.sb(top, "vec", [128, 2, 2, 2, 8])
    gates = P.sb(top, "gates", [128, 2, 2, D])
    ps = [top.enter_context(nc.psum_tensor("ps%d" % i, [128, 512], F32)) for i in range(8)]

    def pk(i):
        return "ps%d" % i

    P.dma("sp", ident[:], k_ident, writes=["ident"])
    P.cp("dve", identb[:], ident[:], ["ident"], ["identb"])
    P.memset("pool", ones[:], 1.0, ["ones"])
    P.dma("sp", X[0:CTX, :], ctx_in, writes=[xk(0), xk(1)], semkey="xinit")
    P.dma("sp", X[CTX:T, :], x_in, writes=[xk(t) for t in range(2, NT)], semkey="xinit")

    with contextlib.ExitStack() as st:
        craw = P.sb(st, "craw", [128, 8, 2])
        P.dma("sp", craw[:, :, 0], c_in[0].rearrange("(k p) -> p k", p=128), writes=["craw"],
              allow_slow_non_contiguous=True)
        P.dma("sp", craw[:, :, 1], cctx_in[0].rearrange("(k p) -> p k", p=128), writes=["craw"],
              allow_slow_non_contiguous=True)
        P.act(cT[:], craw[:], AF.Silu, ["craw"], ["cT"])
        S.wait_all("sp", ["craw"])
        S.wait_all("act", ["craw"])


    def drain(keys):
        for e_ in ("sp", "pe", "act", "dve", "pool"):
            S.wait_all(e_, keys)

    def adaln(i, need_bc_all):
        with contextlib.ExitStack() as st:
            wblk = [P.sb(st, "wblk%d" % j, [128, 8, 512]) for j in range(2)]
            brow = P.sb(st, "brow", [1, 6 * D])
            bT = P.sb(st, "bT", [128, 48])
            g2 = P.sb(st, "g2", [128, 2, 8])
            cbc = P.sb(st, "cbc", [128, 2, 8, 128])
            for s in range(2):
                P.cp("dve", cbc[:, s], cT[:, :, s:s + 1].broadcast_to([128, 8, 128]), ["cT"], ["cbc"])
            P.dma("sp", brow[:], b_ada[i][0:1, :], writes=["brow"])
            P.dma("sp", bT[:], b_ada[i][0].rearrange("(j p) -> p j", p=128), writes=["bT"],
                  allow_slow_non_contiguous=True)
            P.dma("sp", g2[:, 0, :], norm_mix_g[i].rearrange("(j p) -> p j", p=128), writes=["g2"],
                  allow_slow_non_contiguous=True)
            P.dma("sp", g2[:, 1, :], norm_ffn_g[i].rearrange("(j p) -> p j", p=128), writes=["g2"],
                  allow_slow_non_contiguous=True)
            for n in range(12):
                wb = wblk[n % 2]
                wkey = "wblk%d" % (n % 2)
                P.dma("sp", wb[:], w_ada[i][:, n * 512:(n + 1) * 512].rearrange("(k p) n -> p k n", p=128),
                      writes=[wkey])
                for q in range(4):
                    j = n * 4 + q
                    for k in range(8):
                        P.mm(ps[0][:, 2 * j:2 * j + 2], wb[:, k, q * 128:(q + 1) * 128], cT[:, k, :],
                             k == 0, k == 7, [wkey, "cT"], [pk(0)])
                split = n // 2
                if split in (2, 5):
                    which = 0 if split == 2 else 1
                    half = n % 2
                    for s in range(2):
                        bank = 1 + s
                        for k in range(8):
                            P.mm(ps[bank][:, :], cbc[:, s, k, :], wb[:, k, :], k == 0, False,
                                 [wkey, "cbc"], [pk(bank)])
                        P.mm(ps[bank][:, :], ones[0:1, :], brow[0:1, n * 512:(n + 1) * 512], False, True,
                             ["ones", "brow"], [pk(bank)])
                        P.cp("act", gates[:, which, s, half * 512:(half + 1) * 512], ps[bank][:, :],
                             [pk(bank)], [("gates", which, s)])
            psv = ps[0][:, 0:96].rearrange("p (j s) -> p j s", s=2)
            P.tt("dve", modL[:], psv[:, :, 0], bT[:], ALU.add, [pk(0), "bT"], ["modL"])
            P.tt("dve", modC[:], psv[:, :, 1], bT[:], ALU.add, [pk(0), "bT"], ["modC"])
            for which in range(2):
                for s, m in enumerate((modL, modC)):
                    mk = "modL" if s == 0 else "modC"
                    base = which * 24
                    P.stt("dve", vec[:, which, s, 0, :], m[:, base + 8:base + 16], 1.0, g2[:, which, :],
                          ALU.add, ALU.mult, [mk, "g2"], [("vec", which, s)])
                    P.cp("dve", vec[:, which, s, 1, :], m[:, base:base + 8], [mk], [("vec", which, s)])
            S.wait_all("sp", ["wblk0", "wblk1", "brow", "bT", "g2"])
            S.wait_all("pe", ["wblk0", "wblk1", "brow", "cbc"])
            S.wait_all("dve", ["bT", "g2"])

    def norm_tiles(st, tiles, which, hT, hT_key, col0, hook=None, tag="n", colfn=None):
        xt = [P.sb(st, "%s_xt%d" % (tag, j), [128, D]) for j in range(2)]
        h32 = [P.sb(st, "%s_h32%d" % (tag, j), [128, 8, 128]) for j in range(2)]
        st8 = [P.sb(st, "%s_st%d" % (tag, j), [128, 4]) for j in range(2)]
        junk = P.sb(st, "%s_junk" % tag, [128, D], BF16)

        def load(idx):
            t = tiles[idx]
            P.dma("sp", xt[idx % 2][:], X[t * 128:(t + 1) * 128, :], reads=[xk(t)],
                  writes=["%s_xt%d" % (tag, idx % 2)], semkey="%s_xt%d" % (tag, idx % 2))

        load(0)
        for idx, t in enumerate(tiles):
            if idx + 1 < len(tiles):
                load(idx + 1)
            b = idx % 2
            xkey, skey, hkey = "%s_xt%d" % (tag, b), "%s_st%d" % (tag, b), "%s_h32%d" % (tag, b)
            s = 1 if t < 2 else 0
            P.act(junk[:], xt[b][:], AF.Square, [xkey], ["%s_junk" % tag, skey], accum_out=st8[b][:, 0:1])
            P.act(st8[b][:, 1:2], st8[b][:, 0:1], AF.Sqrt, [skey], [skey], bias=EPS, scale=1.0 / D)
            P.recip(st8[b][:, 2:3], st8[b][:, 1:2], [skey], [skey])
            P.ts("dve", xt[b][:], xt[b][:], st8[b][:, 2:3], ALU.mult, [xkey, skey], [xkey])
            for c in range(8):
                bank = 6 + c // 4
                P.tr(ps[bank][:, (c % 4) * 128:(c % 4 + 1) * 128], xt[b][:, c * 128:(c + 1) * 128], ident[:],
                     [xkey, "ident"], [pk(bank)])
            c0 = col0 + idx * 128 if colfn is None else colfn(idx)
            hTk = hT_key if colfn is None else (hT_key, idx % 2)
            if hook is None:
                for c in range(8):
                    bank = 6 + c // 4
                    P.act(hT[:, c, c0:c0 + 128], ps[bank][:, (c % 4) * 128:(c % 4 + 1) * 128], AF.Identity,
                          [pk(bank), ("vec", which, s)], [hTk],
                          bias=vec[:, which, s, 1, c:c + 1], scale=vec[:, which, s, 0, c:c + 1])
            else:
                for c in range(8):
                    bank = 6 + c // 4
                    P.act(h32[b][:, c, :], ps[bank][:, (c % 4) * 128:(c % 4 + 1) * 128], AF.Identity,
                          [pk(bank), ("vec", which, s)], [hkey],
                          bias=vec[:, which, s, 1, c:c + 1], scale=vec[:, which, s, 0, c:c + 1])
                P.cp("dve", hT[:, :, c0:c0 + 128], h32[b][:], [hkey], [hTk])
            if hook is not None:
                hook(idx, t, h32[b], hkey)

    def moe(i, tiles_all):
        nhalf = 2
        per = len(tiles_all) // nhalf
        for hf in range(nhalf):
            tiles = tiles_all[hf * per:(hf + 1) * per]
            ntk = per * 128
            with contextlib.ExitStack() as st:
                hT = P.sb(st, "m_hT", [128, 8, ntk], BF16)
                Y = P.sb(st, "m_Y", [128, per, D])
                Wt = P.sb(st, "m_Wt", [128, per, NE])
                wr = P.sb(st, "m_wr", [128, 8, 36])
                wgu = [P.sb(st, "m_wgu%d" % j, [128, 8, 2 * FH], BF16) for j in range(2)]
                wdn = [P.sb(st, "m_wdn%d" % j, [128, 4, D], BF16) for j in range(2)]
                sg = [P.sb(st, "m_sg%d" % j, [128, 512], BF16) for j in range(2)]
                aT = [P.sb(st, "m_a%d" % j, [128, 4, 512], BF16) for j in range(2)]
                rt = P.sb(st, "m_rt", [128, 160])

                def loadw(e):
                    b = e % 2
                    P.dma("pool", wgu[b][:], moe_w_gu[i][e * 128:(e + 1) * 128, :].rearrange("p (k n) -> p k n", k=8),
                          writes=["m_wgu%d" % b])
                    P.dma("pool", wdn[b][:], moe_w_dn[i][e * 128:(e + 1) * 128, :].rearrange("p (k n) -> p k n", k=4),
                          writes=["m_wdn%d" % b])

                P.dma("sp", wr[:], moe_wr[i].rearrange("(k p) n -> p k n", p=128), writes=["m_wr"])
                loadw(0)

                def router(idx, t, h32, hkey):
                    lg = ps[5]
                    for k in range(8):
                        P.mm(lg[:, 0:36], h32[:, k, :], wr[:, k, :], k == 0, k == 7, [hkey, "m_wr"], [pk(5)])
                    R = "m_rt"
                    lgs = rt[:, 0:36]
                    P.cp("dve", lgs, lg[:, 0:36], [pk(5)], [R])
                    gmax, ngmax, gsum, gate = rt[:, 36:37], rt[:, 37:38], rt[:, 38:39], rt[:, 39:40]
                    P.red("dve", gmax, rt[:, 0:4], ALU.max, [R], [R])
                    P.ts("dve", ngmax, gmax, -1.0, ALU.mult, [R], [R])
                    P.act(rt[:, 40:44], rt[:, 0:4], AF.Exp, [R], [R], bias=ngmax, scale=1.0, accum_out=gsum)
                    P.recip(gate, gsum, [R], [R])
                    pen = rt[:, 44:48]
                    P.ts("dve", pen, rt[:, 0:4], gmax, ALU.is_ge, [R], [R])
                    P.ts("dve", pen, pen, -1.0, ALU.add, [R], [R], s2=BIG, op1=ALU.mult)
                    le = rt[:, 48:80]
                    P.tt("dve", le.rearrange("p (g j) -> p g j", g=4), rt[:, 4:36].rearrange("p (g j) -> p g j", g=4),
                         pen.unsqueeze(2).broadcast_to([128, 4, 8]), ALU.add, [R], [R])
                    m1, m2 = rt[:, 80:81], rt[:, 81:82]
                    P.red("dve", m1, le, ALU.max, [R], [R])
                    oh1, oh2, le2 = rt[:, 84:116], rt[:, 116:148], rt[:, 4:36]
                    P.ts("dve", oh1, le, m1, ALU.is_ge, [R], [R])
                    P.stt("dve", le2, oh1, -BIG, le, ALU.mult, ALU.add, [R], [R])
                    P.red("dve", m2, le2, ALU.max, [R], [R])
                    P.ts("dve", oh2, le2, m2, ALU.is_ge, [R], [R])
                    dd, ee, p1, p2 = rt[:, 148:149], rt[:, 149:150], rt[:, 150:151], rt[:, 151:152]
                    P.tt("dve", dd, m2, m1, ALU.subtract, [R], [R])
                    P.act(ee, dd, AF.Exp, [R], [R])
                    P.ts("dve", p1, ee, 1.0, ALU.add, [R], [R])
                    P.recip(p1, p1, [R], [R])
                    P.tt("dve", p2, ee, p1, ALU.mult, [R], [R])
                    P.tt("dve", p1, p1, gate, ALU.mult, [R], [R])
                    P.tt("dve", p2, p2, gate, ALU.mult, [R], [R])
                    P.ts("dve", oh1, oh1, p1, ALU.mult, [R], [R])
                    P.stt("dve", Wt[:, idx, :], oh2, p2, oh1, ALU.mult, ALU.add, [R], [("m_Wt", idx)])

                with contextlib.ExitStack() as st2:
                    norm_tiles(st2, tiles, 1, hT, "m_hT", 0, hook=router, tag="mn")
                    S.wait_all("sp", ["mn_xt0", "mn_xt1"])
                    S.wait_all("act", ["mn_xt0", "mn_xt1", "mn_junk", "mn_st0", "mn_st1", "mn_h320", "mn_h321"])
                    S.wait_all("dve", ["mn_xt0", "mn_xt1", "mn_st0", "mn_st1"])
                    S.wait_all("pool", ["mn_h320", "mn_h321"])
                    S.wait_all("pe", ["mn_xt0", "mn_xt1", "mn_h320", "mn_h321"])

                blocks = []
                o = 0
                while o < ntk:
                    n_ = min(512, ntk - o)
                    blocks.append((o, n_))
                    o += n_
                cnt = 0
                dcnt = 0
                for e in range(NE):
                    if e + 1 < NE:
                        loadw(e + 1)
                    b = e % 2
                    gk, dk = "m_wgu%d" % b, "m_wdn%d" % b
                    for (o, n_) in blocks:
                        ab = cnt % 2
                        akey = "m_a%d" % ab
                        for j in range(4):
                            gb, ub = (j % 2), 2 + (j % 2)
                            for k in range(8):
                                P.mm(ps[gb][:, 0:n_], wgu[b][:, k, j * 128:(j + 1) * 128], hT[:, k, o:o + n_],
                                     k == 0, k == 7, [gk, "m_hT"], [pk(gb)])
                            for k in range(8):
                                P.mm(ps[ub][:, 0:n_], wgu[b][:, k, FH + j * 128:FH + (j + 1) * 128],
                                     hT[:, k, o:o + n_], k == 0, k == 7, [gk, "m_hT"], [pk(ub)])
                            sk = "m_sg%d" % (j % 2)
                            P.act(sg[j % 2][:, 0:n_], ps[gb][:, 0:n_], AF.Silu, [pk(gb)], [sk])
                            P.tt("dve", aT[ab][:, j, 0:n_], sg[j % 2][:, 0:n_], ps[ub][:, 0:n_], ALU.mult,
                                 [sk, pk(ub)], [(akey, j)])
                        for tt_ in range(n_ // 128):
                            tidx = o // 128 + tt_
                            for half in range(2):
                                db = 4 + dcnt % 2
                                dcnt += 1
                                for j in range(4):
                                    P.mm(ps[db][:, :], aT[ab][:, j, tt_ * 128:(tt_ + 1) * 128],
                                         wdn[b][:, j, half * 512:(half + 1) * 512], j == 0, j == 3,
                                         [(akey, j), dk], [pk(db)])
                                yk = ("m_Y", tidx, half)
                                ysl = Y[:, tidx, half * 512:(half + 1) * 512]
                                if e == 0:
                                    P.ts("dve", ysl, ps[db][:, :], Wt[:, tidx, e:e + 1], ALU.mult,
                                         [pk(db), ("m_Wt", tidx)], [yk])
                                else:
                                    P.stt("dve", ysl, ps[db][:, :], Wt[:, tidx, e:e + 1], ysl, ALU.mult, ALU.add,
                                          [pk(db), ("m_Wt", tidx), yk], [yk])
                        cnt += 1
                xo = [P.sb(st, "m_xo%d" % j, [128, D]) for j in range(2)]
                for idx, t in enumerate(tiles):
                    b = idx % 2
                    s = 1 if t < 2 else 0
                    ok = "m_xo%d" % b
                    P.dma("sp", xo[b][:], X[t * 128:(t + 1) * 128, :], reads=[xk(t)], writes=[ok], semkey=ok)
                    P.tt("pool", Y[:, idx, :], Y[:, idx, :], gates[:, 1, s, :], ALU.mult,
                         [("m_Y", idx, 0), ("m_Y", idx, 1), ("gates", 1, s)], [("m_Y", idx, 0), ("m_Y", idx, 1)])
                    P.tt("dve", xo[b][:], xo[b][:], Y[:, idx, :], ALU.add,
                         [ok, ("m_Y", idx, 0), ("m_Y", idx, 1)], [ok])
                    P.dma("sp", X[t * 128:(t + 1) * 128, :], xo[b][:], reads=[ok], writes=[xk(t)], semkey=ok)
                keys = ["m_hT", "m_wr", "m_wgu0", "m_wgu1", "m_wdn0", "m_wdn1", "m_sg0", "m_sg1", "m_rt",
                        "m_xo0", "m_xo1"]
                keys += [("m_a%d" % a, j) for a in range(2) for j in range(4)]
                keys += [("m_Y", idx, h) for idx in range(per) for h in range(2)]
                keys += [("m_Wt", idx) for idx in range(per)]
                for e_ in ("sp", "pe", "act", "dve", "pool"):
                    S.wait_all(e_, keys)


    def attention(i, j, ctx_out):
        QD = P.dscratch("QD%d" % i, [64, 16, T], BF16)
        with contextlib.ExitStack() as st:
            KT = P.sb(st, "a_KT", [128, 4, T], BF16)
            Vg = P.sb(st, "a_V", [128, NT, 4, 128], BF16)
            P.memset("dve", Vg[:], 0.0, ["a_V"])
            P.memset("dve", Vg[:, :, :, 64:66], 1.0, ["a_V"])
            with contextlib.ExitStack() as s1:
                hT = P.sb(s1, "a_hT", [128, 8, T], BF16)
                wqkv = P.sb(s1, "a_wqkv", [128, 8, 1536], BF16)
                gqk = P.sb(s1, "a_gqk", [128, 2, 64])
                P.dma("pool", wqkv[:], attn_w_qkv[j].rearrange("(k p) n -> p k n", p=128), writes=["a_wqkv"])
                P.dma("sp", gqk[:, 0, :], attn_qg[j:j + 1, :].broadcast_to([128, 64]), writes=["a_gqk"])
                P.dma("sp", gqk[:, 1, :], attn_kg[j:j + 1, :].broadcast_to([128, 64]), writes=["a_gqk"])
                with contextlib.ExitStack() as s2:
                    norm_tiles(s2, ALL, 0, hT, "a_hT", 0, tag="an")
                    drain(["an_xt0", "an_xt1", "an_junk", "an_st0", "an_st1", "an_h320", "an_h321"])
                qk_ = P.sb(s1, "a_qk0", [128, 20, 64])
                qk = [qk_, qk_]
                T4 = P.sb(s1, "a_T4", [128, 4 * 512])
                tmp = [T4[:, b * 512:(b + 1) * 512].rearrange("p (h i) -> p h i", i=32) for b in range(4)]
                sq = T4[:, 0:1280].rearrange("p (h d) -> p h d", d=64)
                qr = [P.sb(s1, "a_qr%d" % b, [128, 20, 64], BF16) for b in range(2)]
                ss = [P.sb(s1, "a_ss%d" % b, [128, 20]) for b in range(2)]
                cs = [P.sb(s1, "a_cs%d" % b, [128, 64]) for b in range(2)]
                tab = [P.sb(s1, "a_tab%d" % b, [128, 2, 4, 32]) for b in range(2)]
                QTt_ = P.sb(s1, "a_QTt0", [64, 16, 128], BF16)
                QTt = [QTt_, QTt_]
                gq4 = gqk[:].rearrange("p w (i two) -> p w i two", two=2)
                for t in range(NT):
                    b = t % 2
                    qkk, ssk, csk, tabk, qrk, qtk = ("a_qk0", "a_ss%d" % b, "a_cs%d" % b, "a_tab%d" % b,
                                                     "a_qr%d" % b, "a_QTt0")
                    if cfg.get("a1_lvl", 9) < 0.2:
                        continue
                    P.dma("sp", cs[b][:], k_rope[t * 128:(t + 1) * 128, :], writes=[csk])
                    if cfg.get("a1_lvl", 9) < 0.4:
                        continue
                    for nb in range(3):
                        for k in range(8):
                            P.mm(ps[nb][:, :], hT[:, k, t * 128:(t + 1) * 128], wqkv[:, k, nb * 512:(nb + 1) * 512],
                                 k == 0, k == 7, ["a_hT", "a_wqkv"], [pk(nb)])
                    if cfg.get("a1_lvl", 9) < 0.6:
                        continue
                    P.cp("act", qk[b][:, 0:8, :], ps[0][:, :].rearrange("p (h d) -> p h d", d=64), [pk(0)], [qkk])
                    P.cp("act", qk[b][:, 8:16, :], ps[1][:, :].rearrange("p (h d) -> p h d", d=64), [pk(1)], [qkk])
                    P.cp("act", qk[b][:, 16:20, :], ps[2][:, 0:256].rearrange("p (h d) -> p h d", d=64), [pk(2)], [qkk])
                    if cfg.get("a1_lvl", 9) < 0.8:
                        continue
                    P.cp("act", Vg[:, t, :, 0:64], ps[2][:, 256:512].rearrange("p (h d) -> p h d", d=64),
                         [pk(2)], [("a_V", t)])
                    if cfg.get("a1_lvl", 9) < 2:
                        continue
                    P.tt("pool", sq, qk[b][:], qk[b][:], ALU.mult, [qkk], ["a_sq", "a_t0", "a_t1", "a_t2"])
                    P.red("dve", ss[b][:], sq, ALU.add, ["a_sq", "a_t0", "a_t1", "a_t2"], [ssk])
                    P.act(ss[b][:], ss[b][:], AF.Sqrt, [ssk], [ssk], bias=EPS, scale=1.0 / HD)
                    P.recip(ss[b][:], ss[b][:], [ssk], [ssk])
                    P.tt("dve", qk[b][:], qk[b][:], ss[b][:].unsqueeze(2).broadcast_to([128, 20, 64]), ALU.mult,
                         [qkk, ssk], [qkk])
                    if cfg.get("a1_lvl", 9) < 3:
                        continue
                    for w in range(2):
                        P.tt("pool", tab[b][:, w, 0, :], cs[b][:, 0:32], gq4[:, w, :, 0], ALU.mult, [csk, "a_gqk"], [tabk])
                        P.tt("pool", tab[b][:, w, 1, :], cs[b][:, 32:64], gq4[:, w, :, 1], ALU.mult, [csk, "a_gqk"], [tabk])
                        P.tt("pool", tab[b][:, w, 2, :], cs[b][:, 32:64], gq4[:, w, :, 0], ALU.mult, [csk, "a_gqk"], [tabk])
                        P.tt("pool", tab[b][:, w, 3, :], cs[b][:, 0:32], gq4[:, w, :, 1], ALU.mult, [csk, "a_gqk"], [tabk])
                    qk4 = qk[b][:].rearrange("p h (i two) -> p h i two", two=2)
                    qr4 = qr[b][:].rearrange("p h (i two) -> p h i two", two=2)
                    for w, (h0, h1) in enumerate(((0, 16), (16, 20))):
                        nh = h1 - h0
                        x0, x1 = qk4[:, h0:h1, :, 0], qk4[:, h0:h1, :, 1]
                        tb = lambda kind: tab[b][:, w, kind, :].unsqueeze(1).broadcast_to([128, nh, 32])
                        P.tt("pool", tmp[0][:, 0:nh, :], x0, tb(0), ALU.mult, [qkk, tabk, "a_sq"], ["a_t0"])
                        P.tt("dve", tmp[1][:, 0:nh, :], x1, tb(1), ALU.mult, [qkk, tabk, "a_sq"], ["a_t1"])
                        P.tt("pool", tmp[2][:, 0:nh, :], x0, tb(2), ALU.mult, [qkk, tabk, "a_sq"], ["a_t2"])
                        P.tt("dve", tmp[3][:, 0:nh, :], x1, tb(3), ALU.mult, [qkk, tabk], ["a_t3"])
                        P.tt("dve", qr4[:, h0:h1, :, 0], tmp[0][:, 0:nh, :], tmp[1][:, 0:nh, :], ALU.subtract,
                             ["a_t0", "a_t1"], [qrk])
                        P.tt("pool", qr4[:, h0:h1, :, 1], tmp[2][:, 0:nh, :], tmp[3][:, 0:nh, :], ALU.add,
                             ["a_t2", "a_t3"], [qrk])
                    if cfg.get("a1_lvl", 9) < 4:
                        continue
                    for hh in range(20):
                        bank = 3 + hh // 8
                        pv = ps[bank][:, :].bitcast(BF16)
                        P.tr(pv[0:64, (hh % 8) * 128:(hh % 8 + 1) * 128], qr[b][:, hh, :], identb[:],
                             [qrk, "identb"], [pk(bank)])
                    if cfg.get("a1_lvl", 9) < 5:
                        continue
                    P.cp("act", QTt[b][:, 0:8, :], ps[3][:, :].bitcast(BF16)[0:64, :].rearrange("p (h t) -> p h t", t=128),
                         [pk(3)], [qtk])
                    P.cp("act", QTt[b][:, 8:16, :], ps[4][:, :].bitcast(BF16)[0:64, :].rearrange("p (h t) -> p h t", t=128),
                         [pk(4)], [qtk])
                    P.cp("act", KT[0:64, :, t * 128:(t + 1) * 128],
                         ps[5][:, :].bitcast(BF16)[0:64, 0:512].rearrange("p (h t) -> p h t", t=128),
                         [pk(5)], [("a_KT", t)])
                    if cfg.get("a1_lvl", 9) < 6:
                        continue
                    P.dma("sp", QD[:, :, t * 128:(t + 1) * 128], QTt[b][:], reads=[qtk], writes=[("QD", t)], semkey=qtk)
                keys = ["a_hT", "a_wqkv", "a_gqk", "a_sq"] + ["a_t%d" % b for b in range(4)]
                for b in range(2):
                    keys += ["a_qk%d" % b, "a_ss%d" % b, "a_cs%d" % b, "a_tab%d" % b, "a_qr%d" % b, "a_QTt%d" % b]
                drain(keys)
            with contextlib.ExitStack() as s1:
                if cfg.get("skip_a2"):
                    return
                wo = P.sb(s1, "a_wo", [64, 16, D], BF16)
                P.dma("pool", wo[:], attn_w_o[j].rearrange("(h d) n -> d h n", d=64), writes=["a_wo"])
                for kv_ in range(4):
                    P.memset("pool", KT[64:128, kv_, :], 0.0, ["a_KTz"])
                QTb = [P.sb(s1, "a_QTb%d" % b, [128, 16, 512], BF16) for b in range(2)]
                for b_ in range(2):
                    P.memset("pool", QTb[b_][64:128, :, :], 0.0, ["a_QTbz"])
                aT = [P.sb(s1, "a_aT%d" % b, [64, 16, 512], BF16) for b in range(2)]
                Pb = [P.sb(s1, "a_P%d" % b, [128, 512], BF16) for b in range(3)]
                rec = P.sb(s1, "a_rec", [65, 512])
                bcs = P.sb(s1, "a_bcs", [64, 512])
                xo = [P.sb(s1, "a_xo%d" % b, [128, D]) for b in range(2)]
                tm = [P.sb(s1, "a_tm%d" % b, [128, D]) for b in range(2)]
                qblocks = []
                if ctx_out:
                    qblocks.append((0, CTX, [0, 1]))
                for qb in range(SEQ // 512):
                    qblocks.append((CTX + qb * 512, 512, list(range(NT))))
                pcnt = 0
                xcnt = 0
                for bi, (qo, nq, ktiles) in enumerate(qblocks):
                    b = bi % 2
                    qbk, atk = "a_QTb%d" % b, "a_aT%d" % b
                    P.dma("sp", QTb[b][0:64, :, 0:nq], QD[:, :, qo:qo + nq],
                          reads=[("QD", t) for t in range(qo // 128, (qo + nq) // 128)], writes=[qbk], semkey=qbk)
                    items = [(h, idx, kt) for h in range(NH) for idx, kt in enumerate(ktiles)]
                    LOOK = 2

                    def emit_S(n):
                        h_, idx_, kt_ = items[n]
                        P.mm(ps[n % 3][:, 0:nq], KT[:, h_ // 4, kt_ * 128:(kt_ + 1) * 128], QTb[b][:, h_, 0:nq], True, True,
                             [("a_KT", kt_), "a_KTz", "a_QTbz", qbk], [pk(n % 3)])

                    def fin1(h_):
                        ob_ = 3 + h_ % 2
                        P.recip(rec[64:65, 0:nq], ps[ob_][64:65, 0:nq], [pk(ob_)], ["a_rec"])

                    def fin2(h_):
                        ob_ = 3 + h_ % 2
                        P.mm(ps[5][0:64, 0:nq], ones[64:65, 0:64], rec[64:65, 0:nq], True, True, ["ones", "a_rec"], [pk(5)])
                        P.cp("dve", bcs[:, 0:nq], ps[5][0:64, 0:nq], [pk(5)], ["a_bcs"])
                        P.tt("dve", aT[b][:, h_, 0:nq], ps[ob_][0:64, 0:nq], bcs[:, 0:nq], ALU.mult,
                             [pk(ob_), "a_bcs"], [(atk, h_)])

                    for n in range(min(LOOK, len(items))):
                        emit_S(n)
                    pend = None
                    for n, (h, idx, kt) in enumerate(items):
                        if n + LOOK < len(items):
                            emit_S(n + LOOK)
                        kv = h // 4
                        ob = 3 + h % 2
                        pb = n % 3
                        P.act(Pb[pb][:, 0:nq], ps[n % 3][:, 0:nq], AF.Exp, [pk(n % 3)], ["a_P%d" % pb], scale=HD ** -0.5)
                        P.mm(ps[ob][0:65, 0:nq], Vg[:, kt, kv, 0:65], Pb[pb][:, 0:nq], idx == 0, idx == len(ktiles) - 1,
                             [("a_V", kt), "a_V", "a_P%d" % pb], [pk(ob)])
                        if pend is not None and (idx == min(3, len(ktiles) - 1)):
                            fin2(pend)
                            pend = None
                        if idx == len(ktiles) - 1:
                            fin1(h)
                            pend = h
                    if pend is not None:
                        fin2(pend)
                    for tt_ in range(nq // 128):
                        t = qo // 128 + tt_
                        s = 1 if t < 2 else 0
                        xb = xcnt % 2
                        xcnt += 1
                        xok, tmk = "a_xo%d" % xb, "a_tm%d" % xb
                        P.dma("sp", xo[xb][:], X[t * 128:(t + 1) * 128, :], reads=[xk(t)], writes=[xok], semkey=xok)
                        for half in range(2):
                            bank = 6 + half
                            for h in range(NH):
                                P.mm(ps[bank][:, :], aT[b][:, h, tt_ * 128:(tt_ + 1) * 128],
                                     wo[:, h, half * 512:(half + 1) * 512], h == 0, h == NH - 1,
                                     [(atk, h), "a_wo"], [pk(bank)])
                            P.tt("dve", tm[xb][:, half * 512:(half + 1) * 512], ps[bank][:, :],
                                 gates[:, 0, s, half * 512:(half + 1) * 512], ALU.mult,
                                 [pk(bank), ("gates", 0, s)], [tmk])
                        P.tt("pool", xo[xb][:], xo[xb][:], tm[xb][:], ALU.add, [xok, tmk], [xok])
                        P.dma("sp", X[t * 128:(t + 1) * 128, :], xo[xb][:], reads=[xok], writes=[xk(t)], semkey=xok)
                if dbg is not None:
                    dt_ = tm[0]
                    P.cp("dve", dt_[:, 0:512], KT[:, 0, 0:512], ["a_KTz"] + [("a_KT", t) for t in range(4)], ["a_dbgt", "a_tm0"])
                    P.cp("dve", dt_[:, 512:1024], QTb[0][:, 0, 0:512], ["a_QTbz", "a_QTb0"], ["a_dbgt", "a_tm0"])
                    P.dma("sp", dbg, dt_[:], reads=["a_dbgt"], writes=["dbg"])
                    drain(["a_dbgt", "a_tm0"])
                keys = ["a_wo", "a_rec", "a_bcs", "a_V", "a_KTz", "a_QTbz"] + ["a_P%d" % b for b in range(3)]
                for b in range(2):
                    keys += ["a_QTb%d" % b, "a_xo%d" % b, "a_tm%d" % b] + [("a_aT%d" % b, h) for h in range(NH)]
                keys += [("a_KT", t) for t in range(NT)] + [("a_V", t) for t in range(NT)]
                drain(keys)


    def pool_mixer(i):
        with contextlib.ExitStack() as st:
            bcm = P.sb(st, "p_bcm", [128, 2, 2, D])
            gmb = P.sb(st, "p_gmb", [128, 2, D])
            psg = P.sb(st, "p_psg", [128, 2, D])
            band = P.sb(st, "p_band", [128, 4, 5, 128])
            wp = P.sb(st, "p_wp", [128, 4, 2, 256])
            P.dma("sp", band[:], k_band.rearrange("w k p n -> p w k n"), writes=["p_band"])
            P.dma("sp", wp[:], pool_w[0].rearrange("g (cc p) n -> p g cc n", p=128), writes=["p_wp"])
            with contextlib.ExitStack() as s1:
                wblk = [P.sb(s1, "p_wblk%d" % b, [128, 8, 512]) for b in range(2)]
                cbc = P.sb(s1, "p_cbc", [128, 2, 8, 128])
                brow = P.sb(s1, "p_brow", [1, 2 * D])
                gb = P.sb(s1, "p_gb", [128, D])
                psb = P.sb(s1, "p_psb", [128, D])
                P.dma("sp", brow[:], b_ada[i][0:1, 0:2 * D], writes=["p_brow"])
                P.dma("sp", gb[:], norm_mix_g[i:i + 1, :].broadcast_to([128, D]), writes=["p_gb"])
                P.dma("sp", psb[:], pool_scale[0:1, :].broadcast_to([128, D]), writes=["p_psb"])
                for s_ in range(2):
                    P.cp("dve", cbc[:, s_], cT[:, :, s_:s_ + 1].broadcast_to([128, 8, 128]), ["cT"], ["p_cbc"])
                for n in range(4):
                    wb, wkey = wblk[n % 2], "p_wblk%d" % (n % 2)
                    P.dma("sp", wb[:], w_ada[i][:, n * 512:(n + 1) * 512].rearrange("(k p) n -> p k n", p=128),
                          writes=[wkey])
                    for s_ in range(2):
                        bank = 1 + s_
                        for k in range(8):
                            P.mm(ps[bank][:, :], cbc[:, s_, k, :], wb[:, k, :], k == 0, False, [wkey, "p_cbc"], [pk(bank)])
                        P.mm(ps[bank][:, :], ones[0:1, :], brow[0:1, n * 512:(n + 1) * 512], False, True,
                             ["ones", "p_brow"], [pk(bank)])
                        P.cp("act", bcm[:, s_, n // 2, (n % 2) * 512:(n % 2 + 1) * 512], ps[bank][:, :],
                             [pk(bank)], ["p_bcm"])
                for s_ in range(2):
                    P.stt("dve", gmb[:, s_, :], bcm[:, s_, 1, :], 1.0, gb[:], ALU.add, ALU.mult, ["p_bcm", "p_gb"], ["p_gmb"])
                    P.tt("pool", psg[:, s_, :], psb[:], gates[:, 0, s_, :], ALU.mult, ["p_psb", ("gates", 0, s_)], ["p_psg"])
                drain(["p_wblk0", "p_wblk1", "p_cbc", "p_brow", "p_gb", "p_psb"])
            xt = [P.sb(st, "p_x%d" % b, [128, D]) for b in range(4)]
            hh = [P.sb(st, "p_h%d" % b, [128, D]) for b in range(4)]
            dT = [P.sb(st, "p_dT%d" % b, [128, 8, 128]) for b in range(2)]
            tm = [P.sb(st, "p_tm%d" % b, [128, D]) for b in range(2)]
            st8 = [P.sb(st, "p_st%d" % b, [128, 4]) for b in range(4)]
            junk = P.sb(st, "p_junk", [128, D], BF16)

            def compute_h(t):
                b = t % 4
                s_ = 1 if t < 2 else 0
                xkey, hkey, skey = "p_x%d" % b, "p_h%d" % b, "p_st%d" % b
                P.dma("sp", xt[b][:], X[t * 128:(t + 1) * 128, :], reads=[xk(t)], writes=[xkey], semkey=xkey)
                P.act(junk[:], xt[b][:], AF.Square, [xkey], ["p_junk", skey], accum_out=st8[b][:, 0:1])
                P.act(st8[b][:, 1:2], st8[b][:, 0:1], AF.Sqrt, [skey], [skey], bias=EPS, scale=1.0 / D)
                P.recip(st8[b][:, 2:3], st8[b][:, 1:2], [skey], [skey])
                P.stt("dve", hh[b][:], xt[b][:], st8[b][:, 2:3], gmb[:, s_, :], ALU.mult, ALU.mult,
                      [xkey, skey, "p_gmb"], [hkey])
                P.tt("pool", hh[b][:], hh[b][:], bcm[:, s_, 0, :], ALU.add, [hkey, "p_bcm"], [hkey])

            done = set()
            for t in range(NT):
                first = t in (0, 2)
                last = t in (1, NT - 1)
                need = [t] + ([] if first else [t - 1]) + ([] if last else [t + 1])
                for tt_ in sorted(need):
                    if tt_ not in done:
                        compute_h(tt_)
                        done.add(tt_)
                s_ = 1 if t < 2 else 0
                db = t % 2
                dkey, tmk = "p_dT%d" % db, "p_tm%d" % db
                for c in range(8):
                    wi = c // 2
                    bank = c // 4
                    col = (c % 4) * 128
                    srcs = []
                    if not first:
                        srcs.append((t - 1, 0))
                    srcs.append((t, 1 if first else (3 if last else 2)))
                    if not last:
                        srcs.append((t + 1, 4))
                    for si, (tt_, kind) in enumerate(srcs):
                        P.mm(ps[bank][:, col:col + 128], hh[tt_ % 4][:, c * 128:(c + 1) * 128], band[:, wi, kind, :],
                             si == 0, si == len(srcs) - 1, ["p_h%d" % (tt_ % 4), "p_band"], [pk(bank)])
                for bank in range(2):
                    P.cp("act", dT[db][:, bank * 4:(bank + 1) * 4, :], ps[bank][:, :].rearrange("p (c t) -> p c t", t=128),
                         [pk(bank)], [dkey])
                for g in range(4):
                    bank = 2 + g // 2
                    for cc in range(2):
                        P.mm(ps[bank][:, (g % 2) * 256:(g % 2 + 1) * 256], dT[db][:, 2 * g + cc, :], wp[:, g, cc, :],
                             cc == 0, cc == 1, [dkey, "p_wp"], [pk(bank)])
                xb = t % 4
                for half in range(2):
                    P.tt("dve", tm[db][:, half * 512:(half + 1) * 512], ps[2 + half][:, :],
                         psg[:, s_, half * 512:(half + 1) * 512], ALU.mult, [pk(2 + half), "p_psg"], [tmk])
                P.tt("pool", tm[db][:], tm[db][:], xt[xb][:], ALU.add, [tmk, "p_x%d" % xb], [tmk])
                P.dma("sp", X[t * 128:(t + 1) * 128, :], tm[db][:], reads=[tmk], writes=[xk(t)], semkey=tmk)
            keys = ["p_bcm", "p_gmb", "p_psg", "p_band", "p_wp", "p_junk", "p_dT0", "p_dT1", "p_tm0", "p_tm1"]
            for b in range(4):
                keys += ["p_x%d" % b, "p_h%d" % b, "p_st%d" % b]
            drain(keys)


    def ssd_mixer(i):
        XT = P.dscratch("s_XT", [T, SSM_DI], BF16)
        BTK = P.dscratch("s_BTK", [T, 512], BF16)
        BF = P.dscratch("s_BF", [4, 128, T], BF16)
        CF = P.dscratch("s_CF", [4, 128, T], BF16)
        Yd = P.dscratch("s_Yd", [2, T, SSM_DI])
        w_in = ssm_w_in[0]
        NU = T + 3

        def xcol(t):
            return t * 128 if t < 2 else 259 + (t - 2) * 128

        ZG = P.dscratch("s_ZG", [T, SSM_DI], BF16)
        with contextlib.ExitStack() as st:
            with contextlib.ExitStack() as sA:
                dtA = P.sb(sA, "s_dtA", [128, NT, 64])
                LA = P.sb(sA, "s_LA", [128, NT, 64])
                sH = contextlib.ExitStack()
                hT = P.sb(sH, "s_hT", [128, 8, T], BF16)
                with contextlib.ExitStack() as s1:
                    wdt = P.sb(s1, "s_wdt", [128, 8, 64])
                    dtb = P.sb(s1, "s_dtb", [128, 64])
                    aB = P.sb(s1, "s_aB", [128, 64])
                    sp_ = P.sb(s1, "s_sp", [128, 4, 64])
                    P.dma("sp", wdt[:], w_in[:, 5120:5184].rearrange("(k p) n -> p k n", p=128), writes=["s_wdt"])
                    P.dma("sp", dtb[:], ssm_dt_bias[0:1, :].broadcast_to([128, 64]), writes=["s_dtb"])
                    P.dma("sp", aB[:], ssm_a_log[0:1, :].broadcast_to([128, 64]), writes=["s_aB"])
                    P.act(aB[:], aB[:], AF.Exp, ["s_aB"], ["s_aB"])
                    P.ts("dve", aB[:], aB[:], -1.0, ALU.mult, ["s_aB"], ["s_aB"])

                    def dthook(idx, t, h32, hkey):
                        for k in range(8):
                            P.mm(ps[5][:, 0:64], h32[:, k, :], wdt[:, k, :], k == 0, k == 7, [hkey, "s_wdt"], [pk(5)])
                        K_ = "s_sp"
                        xr, ab, ee = sp_[:, 0, :], sp_[:, 1, :], sp_[:, 2, :]
                        P.tt("dve", xr, ps[5][:, 0:64], dtb[:], ALU.add, [pk(5), "s_dtb"], [K_])
                        P.ts("dve", sp_[:, 3, :], xr, -1.0, ALU.mult, [K_], [K_])
                        P.tt("dve", ab, xr, sp_[:, 3, :], ALU.max, [K_], [K_])
                        P.act(ee, ab, AF.Exp, [K_], [K_], scale=-1.0)
                        P.act(ee, ee, AF.Ln, [K_], [K_], bias=1.0, scale=1.0)
                        P.stt("dve", dtA[:, t, :], xr, 0.0, ee, ALU.max, ALU.add, [K_], [("s_dtA", t)])
                        P.tt("pool", LA[:, t, :], dtA[:, t, :], aB[:], ALU.mult, [("s_dtA", t), "s_aB"], [("s_LA", t)])

                    with contextlib.ExitStack() as s2:
                        norm_tiles(s2, ALL, 0, hT, "s_hT", 0, hook=dthook, tag="sn")
                        drain(["sn_xt0", "sn_xt1", "sn_junk", "sn_st0", "sn_st1", "sn_h320", "sn_h321"])
                    drain(["s_wdt", "s_dtb", "s_sp"])
                with contextlib.ExitStack() as s1:
                    cwA = P.sb(s1, "s_cwA", [128, 24, 4])
                    cbA = P.sb(s1, "s_cbA", [128, 24])
                    U = P.sb(s1, "s_U", [128, T + 8])
                    acc = P.sb(s1, "s_acc", [128, NU])
                    xc = [P.sb(s1, "s_xc%d" % b, [128, NU], BF16) for b in range(2)]
                    wc = [P.sb(s1, "s_wc%d" % b, [128, 8, 128], BF16) for b in range(2)]
                    stg = [P.sb(s1, "s_stg%d" % b, [128, NT, 128], BF16) for b in range(2)]
                    for k in range(4):
                        P.dma("sp", cwA[:, :, k], ssm_conv_w[0, k].rearrange("(c p) -> p c", p=128), writes=["s_cwA"],
                              allow_slow_non_contiguous=True)
                    P.dma("sp", cbA[:], ssm_conv_b[0].rearrange("(c p) -> p c", p=128), writes=["s_cbA"],
                          allow_slow_non_contiguous=True)
                    P.memset("dve", U[:], 0.0, ["s_U"])
                    blocks = [(0, 256, 2)] + [(256 + b * 512, 512, 261 + b * 512) for b in range(8)]
                    mcnt = 0
                    for cc in range(24):
                        b = cc % 2
                        wck, xck, stk = "s_wc%d" % b, "s_xc%d" % b, "s_stg%d" % b
                        P.dma("pool", wc[b][:], w_in[:, 2048 + cc * 128:2048 + (cc + 1) * 128].rearrange("(k p) n -> p k n", p=128),
                              writes=[wck])
                        for (t0, n_, uo) in blocks:
                            bank = mcnt % 2
                            mcnt += 1
                            for k in range(8):
                                P.mm(ps[bank][:, 0:n_], wc[b][:, k, :], hT[:, k, t0:t0 + n_], k == 0, k == 7,
                                     [wck, "s_hT"], [pk(bank)])
                            P.cp("act", U[:, uo:uo + n_], ps[bank][:, 0:n_], [pk(bank)], ["s_U"])
                        ce = "dve"
                        P.ts(ce, acc[:], U[:, 0:NU], cwA[:, cc, 0:1], ALU.mult, ["s_U", "s_cwA"], ["s_acc"])
                        for k in range(1, 4):
                            P.stt(ce, acc[:], U[:, k:k + NU], cwA[:, cc, k:k + 1], acc[:], ALU.mult, ALU.add,
                                  ["s_U", "s_cwA", "s_acc"], ["s_acc"])
                        P.act(xc[b][:], acc[:], AF.Silu, ["s_acc", "s_cbA"], [xck], bias=cbA[:, cc:cc + 1], scale=1.0)
                        if cc >= 16:
                            g = (cc - 16) % 4
                            dst = BF if cc < 20 else CF
                            dk = "BF" if cc < 20 else "CF"
                            P.dma("sp", dst[g, :, 0:CTX], xc[b][:, 0:CTX], reads=[xck], writes=[(dk, g)], semkey=xck)
                            P.dma("sp", dst[g, :, CTX:T], xc[b][:, 259:259 + SEQ], reads=[xck], writes=[(dk, g)], semkey=xck)
                        if cc < 20:
                            for t in range(NT):
                                bank = 2 + (t // 8) % 4
                                pv = ps[bank][:, :].bitcast(BF16)
                                P.tr(pv[:, (t % 8) * 128:(t % 8 + 1) * 128], xc[b][:, xcol(t):xcol(t) + 128], identb[:],
                                     [xck, "identb"], [pk(bank)])
                                if t % 8 == 7 or t == NT - 1:
                                    t0 = (t // 8) * 8
                                    nt_ = t - t0 + 1
                                    P.cp("act", stg[b][:, t0:t0 + nt_, :],
                                         pv[:, 0:nt_ * 128].rearrange("p (t c) -> p t c", c=128), [pk(bank)], [stk])
                            if cc < 16:
                                dv = XT.rearrange("(t p) c -> p t c", p=128)[:, :, cc * 128:(cc + 1) * 128]
                                wk = ("XT", cc)
                            else:
                                dv = BTK.rearrange("(t p) c -> p t c", p=128)[:, :, (cc - 16) * 128:(cc - 15) * 128]
                                wk = ("BTK", cc - 16)
                            P.dma("sp", dv[:, 0:17, :], stg[b][:, 0:17, :], reads=[stk], writes=[wk], semkey=stk)
                            P.dma("sp", dv[:, 17:NT, :], stg[b][:, 17:NT, :], reads=[stk], writes=[wk], semkey=stk)
                    drain(["s_cwA", "s_cbA", "s_U", "s_acc", "s_xc0", "s_xc1", "s_wc0", "s_wc1", "s_stg0", "s_stg1"])
                with contextlib.ExitStack() as s1:
                    wz = P.sb(s1, "s_wz", [128, 8, SSM_DI], BF16)
                    szb = [P.sb(s1, "s_szb%d" % b, [128, SSM_DI], BF16) for b in range(2)]
                    P.dma("pool", wz[:], w_in[:, 0:SSM_DI].rearrange("(k p) n -> p k n", p=128), writes=["s_wz"])
                    for t in range(NT):
                        b = t % 2
                        for nb in range(4):
                            bank = (t * 4 + nb) % 8
                            for k in range(8):
                                P.mm(ps[bank][:, :], hT[:, k, t * 128:(t + 1) * 128], wz[:, k, nb * 512:(nb + 1) * 512],
                                     k == 0, k == 7, ["s_hT", "s_wz"], [pk(bank)])
                            P.act(szb[b][:, nb * 512:(nb + 1) * 512], ps[bank][:, :], AF.Silu, [pk(bank)], ["s_szb%d" % b])
                        P.dma("sp", ZG[t * 128:(t + 1) * 128, :], szb[b][:], reads=["s_szb%d" % b], writes=[("ZG", t)],
                              semkey="s_szb%d" % b)
                    drain(["s_wz", "s_szb0", "s_szb1", "s_hT"])
                sH.close()
                with contextlib.ExitStack() as s1:
                    tri = P.sb(s1, "s_tri", [128, 4, 128])
                    state = P.sb(s1, "s_state", [128, 32, 64])
                    stbf = P.sb(s1, "s_stbf", [128, 32, 64], BF16)
                    xk_ = [P.sb(s1, "s_xk%d" % b, [128, 32, 64], BF16) for b in range(2)]
                    btk = [P.sb(s1, "s_btk%d" % b, [128, 512], BF16) for b in range(2)]
                    bfc = [P.sb(s1, "s_bfc%d" % b, [128, 4, 128], BF16) for b in range(2)]
                    cfc = [P.sb(s1, "s_cfc%d" % b, [128, 4, 128], BF16) for b in range(2)]
                    xdt = P.sb(s1, "s_xdt", [128, 32, 64], BF16)
                    xsd = P.sb(s1, "s_xsd", [128, 32, 64], BF16)
                    laB = P.sb(s1, "s_laB", [128, 32, 128])
                    CM = P.sb(s1, "s_CM", [128, 32, 128])
                    sm = P.sb(s1, "s_sm", [128, 6, 32])
                    GTs = [P.sb(s1, "s_GTs%d" % b, [128, 128]) for b in range(2)]
                    Lg = [P.sb(s1, "s_Lg%d" % b, [128, 4, 128]) for b in range(2)]
                    WT = [P.sb(s1, "s_WT%d" % b, [128, 4, 128], BF16) for b in range(2)]
                    yoff = [P.sb(s1, "s_yoff%d" % b, [128, 8, 64]) for b in range(2)]
                    ybuf = [P.sb(s1, "s_ybuf%d" % b, [128, 32, 64]) for b in range(2)]
                    P.dma("sp", tri[:], k_tri.rearrange("w p n -> p w n"), writes=["s_tri"])
                    ccount = 0
                    for d in range(2):
                        order = list(range(NT)) if d == 0 else [1, 0] + list(range(NT - 1, 1, -1))
                        Ut, Mneg = tri[:, 2 * d, :], tri[:, 2 * d + 1, :]
                        P.memset("pool", state[:], 0.0, [("s_state", g_) for g_ in range(4)])
                        for c in order:
                            b = ccount % 2
                            ccount += 1
                            xkk, btkk, bfk, cfk, ybk = "s_xk%d" % b, "s_btk%d" % b, "s_bfc%d" % b, "s_cfc%d" % b, "s_ybuf%d" % b
                            P.dma("sp", xk_[b][:], XT[c * 128:(c + 1) * 128, :].rearrange("p (h d) -> p h d", d=64),
                                  reads=[("XT", q) for q in range(16)], writes=[xkk], semkey=xkk)
                            P.dma("sp", btk[b][:], BTK[c * 128:(c + 1) * 128, :], reads=[("BTK", q) for q in range(4)],
                                  writes=[btkk], semkey=btkk)
                            P.dma("sp", bfc[b][:], BF[:, :, c * 128:(c + 1) * 128].rearrange("g n t -> n g t"),
                                  reads=[("BF", q) for q in range(4)], writes=[bfk], semkey=bfk)
                            P.dma("sp", cfc[b][:], CF[:, :, c * 128:(c + 1) * 128].rearrange("g n t -> n g t"),
                                  reads=[("CF", q) for q in range(4)], writes=[cfk], semkey=cfk)
                            la_c = LA[:, c, d * 32:(d + 1) * 32]
                            dt_c = dtA[:, c, d * 32:(d + 1) * 32]
                            lak, dtk = ("s_LA", c), ("s_dtA", c)
                            SM = "s_sm"
                            csc, tot, ff, fx, ecs, dec = (sm[:, q, :] for q in range(6))
                            P.mm(ps[0][:, 0:32], Ut, la_c, True, True, ["s_tri", lak], [pk(0)])
                            P.mm(ps[0][:, 32:64], ones[:], la_c, True, True, ["ones", lak], [pk(0)])
                            P.cp("dve", sm[:, 0:2, :], ps[0][:, 0:64].rearrange("p (a h) -> p a h", h=32), [pk(0)], [SM])
                            P.tt("dve", ff, tot, csc, ALU.subtract, [SM], [SM])
                            P.act(ff, ff, AF.Exp, [SM], [SM])
                            P.act(ecs, csc, AF.Exp, [SM], [SM])
                            P.act(dec, tot, AF.Exp, [SM], [SM])
                            P.tt("dve", fx, ff, dt_c, ALU.mult, [SM, dtk], [SM])
                            P.tt("pool", xdt[:], xk_[b][:], dt_c.unsqueeze(2).broadcast_to([128, 32, 64]), ALU.mult,
                                 [xkk, dtk], ["s_xdt"])
                            P.tt("pool", xsd[:], xk_[b][:], fx.unsqueeze(2).broadcast_to([128, 32, 64]), ALU.mult,
                                 [xkk, SM], ["s_xsd"])
                            P.cp("dve", laB[:], la_c.unsqueeze(2).broadcast_to([128, 32, 128]), [lak], ["s_laB"])
                            P.cp("act", stbf[:], state[:], [("s_state", g_) for g_ in range(4)], ["s_stbf"])
                            P.tt("dve", CM[:], csc.unsqueeze(2).broadcast_to([128, 32, 128]),
                                 Mneg.unsqueeze(1).broadcast_to([128, 32, 128]), ALU.subtract, [SM, "s_tri"], ["s_CM"])
                            def grp_begin(g):
                                P.mm(ps[1][:, 0:128], bfc[b][:, g, :], cfc[b][:, g, :], True, True, [bfk, cfk], [pk(1)])
                                P.cp("act", GTs[g % 2][:], ps[1][:, 0:128], [pk(1)], ["s_GTs%d" % (g % 2)])
                                P.mm(ps[2][:, :], cfc[b][:, g, :], stbf[:, g * 8:(g + 1) * 8, :].rearrange("p h d -> p (h d)"),
                                     True, True, [cfk, "s_stbf"], [pk(2)])
                                P.cp("act", yoff[g % 2][:], ps[2][:, :].rearrange("p (h d) -> p h d", d=64), [pk(2)],
                                     ["s_yoff%d" % (g % 2)])

                            def quad_cs(n):
                                g, q = n // 2, n % 2
                                if q == 0:
                                    grp_begin(g)
                                cb_ = 3 + n % 2
                                for j in range(4):
                                    P.mm(ps[cb_][:, j * 128:(j + 1) * 128], laB[:, g * 8 + q * 4 + j, :], Ut, True, True,
                                         ["s_laB", "s_tri"], [pk(cb_)])

                            def grp_end(g):
                                yb_ = 5 + g % 2
                                yo, yok = yoff[g % 2], "s_yoff%d" % (g % 2)
                                P.tt("pool", yo[:], yo[:], ecs[:, g * 8:(g + 1) * 8].unsqueeze(2).broadcast_to([128, 8, 64]),
                                     ALU.mult, [yok, SM], [yok])
                                P.tt("dve", ybuf[b][:, g * 8:(g + 1) * 8, :], yo[:],
                                     ps[yb_][:, :].rearrange("p (h d) -> p h d", d=64), ALU.add, [yok, pk(yb_)], [ybk])
                                P.mm(ps[7][:, :], btk[b][:, g * 128:(g + 1) * 128],
                                     xsd[:, g * 8:(g + 1) * 8, :].rearrange("p h d -> p (h d)"), True, True,
                                     [btkk, "s_xsd"], [pk(7)])
                                P.tt("pool", state[:, g * 8:(g + 1) * 8, :], state[:, g * 8:(g + 1) * 8, :],
                                     dec[:, g * 8:(g + 1) * 8].unsqueeze(2).broadcast_to([128, 8, 64]), ALU.mult,
                                     [("s_state", g), SM], [("s_state", g)])
                                P.tt("dve", state[:, g * 8:(g + 1) * 8, :], state[:, g * 8:(g + 1) * 8, :],
                                     ps[7][:, :].rearrange("p (h d) -> p h d", d=64), ALU.add, [("s_state", g), pk(7)],
                                     [("s_state", g)])

                            quad_cs(0)
                            for n in range(8):
                                g, q = n // 2, n % 2
                                h0 = g * 8 + q * 4
                                if n + 1 < 8:
                                    quad_cs(n + 1)
                                cb_ = 3 + n % 2
                                lb = n % 2
                                lgk, wtk = "s_Lg%d" % lb, "s_WT%d" % lb
                                csv = ps[cb_][:, :].rearrange("p (j l) -> p j l", l=128)
                                P.tt("dve", Lg[lb][:], csv, CM[:, h0:h0 + 4, :], ALU.subtract, [pk(cb_), "s_CM"], [lgk])
                                P.act(Lg[lb][:], Lg[lb][:], AF.Exp, [lgk], [lgk])
                                P.tt("dve", WT[lb][:], Lg[lb][:], GTs[g % 2][:].unsqueeze(1).broadcast_to([128, 4, 128]),
                                     ALU.mult, [lgk, "s_GTs%d" % (g % 2)], [wtk])
                                yb_ = 5 + g % 2
                                for j in range(4):
                                    P.mm(ps[yb_][:, (q * 4 + j) * 64:(q * 4 + j + 1) * 64], WT[lb][:, j, :], xdt[:, h0 + j, :],
                                         True, True, [wtk, "s_xdt"], [pk(yb_)])
                                if q == 1:
                                    grp_end(g)
                            P.dma("sp", Yd[d, c * 128:(c + 1) * 128, :], ybuf[b][:].rearrange("p h d -> p (h d)"),
                                  reads=[ybk], writes=[("Yd", d, c)], semkey=ybk)
                    keys = ["s_tri", "s_stbf", "s_xdt", "s_xsd", "s_laB", "s_CM", "s_sm", "s_GTs0", "s_GTs1", "s_yoff0", "s_yoff1"]
                    keys += [("s_state", g_) for g_ in range(4)]
                    for b in range(2):
                        keys += ["s_xk%d" % b, "s_btk%d" % b, "s_bfc%d" % b, "s_cfc%d" % b, "s_Lg%d" % b, "s_WT%d" % b,
                                 "s_ybuf%d" % b]
                    keys += [("s_dtA", t) for t in range(NT)] + [("s_LA", t) for t in range(NT)] + ["s_aB"]
                    drain(keys)
            with contextlib.ExitStack() as s1:
                wo = P.sb(s1, "s_wo", [128, 16, D], BF16)
                ng = P.sb(s1, "s_ng", [128, SSM_DI])
                dsk = P.sb(s1, "s_dsk", [128, 64])
                yf = [P.sb(s1, "s_yf%d" % b, [128, 32, 64]) for b in range(2)]
                yb2 = [P.sb(s1, "s_yb2%d" % b, [128, 32, 64]) for b in range(2)]
                xk3 = [P.sb(s1, "s_xk3%d" % b, [128, 32, 64], BF16) for b in range(2)]
                zg = [P.sb(s1, "s_zg%d" % b, [128, SSM_DI], BF16) for b in range(2)]
                xo = [P.sb(s1, "s_xo%d" % b, [128, D]) for b in range(2)]
                sz_ = [P.sb(s1, "s_sz%d" % b, [128, SSM_DI]) for b in range(2)]
                gbf_ = [P.sb(s1, "s_gbf%d" % b, [128, SSM_DI], BF16) for b in range(2)]
                gT = [P.sb(s1, "s_gT%d" % b, [128, 16, 128], BF16) for b in range(2)]
                g4_ = [P.sb(s1, "s_g4%d" % b, [128, 12]) for b in range(2)]
                junk = P.sb(s1, "s_junk3", [128, 512], BF16)
                tm = [P.sb(s1, "s_tm%d" % b, [128, D]) for b in range(2)]
                P.dma("pool", wo[:], ssm_w_out[0].rearrange("(k p) n -> p k n", p=128), writes=["s_wo"])
                P.dma("sp", ng[:], ssm_norm_g[0:1, :].broadcast_to([128, SSM_DI]), writes=["s_ng"])
                P.dma("sp", dsk[:], ssm_d[0:1, :].broadcast_to([128, 64]), writes=["s_dsk"])
                P.tt("dve", dsk[:, 0:32], dsk[:, 0:32], dsk[:, 32:64], ALU.add, ["s_dsk"], ["s_dsk"])

                def s3load(t):
                    b = t % 2
                    P.dma("sp", yf[b][:], Yd[0, t * 128:(t + 1) * 128, :].rearrange("p (h d) -> p h d", d=64),
                          reads=[("Yd", 0, t)], writes=["s_yf%d" % b], semkey="s_yf%d" % b)
                    P.dma("sp", yb2[b][:], Yd[1, t * 128:(t + 1) * 128, :].rearrange("p (h d) -> p h d", d=64),
                          reads=[("Yd", 1, t)], writes=["s_yb2%d" % b], semkey="s_yb2%d" % b)
                    P.dma("sp", xk3[b][:], XT[t * 128:(t + 1) * 128, :].rearrange("p (h d) -> p h d", d=64),
                          reads=[("XT", q) for q in range(16)], writes=["s_xk3%d" % b], semkey="s_xk3%d" % b)
                    P.dma("sp", zg[b][:], ZG[t * 128:(t + 1) * 128, :], reads=[("ZG", t)], writes=["s_zg%d" % b],
                          semkey="s_zg%d" % b)
                    P.dma("sp", xo[b][:], X[t * 128:(t + 1) * 128, :], reads=[xk(t)], writes=["s_xo%d" % b], semkey="s_xo%d" % b)

                s3load(0)
                for t in range(NT):
                    if t + 1 < NT:
                        s3load(t + 1)
                    b = t % 2
                    s_ = 1 if t < 2 else 0
                    yfk, ybk, xkk, zgk, xok, gtk, tmk = ("s_yf%d" % b, "s_yb2%d" % b, "s_xk3%d" % b, "s_zg%d" % b, "s_xo%d" % b,
                                                         "s_gT%d" % b, "s_tm%d" % b)
                    P.tt("dve", yf[b][:], yf[b][:], yb2[b][:], ALU.add, [yfk, ybk], [yfk])
                    P.tt("pool", yb2[b][:], xk3[b][:], dsk[:, 0:32].unsqueeze(2).broadcast_to([128, 32, 64]), ALU.mult,
                         [xkk, "s_dsk"], [ybk])
                    P.tt("pool", yf[b][:], yf[b][:], yb2[b][:], ALU.add, [yfk, ybk], [yfk])
                    sz, gbf, g4 = sz_[b], gbf_[b], g4_[b]
                    szk, gbk, g4k = "s_sz%d" % b, "s_gbf%d" % b, "s_g4%d" % b
                    yfl = yf[b][:].rearrange("p h d -> p (h d)")
                    P.tt("dve", sz[:], zg[b][:], yfl, ALU.mult, [zgk, yfk], [szk])
                    for q in range(4):
                        P.act(junk[:], sz[:, q * 512:(q + 1) * 512], AF.Square, [szk], ["s_junk3", g4k],
                              accum_out=g4[:, q:q + 1])
                    P.act(g4[:, 4:8], g4[:, 0:4], AF.Sqrt, [g4k], [g4k], bias=EPS, scale=1.0 / 512)
                    P.recip(g4[:, 8:12], g4[:, 4:8], [g4k], [g4k])
                    for q in range(4):
                        P.stt("dve", gbf[:, q * 512:(q + 1) * 512], sz[:, q * 512:(q + 1) * 512], g4[:, 8 + q:9 + q],
                              ng[:, q * 512:(q + 1) * 512], ALU.mult, ALU.mult, [szk, g4k, "s_ng"], [gbk])
                    for k in range(16):
                        bank = (0 if b == 0 else 2) + k // 8
                        pv = ps[bank][:, :].bitcast(BF16)
                        P.tr(pv[:, (k % 8) * 128:(k % 8 + 1) * 128], gbf[:, k * 128:(k + 1) * 128], identb[:],
                             [gbk, "identb"], [pk(bank)])
                    for q in range(2):
                        bank = (0 if b == 0 else 2) + q
                        P.cp("act", gT[b][:, q * 8:(q + 1) * 8, :],
                             ps[bank][:, :].bitcast(BF16).rearrange("p (k t) -> p k t", t=128), [pk(bank)], [gtk])
                    for half in range(2):
                        bank = 4 + 2 * b + half
                        for k in range(16):
                            P.mm(ps[bank][:, :], gT[b][:, k, :], wo[:, k, half * 512:(half + 1) * 512], k == 0, k == 15,
                                 [gtk, "s_wo"], [pk(bank)])
                        P.tt("dve", tm[b][:, half * 512:(half + 1) * 512], ps[bank][:, :],
                             gates[:, 0, s_, half * 512:(half + 1) * 512], ALU.mult, [pk(bank), ("gates", 0, s_)], [tmk])
                    P.tt("pool", xo[b][:], xo[b][:], tm[b][:], ALU.add, [xok, tmk], [xok])
                    P.dma("sp", X[t * 128:(t + 1) * 128, :], xo[b][:], reads=[xok], writes=[xk(t)], semkey=xok)
                keys = ["s_wo", "s_ng", "s_dsk", "s_junk3"]
                for b in range(2):
                    keys += ["s_sz%d" % b, "s_gbf%d" % b, "s_g4%d" % b]
                    keys += ["s_yf%d" % b, "s_yb2%d" % b, "s_xk3%d" % b, "s_zg%d" % b, "s_xo%d" % b, "s_gT%d" % b, "s_tm%d" % b]
                drain(keys)

    def moe_sparse(i, tiles):
        ntl = len(tiles)
        Hs = P.dscratch("ms_Hs%d" % i, [NSLOT, D], BF16)
        Z = P.dscratch("ms_Z%d" % i, [NSLOT, D])
        wgu_rows = moe_w_gu[i]
        wdn_rows = moe_w_dn[i]
        IOA = bass.IndirectOffsetOnAxis
        with contextlib.ExitStack() as st:
            WW = P.sb(st, "q_WW", [128, ntl, 2])
            POSI = P.sb(st, "q_POSI", [128, ntl, 2], I32)
            OFFGU = P.sb(st, "q_OFFGU", [128, NBLK], I32)
            pst = P.sb(st, "q_pst", [128, NE])
            km = P.sb(st, "q_km", [128, 128])
            P.dma("sp", km[:], k_moe, writes=["q_km"])
            with contextlib.ExitStack() as s1:
                HTOK = P.sb(s1, "q_HTOK", [128, ntl, D], BF16)
                OH = P.sb(s1, "q_OH", [128, ntl, 3, NE])
                wr = P.sb(s1, "q_wr", [128, 8, 36])
                rt = P.sb(s1, "q_rt", [128, 160])
                hT2 = P.sb(s1, "q_hT2", [128, 8, 256], BF16)
                P.dma("sp", wr[:], moe_wr[i].rearrange("(k p) n -> p k n", p=128), writes=["q_wr"])

                LG = P.sb(s1, "q_LG", [128, ntl, 36])

                def router(idx, t, h32, hkey):
                    lg = ps[5]
                    for k in range(8):
                        P.mm(lg[:, 0:36], h32[:, k, :], wr[:, k, :], k == 0, k == 7, [hkey, "q_wr"], [pk(5)])
                    P.cp("dve", LG[:, idx, :], lg[:, 0:36], [pk(5)], [("q_LG", idx)])
                    c0 = (idx % 2) * 128
                    pv = ps[3][:, :].bitcast(BF16)
                    for c in range(8):
                        P.tr(pv[:, c * 128:(c + 1) * 128], hT2[:, c, c0:c0 + 128], identb[:],
                             [("q_hT2", idx % 2), "identb"], [pk(3)])
                    P.cp("act", HTOK[:, idx, :], pv[:, :], [pk(3)], [("q_HTOK", idx)])

                with contextlib.ExitStack() as s2:
                    norm_tiles(s2, tiles, 1, hT2, "q_hT2", 0, hook=router, tag="qn", colfn=lambda idx: (idx % 2) * 128)
                    drain(["qn_xt0", "qn_xt1", "qn_junk", "qn_st0", "qn_st1", "qn_h320", "qn_h321"])
                R = "q_rt"
                rb = P.sb(s1, "q_rb", [128, 8, ntl])
                g4 = P.sb(s1, "q_g4", [128, 2, ntl, 4])
                le = P.sb(s1, "q_le", [128, 2, ntl, NE])
                lgk = [("q_LG", idx) for idx in range(ntl)]
                ohk = [("q_OH", idx) for idx in range(ntl)]
                wwk = [("q_WW", idx) for idx in range(ntl)]
                LGg, LGe = LG[:, :, 0:4], LG[:, :, 4:36]
                gmax, gate, m1, m2, dd, p1, p2 = (rb[:, q, :] for q in range(7))
                bc4 = lambda v: v.unsqueeze(2).broadcast_to([128, ntl, 4])
                bc32 = lambda v: v.unsqueeze(2).broadcast_to([128, ntl, NE])
                P.red("dve", gmax, LGg, ALU.max, lgk, [R])
                P.tt("dve", g4[:, 0], LGg, bc4(gmax), ALU.subtract, lgk + [R], [R])
                P.act(g4[:, 0], g4[:, 0], AF.Exp, [R], [R])
                P.red("dve", gate, g4[:, 0], ALU.add, [R], [R])
                P.recip(gate, gate, [R], [R])
                P.tt("dve", g4[:, 1], LGg, bc4(gmax), ALU.is_ge, lgk + [R], [R])
                P.ts("dve", g4[:, 1], g4[:, 1], -1.0, ALU.add, [R], [R], s2=BIG, op1=ALU.mult)
                P.tt("dve", le[:, 0].rearrange("p t (g j) -> p t g j", g=4), LGe.rearrange("p t (g j) -> p t g j", g=4),
                     g4[:, 1].unsqueeze(3).broadcast_to([128, ntl, 4, 8]), ALU.add, lgk + [R], [R])
                P.red("dve", m1, le[:, 0], ALU.max, [R], [R])
                oh1, oh2, oha = OH[:, :, 0, :], OH[:, :, 1, :], OH[:, :, 2, :]
                P.tt("dve", oh1, le[:, 0], bc32(m1), ALU.is_ge, [R], ohk)
                P.stt("dve", le[:, 1], oh1, -BIG, le[:, 0], ALU.mult, ALU.add, [R] + ohk, [R])
                P.red("dve", m2, le[:, 1], ALU.max, [R], [R])
                P.tt("dve", oh2, le[:, 1], bc32(m2), ALU.is_ge, [R], ohk)
                P.tt("pool", oha, oh1, oh2, ALU.add, ohk, ohk)
                P.tt("dve", dd, m2, m1, ALU.subtract, [R], [R])
                P.act(dd, dd, AF.Exp, [R], [R])
                P.ts("dve", p1, dd, 1.0, ALU.add, [R], [R])
                P.recip(p1, p1, [R], [R])
                P.tt("dve", p2, dd, p1, ALU.mult, [R], [R])
                P.tt("dve", WW[:, :, 0], p1, gate, ALU.mult, [R], wwk)
                P.tt("dve", WW[:, :, 1], p2, gate, ALU.mult, [R], wwk)
                for idx in range(ntl):
                    P.mm(ps[4][:, 0:NE], ones[:], OH[:, idx, 2, :], idx == 0, idx == ntl - 1, ["ones", ("q_OH", idx)], [pk(4)])
                ob = P.sb(s1, "q_ob", [128, 8, NE])
                c3 = P.sb(s1, "q_c3", [128, NBLK, NE])
                be = P.sb(s1, "q_be", [128, NBLK])
                O_ = "q_ob"
                cnt, nb_, pend, tmpa = ob[:, 0, :], ob[:, 1, :], ob[:, 2, :], ob[:, 3, :]
                P.cp("dve", cnt, ps[4][:, 0:NE], [pk(4)], [O_])
                P.tt("dve", c3[:, 0:NE, 0:18].rearrange("p e m -> p e m") if False else c3[:, 0:18, :],
                     cnt.unsqueeze(1).broadcast_to([128, 18, NE]), km[:, 64:82].unsqueeze(2).broadcast_to([128, 18, NE]),
                     ALU.is_gt, [O_, "q_km"], ["q_c3"])
                P.red("dve", nb_, c3[:, 0:18, :].rearrange("p m e -> p e m"), ALU.add, ["q_c3"], [O_])
                P.ts("dve", nb_, nb_, float(BLK), ALU.mult, [O_], [O_])
                P.cp("dve", pend, nb_, [O_], [O_])
                src, dst = pend, tmpa
                for sft in (1, 2, 4, 8, 16):
                    P.cp("dve", dst[:, 0:sft], src[:, 0:sft], [O_], [O_])
                    P.tt("dve", dst[:, sft:NE], src[:, sft:NE], src[:, 0:NE - sft], ALU.add, [O_], [O_])
                    src, dst = dst, src
                pend_f = src
                P.tt("dve", pst[:], pend_f, nb_, ALU.subtract, [O_], ["q_pst"])
                P.tt("dve", c3[:], pend_f.unsqueeze(1).broadcast_to([128, NBLK, NE]),
                     km[:, 0:NBLK].unsqueeze(2).broadcast_to([128, NBLK, NE]), ALU.is_le, [O_, "q_km"], ["q_c3"])
                P.red("dve", be[:], c3[:], ALU.add, ["q_c3"], ["q_be"])
                P.ts("dve", be[:], be[:], float(NE - 1), ALU.min, ["q_be"], ["q_be"])
                P.ts("dve", be[:], be[:], 128.0, ALU.mult, ["q_be"], ["q_be"])
                P.tt("dve", be[:], be[:], km[:, 96:97].broadcast_to([128, NBLK]), ALU.add, ["q_be", "q_km"], ["q_be"])
                sk_ = c3[:, 0:2, :].rearrange("p a e -> p (a e)")[:, 0:NBLK - 1]
                P.tt("dve", sk_, be[:, 1:NBLK], be[:, 0:NBLK - 1], ALU.is_equal, ["q_be"], ["q_c3"])
                P.stt("dve", be[:, 1:NBLK], sk_, 1.0e6, be[:, 1:NBLK], ALU.mult, ALU.add, ["q_c3", "q_be"], ["q_be"])
                P.cp("dve", OFFGU[:], be[:], ["q_be"], ["q_OFFGU"])
                ustr = P.sb(s1, "q_ustr", [128, 128])
                trl = P.sb(s1, "q_trl", [128, 128])
                P.dma("sp", trl[:], k_tri[0], writes=["q_trl"])
                P.tt("dve", ustr[:], trl[:], ident[:], ALU.subtract, ["q_trl", "ident"], ["q_ustr"])
                rk = P.sb(s1, "q_rk", [128, 3, ntl, NE])
                posf = P.sb(s1, "q_posf", [128, ntl, 2])
                ohk2 = [("q_OH", idx) for idx in range(ntl)]
                for idx in range(ntl):
                    bank, col = idx // 16, (idx % 16) * NE
                    P.mm(ps[bank][:, col:col + NE], ustr[:], OH[:, idx, 2, :], True, True, ["q_ustr", ("q_OH", idx)], [pk(bank)])
                    P.mm(ps[3 + bank][:, col:col + NE], ones[:], OH[:, idx, 2, :], True, True, ["ones", ("q_OH", idx)], [pk(3 + bank)])
                for bank in range((ntl + 15) // 16):
                    n_ = min(16, ntl - bank * 16)
                    P.cp("act", rk[:, 0, bank * 16:bank * 16 + n_, :], ps[bank][:, 0:n_ * NE].rearrange("p (t e) -> p t e", e=NE),
                         [pk(bank)], ["q_rk0"])
                    P.cp("dve", rk[:, 1, bank * 16:bank * 16 + n_, :], ps[3 + bank][:, 0:n_ * NE].rearrange("p (t e) -> p t e", e=NE),
                         [pk(3 + bank)], ["q_rk1"])
                src, dst, sk1, dk1 = 1, 2, "q_rk1", "q_rk2"
                sft = 1
                while sft < ntl:
                    P.cp("dve", rk[:, dst, 0:sft, :], rk[:, src, 0:sft, :], [sk1], [dk1])
                    P.tt("dve", rk[:, dst, sft:ntl, :], rk[:, src, sft:ntl, :], rk[:, src, 0:ntl - sft, :], ALU.add, [sk1], [dk1])
                    src, dst, sk1, dk1 = dst, src, dk1, sk1
                    sft *= 2
                for bank in range((ntl + 15) // 16):
                    n_ = min(16, ntl - bank * 16)
                    P.tt("dve", rk[:, src, bank * 16:bank * 16 + n_, :], rk[:, src, bank * 16:bank * 16 + n_, :],
                         ps[3 + bank][:, 0:n_ * NE].rearrange("p (t e) -> p t e", e=NE), ALU.subtract, [sk1, pk(3 + bank)], [sk1])
                P.tt("dve", rk[:, 0], rk[:, 0], rk[:, src], ALU.add, ["q_rk0", sk1], ["q_rk0"])
                P.tt("dve", rk[:, 0], rk[:, 0], pst[:].unsqueeze(1).broadcast_to([128, ntl, NE]), ALU.add, ["q_rk0", "q_pst"], ["q_rk0"])
                for k2 in range(2):
                    P.tt("dve", rk[:, dst], rk[:, 0], OH[:, :, k2, :], ALU.mult, ["q_rk0", dk1] + ohk2, [dk1])
                    P.red("dve", posf[:, :, k2], rk[:, dst], ALU.add, [dk1], ["q_posf"])
                P.cp("dve", POSI[:], posf[:], ["q_posf"], ["q_POSI"])
                for idx in range(ntl):
                    for k2 in range(2):
                        S.idma(Hs[:, :], IOA(ap=POSI[:, idx, k2:k2 + 1], axis=0), HTOK[:, idx, :], None, NSLOT - 1,
                               reads=[("q_HTOK", idx), "q_POSI"], writes=["Hs"], semkey="q_scat")
                drain(["q_wr", "q_rt", "q_rb", "q_g4", "q_le"] + [("q_LG", idx) for idx in range(ntl)] + ["q_hT2", ("q_hT2", 0), ("q_hT2", 1), "q_ob", "q_c3", "q_be", "q_ustr", "q_trl",
                       "q_rk0", "q_rk1", "q_rk2", "q_posf"] + [("q_HTOK", idx) for idx in range(ntl)] + [("q_OH", idx) for idx in range(ntl)])
            with contextlib.ExitStack() as s1:
                w32g = P.sb(s1, "q_w32g", [128, 8, 2 * FH])
                w32d = P.sb(s1, "q_w32d", [128, 4, D])
                wgu = [P.sb(s1, "q_wgu%d" % b, [128, 8, 2 * FH], BF16) for b in range(2)]
                wdn = [P.sb(s1, "q_wdn%d" % b, [128, 4, D], BF16) for b in range(2)]
                hs = [P.sb(s1, "q_hs%d" % b, [128, 4, D], BF16) for b in range(2)]
                hTs = P.sb(s1, "q_hTs", [128, 8, BLK], BF16)
                sg = [P.sb(s1, "q_sg%d" % b, [128, BLK], BF16) for b in range(2)]
                aT = P.sb(s1, "q_aT", [128, 4, BLK], BF16)
                zt = [P.sb(s1, "q_zt%d" % b, [128, D]) for b in range(2)]

                def gatherw(j):
                    b = j % 2
                    S.idma(w32g[:].rearrange("p k n -> p (k n)"), None, wgu_rows, IOA(ap=OFFGU[:, j:j + 1], axis=0), NE * 128 - 1,
                           reads=["q_OFFGU"], writes=[("q_w32g", k) for k in range(8)], semkey="q_w32g")
                    S.idma(w32d[:].rearrange("p k n -> p (k n)"), None, wdn_rows, IOA(ap=OFFGU[:, j:j + 1], axis=0), NE * 128 - 1,
                           reads=["q_OFFGU"], writes=[("q_w32d", k) for k in range(4)], semkey="q_w32d")
                    P.dma("sp", hs[b][:], Hs[j * BLK:(j + 1) * BLK, :].rearrange("(s p) d -> p s d", p=128),
                          reads=["Hs"], writes=["q_hs%d" % b], semkey="q_hs%d" % b)

                def castw(j):
                    b = j % 2
                    for k in range(8):
                        eng_ = "act" if k % 2 == 0 else "dve"
                        P.cp(eng_, wgu[b][:, k, :], w32g[:, k, :], [("q_w32g", k)], [("q_wgu%d" % b, k)])
                    for k in range(4):
                        P.cp("act" if k % 2 == 0 else "dve", wdn[b][:, k, :], w32d[:, k, :], [("q_w32d", k)],
                             [("q_wdn%d" % b, k)])

                gatherw(0)
                castw(0)
                zc = 0
                for j in range(NBLK):
                    b = j % 2
                    if j + 1 < NBLK:
                        gatherw(j + 1)
                    gkeys = [("q_wgu%d" % b, k) for k in range(8)]
                    dkeys = [("q_wdn%d" % b, k) for k in range(4)]
                    for s_ in range(4):
                        bank = 4 + s_
                        pv = ps[bank][:, :].bitcast(BF16)
                        for c in range(8):
                            P.tr(pv[:, c * 128:(c + 1) * 128], hs[b][:, s_, c * 128:(c + 1) * 128], identb[:],
                                 ["q_hs%d" % b, "identb"], [pk(bank)])
                        P.cp("act" if s_ % 2 == 0 else "dve", hTs[:, :, s_ * 128:(s_ + 1) * 128],
                             pv[:, :].rearrange("p (c t) -> p c t", t=128), [pk(bank)], [("q_hTs", s_)])
                    hkeys = [("q_hTs", s_) for s_ in range(4)]
                    for jj in range(4):
                        gb, ub = (jj % 2), 2 + (jj % 2)
                        for k in range(8):
                            P.mm(ps[gb][:, :], wgu[b][:, k, jj * 128:(jj + 1) * 128], hTs[:, k, :], k == 0, k == 7,
                                 [gkeys[k]] + hkeys, [pk(gb)])
                        for k in range(8):
                            P.mm(ps[ub][:, :], wgu[b][:, k, FH + jj * 128:FH + (jj + 1) * 128], hTs[:, k, :], k == 0, k == 7,
                                 [gkeys[k]] + hkeys, [pk(ub)])
                        sk = "q_sg%d" % (jj % 2)
                        P.act(sg[jj % 2][:], ps[gb][:, :], AF.Silu, [pk(gb)], [sk])
                        P.tt("dve", aT[:, jj, :], sg[jj % 2][:], ps[ub][:, :], ALU.mult, [sk, pk(ub)], [("q_aT", jj)])
                    for tt_ in range(4):
                        zb = zc % 2
                        zc += 1
                        zk = "q_zt%d" % zb
                        for half in range(2):
                            db = 4 + half
                            for jj in range(4):
                                P.mm(ps[db][:, :], aT[:, jj, tt_ * 128:(tt_ + 1) * 128], wdn[b][:, jj, half * 512:(half + 1) * 512],
                                     jj == 0, jj == 3, [("q_aT", jj), dkeys[jj]], [pk(db)])
                            P.cp("act" if half == 0 else "dve", zt[zb][:, half * 512:(half + 1) * 512], ps[db][:, :],
                                 [pk(db)], [zk])
                        r0 = j * BLK + tt_ * 128
                        P.dma("sp", Z[r0:r0 + 128, :], zt[zb][:], reads=[zk], writes=["Z"], semkey=zk)
                    if j + 1 < NBLK:
                        castw(j + 1)
                keys = ["q_hTs", "q_sg0", "q_sg1", "q_zt0", "q_zt1", "q_hs0", "q_hs1"]
                keys += [("q_w32g", k) for k in range(8)] + [("q_w32d", k) for k in range(4)]
                keys += [("q_wgu%d" % b, k) for b in range(2) for k in range(8)]
                keys += [("q_wdn%d" % b, k) for b in range(2) for k in range(4)]
                keys += [("q_aT", jj) for jj in range(4)] + [("q_hTs", s_) for s_ in range(4)]
                drain(keys)
            with contextlib.ExitStack() as s1:
                NBUF = 4
                z1 = [P.sb(s1, "q_z1%d" % b, [128, D]) for b in range(NBUF)]
                z2 = [P.sb(s1, "q_z2%d" % b, [128, D]) for b in range(NBUF)]
                xo = [P.sb(s1, "q_xo%d" % b, [128, D]) for b in range(NBUF)]

                def cload(idx):
                    b = idx % NBUF
                    t = tiles[idx]
                    S.idma(z1[b][:], None, Z[:, :], IOA(ap=POSI[:, idx, 0:1], axis=0), NSLOT - 1,
                           reads=["Z", "q_POSI"], writes=["q_z1%d" % b], semkey="q_z1%d" % b)
                    S.idma(z2[b][:], None, Z[:, :], IOA(ap=POSI[:, idx, 1:2], axis=0), NSLOT - 1,
                           reads=["Z", "q_POSI"], writes=["q_z2%d" % b], semkey="q_z2%d" % b)
                    P.dma("sp", xo[b][:], X[t * 128:(t + 1) * 128, :], reads=[xk(t)], writes=["q_xo%d" % b], semkey="q_xo%d" % b)

                for idx in range(min(NBUF - 1, ntl)):
                    cload(idx)
                for idx, t in enumerate(tiles):
                    if idx + NBUF - 1 < ntl:
                        cload(idx + NBUF - 1)
                    b = idx % NBUF
                    s_ = 1 if t < 2 else 0
                    k1, k2_, ok = "q_z1%d" % b, "q_z2%d" % b, "q_xo%d" % b
                    P.ts("dve", z1[b][:], z1[b][:], WW[:, idx, 0:1], ALU.mult, [k1, ("q_WW", idx)], [k1])
                    P.stt("dve", z1[b][:], z2[b][:], WW[:, idx, 1:2], z1[b][:], ALU.mult, ALU.add, [k1, k2_, ("q_WW", idx)], [k1])
                    P.tt("pool", z1[b][:], z1[b][:], gates[:, 1, s_, :], ALU.mult, [k1, ("gates", 1, s_)], [k1])
                    P.tt("dve", xo[b][:], xo[b][:], z1[b][:], ALU.add, [ok, k1], [ok])
                    P.dma("sp", X[t * 128:(t + 1) * 128, :], xo[b][:], reads=[ok], writes=[xk(t)], semkey=ok)
                drain(["q_z1%d" % b for b in range(4)] + ["q_z2%d" % b for b in range(4)] + ["q_xo%d" % b for b in range(4)] + ["q_POSI", "q_OFFGU", "q_pst", "q_km",
                       "Hs", "Z"] + [("q_WW", idx) for idx in range(ntl)])

    def final_norm():
        with contextlib.ExitStack() as st:
            gb = P.sb(st, "f_g", [128, D])
            xt = [P.sb(st, "f_x%d" % j, [128, D]) for j in range(2)]
            junk = P.sb(st, "f_junk", [128, D])
            st8 = [P.sb(st, "f_st%d" % j, [128, 4]) for j in range(2)]
            P.dma("sp", gb[:], final_g.partition_broadcast(128) if False else final_g[0:1, :].broadcast_to([128, D]),
                  writes=["f_g"])
            for idx in range(SEQ // 128):
                t = idx + 2
                b = idx % 2
                xkey, skey = "f_x%d" % b, "f_st%d" % b
                P.dma("sp", xt[b][:], X[t * 128:(t + 1) * 128, :], reads=[xk(t)], writes=[xkey], semkey=xkey)
                P.act(junk[:], xt[b][:], AF.Square, [xkey], ["f_junk", skey], accum_out=st8[b][:, 0:1])
                P.act(st8[b][:, 1:2], st8[b][:, 0:1], AF.Sqrt, [skey], [skey], bias=EPS, scale=1.0 / D)
                P.recip(st8[b][:, 2:3], st8[b][:, 1:2], [skey], [skey])
                P.stt("dve", xt[b][:], xt[b][:], st8[b][:, 2:3], gb[:], ALU.mult, ALU.mult, [xkey, skey, "f_g"], [xkey])
                P.dma("sp", out[idx * 128:(idx + 1) * 128, :], xt[b][:], reads=[xkey], writes=[("out", idx)],
                      semkey=xkey)
            for e_ in ("sp", "act", "dve"):
                S.wait_all(e_, ["f_g", "f_x0", "f_x1", "f_junk", "f_st0", "f_st1"])

    ALL = list(range(NT))
    LAT = list(range(2, NT))
    for i in layers:
        kind = i % 3
        ctx_out = i < DEPTH - 1
        adaln(i, kind == 1)
        S.recycle()
        if mixers_on:
            if kind == 0:
                attention(i, i // 3, ctx_out)
            elif kind == 1:
                pool_mixer(i)
            else:
                ssd_mixer(i)
            S.recycle()
        if moe_on:
            if cfg.get("dense_moe"):
                moe(i, ALL if ctx_out else LAT)
            else:
                moe_sparse(i, ALL if ctx_out else LAT)
            S.recycle()
    final_norm()
    for e_ in ("sp", "pe", "act", "dve", "pool"):
        S.wait_all(e_, list(S.wr.keys()))
    P.es.close()
    return P


def host_consts():
    ident = np.eye(128, dtype=np.float32)
    n_freq = HD // 4
    inv = (10000.0 ** (-np.arange(n_freq, dtype=np.float32) / n_freq)).astype(np.float32)
    tok = np.arange(SEQ)
    row = (tok // GRID_W).astype(np.float32)
    col = (tok % GRID_W).astype(np.float32)
    ang = np.concatenate([row[:, None] * inv[None, :], col[:, None] * inv[None, :]], axis=-1).astype(np.float32)
    rope = np.zeros((T, 64), np.float32)
    rope[:CTX, :32] = 1.0
    rope[CTX:, :32] = np.cos(ang)
    rope[CTX:, 32:] = np.sin(ang)
    band = np.zeros((4, 5, 128, 128), np.float32)
    n = 128 * 4
    for wi, w in enumerate(POOL_WINDOWS):
        M = np.zeros((n, n), np.float64)
        for t in range(n):
            lo = max(t - w // 2, 0)
            hi = min(t + w // 2, n)
            M[lo:hi, t] = 1.0 / (hi - lo)
            M[t, t] -= 1.0
        band[wi, 0] = M[0:128, 128:256]
        band[wi, 1] = M[0:128, 0:128]
        band[wi, 2] = M[128:256, 128:256]
        band[wi, 3] = M[384:512, 384:512]
        band[wi, 4] = M[256:384, 128:256]
    tri = np.zeros((4, 128, 128), np.float32)
    s = np.arange(128)[:, None]
    l = np.arange(128)[None, :]
    tri[0] = (s <= l)
    tri[1] = np.where(s <= l, 0.0, -1.0e4)
    tri[2] = (s >= l)
    tri[3] = np.where(s >= l, 0.0, -1.0e4)
    kmoe = np.zeros((128, 128), np.float32)
    kmoe[:, 0:NBLK] = (np.arange(NBLK) * BLK)[None, :]
    kmoe[:, 64:82] = (np.arange(18) * BLK)[None, :]
    kmoe[:, 96:104] = np.arange(8)[None, :] * 128 + np.arange(128)[:, None]
    return {"k_ident": ident, "k_rope": rope, "k_band": band, "k_tri": tri, "k_moe": kmoe}


_CACHE = {}


def kernel(**inputs):
    cfg = inputs.pop("_cfg", {})
    key = repr(sorted(cfg.items()))
    if key not in _CACHE:
        _CACHE[key] = build(cfg)
    P = _CACHE[key]
    f = lambda a: np.ascontiguousarray(np.asarray(a, dtype=np.float32))
    consts = host_consts()
    shared = {}
    for name in ("norm_mix_g", "norm_ffn_g",
                 "attn_q_norm_g", "attn_k_norm_g", "pool_w", "pool_scale", "ssm_w_in", "ssm_conv_w",
                 "ssm_conv_b", "ssm_norm_g", "ssm_w_out"):
        shared[name] = f(inputs[name])
    wrc = np.concatenate([f(inputs["moe_w_router_group"]), f(inputs["moe_w_router_expert"])], axis=-1)
    for i in range(DEPTH):
        if "w_ada_%d" % i in P.inp:
            shared["w_ada_%d" % i] = f(inputs["w_ada"][i])
            shared["b_ada_%d" % i] = f(inputs["b_ada"][i]).reshape(1, 6 * D)
        if "moe_w_gu_%d" % i in P.inp:
            shared["moe_w_gu_%d" % i] = np.ascontiguousarray(
                f(inputs["moe_w_gate_up"][i]).reshape(NE, 8, 128, 2 * FH).transpose(0, 2, 1, 3)).reshape(NE * 128, 8 * 2 * FH)
            shared["moe_w_dn_%d" % i] = np.ascontiguousarray(
                f(inputs["moe_w_down"][i]).reshape(NE, 4, 128, D).transpose(0, 2, 1, 3)).reshape(NE * 128, 4 * D)
            shared["moe_wr_%d" % i] = np.ascontiguousarray(wrc[i])
    for j in range(2):
        if "attn_w_qkv_%d" % j in P.inp:
            shared["attn_w_qkv_%d" % j] = f(inputs["attn_w_qkv"][j])
            shared["attn_w_o_%d" % j] = f(inputs["attn_w_o"][j])
    shared["final_norm_g"] = f(inputs["final_norm_g"]).reshape(1, D)
    shared["c_ctx"] = f(inputs["c_ctx"]).reshape(1, D)
    for name in ("ssm_dt_bias", "ssm_a_log", "ssm_d"):
        shared[name] = f(inputs[name]).reshape(1, 2 * SSM_H)
    shared.update(consts)
    x = f(inputs["x"])
    ctx = f(inputs["ctx"])
    c = f(inputs["c"])
    in_maps = []
    ncores = cfg.get("cores", 8)
    for core in range(ncores):
        b = core % NB
        m = dict(shared)
        m["x"] = x[b]
        m["ctx"] = ctx[b]
        m["c"] = c[b:b + 1]
        in_maps.append({k: v for k, v in m.items() if k in P.inp})
    if cfg.get("trace"):
        res = run_bass_kernel_spmd(P.nc, in_maps, core_ids=list(range(ncores)), trace=True)
        kernel.exec_ns = res.exec_time_ns
    else:
        res = run_bass_kernel_spmd(P.nc, in_maps, core_ids=list(range(ncores)))
    nb = min(NB, ncores)
    outs = np.stack([np.asarray(res.results[b]["out"], dtype=np.float32) for b in range(nb)], axis=0)
    if cfg.get("dbg"):
        kernel.dbg = [np.asarray(res.results[b]["dbg"]) for b in range(nb)]
    return outs
```

```python
import contextlib
import numpy as np
import concourse.bass as bass
import concourse.mybir as mybir
from concourse.bass_utils import run_bass_kernel_spmd

F32 = mybir.dt.float32
BF16 = mybir.dt.bfloat16
I32 = mybir.dt.int32
ALU = mybir.AluOpType
AF = mybir.ActivationFunctionType
AX = mybir.AxisListType

D = 1024
NB = 4
SEQ = 4096
CTX = 256
T = SEQ + CTX
NT = T // 128
DEPTH = 4
EPS = 1e-6
GRID_W = 64
NH, NKV, HD = 16, 4, 64
POOL_WINDOWS = (2, 4, 8, 16)
SSM_DI, SSM_H, SSM_P, SSM_G, SSM_N = 2048, 32, 64, 4, 128
SSM_CONV_DIM = SSM_DI + 2 * SSM_G * SSM_N
SSM_IN = SSM_DI + SSM_CONV_DIM + 2 * SSM_H
NE, EPG, FH = 32, 8, 512
BIG = 1.0e30
BLK = 512
NBLK = (2 * T + BLK - 1) // BLK + NE
NSLOT = NBLK * BLK

SAME_ENGINE_SYNC = ("act", "dve", "pool")


class Sched:
    def __init__(self, nc):
        self.nc = nc
        self.eng = {"pe": nc.tensor, "act": nc.scalar, "dve": nc.vector,
                    "pool": nc.gpsimd, "sp": nc.sync}
        self.esem = {}
        self.ecnt = {}
        for e in ("pe", "act", "dve", "pool"):
            self.esem[e] = nc.alloc_semaphore("s_" + e)
            self.ecnt[e] = 0
        self.seen = {e: {} for e in self.eng}
        self.wr = {}
        self.rd = {}
        self.dsem = {}
        self.dfree = []
        self.nsem = 0
        self.bregs = {}
        self.n_inst = 0

    def _wait(self, e, toks):
        eng = self.eng[e]
        for name, (sem, val, src) in toks.items():
            if src == e and e not in SAME_ENGINE_SYNC:
                continue
            if self.seen[e].get(name, 0) >= val:
                continue
            eng.wait_ge(sem, val)
            self.seen[e][name] = val

    def _deps(self, e, reads, writes):
        for k in reads:
            self._wait(e, self.wr.get(k, {}))
        for k in writes:
            self._wait(e, self.wr.get(k, {}))
            self._wait(e, self.rd.get(k, {}))

    def _commit(self, name, tok, reads, writes):
        for k in reads:
            self.rd.setdefault(k, {})[name] = tok
        for k in writes:
            self.wr[k] = {name: tok}
            self.rd[k] = {}

    def op(self, e, fn, reads=(), writes=()):
        self._deps(e, reads, writes)
        ins = fn(self.eng[e])
        self.ecnt[e] += 1
        ins.then_inc(self.esem[e], 1)
        self._commit("s_" + e, (self.esem[e], self.ecnt[e], e), reads, writes)
        self.n_inst += 1
        return ins

    def dma(self, q, out, in_, reads=(), writes=(), semkey=None, **kw):
        if semkey is None:
            semkey = tuple(writes) + tuple(reads)
        ent = self._dsem_get(semkey)
        self._deps(q, reads, writes)
        ins = self.eng[q].dma_start(out=out, in_=in_, **kw)
        ent[1] += 16
        ins.then_inc(ent[0], 16)
        self._commit(ent[2], (ent[0], ent[1], "dma"), reads, writes)
        self.n_inst += 1

    def idma(self, out, out_off, in_, in_off, bounds, reads=(), writes=(), semkey=None):
        q = "pool"
        ent = self._dsem_get(semkey)
        self._deps(q, reads, writes)
        if bounds not in self.bregs:
            self.bregs[bounds] = self.eng[q].to_reg(bounds)
        ins = self.eng[q].indirect_dma_start(out=out, out_offset=out_off, in_=in_, in_offset=in_off,
                                             bounds_check=self.bregs[bounds], oob_is_err=False)
        ent[1] += 16
        ins.then_inc(ent[0], 16)
        self._commit(ent[2], (ent[0], ent[1], "dma"), reads, writes)
        self.n_inst += 1

    def recycle(self):
        for key, ent in list(self.dsem.items()):
            for e in self.eng:
                if self.seen[e].get(ent[2], 0) < ent[1]:
                    self.eng[e].wait_ge(ent[0], ent[1])
                    self.seen[e][ent[2]] = ent[1]
            self.dfree.append(ent)
            del self.dsem[key]

    def _dsem_get(self, semkey):
        if semkey not in self.dsem:
            if self.dfree:
                self.dsem[semkey] = self.dfree.pop()
            else:
                nm = "d%d" % self.nsem
                self.nsem += 1
                self.dsem[semkey] = [self.nc.alloc_semaphore(nm), 0, nm]
        return self.dsem[semkey]

    def wait_all(self, e, keys):
        for k in keys:
            self._wait(e, self.wr.get(k, {}))
            self._wait(e, self.rd.get(k, {}))


class Prog:
    def __init__(self, cfg):
        self.cfg = cfg
        self.nc = nc = bass.Bass("TRN2", target_bir_lowering=False)
        self.S = Sched(nc)
        self.es = contextlib.ExitStack()
        self.inp = {}
        self.uid = 0

    def din(self, name, shape, dt=F32):
        t = self.nc.dram_tensor(name, list(shape), dt, kind="ExternalInput").ap()
        self.inp[name] = t
        return t

    def dscratch(self, name, shape, dt=F32):
        return self.nc.dram_tensor(name, list(shape), dt, kind="Internal").ap()

    def sb(self, stack, name, shape, dt=F32):
        self.uid += 1
        return stack.enter_context(self.nc.sbuf_tensor("%s_u%d" % (name, self.uid), list(shape), dt))

    def mm(self, out, lhsT, rhs, start, stop, reads, writes):
        self.S.op("pe", lambda e: e.matmul(out, lhsT=lhsT, rhs=rhs, start=start, stop=stop),
                  reads=reads, writes=writes)

    def tr(self, out, in_, ident, reads, writes):
        self.S.op("pe", lambda e: e.transpose(out, in_, ident), reads=reads, writes=writes)

    def act(self, out, in_, func, reads, writes, bias=None, scale=None, accum_out=None):
        kw = {}
        if bias is not None:
            kw["bias"] = bias
        if scale is not None:
            kw["scale"] = scale
        if accum_out is not None:
            kw["accum_out"] = accum_out
        self.S.op("act", lambda e: e.activation(out=out, in_=in_, func=func, **kw),
                  reads=reads, writes=writes)

    def tt(self, e, out, in0, in1, op, reads, writes):
        self.S.op(e, lambda g: g.tensor_tensor(out=out, in0=in0, in1=in1, op=op),
                  reads=reads, writes=writes)

    def ts(self, e, out, in0, s1, op0, reads, writes, s2=None, op1=None, accum_out=None):
        kw = {}
        if op1 is not None:
            kw["op1"] = op1
        if accum_out is not None:
            kw["accum_out"] = accum_out
        self.S.op(e, lambda g: g.tensor_scalar(out=out, in0=in0, scalar1=s1, scalar2=s2, op0=op0, **kw),
                  reads=reads, writes=writes)

    def stt(self, e, out, in0, scalar, in1, op0, op1, reads, writes):
        self.S.op(e, lambda g: g.scalar_tensor_tensor(out=out, in0=in0, scalar=scalar, in1=in1,
                                                      op0=op0, op1=op1),
                  reads=reads, writes=writes)

    def cp(self, e, out, in_, reads, writes):
        if e == "act":
            self.S.op("act", lambda g: g.copy(out=out, in_=in_), reads=reads, writes=writes)
        else:
            self.S.op(e, lambda g: g.tensor_copy(out=out, in_=in_), reads=reads, writes=writes)

    def red(self, e, out, in_, op, reads, writes, axis=AX.X):
        self.S.op(e, lambda g: g.tensor_reduce(out=out, in_=in_, axis=axis, op=op),
                  reads=reads, writes=writes)

    def recip(self, out, in_, reads, writes):
        self.S.op("dve", lambda g: g.reciprocal(out=out, in_=in_), reads=reads, writes=writes)

    def memset(self, e, ap, val, writes):
        self.S.op(e, lambda g: g.memset(ap, val), writes=writes)

    def dma(self, q, out, in_, reads=(), writes=(), semkey=None, **kw):
        self.S.dma(q, out, in_, reads=reads, writes=writes, semkey=semkey, **kw)


def build(cfg):
    P = Prog(cfg)
    nc, S = P.nc, P.S
    layers = cfg.get("layers", list(range(DEPTH)))
    mixers_on = cfg.get("mixers", True)
    moe_on = cfg.get("moe", True)

    x_in = P.din("x", [SEQ, D])
    ctx_in = P.din("ctx", [CTX, D])
    c_in = P.din("c", [1, D])
    cctx_in = P.din("c_ctx", [1, D])
    w_ada = {i: P.din("w_ada_%d" % i, [D, 6 * D]) for i in layers}
    b_ada = {i: P.din("b_ada_%d" % i, [1, 6 * D]) for i in layers}
    norm_mix_g = P.din("norm_mix_g", [DEPTH, D])
    norm_ffn_g = P.din("norm_ffn_g", [DEPTH, D])
    final_g = P.din("final_norm_g", [1, D])
    attn_w_qkv = {j: P.din("attn_w_qkv_%d" % j, [D, 1536]) for j in range(2) if 3 * j in layers and mixers_on}
    attn_w_o = {j: P.din("attn_w_o_%d" % j, [D, D]) for j in range(2) if 3 * j in layers and mixers_on}
    attn_qg = P.din("attn_q_norm_g", [2, HD])
    attn_kg = P.din("attn_k_norm_g", [2, HD])
    pool_w = P.din("pool_w", [1, 4, 256, 256])
    pool_scale = P.din("pool_scale", [1, D])
    ssm_on = 2 in layers and mixers_on
    ssm_w_in = P.din("ssm_w_in", [1, D, SSM_IN]) if ssm_on else None
    ssm_conv_w = P.din("ssm_conv_w", [1, 4, SSM_CONV_DIM])
    ssm_conv_b = P.din("ssm_conv_b", [1, SSM_CONV_DIM])
    ssm_dt_bias = P.din("ssm_dt_bias", [1, 2 * SSM_H])
    ssm_a_log = P.din("ssm_a_log", [1, 2 * SSM_H])
    ssm_d = P.din("ssm_d", [1, 2 * SSM_H])
    ssm_norm_g = P.din("ssm_norm_g", [1, SSM_DI])
    ssm_w_out = P.din("ssm_w_out", [1, SSM_DI, D]) if ssm_on else None
    moe_l = [i for i in layers if moe_on]
    moe_wr = {i: P.din("moe_wr_%d" % i, [D, 36]) for i in moe_l}
    moe_w_gu = {i: P.din("moe_w_gu_%d" % i, [NE * 128, 8 * 2 * FH]) for i in moe_l}
    moe_w_dn = {i: P.din("moe_w_dn_%d" % i, [NE * 128, 4 * D]) for i in moe_l}
    k_ident = P.din("k_ident", [128, 128])
    k_rope = P.din("k_rope", [T, 64])
    k_band = P.din("k_band", [4, 5, 128, 128])
    k_tri = P.din("k_tri", [4, 128, 128])
    k_moe = P.din("k_moe", [128, 128])
    out = nc.dram_tensor("out", [SEQ, D], F32, kind="ExternalOutput").ap()
    dbg = None
    if cfg.get("dbg"):
        dbg = nc.dram_tensor("dbg", list(cfg["dbg"]), F32, kind="ExternalOutput").ap()

    X = P.dscratch("X", [T, D])

    def xk(t):
        return ("X", t)

    top = P.es
    ident = P.sb(top, "ident", [128, 128])
    identb = P.sb(top, "identb", [128, 128], BF16)
    ones = P.sb(top, "ones", [128, 128])
    cT = P.sb(top, "cT", [128, 8, 2])
    modL = P.sb(top, "modL", [128, 48])
    modC = P.sb(top, "modC", [128, 48])
    vec = P.sb(top, "vec", [128, 2, 2, 2, 8])
    gates = P.sb(top, "gates", [128, 2, 2, D])
    ps = [top.enter_context(nc.psum_tensor("ps%d" % i, [128, 512], F32)) for i in range(8)]

    def pk(i):
        return "ps%d" % i

    P.dma("sp", ident[:], k_ident, writes=["ident"])
    P.cp("dve", identb[:], ident[:], ["ident"], ["identb"])
    P.memset("pool", ones[:], 1.0, ["ones"])
    P.dma("sp", X[0:CTX, :], ctx_in, writes=[xk(0), xk(1)], semkey="xinit")
    P.dma("sp", X[CTX:T, :], x_in, writes=[xk(t) for t in range(2, NT)], semkey="xinit")

    with contextlib.ExitStack() as st:
        craw = P.sb(st, "craw", [128, 8, 2])
        P.dma("sp", craw[:, :, 0], c_in[0].rearrange("(k p) -> p k", p=128), writes=["craw"],
              allow_slow_non_contiguous=True)
        P.dma("sp", craw[:, :, 1], cctx_in[0].rearrange("(k p) -> p k", p=128), writes=["craw"],
              allow_slow_non_contiguous=True)
        P.act(cT[:], craw[:], AF.Silu, ["craw"], ["cT"])
        S.wait_all("sp", ["craw"])
        S.wait_all("act", ["craw"])


    def drain(keys):
        for e_ in ("sp", "pe", "act", "dve", "pool"):
            S.wait_all(e_, keys)

    def adaln(i, need_bc_all):
        with contextlib.ExitStack() as st:
            wblk = [P.sb(st, "wblk%d" % j, [128, 8, 512]) for j in range(2)]
            brow = P.sb(st, "brow", [1, 6 * D])
            bT = P.sb(st, "bT", [128, 48])
            g2 = P.sb(st, "g2", [128, 2, 8])
            cbc = P.sb(st, "cbc", [128, 2, 8, 128])
            for s in range(2):
                P.cp("dve", cbc[:, s], cT[:, :, s:s + 1].broadcast_to([128, 8, 128]), ["cT"], ["cbc"])
            P.dma("sp", brow[:], b_ada[i][0:1, :], writes=["brow"])
            P.dma("sp", bT[:], b_ada[i][0].rearrange("(j p) -> p j", p=128), writes=["bT"],
                  allow_slow_non_contiguous=True)
            P.dma("sp", g2[:, 0, :], norm_mix_g[i].rearrange("(j p) -> p j", p=128), writes=["g2"],
                  allow_slow_non_contiguous=True)
            P.dma("sp", g2[:, 1, :], norm_ffn_g[i].rearrange("(j p) -> p j", p=128), writes=["g2"],
                  allow_slow_non_contiguous=True)
            for n in range(12):
                wb = wblk[n % 2]
                wkey = "wblk%d" % (n % 2)
                P.dma("sp", wb[:], w_ada[i][:, n * 512:(n + 1) * 512].rearrange("(k p) n -> p k n", p=128),
                      writes=[wkey])
                for q in range(4):
                    j = n * 4 + q
                    for k in range(8):
                        P.mm(ps[0][:, 2 * j:2 * j + 2], wb[:, k, q * 128:(q + 1) * 128], cT[:, k, :],
                             k == 0, k == 7, [wkey, "cT"], [pk(0)])
                split = n // 2
                if split in (2, 5):
                    which = 0 if split == 2 else 1
                    half = n % 2
                    for s in range(2):
                        bank = 1 + s
                        for k in range(8):
                            P.mm(ps[bank][:, :], cbc[:, s, k, :], wb[:, k, :], k == 0, False,
                                 [wkey, "cbc"], [pk(bank)])
                        P.mm(ps[bank][:, :], ones[0:1, :], brow[0:1, n * 512:(n + 1) * 512], False, True,
                             ["ones", "brow"], [pk(bank)])
                        P.cp("act", gates[:, which, s, half * 512:(half + 1) * 512], ps[bank][:, :],
                             [pk(bank)], [("gates", which, s)])
            psv = ps[0][:, 0:96].rearrange("p (j s) -> p j s", s=2)
            P.tt("dve", modL[:], psv[:, :, 0], bT[:], ALU.add, [pk(0), "bT"], ["modL"])
            P.tt("dve", modC[:], psv[:, :, 1], bT[:], ALU.add, [pk(0), "bT"], ["modC"])
            for which in range(2):
                for s, m in enumerate((modL, modC)):
                    mk = "modL" if s == 0 else "modC"
                    base = which * 24
                    P.stt("dve", vec[:, which, s, 0, :], m[:, base + 8:base + 16], 1.0, g2[:, which, :],
                          ALU.add, ALU.mult, [mk, "g2"], [("vec", which, s)])
                    P.cp("dve", vec[:, which, s, 1, :], m[:, base:base + 8], [mk], [("vec", which, s)])
            S.wait_all("sp", ["wblk0", "wblk1", "brow", "bT", "g2"])
            S.wait_all("pe", ["wblk0", "wblk1", "brow", "cbc"])
            S.wait_all("dve", ["bT", "g2"])

    def norm_tiles(st, tiles, which, hT, hT_key, col0, hook=None, tag="n", colfn=None):
        xt = [P.sb(st, "%s_xt%d" % (tag, j), [128, D]) for j in range(2)]
        h32 = [P.sb(st, "%s_h32%d" % (tag, j), [128, 8, 128]) for j in range(2)]
        st8 = [P.sb(st, "%s_st%d" % (tag, j), [128, 4]) for j in range(2)]
        junk = P.sb(st, "%s_junk" % tag, [128, D], BF16)

        def load(idx):
            t = tiles[idx]
            P.dma("sp", xt[idx % 2][:], X[t * 128:(t + 1) * 128, :], reads=[xk(t)],
                  writes=["%s_xt%d" % (tag, idx % 2)], semkey="%s_xt%d" % (tag, idx % 2))

        load(0)
        for idx, t in enumerate(tiles):
            if idx + 1 < len(tiles):
                load(idx + 1)
            b = idx % 2
            xkey, skey, hkey = "%s_xt%d" % (tag, b), "%s_st%d" % (tag, b), "%s_h32%d" % (tag, b)
            s = 1 if t < 2 else 0
            P.act(junk[:], xt[b][:], AF.Square, [xkey], ["%s_junk" % tag, skey], accum_out=st8[b][:, 0:1])
            P.act(st8[b][:, 1:2], st8[b][:, 0:1], AF.Sqrt, [skey], [skey], bias=EPS, scale=1.0 / D)
            P.recip(st8[b][:, 2:3], st8[b][:, 1:2], [skey], [skey])
            P.ts("dve", xt[b][:], xt[b][:], st8[b][:, 2:3], ALU.mult, [xkey, skey], [xkey])
            for c in range(8):
                bank = 6 + c // 4
                P.tr(ps[bank][:, (c % 4) * 128:(c % 4 + 1) * 128], xt[b][:, c * 128:(c + 1) * 128], ident[:],
                     [xkey, "ident"], [pk(bank)])
            c0 = col0 + idx * 128 if colfn is None else colfn(idx)
            hTk = hT_key if colfn is None else (hT_key, idx % 2)
            if hook is None:
                for c in range(8):
                    bank = 6 + c // 4
                    P.act(hT[:, c, c0:c0 + 128], ps[bank][:, (c % 4) * 128:(c % 4 + 1) * 128], AF.Identity,
                          [pk(bank), ("vec", which, s)], [hTk],
                          bias=vec[:, which, s, 1, c:c + 1], scale=vec[:, which, s, 0, c:c + 1])
            else:
                for c in range(8):
                    bank = 6 + c // 4
                    P.act(h32[b][:, c, :], ps[bank][:, (c % 4) * 128:(c % 4 + 1) * 128], AF.Identity,
                          [pk(bank), ("vec", which, s)], [hkey],
                          bias=vec[:, which, s, 1, c:c + 1], scale=vec[:, which, s, 0, c:c + 1])
                P.cp("dve", hT[:, :, c0:c0 + 128], h32[b][:], [hkey], [hTk])
            if hook is not None:
                hook(idx, t, h32[b], hkey)

    def moe(i, tiles_all):
        nhalf = 2
        per = len(tiles_all) // nhalf
        for hf in range(nhalf):
            tiles = tiles_all[hf * per:(hf + 1) * per]
            ntk = per * 128
            with contextlib.ExitStack() as st:
                hT = P.sb(st, "m_hT", [128, 8, ntk], BF16)
                Y = P.sb(st, "m_Y", [128, per, D])
                Wt = P.sb(st, "m_Wt", [128, per, NE])
                wr = P.sb(st, "m_wr", [128, 8, 36])
                wgu = [P.sb(st, "m_wgu%d" % j, [128, 8, 2 * FH], BF16) for j in range(2)]
                wdn = [P.sb(st, "m_wdn%d" % j, [128, 4, D], BF16) for j in range(2)]
                sg = [P.sb(st, "m_sg%d" % j, [128, 512], BF16) for j in range(2)]
                aT = [P.sb(st, "m_a%d" % j, [128, 4, 512], BF16) for j in range(2)]
                rt = P.sb(st, "m_rt", [128, 160])

                def loadw(e):
                    b = e % 2
                    P.dma("pool", wgu[b][:], moe_w_gu[i][e * 128:(e + 1) * 128, :].rearrange("p (k n) -> p k n", k=8),
                          writes=["m_wgu%d" % b])
                    P.dma("pool", wdn[b][:], moe_w_dn[i][e * 128:(e + 1) * 128, :].rearrange("p (k n) -> p k n", k=4),
                          writes=["m_wdn%d" % b])

                P.dma("sp", wr[:], moe_wr[i].rearrange("(k p) n -> p k n", p=128), writes=["m_wr"])
                loadw(0)

                def router(idx, t, h32, hkey):
                    lg = ps[5]
                    for k in range(8):
                        P.mm(lg[:, 0:36], h32[:, k, :], wr[:, k, :], k == 0, k == 7, [hkey, "m_wr"], [pk(5)])
                    R = "m_rt"
                    lgs = rt[:, 0:36]
                    P.cp("dve", lgs, lg[:, 0:36], [pk(5)], [R])
                    gmax, ngmax, gsum, gate = rt[:, 36:37], rt[:, 37:38], rt[:, 38:39], rt[:, 39:40]
                    P.red("dve", gmax, rt[:, 0:4], ALU.max, [R], [R])
                    P.ts("dve", ngmax, gmax, -1.0, ALU.mult, [R], [R])
                    P.act(rt[:, 40:44], rt[:, 0:4], AF.Exp, [R], [R], bias=ngmax, scale=1.0, accum_out=gsum)
                    P.recip(gate, gsum, [R], [R])
                    pen = rt[:, 44:48]
                    P.ts("dve", pen, rt[:, 0:4], gmax, ALU.is_ge, [R], [R])
                    P.ts("dve", pen, pen, -1.0, ALU.add, [R], [R], s2=BIG, op1=ALU.mult)
                    le = rt[:, 48:80]
                    P.tt("dve", le.rearrange("p (g j) -> p g j", g=4), rt[:, 4:36].rearrange("p (g j) -> p g j", g=4),
                         pen.unsqueeze(2).broadcast_to([128, 4, 8]), ALU.add, [R], [R])
                    m1, m2 = rt[:, 80:81], rt[:, 81:82]
                    P.red("dve", m1, le, ALU.max, [R], [R])
                    oh1, oh2, le2 = rt[:, 84:116], rt[:, 116:148], rt[:, 4:36]
                    P.ts("dve", oh1, le, m1, ALU.is_ge, [R], [R])
                    P.stt("dve", le2, oh1, -BIG, le, ALU.mult, ALU.add, [R], [R])
                    P.red("dve", m2, le2, ALU.max, [R], [R])
                    P.ts("dve", oh2, le2, m2, ALU.is_ge, [R], [R])
                    dd, ee, p1, p2 = rt[:, 148:149], rt[:, 149:150], rt[:, 150:151], rt[:, 151:152]
                    P.tt("dve", dd, m2, m1, ALU.subtract, [R], [R])
                    P.act(ee, dd, AF.Exp, [R], [R])
                    P.ts("dve", p1, ee, 1.0, ALU.add, [R], [R])
                    P.recip(p1, p1, [R], [R])
                    P.tt("dve", p2, ee, p1, ALU.mult, [R], [R])
                    P.tt("dve", p1, p1, gate, ALU.mult, [R], [R])
                    P.tt("dve", p2, p2, gate, ALU.mult, [R], [R])
                    P.ts("dve", oh1, oh1, p1, ALU.mult, [R], [R])
                    P.stt("dve", Wt[:, idx, :], oh2, p2, oh1, ALU.mult, ALU.add, [R], [("m_Wt", idx)])

                with contextlib.ExitStack() as st2:
                    norm_tiles(st2, tiles, 1, hT, "m_hT", 0, hook=router, tag="mn")
                    S.wait_all("sp", ["mn_xt0", "mn_xt1"])
                    S.wait_all("act", ["mn_xt0", "mn_xt1", "mn_junk", "mn_st0", "mn_st1", "mn_h320", "mn_h321"])
                    S.wait_all("dve", ["mn_xt0", "mn_xt1", "mn_st0", "mn_st1"])
                    S.wait_all("pool", ["mn_h320", "mn_h321"])
                    S.wait_all("pe", ["mn_xt0", "mn_xt1", "mn_h320", "mn_h321"])

                blocks = []
                o = 0
                while o < ntk:
                    n_ = min(512, ntk - o)
                    blocks.append((o, n_))
                    o += n_
                cnt = 0
                dcnt = 0
                for e in range(NE):
                    if e + 1 < NE:
                        loadw(e + 1)
                    b = e % 2
                    gk, dk = "m_wgu%d" % b, "m_wdn%d" % b
                    for (o, n_) in blocks:
                        ab = cnt % 2
                        akey = "m_a%d" % ab
                        for j in range(4):
                            gb, ub = (j % 2), 2 + (j % 2)
                            for k in range(8):
                                P.mm(ps[gb][:, 0:n_], wgu[b][:, k, j * 128:(j + 1) * 128], hT[:, k, o:o + n_],
                                     k == 0, k == 7, [gk, "m_hT"], [pk(gb)])
                            for k in range(8):
                                P.mm(ps[ub][:, 0:n_], wgu[b][:, k, FH + j * 128:FH + (j + 1) * 128],
                                     hT[:, k, o:o + n_], k == 0, k == 7, [gk, "m_hT"], [pk(ub)])
                            sk = "m_sg%d" % (j % 2)
                            P.act(sg[j % 2][:, 0:n_], ps[gb][:, 0:n_], AF.Silu, [pk(gb)], [sk])
                            P.tt("dve", aT[ab][:, j, 0:n_], sg[j % 2][:, 0:n_], ps[ub][:, 0:n_], ALU.mult,
                                 [sk, pk(ub)], [(akey, j)])
                        for tt_ in range(n_ // 128):
                            tidx = o // 128 + tt_
                            for half in range(2):
                                db = 4 + dcnt % 2
                                dcnt += 1
                                for j in range(4):
                                    P.mm(ps[db][:, :], aT[ab][:, j, tt_ * 128:(tt_ + 1) * 128],
                                         wdn[b][:, j, half * 512:(half + 1) * 512], j == 0, j == 3,
                                         [(akey, j), dk], [pk(db)])
                                yk = ("m_Y", tidx, half)
                                ysl = Y[:, tidx, half * 512:(half + 1) * 512]
                                if e == 0:
                                    P.ts("dve", ysl, ps[db][:, :], Wt[:, tidx, e:e + 1], ALU.mult,
                                         [pk(db), ("m_Wt", tidx)], [yk])
                                else:
                                    P.stt("dve", ysl, ps[db][:, :], Wt[:, tidx, e:e + 1], ysl, ALU.mult, ALU.add,
                                          [pk(db), ("m_Wt", tidx), yk], [yk])
                        cnt += 1
                xo = [P.sb(st, "m_xo%d" % j, [128, D]) for j in range(2)]
                for idx, t in enumerate(tiles):
                    b = idx % 2
                    s = 1 if t < 2 else 0
                    ok = "m_xo%d" % b
                    P.dma("sp", xo[b][:], X[t * 128:(t + 1) * 128, :], reads=[xk(t)], writes=[ok], semkey=ok)
                    P.tt("pool", Y[:, idx, :], Y[:, idx, :], gates[:, 1, s, :], ALU.mult,
                         [("m_Y", idx, 0), ("m_Y", idx, 1), ("gates", 1, s)], [("m_Y", idx, 0), ("m_Y", idx, 1)])
                    P.tt("dve", xo[b][:], xo[b][:], Y[:, idx, :], ALU.add,
                         [ok, ("m_Y", idx, 0), ("m_Y", idx, 1)], [ok])
                    P.dma("sp", X[t * 128:(t + 1) * 128, :], xo[b][:], reads=[ok], writes=[xk(t)], semkey=ok)
                keys = ["m_hT", "m_wr", "m_wgu0", "m_wgu1", "m_wdn0", "m_wdn1", "m_sg0", "m_sg1", "m_rt",
                        "m_xo0", "m_xo1"]
                keys += [("m_a%d" % a, j) for a in range(2) for j in range(4)]
                keys += [("m_Y", idx, h) for idx in range(per) for h in range(2)]
                keys += [("m_Wt", idx) for idx in range(per)]
                for e_ in ("sp", "pe", "act", "dve", "pool"):
                    S.wait_all(e_, keys)


    def attention(i, j, ctx_out):
        QD = P.dscratch("QD%d" % i, [64, 16, T], BF16)
        with contextlib.ExitStack() as st:
            KT = P.sb(st, "a_KT", [128, 4, T], BF16)
            Vg = P.sb(st, "a_V", [128, NT, 4, 128], BF16)
            P.memset("dve", Vg[:], 0.0, ["a_V"])
            P.memset("dve", Vg[:, :, :, 64:66], 1.0, ["a_V"])
            with contextlib.ExitStack() as s1:
                hT = P.sb(s1, "a_hT", [128, 8, T], BF16)
                wqkv = P.sb(s1, "a_wqkv", [128, 8, 1536], BF16)
                gqk = P.sb(s1, "a_gqk", [128, 2, 64])
                P.dma("pool", wqkv[:], attn_w_qkv[j].rearrange("(k p) n -> p k n", p=128), writes=["a_wqkv"])
                P.dma("sp", gqk[:, 0, :], attn_qg[j:j + 1, :].broadcast_to([128, 64]), writes=["a_gqk"])
                P.dma("sp", gqk[:, 1, :], attn_kg[j:j + 1, :].broadcast_to([128, 64]), writes=["a_gqk"])
                with contextlib.ExitStack() as s2:
                    norm_tiles(s2, ALL, 0, hT, "a_hT", 0, tag="an")
                    drain(["an_xt0", "an_xt1", "an_junk", "an_st0", "an_st1", "an_h320", "an_h321"])
                qk_ = P.sb(s1, "a_qk0", [128, 20, 64])
                qk = [qk_, qk_]
                T4 = P.sb(s1, "a_T4", [128, 4 * 512])
                tmp = [T4[:, b * 512:(b + 1) * 512].rearrange("p (h i) -> p h i", i=32) for b in range(4)]
                sq = T4[:, 0:1280].rearrange("p (h d) -> p h d", d=64)
                qr = [P.sb(s1, "a_qr%d" % b, [128, 20, 64], BF16) for b in range(2)]
                ss = [P.sb(s1, "a_ss%d" % b, [128, 20]) for b in range(2)]
                cs = [P.sb(s1, "a_cs%d" % b, [128, 64]) for b in range(2)]
                tab = [P.sb(s1, "a_tab%d" % b, [128, 2, 4, 32]) for b in range(2)]
                QTt_ = P.sb(s1, "a_QTt0", [64, 16, 128], BF16)
                QTt = [QTt_, QTt_]
                gq4 = gqk[:].rearrange("p w (i two) -> p w i two", two=2)
                for t in range(NT):
                    b = t % 2
                    qkk, ssk, csk, tabk, qrk, qtk = ("a_qk0", "a_ss%d" % b, "a_cs%d" % b, "a_tab%d" % b,
                                                     "a_qr%d" % b, "a_QTt0")
                    if cfg.get("a1_lvl", 9) < 0.2:
                        continue
                    P.dma("sp", cs[b][:], k_rope[t * 128:(t + 1) * 128, :], writes=[csk])
                    if cfg.get("a1_lvl", 9) < 0.4:
                        continue
                    for nb in range(3):
                        for k in range(8):
                            P.mm(ps[nb][:, :], hT[:, k, t * 128:(t + 1) * 128], wqkv[:, k, nb * 512:(nb + 1) * 512],
                                 k == 0, k == 7, ["a_hT", "a_wqkv"], [pk(nb)])
                    if cfg.get("a1_lvl", 9) < 0.6:
                        continue
                    P.cp("act", qk[b][:, 0:8, :], ps[0][:, :].rearrange("p (h d) -> p h d", d=64), [pk(0)], [qkk])
                    P.cp("act", qk[b][:, 8:16, :], ps[1][:, :].rearrange("p (h d) -> p h d", d=64), [pk(1)], [qkk])
                    P.cp("act", qk[b][:, 16:20, :], ps[2][:, 0:256].rearrange("p (h d) -> p h d", d=64), [pk(2)], [qkk])
                    if cfg.get("a1_lvl", 9) < 0.8:
                        continue
                    P.cp("act", Vg[:, t, :, 0:64], ps[2][:, 256:512].rearrange("p (h d) -> p h d", d=64),
                         [pk(2)], [("a_V", t)])
                    if cfg.get("a1_lvl", 9) < 2:
                        continue
                    P.tt("pool", sq, qk[b][:], qk[b][:], ALU.mult, [qkk], ["a_sq", "a_t0", "a_t1", "a_t2"])
                    P.red("dve", ss[b][:], sq, ALU.add, ["a_sq", "a_t0", "a_t1", "a_t2"], [ssk])
                    P.act(ss[b][:], ss[b][:], AF.Sqrt, [ssk], [ssk], bias=EPS, scale=1.0 / HD)
                    P.recip(ss[b][:], ss[b][:], [ssk], [ssk])
                    P.tt("dve", qk[b][:], qk[b][:], ss[b][:].unsqueeze(2).broadcast_to([128, 20, 64]), ALU.mult,
                         [qkk, ssk], [qkk])
                    if cfg.get("a1_lvl", 9) < 3:
                        continue
                    for w in range(2):
                        P.tt("pool", tab[b][:, w, 0, :], cs[b][:, 0:32], gq4[:, w, :, 0], ALU.mult, [csk, "a_gqk"], [tabk])
                        P.tt("pool", tab[b][:, w, 1, :], cs[b][:, 32:64], gq4[:, w, :, 1], ALU.mult, [csk, "a_gqk"], [tabk])
                        P.tt("pool", tab[b][:, w, 2, :], cs[b][:, 32:64], gq4[:, w, :, 0], ALU.mult, [csk, "a_gqk"], [tabk])
                        P.tt("pool", tab[b][:, w, 3, :], cs[b][:, 0:32], gq4[:, w, :, 1], ALU.mult, [csk, "a_gqk"], [tabk])
                    qk4 = qk[b][:].rearrange("p h (i two) -> p h i two", two=2)
                    qr4 = qr[b][:].rearrange("p h (i two) -> p h i two", two=2)
                    for w, (h0, h1) in enumerate(((0, 16), (16, 20))):
                        nh = h1 - h0
                        x0, x1 = qk4[:, h0:h1, :, 0], qk4[:, h0:h1, :, 1]
                        tb = lambda kind: tab[b][:, w, kind, :].unsqueeze(1).broadcast_to([128, nh, 32])
                        P.tt("pool", tmp[0][:, 0:nh, :], x0, tb(0), ALU.mult, [qkk, tabk, "a_sq"], ["a_t0"])
                        P.tt("dve", tmp[1][:, 0:nh, :], x1, tb(1), ALU.mult, [qkk, tabk, "a_sq"], ["a_t1"])
                        P.tt("pool", tmp[2][:, 0:nh, :], x0, tb(2), ALU.mult, [qkk, tabk, "a_sq"], ["a_t2"])
                        P.tt("dve", tmp[3][:, 0:nh, :], x1, tb(3), ALU.mult, [qkk, tabk], ["a_t3"])
                        P.tt("dve", qr4[:, h0:h1, :, 0], tmp[0][:, 0:nh, :], tmp[1][:, 0:nh, :], ALU.subtract,
                             ["a_t0", "a_t1"], [qrk])
                        P.tt("pool", qr4[:, h0:h1, :, 1], tmp[2][:, 0:nh, :], tmp[3][:, 0:nh, :], ALU.add,
                             ["a_t2", "a_t3"], [qrk])
                    if cfg.get("a1_lvl", 9) < 4:
                        continue
                    for hh in range(20):
                        bank = 3 + hh // 8
                        pv = ps[bank][:, :].bitcast(BF16)
                        P.tr(pv[0:64, (hh % 8) * 128:(hh % 8 + 1) * 128], qr[b][:, hh, :], identb[:],
                             [qrk, "identb"], [pk(bank)])
                    if cfg.get("a1_lvl", 9) < 5:
                        continue
                    P.cp("act", QTt[b][:, 0:8, :], ps[3][:, :].bitcast(BF16)[0:64, :].rearrange("p (h t) -> p h t", t=128),
                         [pk(3)], [qtk])
                    P.cp("act", QTt[b][:, 8:16, :], ps[4][:, :].bitcast(BF16)[0:64, :].rearrange("p (h t) -> p h t", t=128),
                         [pk(4)], [qtk])
                    P.cp("act", KT[0:64, :, t * 128:(t + 1) * 128],
                         ps[5][:, :].bitcast(BF16)[0:64, 0:512].rearrange("p (h t) -> p h t", t=128),
                         [pk(5)], [("a_KT", t)])
                    if cfg.get("a1_lvl", 9) < 6:
                        continue
                    P.dma("sp", QD[:, :, t * 128:(t + 1) * 128], QTt[b][:], reads=[qtk], writes=[("QD", t)], semkey=qtk)
                keys = ["a_hT", "a_wqkv", "a_gqk", "a_sq"] + ["a_t%d" % b for b in range(4)]
                for b in range(2):
                    keys += ["a_qk%d" % b, "a_ss%d" % b, "a_cs%d" % b, "a_tab%d" % b, "a_qr%d" % b, "a_QTt%d" % b]
                drain(keys)
            with contextlib.ExitStack() as s1:
                if cfg.get("skip_a2"):
                    return
                wo = P.sb(s1, "a_wo", [64, 16, D], BF16)
                P.dma("pool", wo[:], attn_w_o[j].rearrange("(h d) n -> d h n", d=64), writes=["a_wo"])
                for kv_ in range(4):
                    P.memset("pool", KT[64:128, kv_, :], 0.0, ["a_KTz"])
                QTb = [P.sb(s1, "a_QTb%d" % b, [128, 16, 512], BF16) for b in range(2)]
                for b_ in range(2):
                    P.memset("pool", QTb[b_][64:128, :, :], 0.0, ["a_QTbz"])
                aT = [P.sb(s1, "a_aT%d" % b, [64, 16, 512], BF16) for b in range(2)]
                Pb = [P.sb(s1, "a_P%d" % b, [128, 512], BF16) for b in range(3)]
                rec = P.sb(s1, "a_rec", [65, 512])
                bcs = P.sb(s1, "a_bcs", [64, 512])
                xo = [P.sb(s1, "a_xo%d" % b, [128, D]) for b in range(2)]
                tm = [P.sb(s1, "a_tm%d" % b, [128, D]) for b in range(2)]
                qblocks = []
                if ctx_out:
                    qblocks.append((0, CTX, [0, 1]))
                for qb in range(SEQ // 512):
                    qblocks.append((CTX + qb * 512, 512, list(range(NT))))
                pcnt = 0
                xcnt = 0
                for bi, (qo, nq, ktiles) in enumerate(qblocks):
                    b = bi % 2
                    qbk, atk = "a_QTb%d" % b, "a_aT%d" % b
                    P.dma("sp", QTb[b][0:64, :, 0:nq], QD[:, :, qo:qo + nq],
                          reads=[("QD", t) for t in range(qo // 128, (qo + nq) // 128)], writes=[qbk], semkey=qbk)
                    items = [(h, idx, kt) for h in range(NH) for idx, kt in enumerate(ktiles)]
                    LOOK = 2

                    def emit_S(n):
                        h_, idx_, kt_ = items[n]
                        P.mm(ps[n % 3][:, 0:nq], KT[:, h_ // 4, kt_ * 128:(kt_ + 1) * 128], QTb[b][:, h_, 0:nq], True, True,
                             [("a_KT", kt_), "a_KTz", "a_QTbz", qbk], [pk(n % 3)])

                    def fin1(h_):
                        ob_ = 3 + h_ % 2
                        P.recip(rec[64:65, 0:nq], ps[ob_][64:65, 0:nq], [pk(ob_)], ["a_rec"])

                    def fin2(h_):
                        ob_ = 3 + h_ % 2
                        P.mm(ps[5][0:64, 0:nq], ones[64:65, 0:64], rec[64:65, 0:nq], True, True, ["ones", "a_rec"], [pk(5)])
                        P.cp("dve", bcs[:, 0:nq], ps[5][0:64, 0:nq], [pk(5)], ["a_bcs"])
                        P.tt("dve", aT[b][:, h_, 0:nq], ps[ob_][0:64, 0:nq], bcs[:, 0:nq], ALU.mult,
                             [pk(ob_), "a_bcs"], [(atk, h_)])

                    for n in range(min(LOOK, len(items))):
                        emit_S(n)
                    pend = None
                    for n, (h, idx, kt) in enumerate(items):
                        if n + LOOK < len(items):
                            emit_S(n + LOOK)
                        kv = h // 4
                        ob = 3 + h % 2
                        pb = n % 3
                        P.act(Pb[pb][:, 0:nq], ps[n % 3][:, 0:nq], AF.Exp, [pk(n % 3)], ["a_P%d" % pb], scale=HD ** -0.5)
                        P.mm(ps[ob][0:65, 0:nq], Vg[:, kt, kv, 0:65], Pb[pb][:, 0:nq], idx == 0, idx == len(ktiles) - 1,
                             [("a_V", kt), "a_V", "a_P%d" % pb], [pk(ob)])
                        if pend is not None and (idx == min(3, len(ktiles) - 1)):
                            fin2(pend)
                            pend = None
                        if idx == len(ktiles) - 1:
                            fin1(h)
                            pend = h
                    if pend is not None:
                        fin2(pend)
                    for tt_ in range(nq // 128):
                        t = qo // 128 + tt_
                        s = 1 if t < 2 else 0
                        xb = xcnt % 2
                        xcnt += 1
                        xok, tmk = "a_xo%d" % xb, "a_tm%d" % xb
                        P.dma("sp", xo[xb][:], X[t * 128:(t + 1) * 128, :], reads=[xk(t)], writes=[xok], semkey=xok)
                        for half in range(2):
                            bank = 6 + half
                            for h in range(NH):
                                P.mm(ps[bank][:, :], aT[b][:, h, tt_ * 128:(tt_ + 1) * 128],
                                     wo[:, h, half * 512:(half + 1) * 512], h == 0, h == NH - 1,
                                     [(atk, h), "a_wo"], [pk(bank)])
                            P.tt("dve", tm[xb][:, half * 512:(half + 1) * 512], ps[bank][:, :],
                                 gates[:, 0, s, half * 512:(half + 1) * 512], ALU.mult,
                                 [pk(bank), ("gates", 0, s)], [tmk])
                        P.tt("pool", xo[xb][:], xo[xb][:], tm[xb][:], ALU.add, [xok, tmk], [xok])
                        P.dma("sp", X[t * 128:(t + 1) * 128, :], xo[xb][:], reads=[xok], writes=[xk(t)], semkey=xok)
                if dbg is not None:
                    dt_ = tm[0]
                    P.cp("dve", dt_[:, 0:512], KT[:, 0, 0:512], ["a_KTz"] + [("a_KT", t) for t in range(4)], ["a_dbgt", "a_tm0"])
                    P.cp("dve", dt_[:, 512:1024], QTb[0][:, 0, 0:512], ["a_QTbz", "a_QTb0"], ["a_dbgt", "a_tm0"])
                    P.dma("sp", dbg, dt_[:], reads=["a_dbgt"], writes=["dbg"])
                    drain(["a_dbgt", "a_tm0"])
                keys = ["a_wo", "a_rec", "a_bcs", "a_V", "a_KTz", "a_QTbz"] + ["a_P%d" % b for b in range(3)]
                for b in range(2):
                    keys += ["a_QTb%d" % b, "a_xo%d" % b, "a_tm%d" % b] + [("a_aT%d" % b, h) for h in range(NH)]
                keys += [("a_KT", t) for t in range(NT)] + [("a_V", t) for t in range(NT)]
                drain(keys)


    def pool_mixer(i):
        with contextlib.ExitStack() as st:
            bcm = P.sb(st, "p_bcm", [128, 2, 2, D])
            gmb = P.sb(st, "p_gmb", [128, 2, D])
            psg = P.sb(st, "p_psg", [128, 2, D])
            band = P.sb(st, "p_band", [128, 4, 5, 128])
            wp = P.sb(st, "p_wp", [128, 4, 2, 256])
            P.dma("sp", band[:], k_band.rearrange("w k p n -> p w k n"), writes=["p_band"])
            P.dma("sp", wp[:], pool_w[0].rearrange("g (cc p) n -> p g cc n", p=128), writes=["p_wp"])
            with contextlib.ExitStack() as s1:
                wblk = [P.sb(s1, "p_wblk%d" % b, [128, 8, 512]) for b in range(2)]
                cbc = P.sb(s1, "p_cbc", [128, 2, 8, 128])
                brow = P.sb(s1, "p_brow", [1, 2 * D])
                gb = P.sb(s1, "p_gb", [128, D])
                psb = P.sb(s1, "p_psb", [128, D])
                P.dma("sp", brow[:], b_ada[i][0:1, 0:2 * D], writes=["p_brow"])
                P.dma("sp", gb[:], norm_mix_g[i:i + 1, :].broadcast_to([128, D]), writes=["p_gb"])
                P.dma("sp", psb[:], pool_scale[0:1, :].broadcast_to([128, D]), writes=["p_psb"])
                for s_ in range(2):
                    P.cp("dve", cbc[:, s_], cT[:, :, s_:s_ + 1].broadcast_to([128, 8, 128]), ["cT"], ["p_cbc"])
                for n in range(4):
                    wb, wkey = wblk[n % 2], "p_wblk%d" % (n % 2)
                    P.dma("sp", wb[:], w_ada[i][:, n * 512:(n + 1) * 512].rearrange("(k p) n -> p k n", p=128),
                          writes=[wkey])
                    for s_ in range(2):
                        bank = 1 + s_
                        for k in range(8):
                            P.mm(ps[bank][:, :], cbc[:, s_, k, :], wb[:, k, :], k == 0, False, [wkey, "p_cbc"], [pk(bank)])
                        P.mm(ps[bank][:, :], ones[0:1, :], brow[0:1, n * 512:(n + 1) * 512], False, True,
                             ["ones", "p_brow"], [pk(bank)])
                        P.cp("act", bcm[:, s_, n // 2, (n % 2) * 512:(n % 2 + 1) * 512], ps[bank][:, :],
                             [pk(bank)], ["p_bcm"])
                for s_ in range(2):
                    P.stt("dve", gmb[:, s_, :], bcm[:, s_, 1, :], 1.0, gb[:], ALU.add, ALU.mult, ["p_bcm", "p_gb"], ["p_gmb"])
                    P.tt("pool", psg[:, s_, :], psb[:], gates[:, 0, s_, :], ALU.mult, ["p_psb", ("gates", 0, s_)], ["p_psg"])
                drain(["p_wblk0", "p_wblk1", "p_cbc", "p_brow", "p_gb", "p_psb"])
            xt = [P.sb(st, "p_x%d" % b, [128, D]) for b in range(4)]
            hh = [P.sb(st, "p_h%d" % b, [128, D]) for b in range(4)]
            dT = [P.sb(st, "p_dT%d" % b, [128, 8, 128]) for b in range(2)]
            tm = [P.sb(st, "p_tm%d" % b, [128, D]) for b in range(2)]
            st8 = [P.sb(st, "p_st%d" % b, [128, 4]) for b in range(4)]
            junk = P.sb(st, "p_junk", [128, D], BF16)

            def compute_h(t):
                b = t % 4
                s_ = 1 if t < 2 else 0
                xkey, hkey, skey = "p_x%d" % b, "p_h%d" % b, "p_st%d" % b
                P.dma("sp", xt[b][:], X[t * 128:(t + 1) * 128, :], reads=[xk(t)], writes=[xkey], semkey=xkey)
                P.act(junk[:], xt[b][:], AF.Square, [xkey], ["p_junk", skey], accum_out=st8[b][:, 0:1])
                P.act(st8[b][:, 1:2], st8[b][:, 0:1], AF.Sqrt, [skey], [skey], bias=EPS, scale=1.0 / D)
                P.recip(st8[b][:, 2:3], st8[b][:, 1:2], [skey], [skey])
                P.stt("dve", hh[b][:], xt[b][:], st8[b][:, 2:3], gmb[:, s_, :], ALU.mult, ALU.mult,
                      [xkey, skey, "p_gmb"], [hkey])
                P.tt("pool", hh[b][:], hh[b][:], bcm[:, s_, 0, :], ALU.add, [hkey, "p_bcm"], [hkey])

            done = set()
            for t in range(NT):
                first = t in (0, 2)
                last = t in (1, NT - 1)
                need = [t] + ([] if first else [t - 1]) + ([] if last else [t + 1])
                for tt_ in sorted(need):
                    if tt_ not in done:
                        compute_h(tt_)
                        done.add(tt_)
                s_ = 1 if t < 2 else 0
                db = t % 2
                dkey, tmk = "p_dT%d" % db, "p_tm%d" % db
                for c in range(8):
                    wi = c // 2
                    bank = c // 4
                    col = (c % 4) * 128
                    srcs = []
                    if not first:
                        srcs.append((t - 1, 0))
                    srcs.append((t, 1 if first else (3 if last else 2)))
                    if not last:
                        srcs.append((t + 1, 4))
                    for si, (tt_, kind) in enumerate(srcs):
                        P.mm(ps[bank][:, col:col + 128], hh[tt_ % 4][:, c * 128:(c + 1) * 128], band[:, wi, kind, :],
                             si == 0, si == len(srcs) - 1, ["p_h%d" % (tt_ % 4), "p_band"], [pk(bank)])
                for bank in range(2):
                    P.cp("act", dT[db][:, bank * 4:(bank + 1) * 4, :], ps[bank][:, :].rearrange("p (c t) -> p c t", t=128),
                         [pk(bank)], [dkey])
                for g in range(4):
                    bank = 2 + g // 2
                    for cc in range(2):
                        P.mm(ps[bank][:, (g % 2) * 256:(g % 2 + 1) * 256], dT[db][:, 2 * g + cc, :], wp[:, g, cc, :],
                             cc == 0, cc == 1, [dkey, "p_wp"], [pk(bank)])
                xb = t % 4
                for half in range(2):
                    P.tt("dve", tm[db][:, half * 512:(half + 1) * 512], ps[2 + half][:, :],
                         psg[:, s_, half * 512:(half + 1) * 512], ALU.mult, [pk(2 + half), "p_psg"], [tmk])
                P.tt("pool", tm[db][:], tm[db][:], xt[xb][:], ALU.add, [tmk, "p_x%d" % xb], [tmk])
                P.dma("sp", X[t * 128:(t + 1) * 128, :], tm[db][:], reads=[tmk], writes=[xk(t)], semkey=tmk)
            keys = ["p_bcm", "p_gmb", "p_psg", "p_band", "p_wp", "p_junk", "p_dT0", "p_dT1", "p_tm0", "p_tm1"]
            for b in range(4):
                keys += ["p_x%d" % b, "p_h%d" % b, "p_st%d" % b]
            drain(keys)


    def ssd_mixer(i):
        XT = P.dscratch("s_XT", [T, SSM_DI], BF16)
        BTK = P.dscratch("s_BTK", [T, 512], BF16)
        BF = P.dscratch("s_BF", [4, 128, T], BF16)
        CF = P.dscratch("s_CF", [4, 128, T], BF16)
        Yd = P.dscratch("s_Yd", [2, T, SSM_DI])
        w_in = ssm_w_in[0]
        NU = T + 3

        def xcol(t):
            return t * 128 if t < 2 else 259 + (t - 2) * 128

        ZG = P.dscratch("s_ZG", [T, SSM_DI], BF16)
        with contextlib.ExitStack() as st:
            with contextlib.ExitStack() as sA:
                dtA = P.sb(sA, "s_dtA", [128, NT, 64])
                LA = P.sb(sA, "s_LA", [128, NT, 64])
                sH = contextlib.ExitStack()
                hT = P.sb(sH, "s_hT", [128, 8, T], BF16)
                with contextlib.ExitStack() as s1:
                    wdt = P.sb(s1, "s_wdt", [128, 8, 64])
                    dtb = P.sb(s1, "s_dtb", [128, 64])
                    aB = P.sb(s1, "s_aB", [128, 64])
                    sp_ = P.sb(s1, "s_sp", [128, 4, 64])
                    P.dma("sp", wdt[:], w_in[:, 5120:5184].rearrange("(k p) n -> p k n", p=128), writes=["s_wdt"])
                    P.dma("sp", dtb[:], ssm_dt_bias[0:1, :].broadcast_to([128, 64]), writes=["s_dtb"])
                    P.dma("sp", aB[:], ssm_a_log[0:1, :].broadcast_to([128, 64]), writes=["s_aB"])
                    P.act(aB[:], aB[:], AF.Exp, ["s_aB"], ["s_aB"])
                    P.ts("dve", aB[:], aB[:], -1.0, ALU.mult, ["s_aB"], ["s_aB"])

                    def dthook(idx, t, h32, hkey):
                        for k in range(8):
                            P.mm(ps[5][:, 0:64], h32[:, k, :], wdt[:, k, :], k == 0, k == 7, [hkey, "s_wdt"], [pk(5)])
                        K_ = "s_sp"
                        xr, ab, ee = sp_[:, 0, :], sp_[:, 1, :], sp_[:, 2, :]
                        P.tt("dve", xr, ps[5][:, 0:64], dtb[:], ALU.add, [pk(5), "s_dtb"], [K_])
                        P.ts("dve", sp_[:, 3, :], xr, -1.0, ALU.mult, [K_], [K_])
                        P.tt("dve", ab, xr, sp_[:, 3, :], ALU.max, [K_], [K_])
                        P.act(ee, ab, AF.Exp, [K_], [K_], scale=-1.0)
                        P.act(ee, ee, AF.Ln, [K_], [K_], bias=1.0, scale=1.0)
                        P.stt("dve", dtA[:, t, :], xr, 0.0, ee, ALU.max, ALU.add, [K_], [("s_dtA", t)])
                        P.tt("pool", LA[:, t, :], dtA[:, t, :], aB[:], ALU.mult, [("s_dtA", t), "s_aB"], [("s_LA", t)])

                    with contextlib.ExitStack() as s2:
                        norm_tiles(s2, ALL, 0, hT, "s_hT", 0, hook=dthook, tag="sn")
                        drain(["sn_xt0", "sn_xt1", "sn_junk", "sn_st0", "sn_st1", "sn_h320", "sn_h321"])
                    drain(["s_wdt", "s_dtb", "s_sp"])
                with contextlib.ExitStack() as s1:
                    cwA = P.sb(s1, "s_cwA", [128, 24, 4])
                    cbA = P.sb(s1, "s_cbA", [128, 24])
                    U2 = [P.sb(s1, "s_U%d" % b, [128, T + 8]) for b in range(2)]
                    acc = P.sb(s1, "s_acc", [128, NU])
                    xc = [P.sb(s1, "s_xc%d" % b, [128, NU], BF16) for b in range(2)]
                    wc = [P.sb(s1, "s_wc%d" % b, [128, 8, 128], BF16) for b in range(2)]
                    stg = [P.sb(s1, "s_stg%d" % b, [128, NT, 128], BF16) for b in range(2)]
                    for k in range(4):
                        P.dma("sp", cwA[:, :, k], ssm_conv_w[0, k].rearrange("(c p) -> p c", p=128), writes=["s_cwA"],
                              allow_slow_non_contiguous=True)
                    P.dma("sp", cbA[:], ssm_conv_b[0].rearrange("(c p) -> p c", p=128), writes=["s_cbA"],
                          allow_slow_non_contiguous=True)
                    for b_ in range(2):
                        P.memset("dve" if b_ == 0 else "pool", U2[b_][:], 0.0, ["s_U%d" % b_])
                    blocks = [(0, 256, 2)] + [(256 + b * 512, 512, 261 + b * 512) for b in range(8)]
                    mcnt = [0]

                    def inproj(cc):
                        b = cc % 2
                        wck = "s_wc%d" % b
                        U, uk = U2[b], "s_U%d" % b
                        P.dma("pool", wc[b][:], w_in[:, 2048 + cc * 128:2048 + (cc + 1) * 128].rearrange("(k p) n -> p k n", p=128),
                              writes=[wck])
                        for (t0, n_, uo) in blocks:
                            bank = mcnt[0] % 2
                            mcnt[0] += 1
                            for k in range(8):
                                P.mm(ps[bank][:, 0:n_], wc[b][:, k, :], hT[:, k, t0:t0 + n_], k == 0, k == 7,
                                     [wck, "s_hT"], [pk(bank)])
                            P.cp("act", U[:, uo:uo + n_], ps[bank][:, 0:n_], [pk(bank)], [uk])

                    inproj(0)
                    for cc in range(24):
                        b = cc % 2
                        wck, xck, stk = "s_wc%d" % b, "s_xc%d" % b, "s_stg%d" % b
                        U, uk = U2[b], "s_U%d" % b
                        if cc + 1 < 24:
                            inproj(cc + 1)
                        ce = "dve"
                        P.ts(ce, acc[:], U[:, 0:NU], cwA[:, cc, 0:1], ALU.mult, [uk, "s_cwA"], ["s_acc"])
                        for k in range(1, 4):
                            P.stt(ce, acc[:], U[:, k:k + NU], cwA[:, cc, k:k + 1], acc[:], ALU.mult, ALU.add,
                                  [uk, "s_cwA", "s_acc"], ["s_acc"])
                        P.act(xc[b][:], acc[:], AF.Silu, ["s_acc", "s_cbA"], [xck], bias=cbA[:, cc:cc + 1], scale=1.0)
                        if cc >= 16:
                            g = (cc - 16) % 4
                            dst = BF if cc < 20 else CF
                            dk = "BF" if cc < 20 else "CF"
                            P.dma("sp", dst[g, :, 0:CTX], xc[b][:, 0:CTX], reads=[xck], writes=[(dk, g)], semkey=xck)
                            P.dma("sp", dst[g, :, CTX:T], xc[b][:, 259:259 + SEQ], reads=[xck], writes=[(dk, g)], semkey=xck)
                        if cc < 20:
                            for t in range(NT):
                                bank = 2 + (t // 8) % 4
                                pv = ps[bank][:, :].bitcast(BF16)
                                P.tr(pv[:, (t % 8) * 128:(t % 8 + 1) * 128], xc[b][:, xcol(t):xcol(t) + 128], identb[:],
                                     [xck, "identb"], [pk(bank)])
                                if t % 8 == 7 or t == NT - 1:
                                    t0 = (t // 8) * 8
                                    nt_ = t - t0 + 1
                                    P.cp("act", stg[b][:, t0:t0 + nt_, :],
                                         pv[:, 0:nt_ * 128].rearrange("p (t c) -> p t c", c=128), [pk(bank)], [stk])
                            if cc < 16:
                                dv = XT.rearrange("(t p) c -> p t c", p=128)[:, :, cc * 128:(cc + 1) * 128]
                                wk = ("XT", cc)
                            else:
                                dv = BTK.rearrange("(t p) c -> p t c", p=128)[:, :, (cc - 16) * 128:(cc - 15) * 128]
                                wk = ("BTK", cc - 16)
                            P.dma("sp", dv[:, 0:17, :], stg[b][:, 0:17, :], reads=[stk], writes=[wk], semkey=stk)
                            P.dma("sp", dv[:, 17:NT, :], stg[b][:, 17:NT, :], reads=[stk], writes=[wk], semkey=stk)
                    drain(["s_cwA", "s_cbA", "s_U0", "s_U1", "s_acc", "s_xc0", "s_xc1", "s_wc0", "s_wc1", "s_stg0", "s_stg1"])
                with contextlib.ExitStack() as s1:
                    wz = P.sb(s1, "s_wz", [128, 8, SSM_DI], BF16)
                    szb = [P.sb(s1, "s_szb%d" % b, [128, SSM_DI], BF16) for b in range(2)]
                    P.dma("pool", wz[:], w_in[:, 0:SSM_DI].rearrange("(k p) n -> p k n", p=128), writes=["s_wz"])
                    for t in range(NT):
                        b = t % 2
                        for nb in range(4):
                            bank = (t * 4 + nb) % 8
                            for k in range(8):
                                P.mm(ps[bank][:, :], hT[:, k, t * 128:(t + 1) * 128], wz[:, k, nb * 512:(nb + 1) * 512],
                                     k == 0, k == 7, ["s_hT", "s_wz"], [pk(bank)])
                            P.act(szb[b][:, nb * 512:(nb + 1) * 512], ps[bank][:, :], AF.Silu, [pk(bank)], ["s_szb%d" % b])
                        P.dma("sp", ZG[t * 128:(t + 1) * 128, :], szb[b][:], reads=["s_szb%d" % b], writes=[("ZG", t)],
                              semkey="s_szb%d" % b)
                    drain(["s_wz", "s_szb0", "s_szb1", "s_hT"])
                sH.close()
                with contextlib.ExitStack() as s1:
                    tri = P.sb(s1, "s_tri", [128, 4, 128])
                    state = P.sb(s1, "s_state", [128, 32, 64])
                    stbf = P.sb(s1, "s_stbf", [128, 32, 64], BF16)
                    xk_ = [P.sb(s1, "s_xk%d" % b, [128, 32, 64], BF16) for b in range(2)]
                    btk = [P.sb(s1, "s_btk%d" % b, [128, 512], BF16) for b in range(2)]
                    bfc = [P.sb(s1, "s_bfc%d" % b, [128, 4, 128], BF16) for b in range(2)]
                    cfc = [P.sb(s1, "s_cfc%d" % b, [128, 4, 128], BF16) for b in range(2)]
                    xdt = P.sb(s1, "s_xdt", [128, 32, 64], BF16)
                    xsd = P.sb(s1, "s_xsd", [128, 32, 64], BF16)
                    laB = P.sb(s1, "s_laB", [128, 32, 128])
                    CM = P.sb(s1, "s_CM", [128, 32, 128])
                    sm = P.sb(s1, "s_sm", [128, 6, 32])
                    GTs = [P.sb(s1, "s_GTs%d" % b, [128, 128]) for b in range(2)]
                    Lg = [P.sb(s1, "s_Lg%d" % b, [128, 4, 128]) for b in range(2)]
                    WT = [P.sb(s1, "s_WT%d" % b, [128, 4, 128], BF16) for b in range(2)]
                    yoff = [P.sb(s1, "s_yoff%d" % b, [128, 8, 64]) for b in range(2)]
                    ybuf = [P.sb(s1, "s_ybuf%d" % b, [128, 32, 64]) for b in range(2)]
                    P.dma("sp", tri[:], k_tri.rearrange("w p n -> p w n"), writes=["s_tri"])
                    ccount = 0
                    for d in range(2):
                        order = list(range(NT)) if d == 0 else [1, 0] + list(range(NT - 1, 1, -1))
                        Ut, Mneg = tri[:, 2 * d, :], tri[:, 2 * d + 1, :]
                        P.memset("pool", state[:], 0.0, [("s_state", g_) for g_ in range(4)])
                        for c in order:
                            b = ccount % 2
                            ccount += 1
                            xkk, btkk, bfk, cfk, ybk = "s_xk%d" % b, "s_btk%d" % b, "s_bfc%d" % b, "s_cfc%d" % b, "s_ybuf%d" % b
                            P.dma("sp", xk_[b][:], XT[c * 128:(c + 1) * 128, :].rearrange("p (h d) -> p h d", d=64),
                                  reads=[("XT", q) for q in range(16)], writes=[xkk], semkey=xkk)
                            P.dma("sp", btk[b][:], BTK[c * 128:(c + 1) * 128, :], reads=[("BTK", q) for q in range(4)],
                                  writes=[btkk], semkey=btkk)
                            P.dma("sp", bfc[b][:], BF[:, :, c * 128:(c + 1) * 128].rearrange("g n t -> n g t"),
                                  reads=[("BF", q) for q in range(4)], writes=[bfk], semkey=bfk)
                            P.dma("sp", cfc[b][:], CF[:, :, c * 128:(c + 1) * 128].rearrange("g n t -> n g t"),
                                  reads=[("CF", q) for q in range(4)], writes=[cfk], semkey=cfk)
                            la_c = LA[:, c, d * 32:(d + 1) * 32]
                            dt_c = dtA[:, c, d * 32:(d + 1) * 32]
                            lak, dtk = ("s_LA", c), ("s_dtA", c)
                            SM = "s_sm"
                            csc, tot, ff, fx, ecs, dec = (sm[:, q, :] for q in range(6))
                            P.mm(ps[0][:, 0:32], Ut, la_c, True, True, ["s_tri", lak], [pk(0)])
                            P.mm(ps[0][:, 32:64], ones[:], la_c, True, True, ["ones", lak], [pk(0)])
                            P.cp("dve", sm[:, 0:2, :], ps[0][:, 0:64].rearrange("p (a h) -> p a h", h=32), [pk(0)], [SM])
                            P.tt("dve", ff, tot, csc, ALU.subtract, [SM], [SM])
                            P.act(ff, ff, AF.Exp, [SM], [SM])
                            P.act(ecs, csc, AF.Exp, [SM], [SM])
                            P.act(dec, tot, AF.Exp, [SM], [SM])
                            P.tt("dve", fx, ff, dt_c, ALU.mult, [SM, dtk], [SM])
                            P.tt("pool", xdt[:], xk_[b][:], dt_c.unsqueeze(2).broadcast_to([128, 32, 64]), ALU.mult,
                                 [xkk, dtk], ["s_xdt"])
                            P.tt("pool", xsd[:], xk_[b][:], fx.unsqueeze(2).broadcast_to([128, 32, 64]), ALU.mult,
                                 [xkk, SM], ["s_xsd"])
                            P.cp("dve", laB[:], la_c.unsqueeze(2).broadcast_to([128, 32, 128]), [lak], ["s_laB"])
                            P.cp("act", stbf[:], state[:], [("s_state", g_) for g_ in range(4)], ["s_stbf"])
                            P.tt("dve", CM[:], csc.unsqueeze(2).broadcast_to([128, 32, 128]),
                                 Mneg.unsqueeze(1).broadcast_to([128, 32, 128]), ALU.subtract, [SM, "s_tri"], ["s_CM"])
                            def grp_begin(g):
                                P.mm(ps[1][:, 0:128], bfc[b][:, g, :], cfc[b][:, g, :], True, True, [bfk, cfk], [pk(1)])
                                P.cp("act", GTs[g % 2][:], ps[1][:, 0:128], [pk(1)], ["s_GTs%d" % (g % 2)])
                                P.mm(ps[2][:, :], cfc[b][:, g, :], stbf[:, g * 8:(g + 1) * 8, :].rearrange("p h d -> p (h d)"),
                                     True, True, [cfk, "s_stbf"], [pk(2)])
                                P.cp("act", yoff[g % 2][:], ps[2][:, :].rearrange("p (h d) -> p h d", d=64), [pk(2)],
                                     ["s_yoff%d" % (g % 2)])

                            def quad_cs(n):
                                g, q = n // 2, n % 2
                                if q == 0:
                                    grp_begin(g)
                                cb_ = 3 + n % 2
                                for j in range(4):
                                    P.mm(ps[cb_][:, j * 128:(j + 1) * 128], laB[:, g * 8 + q * 4 + j, :], Ut, True, True,
                                         ["s_laB", "s_tri"], [pk(cb_)])

                            def grp_end(g):
                                yb_ = 5 + g % 2
                                yo, yok = yoff[g % 2], "s_yoff%d" % (g % 2)
                                P.tt("pool", yo[:], yo[:], ecs[:, g * 8:(g + 1) * 8].unsqueeze(2).broadcast_to([128, 8, 64]),
                                     ALU.mult, [yok, SM], [yok])
                                P.tt("dve", ybuf[b][:, g * 8:(g + 1) * 8, :], yo[:],
                                     ps[yb_][:, :].rearrange("p (h d) -> p h d", d=64), ALU.add, [yok, pk(yb_)], [ybk])
                                P.mm(ps[7][:, :], btk[b][:, g * 128:(g + 1) * 128],
                                     xsd[:, g * 8:(g + 1) * 8, :].rearrange("p h d -> p (h d)"), True, True,
                                     [btkk, "s_xsd"], [pk(7)])
                                P.tt("pool", state[:, g * 8:(g + 1) * 8, :], state[:, g * 8:(g + 1) * 8, :],
                                     dec[:, g * 8:(g + 1) * 8].unsqueeze(2).broadcast_to([128, 8, 64]), ALU.mult,
                                     [("s_state", g), SM], [("s_state", g)])
                                P.tt("dve", state[:, g * 8:(g + 1) * 8, :], state[:, g * 8:(g + 1) * 8, :],
                                     ps[7][:, :].rearrange("p (h d) -> p h d", d=64), ALU.add, [("s_state", g), pk(7)],
                                     [("s_state", g)])

                            quad_cs(0)
                            for n in range(8):
                                g, q = n // 2, n % 2
                                h0 = g * 8 + q * 4
                                if n + 1 < 8:
                                    quad_cs(n + 1)
                                cb_ = 3 + n % 2
                                lb = n % 2
                                lgk, wtk = "s_Lg%d" % lb, "s_WT%d" % lb
                                csv = ps[cb_][:, :].rearrange("p (j l) -> p j l", l=128)
                                P.tt("dve", Lg[lb][:], csv, CM[:, h0:h0 + 4, :], ALU.subtract, [pk(cb_), "s_CM"], [lgk])
                                P.act(Lg[lb][:], Lg[lb][:], AF.Exp, [lgk], [lgk])
                                P.tt("dve", WT[lb][:], Lg[lb][:], GTs[g % 2][:].unsqueeze(1).broadcast_to([128, 4, 128]),
                                     ALU.mult, [lgk, "s_GTs%d" % (g % 2)], [wtk])
                                yb_ = 5 + g % 2
                                for j in range(4):
                                    P.mm(ps[yb_][:, (q * 4 + j) * 64:(q * 4 + j + 1) * 64], WT[lb][:, j, :], xdt[:, h0 + j, :],
                                         True, True, [wtk, "s_xdt"], [pk(yb_)])
                                if q == 1:
                                    grp_end(g)
                            P.dma("sp", Yd[d, c * 128:(c + 1) * 128, :], ybuf[b][:].rearrange("p h d -> p (h d)"),
                                  reads=[ybk], writes=[("Yd", d, c)], semkey=ybk)
                    keys = ["s_tri", "s_stbf", "s_xdt", "s_xsd", "s_laB", "s_CM", "s_sm", "s_GTs0", "s_GTs1", "s_yoff0", "s_yoff1"]
                    keys += [("s_state", g_) for g_ in range(4)]
                    for b in range(2):
                        keys += ["s_xk%d" % b, "s_btk%d" % b, "s_bfc%d" % b, "s_cfc%d" % b, "s_Lg%d" % b, "s_WT%d" % b,
                                 "s_ybuf%d" % b]
                    keys += [("s_dtA", t) for t in range(NT)] + [("s_LA", t) for t in range(NT)] + ["s_aB"]
                    drain(keys)
            with contextlib.ExitStack() as s1:
                wo = P.sb(s1, "s_wo", [128, 16, D], BF16)
                ng = P.sb(s1, "s_ng", [128, SSM_DI])
                dsk = P.sb(s1, "s_dsk", [128, 64])
                yf = [P.sb(s1, "s_yf%d" % b, [128, 32, 64]) for b in range(2)]
                yb2 = [P.sb(s1, "s_yb2%d" % b, [128, 32, 64]) for b in range(2)]
                xk3 = [P.sb(s1, "s_xk3%d" % b, [128, 32, 64], BF16) for b in range(2)]
                zg = [P.sb(s1, "s_zg%d" % b, [128, SSM_DI], BF16) for b in range(2)]
                xo = [P.sb(s1, "s_xo%d" % b, [128, D]) for b in range(2)]
                sz_ = [P.sb(s1, "s_sz%d" % b, [128, SSM_DI]) for b in range(2)]
                gbf_ = [P.sb(s1, "s_gbf%d" % b, [128, SSM_DI], BF16) for b in range(2)]
                gT = [P.sb(s1, "s_gT%d" % b, [128, 16, 128], BF16) for b in range(2)]
                g4_ = [P.sb(s1, "s_g4%d" % b, [128, 12]) for b in range(2)]
                junk = P.sb(s1, "s_junk3", [128, 512], BF16)
                tm = [P.sb(s1, "s_tm%d" % b, [128, D]) for b in range(2)]
                P.dma("pool", wo[:], ssm_w_out[0].rearrange("(k p) n -> p k n", p=128), writes=["s_wo"])
                P.dma("sp", ng[:], ssm_norm_g[0:1, :].broadcast_to([128, SSM_DI]), writes=["s_ng"])
                P.dma("sp", dsk[:], ssm_d[0:1, :].broadcast_to([128, 64]), writes=["s_dsk"])
                P.tt("dve", dsk[:, 0:32], dsk[:, 0:32], dsk[:, 32:64], ALU.add, ["s_dsk"], ["s_dsk"])

                def s3load(t):
                    b = t % 2
                    P.dma("sp", yf[b][:], Yd[0, t * 128:(t + 1) * 128, :].rearrange("p (h d) -> p h d", d=64),
                          reads=[("Yd", 0, t)], writes=["s_yf%d" % b], semkey="s_yf%d" % b)
                    P.dma("sp", yb2[b][:], Yd[1, t * 128:(t + 1) * 128, :].rearrange("p (h d) -> p h d", d=64),
                          reads=[("Yd", 1, t)], writes=["s_yb2%d" % b], semkey="s_yb2%d" % b)
                    P.dma("sp", xk3[b][:], XT[t * 128:(t + 1) * 128, :].rearrange("p (h d) -> p h d", d=64),
                          reads=[("XT", q) for q in range(16)], writes=["s_xk3%d" % b], semkey="s_xk3%d" % b)
                    P.dma("sp", zg[b][:], ZG[t * 128:(t + 1) * 128, :], reads=[("ZG", t)], writes=["s_zg%d" % b],
                          semkey="s_zg%d" % b)
                    P.dma("sp", xo[b][:], X[t * 128:(t + 1) * 128, :], reads=[xk(t)], writes=["s_xo%d" % b], semkey="s_xo%d" % b)

                s3load(0)
                for t in range(NT):
                    if t + 1 < NT:
                        s3load(t + 1)
                    b = t % 2
                    s_ = 1 if t < 2 else 0
                    yfk, ybk, xkk, zgk, xok, gtk, tmk = ("s_yf%d" % b, "s_yb2%d" % b, "s_xk3%d" % b, "s_zg%d" % b, "s_xo%d" % b,
                                                         "s_gT%d" % b, "s_tm%d" % b)
                    P.tt("dve", yf[b][:], yf[b][:], yb2[b][:], ALU.add, [yfk, ybk], [yfk])
                    P.tt("pool", yb2[b][:], xk3[b][:], dsk[:, 0:32].unsqueeze(2).broadcast_to([128, 32, 64]), ALU.mult,
                         [xkk, "s_dsk"], [ybk])
                    P.tt("pool", yf[b][:], yf[b][:], yb2[b][:], ALU.add, [yfk, ybk], [yfk])
                    sz, gbf, g4 = sz_[b], gbf_[b], g4_[b]
                    szk, gbk, g4k = "s_sz%d" % b, "s_gbf%d" % b, "s_g4%d" % b
                    yfl = yf[b][:].rearrange("p h d -> p (h d)")
                    P.tt("dve", sz[:], zg[b][:], yfl, ALU.mult, [zgk, yfk], [szk])
                    for q in range(4):
                        P.act(junk[:], sz[:, q * 512:(q + 1) * 512], AF.Square, [szk], ["s_junk3", g4k],
                              accum_out=g4[:, q:q + 1])
                    P.act(g4[:, 4:8], g4[:, 0:4], AF.Sqrt, [g4k], [g4k], bias=EPS, scale=1.0 / 512)
                    P.recip(g4[:, 8:12], g4[:, 4:8], [g4k], [g4k])
                    for q in range(4):
                        P.stt("dve", gbf[:, q * 512:(q + 1) * 512], sz[:, q * 512:(q + 1) * 512], g4[:, 8 + q:9 + q],
                              ng[:, q * 512:(q + 1) * 512], ALU.mult, ALU.mult, [szk, g4k, "s_ng"], [gbk])
                    for k in range(16):
                        bank = (0 if b == 0 else 2) + k // 8
                        pv = ps[bank][:, :].bitcast(BF16)
                        P.tr(pv[:, (k % 8) * 128:(k % 8 + 1) * 128], gbf[:, k * 128:(k + 1) * 128], identb[:],
                             [gbk, "identb"], [pk(bank)])
                    for q in range(2):
                        bank = (0 if b == 0 else 2) + q
                        P.cp("act", gT[b][:, q * 8:(q + 1) * 8, :],
                             ps[bank][:, :].bitcast(BF16).rearrange("p (k t) -> p k t", t=128), [pk(bank)], [gtk])
                    for half in range(2):
                        bank = 4 + 2 * b + half
                        for k in range(16):
                            P.mm(ps[bank][:, :], gT[b][:, k, :], wo[:, k, half * 512:(half + 1) * 512], k == 0, k == 15,
                                 [gtk, "s_wo"], [pk(bank)])
                        P.tt("dve", tm[b][:, half * 512:(half + 1) * 512], ps[bank][:, :],
                             gates[:, 0, s_, half * 512:(half + 1) * 512], ALU.mult, [pk(bank), ("gates", 0, s_)], [tmk])
                    P.tt("pool", xo[b][:], xo[b][:], tm[b][:], ALU.add, [xok, tmk], [xok])
                    P.dma("sp", X[t * 128:(t + 1) * 128, :], xo[b][:], reads=[xok], writes=[xk(t)], semkey=xok)
                keys = ["s_wo", "s_ng", "s_dsk", "s_junk3"]
                for b in range(2):
                    keys += ["s_sz%d" % b, "s_gbf%d" % b, "s_g4%d" % b]
                    keys += ["s_yf%d" % b, "s_yb2%d" % b, "s_xk3%d" % b, "s_zg%d" % b, "s_xo%d" % b, "s_gT%d" % b, "s_tm%d" % b]
                drain(keys)

    def moe_sparse(i, tiles):
        ntl = len(tiles)
        Hs = P.dscratch("ms_Hs%d" % i, [NSLOT, D], BF16)
        Z = P.dscratch("ms_Z%d" % i, [NSLOT, D])
        wgu_rows = moe_w_gu[i]
        wdn_rows = moe_w_dn[i]
        IOA = bass.IndirectOffsetOnAxis
        with contextlib.ExitStack() as st:
            WW = P.sb(st, "q_WW", [128, ntl, 2])
            POSI = P.sb(st, "q_POSI", [128, ntl, 2], I32)
            OFFGU = P.sb(st, "q_OFFGU", [128, NBLK], I32)
            pst = P.sb(st, "q_pst", [128, NE])
            km = P.sb(st, "q_km", [128, 128])
            P.dma("sp", km[:], k_moe, writes=["q_km"])
            with contextlib.ExitStack() as s1:
                HTOK = P.sb(s1, "q_HTOK", [128, ntl, D], BF16)
                OH = P.sb(s1, "q_OH", [128, ntl, 3, NE])
                wr = P.sb(s1, "q_wr", [128, 8, 36])
                rt = P.sb(s1, "q_rt", [128, 160])
                hT2 = P.sb(s1, "q_hT2", [128, 8, 256], BF16)
                P.dma("sp", wr[:], moe_wr[i].rearrange("(k p) n -> p k n", p=128), writes=["q_wr"])

                LG = P.sb(s1, "q_LG", [128, ntl, 36])

                def router(idx, t, h32, hkey):
                    lg = ps[5]
                    for k in range(8):
                        P.mm(lg[:, 0:36], h32[:, k, :], wr[:, k, :], k == 0, k == 7, [hkey, "q_wr"], [pk(5)])
                    P.cp("dve", LG[:, idx, :], lg[:, 0:36], [pk(5)], [("q_LG", idx)])
                    c0 = (idx % 2) * 128
                    pv = ps[3][:, :].bitcast(BF16)
                    for c in range(8):
                        P.tr(pv[:, c * 128:(c + 1) * 128], hT2[:, c, c0:c0 + 128], identb[:],
                             [("q_hT2", idx % 2), "identb"], [pk(3)])
                    P.cp("act", HTOK[:, idx, :], pv[:, :], [pk(3)], [("q_HTOK", idx)])

                with contextlib.ExitStack() as s2:
                    norm_tiles(s2, tiles, 1, hT2, "q_hT2", 0, hook=router, tag="qn", colfn=lambda idx: (idx % 2) * 128)
                    drain(["qn_xt0", "qn_xt1", "qn_junk", "qn_st0", "qn_st1", "qn_h320", "qn_h321"])
                R = "q_rt"
                rb = P.sb(s1, "q_rb", [128, 8, ntl])
                g4 = P.sb(s1, "q_g4", [128, 2, ntl, 4])
                le = P.sb(s1, "q_le", [128, 2, ntl, NE])
                lgk = [("q_LG", idx) for idx in range(ntl)]
                ohk = [("q_OH", idx) for idx in range(ntl)]
                wwk = [("q_WW", idx) for idx in range(ntl)]
                LGg, LGe = LG[:, :, 0:4], LG[:, :, 4:36]
                gmax, gate, m1, m2, dd, p1, p2 = (rb[:, q, :] for q in range(7))
                bc4 = lambda v: v.unsqueeze(2).broadcast_to([128, ntl, 4])
                bc32 = lambda v: v.unsqueeze(2).broadcast_to([128, ntl, NE])
                P.red("dve", gmax, LGg, ALU.max, lgk, [R])
                P.tt("dve", g4[:, 0], LGg, bc4(gmax), ALU.subtract, lgk + [R], [R])
                P.act(g4[:, 0], g4[:, 0], AF.Exp, [R], [R])
                P.red("dve", gate, g4[:, 0], ALU.add, [R], [R])
                P.recip(gate, gate, [R], [R])
                P.tt("dve", g4[:, 1], LGg, bc4(gmax), ALU.is_ge, lgk + [R], [R])
                P.ts("dve", g4[:, 1], g4[:, 1], -1.0, ALU.add, [R], [R], s2=BIG, op1=ALU.mult)
                P.tt("dve", le[:, 0].rearrange("p t (g j) -> p t g j", g=4), LGe.rearrange("p t (g j) -> p t g j", g=4),
                     g4[:, 1].unsqueeze(3).broadcast_to([128, ntl, 4, 8]), ALU.add, lgk + [R], [R])
                P.red("dve", m1, le[:, 0], ALU.max, [R], [R])
                oh1, oh2, oha = OH[:, :, 0, :], OH[:, :, 1, :], OH[:, :, 2, :]
                P.tt("dve", oh1, le[:, 0], bc32(m1), ALU.is_ge, [R], ohk)
                P.stt("dve", le[:, 1], oh1, -BIG, le[:, 0], ALU.mult, ALU.add, [R] + ohk, [R])
                P.red("dve", m2, le[:, 1], ALU.max, [R], [R])
                P.tt("dve", oh2, le[:, 1], bc32(m2), ALU.is_ge, [R], ohk)
                P.tt("pool", oha, oh1, oh2, ALU.add, ohk, ohk)
                P.tt("dve", dd, m2, m1, ALU.subtract, [R], [R])
                P.act(dd, dd, AF.Exp, [R], [R])
                P.ts("dve", p1, dd, 1.0, ALU.add, [R], [R])
                P.recip(p1, p1, [R], [R])
                P.tt("dve", p2, dd, p1, ALU.mult, [R], [R])
                P.tt("dve", WW[:, :, 0], p1, gate, ALU.mult, [R], wwk)
                P.tt("dve", WW[:, :, 1], p2, gate, ALU.mult, [R], wwk)
                for idx in range(ntl):
                    P.mm(ps[4][:, 0:NE], ones[:], OH[:, idx, 2, :], idx == 0, idx == ntl - 1, ["ones", ("q_OH", idx)], [pk(4)])
                ob = P.sb(s1, "q_ob", [128, 8, NE])
                c3 = P.sb(s1, "q_c3", [128, NBLK, NE])
                be = P.sb(s1, "q_be", [128, NBLK])
                O_ = "q_ob"
                cnt, nb_, pend, tmpa = ob[:, 0, :], ob[:, 1, :], ob[:, 2, :], ob[:, 3, :]
                P.cp("dve", cnt, ps[4][:, 0:NE], [pk(4)], [O_])
                P.tt("dve", c3[:, 0:NE, 0:18].rearrange("p e m -> p e m") if False else c3[:, 0:18, :],
                     cnt.unsqueeze(1).broadcast_to([128, 18, NE]), km[:, 64:82].unsqueeze(2).broadcast_to([128, 18, NE]),
                     ALU.is_gt, [O_, "q_km"], ["q_c3"])
                P.red("dve", nb_, c3[:, 0:18, :].rearrange("p m e -> p e m"), ALU.add, ["q_c3"], [O_])
                P.ts("dve", nb_, nb_, float(BLK), ALU.mult, [O_], [O_])
                P.cp("dve", pend, nb_, [O_], [O_])
                src, dst = pend, tmpa
                for sft in (1, 2, 4, 8, 16):
                    P.cp("dve", dst[:, 0:sft], src[:, 0:sft], [O_], [O_])
                    P.tt("dve", dst[:, sft:NE], src[:, sft:NE], src[:, 0:NE - sft], ALU.add, [O_], [O_])
                    src, dst = dst, src
                pend_f = src
                P.tt("dve", pst[:], pend_f, nb_, ALU.subtract, [O_], ["q_pst"])
                P.tt("dve", c3[:], pend_f.unsqueeze(1).broadcast_to([128, NBLK, NE]),
                     km[:, 0:NBLK].unsqueeze(2).broadcast_to([128, NBLK, NE]), ALU.is_le, [O_, "q_km"], ["q_c3"])
                P.red("dve", be[:], c3[:], ALU.add, ["q_c3"], ["q_be"])
                P.ts("dve", be[:], be[:], float(NE - 1), ALU.min, ["q_be"], ["q_be"])
                P.ts("dve", be[:], be[:], 128.0, ALU.mult, ["q_be"], ["q_be"])
                P.tt("dve", be[:], be[:], km[:, 96:97].broadcast_to([128, NBLK]), ALU.add, ["q_be", "q_km"], ["q_be"])
                sk_ = c3[:, 0:2, :].rearrange("p a e -> p (a e)")[:, 0:NBLK - 1]
                P.tt("dve", sk_, be[:, 1:NBLK], be[:, 0:NBLK - 1], ALU.is_equal, ["q_be"], ["q_c3"])
                P.stt("dve", be[:, 1:NBLK], sk_, 1.0e6, be[:, 1:NBLK], ALU.mult, ALU.add, ["q_c3", "q_be"], ["q_be"])
                P.cp("dve", OFFGU[:], be[:], ["q_be"], ["q_OFFGU"])
                ustr = P.sb(s1, "q_ustr", [128, 128])
                trl = P.sb(s1, "q_trl", [128, 128])
                P.dma("sp", trl[:], k_tri[0], writes=["q_trl"])
                P.tt("dve", ustr[:], trl[:], ident[:], ALU.subtract, ["q_trl", "ident"], ["q_ustr"])
                rk = P.sb(s1, "q_rk", [128, 3, ntl, NE])
                posf = P.sb(s1, "q_posf", [128, ntl, 2])
                ohk2 = [("q_OH", idx) for idx in range(ntl)]
                for idx in range(ntl):
                    bank, col = idx // 16, (idx % 16) * NE
                    P.mm(ps[bank][:, col:col + NE], ustr[:], OH[:, idx, 2, :], True, True, ["q_ustr", ("q_OH", idx)], [pk(bank)])
                    P.mm(ps[3 + bank][:, col:col + NE], ones[:], OH[:, idx, 2, :], True, True, ["ones", ("q_OH", idx)], [pk(3 + bank)])
                for bank in range((ntl + 15) // 16):
                    n_ = min(16, ntl - bank * 16)
                    P.cp("act", rk[:, 0, bank * 16:bank * 16 + n_, :], ps[bank][:, 0:n_ * NE].rearrange("p (t e) -> p t e", e=NE),
                         [pk(bank)], ["q_rk0"])
                    P.cp("dve", rk[:, 1, bank * 16:bank * 16 + n_, :], ps[3 + bank][:, 0:n_ * NE].rearrange("p (t e) -> p t e", e=NE),
                         [pk(3 + bank)], ["q_rk1"])
                src, dst, sk1, dk1 = 1, 2, "q_rk1", "q_rk2"
                sft = 1
                while sft < ntl:
                    P.cp("dve", rk[:, dst, 0:sft, :], rk[:, src, 0:sft, :], [sk1], [dk1])
                    P.tt("dve", rk[:, dst, sft:ntl, :], rk[:, src, sft:ntl, :], rk[:, src, 0:ntl - sft, :], ALU.add, [sk1], [dk1])
                    src, dst, sk1, dk1 = dst, src, dk1, sk1
                    sft *= 2
                for bank in range((ntl + 15) // 16):
                    n_ = min(16, ntl - bank * 16)
                    P.tt("dve", rk[:, src, bank * 16:bank * 16 + n_, :], rk[:, src, bank * 16:bank * 16 + n_, :],
                         ps[3 + bank][:, 0:n_ * NE].rearrange("p (t e) -> p t e", e=NE), ALU.subtract, [sk1, pk(3 + bank)], [sk1])
                P.tt("dve", rk[:, 0], rk[:, 0], rk[:, src], ALU.add, ["q_rk0", sk1], ["q_rk0"])
                P.tt("dve", rk[:, 0], rk[:, 0], pst[:].unsqueeze(1).broadcast_to([128, ntl, NE]), ALU.add, ["q_rk0", "q_pst"], ["q_rk0"])
                for k2 in range(2):
                    P.tt("dve", rk[:, dst], rk[:, 0], OH[:, :, k2, :], ALU.mult, ["q_rk0", dk1] + ohk2, [dk1])
                    P.red("dve", posf[:, :, k2], rk[:, dst], ALU.add, [dk1], ["q_posf"])
                P.cp("dve", POSI[:], posf[:], ["q_posf"], ["q_POSI"])
                for idx in range(ntl):
                    for k2 in range(2):
                        S.idma(Hs[:, :], IOA(ap=POSI[:, idx, k2:k2 + 1], axis=0), HTOK[:, idx, :], None, NSLOT - 1,
                               reads=[("q_HTOK", idx), "q_POSI"], writes=["Hs"], semkey="q_scat")
                drain(["q_wr", "q_rt", "q_rb", "q_g4", "q_le"] + [("q_LG", idx) for idx in range(ntl)] + ["q_hT2", ("q_hT2", 0), ("q_hT2", 1), "q_ob", "q_c3", "q_be", "q_ustr", "q_trl",
                       "q_rk0", "q_rk1", "q_rk2", "q_posf"] + [("q_HTOK", idx) for idx in range(ntl)] + [("q_OH", idx) for idx in range(ntl)])
            with contextlib.ExitStack() as s1:
                w32g = P.sb(s1, "q_w32g", [128, 8, 2 * FH])
                w32d = P.sb(s1, "q_w32d", [128, 4, D])
                wgu = [P.sb(s1, "q_wgu%d" % b, [128, 8, 2 * FH], BF16) for b in range(2)]
                wdn = [P.sb(s1, "q_wdn%d" % b, [128, 4, D], BF16) for b in range(2)]
                hs = [P.sb(s1, "q_hs%d" % b, [128, 4, D], BF16) for b in range(2)]
                hTs = P.sb(s1, "q_hTs", [128, 8, BLK], BF16)
                sg = [P.sb(s1, "q_sg%d" % b, [128, BLK], BF16) for b in range(2)]
                aT = P.sb(s1, "q_aT", [128, 4, BLK], BF16)
                zt = [P.sb(s1, "q_zt%d" % b, [128, D]) for b in range(2)]

                def gatherw(j):
                    b = j % 2
                    S.idma(w32g[:].rearrange("p k n -> p (k n)"), None, wgu_rows, IOA(ap=OFFGU[:, j:j + 1], axis=0), NE * 128 - 1,
                           reads=["q_OFFGU"], writes=[("q_w32g", k) for k in range(8)], semkey="q_w32g")
                    S.idma(w32d[:].rearrange("p k n -> p (k n)"), None, wdn_rows, IOA(ap=OFFGU[:, j:j + 1], axis=0), NE * 128 - 1,
                           reads=["q_OFFGU"], writes=[("q_w32d", k) for k in range(4)], semkey="q_w32d")
                    P.dma("sp", hs[b][:], Hs[j * BLK:(j + 1) * BLK, :].rearrange("(s p) d -> p s d", p=128),
                          reads=["Hs"], writes=["q_hs%d" % b], semkey="q_hs%d" % b)

                def castw(j):
                    b = j % 2
                    for k in range(8):
                        eng_ = "act" if k % 2 == 0 else "dve"
                        P.cp(eng_, wgu[b][:, k, :], w32g[:, k, :], [("q_w32g", k)], [("q_wgu%d" % b, k)])
                    for k in range(4):
                        P.cp("act" if k % 2 == 0 else "dve", wdn[b][:, k, :], w32d[:, k, :], [("q_w32d", k)],
                             [("q_wdn%d" % b, k)])

                def slots_T(j):
                    b = j % 2
                    for s_ in range(4):
                        bank = 4 + s_
                        pv = ps[bank][:, :].bitcast(BF16)
                        for c in range(8):
                            P.tr(pv[:, c * 128:(c + 1) * 128], hs[b][:, s_, c * 128:(c + 1) * 128], identb[:],
                                 ["q_hs%d" % b, "identb"], [pk(bank)])
                        P.cp("act" if s_ % 2 == 0 else "dve", hTs[:, :, s_ * 128:(s_ + 1) * 128],
                             pv[:, :].rearrange("p (c t) -> p c t", t=128), [pk(bank)], [("q_hTs", s_)])

                gatherw(0)
                castw(0)
                slots_T(0)
                zc = 0
                for j in range(NBLK):
                    b = j % 2
                    if j + 1 < NBLK:
                        gatherw(j + 1)
                    gkeys = [("q_wgu%d" % b, k) for k in range(8)]
                    dkeys = [("q_wdn%d" % b, k) for k in range(4)]
                    hkeys = [("q_hTs", s_) for s_ in range(4)]
                    for jj in range(4):
                        gb, ub = (jj % 2), 2 + (jj % 2)
                        for k in range(8):
                            P.mm(ps[gb][:, :], wgu[b][:, k, jj * 128:(jj + 1) * 128], hTs[:, k, :], k == 0, k == 7,
                                 [gkeys[k]] + hkeys, [pk(gb)])
                        for k in range(8):
                            P.mm(ps[ub][:, :], wgu[b][:, k, FH + jj * 128:FH + (jj + 1) * 128], hTs[:, k, :], k == 0, k == 7,
                                 [gkeys[k]] + hkeys, [pk(ub)])
                        sk = "q_sg%d" % (jj % 2)
                        P.act(sg[jj % 2][:], ps[gb][:, :], AF.Silu, [pk(gb)], [sk])
                        P.tt("dve", aT[:, jj, :], sg[jj % 2][:], ps[ub][:, :], ALU.mult, [sk, pk(ub)], [("q_aT", jj)])
                    for tt_ in range(4):
                        zb = zc % 2
                        zc += 1
                        zk = "q_zt%d" % zb
                        for half in range(2):
                            db = 4 + half
                            for jj in range(4):
                                P.mm(ps[db][:, :], aT[:, jj, tt_ * 128:(tt_ + 1) * 128], wdn[b][:, jj, half * 512:(half + 1) * 512],
                                     jj == 0, jj == 3, [("q_aT", jj), dkeys[jj]], [pk(db)])
                            P.cp("act" if half == 0 else "dve", zt[zb][:, half * 512:(half + 1) * 512], ps[db][:, :],
                                 [pk(db)], [zk])
                        r0 = j * BLK + tt_ * 128
                        P.dma("sp", Z[r0:r0 + 128, :], zt[zb][:], reads=[zk], writes=["Z"], semkey=zk)
                    if j + 1 < NBLK:
                        slots_T(j + 1)
                        castw(j + 1)
                keys = ["q_hTs", "q_sg0", "q_sg1", "q_zt0", "q_zt1", "q_hs0", "q_hs1"]
                keys += [("q_w32g", k) for k in range(8)] + [("q_w32d", k) for k in range(4)]
                keys += [("q_wgu%d" % b, k) for b in range(2) for k in range(8)]
                keys += [("q_wdn%d" % b, k) for b in range(2) for k in range(4)]
                keys += [("q_aT", jj) for jj in range(4)] + [("q_hTs", s_) for s_ in range(4)]
                drain(keys)
            with contextlib.ExitStack() as s1:
                NBUF = 4
                z1 = [P.sb(s1, "q_z1%d" % b, [128, D]) for b in range(NBUF)]
                z2 = [P.sb(s1, "q_z2%d" % b, [128, D]) for b in range(NBUF)]
                xo = [P.sb(s1, "q_xo%d" % b, [128, D]) for b in range(NBUF)]

                def cload(idx):
                    b = idx % NBUF
                    t = tiles[idx]
                    S.idma(z1[b][:], None, Z[:, :], IOA(ap=POSI[:, idx, 0:1], axis=0), NSLOT - 1,
                           reads=["Z", "q_POSI"], writes=["q_z1%d" % b], semkey="q_z1%d" % b)
                    S.idma(z2[b][:], None, Z[:, :], IOA(ap=POSI[:, idx, 1:2], axis=0), NSLOT - 1,
                           reads=["Z", "q_POSI"], writes=["q_z2%d" % b], semkey="q_z2%d" % b)
                    P.dma("sp", xo[b][:], X[t * 128:(t + 1) * 128, :], reads=[xk(t)], writes=["q_xo%d" % b], semkey="q_xo%d" % b)

                for idx in range(min(NBUF - 1, ntl)):
                    cload(idx)
                for idx, t in enumerate(tiles):
                    if idx + NBUF - 1 < ntl:
                        cload(idx + NBUF - 1)
                    b = idx % NBUF
                    s_ = 1 if t < 2 else 0
                    k1, k2_, ok = "q_z1%d" % b, "q_z2%d" % b, "q_xo%d" % b
                    P.ts("dve", z1[b][:], z1[b][:], WW[:, idx, 0:1], ALU.mult, [k1, ("q_WW", idx)], [k1])
                    P.stt("dve", z1[b][:], z2[b][:], WW[:, idx, 1:2], z1[b][:], ALU.mult, ALU.add, [k1, k2_, ("q_WW", idx)], [k1])
                    P.tt("pool", z1[b][:], z1[b][:], gates[:, 1, s_, :], ALU.mult, [k1, ("gates", 1, s_)], [k1])
                    P.tt("dve", xo[b][:], xo[b][:], z1[b][:], ALU.add, [ok, k1], [ok])
                    P.dma("sp", X[t * 128:(t + 1) * 128, :], xo[b][:], reads=[ok], writes=[xk(t)], semkey=ok)
                drain(["q_z1%d" % b for b in range(4)] + ["q_z2%d" % b for b in range(4)] + ["q_xo%d" % b for b in range(4)] + ["q_POSI", "q_OFFGU", "q_pst", "q_km",
                       "Hs", "Z"] + [("q_WW", idx) for idx in range(ntl)])

    def final_norm():
        with contextlib.ExitStack() as st:
            gb = P.sb(st, "f_g", [128, D])
            xt = [P.sb(st, "f_x%d" % j, [128, D]) for j in range(2)]
            junk = P.sb(st, "f_junk", [128, D])
            st8 = [P.sb(st, "f_st%d" % j, [128, 4]) for j in range(2)]
            P.dma("sp", gb[:], final_g.partition_broadcast(128) if False else final_g[0:1, :].broadcast_to([128, D]),
                  writes=["f_g"])
            for idx in range(SEQ // 128):
                t = idx + 2
                b = idx % 2
                xkey, skey = "f_x%d" % b, "f_st%d" % b
                P.dma("sp", xt[b][:], X[t * 128:(t + 1) * 128, :], reads=[xk(t)], writes=[xkey], semkey=xkey)
                P.act(junk[:], xt[b][:], AF.Square, [xkey], ["f_junk", skey], accum_out=st8[b][:, 0:1])
                P.act(st8[b][:, 1:2], st8[b][:, 0:1], AF.Sqrt, [skey], [skey], bias=EPS, scale=1.0 / D)
                P.recip(st8[b][:, 2:3], st8[b][:, 1:2], [skey], [skey])
                P.stt("dve", xt[b][:], xt[b][:], st8[b][:, 2:3], gb[:], ALU.mult, ALU.mult, [xkey, skey, "f_g"], [xkey])
                P.dma("sp", out[idx * 128:(idx + 1) * 128, :], xt[b][:], reads=[xkey], writes=[("out", idx)],
                      semkey=xkey)
            for e_ in ("sp", "act", "dve"):
                S.wait_all(e_, ["f_g", "f_x0", "f_x1", "f_junk", "f_st0", "f_st1"])

    ALL = list(range(NT))
    LAT = list(range(2, NT))
    for i in layers:
        kind = i % 3
        ctx_out = i < DEPTH - 1
        adaln(i, kind == 1)
        S.recycle()
        if mixers_on:
            if kind == 0:
                attention(i, i // 3, ctx_out)
            elif kind == 1:
                pool_mixer(i)
            else:
                ssd_mixer(i)
            S.recycle()
        if moe_on:
            if cfg.get("dense_moe"):
                moe(i, ALL if ctx_out else LAT)
            else:
                moe_sparse(i, ALL if ctx_out else LAT)
            S.recycle()
    final_norm()
    for e_ in ("sp", "pe", "act", "dve", "pool"):
        S.wait_all(e_, list(S.wr.keys()))
    P.es.close()
    return P


def host_consts():
    ident = np.eye(128, dtype=np.float32)
    n_freq = HD // 4
    inv = (10000.0 ** (-np.arange(n_freq, dtype=np.float32) / n_freq)).astype(np.float32)
    tok = np.arange(SEQ)
    row = (tok // GRID_W).astype(np.float32)
    col = (tok % GRID_W).astype(np.float32)
    ang = np.concatenate([row[:, None] * inv[None, :], col[:, None] * inv[None, :]], axis=-1).astype(np.float32)
    rope = np.zeros((T, 64), np.float32)
    rope[:CTX, :32] = 1.0
    rope[CTX:, :32] = np.cos(ang)
    rope[CTX:, 32:] = np.sin(ang)
    band = np.zeros((4, 5, 128, 128), np.float32)
    n = 128 * 4
    for wi, w in enumerate(POOL_WINDOWS):
        M = np.zeros((n, n), np.float64)
        for t in range(n):
            lo = max(t - w // 2, 0)
            hi = min(t + w // 2, n)
            M[lo:hi, t] = 1.0 / (hi - lo)
            M[t, t] -= 1.0
        band[wi, 0] = M[0:128, 128:256]
        band[wi, 1] = M[0:128, 0:128]
        band[wi, 2] = M[128:256, 128:256]
        band[wi, 3] = M[384:512, 384:512]
        band[wi, 4] = M[256:384, 128:256]
    tri = np.zeros((4, 128, 128), np.float32)
    s = np.arange(128)[:, None]
    l = np.arange(128)[None, :]
    tri[0] = (s <= l)
    tri[1] = np.where(s <= l, 0.0, -1.0e4)
    tri[2] = (s >= l)
    tri[3] = np.where(s >= l, 0.0, -1.0e4)
    kmoe = np.zeros((128, 128), np.float32)
    kmoe[:, 0:NBLK] = (np.arange(NBLK) * BLK)[None, :]
    kmoe[:, 64:82] = (np.arange(18) * BLK)[None, :]
    kmoe[:, 96:104] = np.arange(8)[None, :] * 128 + np.arange(128)[:, None]
    return {"k_ident": ident, "k_rope": rope, "k_band": band, "k_tri": tri, "k_moe": kmoe}


_CACHE = {}


def kernel(**inputs):
    cfg = inputs.pop("_cfg", {})
    key = repr(sorted(cfg.items()))
    if key not in _CACHE:
        _CACHE[key] = build(cfg)
    P = _CACHE[key]
    f = lambda a: np.ascontiguousarray(np.asarray(a, dtype=np.float32))
    consts = host_consts()
    shared = {}
    for name in ("norm_mix_g", "norm_ffn_g",
                 "attn_q_norm_g", "attn_k_norm_g", "pool_w", "pool_scale", "ssm_w_in", "ssm_conv_w",
                 "ssm_conv_b", "ssm_norm_g", "ssm_w_out"):
        shared[name] = f(inputs[name])
    wrc = np.concatenate([f(inputs["moe_w_router_group"]), f(inputs["moe_w_router_expert"])], axis=-1)
    for i in range(DEPTH):
        if "w_ada_%d" % i in P.inp:
            shared["w_ada_%d" % i] = f(inputs["w_ada"][i])
            shared["b_ada_%d" % i] = f(inputs["b_ada"][i]).reshape(1, 6 * D)
        if "moe_w_gu_%d" % i in P.inp:
            shared["moe_w_gu_%d" % i] = np.ascontiguousarray(
                f(inputs["moe_w_gate_up"][i]).reshape(NE, 8, 128, 2 * FH).transpose(0, 2, 1, 3)).reshape(NE * 128, 8 * 2 * FH)
            shared["moe_w_dn_%d" % i] = np.ascontiguousarray(
                f(inputs["moe_w_down"][i]).reshape(NE, 4, 128, D).transpose(0, 2, 1, 3)).reshape(NE * 128, 4 * D)
            shared["moe_wr_%d" % i] = np.ascontiguousarray(wrc[i])
    for j in range(2):
        if "attn_w_qkv_%d" % j in P.inp:
            shared["attn_w_qkv_%d" % j] = f(inputs["attn_w_qkv"][j])
            shared["attn_w_o_%d" % j] = f(inputs["attn_w_o"][j])
    shared["final_norm_g"] = f(inputs["final_norm_g"]).reshape(1, D)
    shared["c_ctx"] = f(inputs["c_ctx"]).reshape(1, D)
    for name in ("ssm_dt_bias", "ssm_a_log", "ssm_d"):
        shared[name] = f(inputs[name]).reshape(1, 2 * SSM_H)
    shared.update(consts)
    x = f(inputs["x"])
    ctx = f(inputs["ctx"])
    c = f(inputs["c"])
    in_maps = []
    ncores = cfg.get("cores", 8)
    for core in range(ncores):
        b = core % NB
        m = dict(shared)
        m["x"] = x[b]
        m["ctx"] = ctx[b]
        m["c"] = c[b:b + 1]
        in_maps.append({k: v for k, v in m.items() if k in P.inp})
    if cfg.get("trace"):
        res = run_bass_kernel_spmd(P.nc, in_maps, core_ids=list(range(ncores)), trace=True)
        kernel.exec_ns = res.exec_time_ns
    else:
        res = run_bass_kernel_spmd(P.nc, in_maps, core_ids=list(range(ncores)))
    nb = min(NB, ncores)
    outs = np.stack([np.asarray(res.results[b]["out"], dtype=np.float32) for b in range(nb)], axis=0)
    if cfg.get("dbg"):
        kernel.dbg = [np.asarray(res.results[b]["dbg"]) for b in range(nb)]
    return outs
```

```python
import contextlib
import numpy as np
import concourse.bass as bass
import concourse.mybir as mybir
from concourse.bass_utils import run_bass_kernel_spmd

F32 = mybir.dt.float32
BF16 = mybir.dt.bfloat16
I32 = mybir.dt.int32
ALU = mybir.AluOpType
AF = mybir.ActivationFunctionType
AX = mybir.AxisListType

D = 1024
NB = 4
SEQ = 4096
CTX = 256
T = SEQ + CTX
NT = T // 128
DEPTH = 4
EPS = 1e-6
GRID_W = 64
NH, NKV, HD = 16, 4, 64
POOL_WINDOWS = (2, 4, 8, 16)
SSM_DI, SSM_H, SSM_P, SSM_G, SSM_N = 2048, 32, 64, 4, 128
SSM_CONV_DIM = SSM_DI + 2 * SSM_G * SSM_N
SSM_IN = SSM_DI + SSM_CONV_DIM + 2 * SSM_H
NE, EPG, FH = 32, 8, 512
BIG = 1.0e30
BLK = 256
NSB = BLK // 128
NTH = (2 * T + BLK - 1) // BLK + 1
NBLK = (2 * T + BLK - 1) // BLK + NE
NSLOT = NBLK * BLK

SAME_ENGINE_SYNC = ("act", "dve", "pool")


class Sched:
    def __init__(self, nc):
        self.nc = nc
        self.eng = {"pe": nc.tensor, "act": nc.scalar, "dve": nc.vector,
                    "pool": nc.gpsimd, "sp": nc.sync}
        self.esem = {}
        self.ecnt = {}
        for e in ("pe", "act", "dve", "pool"):
            self.esem[e] = nc.alloc_semaphore("s_" + e)
            self.ecnt[e] = 0
        self.seen = {e: {} for e in self.eng}
        self.wr = {}
        self.rd = {}
        self.dsem = {}
        self.dfree = []
        self.nsem = 0
        self.bregs = {}
        self.n_inst = 0

    def _wait(self, e, toks):
        eng = self.eng[e]
        for name, (sem, val, src) in toks.items():
            if src == e and e not in SAME_ENGINE_SYNC:
                continue
            if self.seen[e].get(name, 0) >= val:
                continue
            eng.wait_ge(sem, val)
            self.seen[e][name] = val

    def _deps(self, e, reads, writes):
        for k in reads:
            self._wait(e, self.wr.get(k, {}))
        for k in writes:
            self._wait(e, self.wr.get(k, {}))
            self._wait(e, self.rd.get(k, {}))

    def _commit(self, name, tok, reads, writes):
        for k in reads:
            self.rd.setdefault(k, {})[name] = tok
        for k in writes:
            self.wr[k] = {name: tok}
            self.rd[k] = {}

    def op(self, e, fn, reads=(), writes=()):
        self._deps(e, reads, writes)
        ins = fn(self.eng[e])
        self.ecnt[e] += 1
        ins.then_inc(self.esem[e], 1)
        self._commit("s_" + e, (self.esem[e], self.ecnt[e], e), reads, writes)
        self.n_inst += 1
        return ins

    def dma(self, q, out, in_, reads=(), writes=(), semkey=None, **kw):
        if semkey is None:
            semkey = tuple(writes) + tuple(reads)
        ent = self._dsem_get(semkey)
        self._deps(q, reads, writes)
        ins = self.eng[q].dma_start(out=out, in_=in_, **kw)
        ent[1] += 16
        ins.then_inc(ent[0], 16)
        self._commit(ent[2], (ent[0], ent[1], "dma"), reads, writes)
        self.n_inst += 1

    def idma(self, out, out_off, in_, in_off, bounds, reads=(), writes=(), semkey=None):
        q = "pool"
        ent = self._dsem_get(semkey)
        self._deps(q, reads, writes)
        if bounds not in self.bregs:
            self.bregs[bounds] = self.eng[q].to_reg(bounds)
        ins = self.eng[q].indirect_dma_start(out=out, out_offset=out_off, in_=in_, in_offset=in_off,
                                             bounds_check=self.bregs[bounds], oob_is_err=False)
        ent[1] += 16
        ins.then_inc(ent[0], 16)
        self._commit(ent[2], (ent[0], ent[1], "dma"), reads, writes)
        self.n_inst += 1

    def recycle(self):
        for key, ent in list(self.dsem.items()):
            for e in self.eng:
                if self.seen[e].get(ent[2], 0) < ent[1]:
                    self.eng[e].wait_ge(ent[0], ent[1])
                    self.seen[e][ent[2]] = ent[1]
            self.dfree.append(ent)
            del self.dsem[key]

    def _dsem_get(self, semkey):
        if semkey not in self.dsem:
            if self.dfree:
                self.dsem[semkey] = self.dfree.pop()
            else:
                nm = "d%d" % self.nsem
                self.nsem += 1
                self.dsem[semkey] = [self.nc.alloc_semaphore(nm), 0, nm]
        return self.dsem[semkey]

    def wait_all(self, e, keys):
        for k in keys:
            self._wait(e, self.wr.get(k, {}))
            self._wait(e, self.rd.get(k, {}))


class Prog:
    def __init__(self, cfg):
        self.cfg = cfg
        self.nc = nc = bass.Bass("TRN2", target_bir_lowering=False)
        self.S = Sched(nc)
        self.es = contextlib.ExitStack()
        self.inp = {}
        self.uid = 0

    def din(self, name, shape, dt=F32):
        t = self.nc.dram_tensor(name, list(shape), dt, kind="ExternalInput").ap()
        self.inp[name] = t
        return t

    def dscratch(self, name, shape, dt=F32):
        return self.nc.dram_tensor(name, list(shape), dt, kind="Internal").ap()

    def sb(self, stack, name, shape, dt=F32):
        self.uid += 1
        return stack.enter_context(self.nc.sbuf_tensor("%s_u%d" % (name, self.uid), list(shape), dt))

    def mm(self, out, lhsT, rhs, start, stop, reads, writes):
        self.S.op("pe", lambda e: e.matmul(out, lhsT=lhsT, rhs=rhs, start=start, stop=stop),
                  reads=reads, writes=writes)

    def tr(self, out, in_, ident, reads, writes):
        self.S.op("pe", lambda e: e.transpose(out, in_, ident), reads=reads, writes=writes)

    def act(self, out, in_, func, reads, writes, bias=None, scale=None, accum_out=None):
        kw = {}
        if bias is not None:
            kw["bias"] = bias
        if scale is not None:
            kw["scale"] = scale
        if accum_out is not None:
            kw["accum_out"] = accum_out
        self.S.op("act", lambda e: e.activation(out=out, in_=in_, func=func, **kw),
                  reads=reads, writes=writes)

    def tt(self, e, out, in0, in1, op, reads, writes):
        self.S.op(e, lambda g: g.tensor_tensor(out=out, in0=in0, in1=in1, op=op),
                  reads=reads, writes=writes)

    def ts(self, e, out, in0, s1, op0, reads, writes, s2=None, op1=None, accum_out=None):
        kw = {}
        if op1 is not None:
            kw["op1"] = op1
        if accum_out is not None:
            kw["accum_out"] = accum_out
        self.S.op(e, lambda g: g.tensor_scalar(out=out, in0=in0, scalar1=s1, scalar2=s2, op0=op0, **kw),
                  reads=reads, writes=writes)

    def stt(self, e, out, in0, scalar, in1, op0, op1, reads, writes):
        self.S.op(e, lambda g: g.scalar_tensor_tensor(out=out, in0=in0, scalar=scalar, in1=in1,
                                                      op0=op0, op1=op1),
                  reads=reads, writes=writes)

    def cp(self, e, out, in_, reads, writes):
        if e == "act":
            self.S.op("act", lambda g: g.copy(out=out, in_=in_), reads=reads, writes=writes)
        else:
            self.S.op(e, lambda g: g.tensor_copy(out=out, in_=in_), reads=reads, writes=writes)

    def red(self, e, out, in_, op, reads, writes, axis=AX.X):
        self.S.op(e, lambda g: g.tensor_reduce(out=out, in_=in_, axis=axis, op=op),
                  reads=reads, writes=writes)

    def recip(self, out, in_, reads, writes):
        self.S.op("dve", lambda g: g.reciprocal(out=out, in_=in_), reads=reads, writes=writes)

    def memset(self, e, ap, val, writes):
        self.S.op(e, lambda g: g.memset(ap, val), writes=writes)

    def dma(self, q, out, in_, reads=(), writes=(), semkey=None, **kw):
        self.S.dma(q, out, in_, reads=reads, writes=writes, semkey=semkey, **kw)


def build(cfg):
    P = Prog(cfg)
    nc, S = P.nc, P.S
    layers = cfg.get("layers", list(range(DEPTH)))
    mixers_on = cfg.get("mixers", True)
    moe_on = cfg.get("moe", True)

    x_in = P.din("x", [SEQ, D])
    ctx_in = P.din("ctx", [CTX, D])
    c_in = P.din("c", [1, D])
    cctx_in = P.din("c_ctx", [1, D])
    w_ada = {i: P.din("w_ada_%d" % i, [D, 6 * D]) for i in layers}
    b_ada = {i: P.din("b_ada_%d" % i, [1, 6 * D]) for i in layers}
    norm_mix_g = P.din("norm_mix_g", [DEPTH, D])
    norm_ffn_g = P.din("norm_ffn_g", [DEPTH, D])
    final_g = P.din("final_norm_g", [1, D])
    attn_w_qkv = {j: P.din("attn_w_qkv_%d" % j, [D, 1536]) for j in range(2) if 3 * j in layers and mixers_on}
    attn_w_o = {j: P.din("attn_w_o_%d" % j, [D, D]) for j in range(2) if 3 * j in layers and mixers_on}
    attn_qg = P.din("attn_q_norm_g", [2, HD])
    attn_kg = P.din("attn_k_norm_g", [2, HD])
    pool_w = P.din("pool_w", [1, 4, 256, 256])
    pool_scale = P.din("pool_scale", [1, D])
    ssm_on = 2 in layers and mixers_on
    ssm_w_in = P.din("ssm_w_in", [1, D, SSM_IN]) if ssm_on else None
    ssm_conv_w = P.din("ssm_conv_w", [1, 4, SSM_CONV_DIM])
    ssm_conv_b = P.din("ssm_conv_b", [1, SSM_CONV_DIM])
    ssm_dt_bias = P.din("ssm_dt_bias", [1, 2 * SSM_H])
    ssm_a_log = P.din("ssm_a_log", [1, 2 * SSM_H])
    ssm_d = P.din("ssm_d", [1, 2 * SSM_H])
    ssm_norm_g = P.din("ssm_norm_g", [1, SSM_DI])
    ssm_w_out = P.din("ssm_w_out", [1, SSM_DI, D]) if ssm_on else None
    moe_l = [i for i in layers if moe_on]
    moe_wr = {i: P.din("moe_wr_%d" % i, [D, 36]) for i in moe_l}
    moe_w_gu = {i: P.din("moe_w_gu_%d" % i, [NE * 128, 8 * 2 * FH]) for i in moe_l}
    moe_w_dn = {i: P.din("moe_w_dn_%d" % i, [NE * 128, 4 * D]) for i in moe_l}
    k_ident = P.din("k_ident", [128, 128])
    k_rope = P.din("k_rope", [T, 64])
    k_band = P.din("k_band", [4, 5, 128, 128])
    k_tri = P.din("k_tri", [4, 128, 128])
    k_moe = P.din("k_moe", [128, 256])
    out = nc.dram_tensor("out", [SEQ, D], F32, kind="ExternalOutput").ap()
    dbg = None
    if cfg.get("dbg"):
        dbg = nc.dram_tensor("dbg", list(cfg["dbg"]), F32, kind="ExternalOutput").ap()

    X = P.dscratch("X", [T, D])

    def xk(t):
        return ("X", t)

    top = P.es
    ident = P.sb(top, "ident", [128, 128])
    identb = P.sb(top, "identb", [128, 128], BF16)
    ones = P.sb(top, "ones", [128, 128])
    cT = P.sb(top, "cT", [128, 8, 2])
    modL = P.sb(top, "modL", [128, 48])
    modC = P.sb(top, "modC", [128, 48])
    vec = P.sb(top, "vec", [128, 2, 2, 2, 8])
    gates = P.sb(top, "gates", [128, 2, 2, D])
    ps = [top.enter_context(nc.psum_tensor("ps%d" % i, [128, 512], F32)) for i in range(8)]

    def pk(i):
        return "ps%d" % i

    P.dma("sp", ident[:], k_ident, writes=["ident"])
    P.cp("dve", identb[:], ident[:], ["ident"], ["identb"])
    P.memset("pool", ones[:], 1.0, ["ones"])
    P.dma("sp", X[0:CTX, :], ctx_in, writes=[xk(0), xk(1)], semkey="xinit")
    P.dma("sp", X[CTX:T, :], x_in, writes=[xk(t) for t in range(2, NT)], semkey="xinit")

    with contextlib.ExitStack() as st:
        craw = P.sb(st, "craw", [128, 8, 2])
        P.dma("sp", craw[:, :, 0], c_in[0].rearrange("(k p) -> p k", p=128), writes=["craw"],
              allow_slow_non_contiguous=True)
        P.dma("sp", craw[:, :, 1], cctx_in[0].rearrange("(k p) -> p k", p=128), writes=["craw"],
              allow_slow_non_contiguous=True)
        P.act(cT[:], craw[:], AF.Silu, ["craw"], ["cT"])
        S.wait_all("sp", ["craw"])
        S.wait_all("act", ["craw"])


    def drain(keys):
        for e_ in ("sp", "pe", "act", "dve", "pool"):
            S.wait_all(e_, keys)

    def adaln(i, need_bc_all):
        with contextlib.ExitStack() as st:
            wblk = [P.sb(st, "wblk%d" % j, [128, 8, 512]) for j in range(2)]
            brow = P.sb(st, "brow", [1, 6 * D])
            bT = P.sb(st, "bT", [128, 48])
            g2 = P.sb(st, "g2", [128, 2, 8])
            cbc = P.sb(st, "cbc", [128, 2, 8, 128])
            for s in range(2):
                P.cp("dve", cbc[:, s], cT[:, :, s:s + 1].broadcast_to([128, 8, 128]), ["cT"], ["cbc"])
            P.dma("sp", brow[:], b_ada[i][0:1, :], writes=["brow"])
            P.dma("sp", bT[:], b_ada[i][0].rearrange("(j p) -> p j", p=128), writes=["bT"],
                  allow_slow_non_contiguous=True)
            P.dma("sp", g2[:, 0, :], norm_mix_g[i].rearrange("(j p) -> p j", p=128), writes=["g2"],
                  allow_slow_non_contiguous=True)
            P.dma("sp", g2[:, 1, :], norm_ffn_g[i].rearrange("(j p) -> p j", p=128), writes=["g2"],
                  allow_slow_non_contiguous=True)
            for n in range(12):
                wb = wblk[n % 2]
                wkey = "wblk%d" % (n % 2)
                P.dma("sp", wb[:], w_ada[i][:, n * 512:(n + 1) * 512].rearrange("(k p) n -> p k n", p=128),
                      writes=[wkey])
                for q in range(4):
                    j = n * 4 + q
                    for k in range(8):
                        P.mm(ps[0][:, 2 * j:2 * j + 2], wb[:, k, q * 128:(q + 1) * 128], cT[:, k, :],
                             k == 0, k == 7, [wkey, "cT"], [pk(0)])
                split = n // 2
                if split in (2, 5):
                    which = 0 if split == 2 else 1
                    half = n % 2
                    for s in range(2):
                        bank = 1 + s
                        for k in range(8):
                            P.mm(ps[bank][:, :], cbc[:, s, k, :], wb[:, k, :], k == 0, False,
                                 [wkey, "cbc"], [pk(bank)])
                        P.mm(ps[bank][:, :], ones[0:1, :], brow[0:1, n * 512:(n + 1) * 512], False, True,
                             ["ones", "brow"], [pk(bank)])
                        P.cp("act", gates[:, which, s, half * 512:(half + 1) * 512], ps[bank][:, :],
                             [pk(bank)], [("gates", which, s)])
            psv = ps[0][:, 0:96].rearrange("p (j s) -> p j s", s=2)
            P.tt("dve", modL[:], psv[:, :, 0], bT[:], ALU.add, [pk(0), "bT"], ["modL"])
            P.tt("dve", modC[:], psv[:, :, 1], bT[:], ALU.add, [pk(0), "bT"], ["modC"])
            for which in range(2):
                for s, m in enumerate((modL, modC)):
                    mk = "modL" if s == 0 else "modC"
                    base = which * 24
                    P.stt("dve", vec[:, which, s, 0, :], m[:, base + 8:base + 16], 1.0, g2[:, which, :],
                          ALU.add, ALU.mult, [mk, "g2"], [("vec", which, s)])
                    P.cp("dve", vec[:, which, s, 1, :], m[:, base:base + 8], [mk], [("vec", which, s)])
            S.wait_all("sp", ["wblk0", "wblk1", "brow", "bT", "g2"])
            S.wait_all("pe", ["wblk0", "wblk1", "brow", "cbc"])
            S.wait_all("dve", ["bT", "g2"])

    def norm_tiles(st, tiles, which, hT, hT_key, col0, hook=None, tag="n", colfn=None):
        xt = [P.sb(st, "%s_xt%d" % (tag, j), [128, D]) for j in range(2)]
        h32 = [P.sb(st, "%s_h32%d" % (tag, j), [128, 8, 128]) for j in range(2)]
        st8 = [P.sb(st, "%s_st%d" % (tag, j), [128, 4]) for j in range(2)]
        junk = P.sb(st, "%s_junk" % tag, [128, D], BF16)

        def load(idx):
            t = tiles[idx]
            P.dma("sp", xt[idx % 2][:], X[t * 128:(t + 1) * 128, :], reads=[xk(t)],
                  writes=["%s_xt%d" % (tag, idx % 2)], semkey="%s_xt%d" % (tag, idx % 2))

        load(0)
        for idx, t in enumerate(tiles):
            if idx + 1 < len(tiles):
                load(idx + 1)
            b = idx % 2
            xkey, skey, hkey = "%s_xt%d" % (tag, b), "%s_st%d" % (tag, b), "%s_h32%d" % (tag, b)
            s = 1 if t < 2 else 0
            P.act(junk[:], xt[b][:], AF.Square, [xkey], ["%s_junk" % tag, skey], accum_out=st8[b][:, 0:1])
            P.act(st8[b][:, 1:2], st8[b][:, 0:1], AF.Sqrt, [skey], [skey], bias=EPS, scale=1.0 / D)
            P.recip(st8[b][:, 2:3], st8[b][:, 1:2], [skey], [skey])
            P.ts("dve", xt[b][:], xt[b][:], st8[b][:, 2:3], ALU.mult, [xkey, skey], [xkey])
            for c in range(8):
                bank = 6 + c // 4
                P.tr(ps[bank][:, (c % 4) * 128:(c % 4 + 1) * 128], xt[b][:, c * 128:(c + 1) * 128], ident[:],
                     [xkey, "ident"], [pk(bank)])
            c0 = col0 + idx * 128 if colfn is None else colfn(idx)
            hTk = hT_key if colfn is None else (hT_key, idx % 2)
            if hook is None:
                for c in range(8):
                    bank = 6 + c // 4
                    P.act(hT[:, c, c0:c0 + 128], ps[bank][:, (c % 4) * 128:(c % 4 + 1) * 128], AF.Identity,
                          [pk(bank), ("vec", which, s)], [hTk],
                          bias=vec[:, which, s, 1, c:c + 1], scale=vec[:, which, s, 0, c:c + 1])
            else:
                for c in range(8):
                    bank = 6 + c // 4
                    P.act(h32[b][:, c, :], ps[bank][:, (c % 4) * 128:(c % 4 + 1) * 128], AF.Identity,
                          [pk(bank), ("vec", which, s)], [hkey],
                          bias=vec[:, which, s, 1, c:c + 1], scale=vec[:, which, s, 0, c:c + 1])
                P.cp("dve", hT[:, :, c0:c0 + 128], h32[b][:], [hkey], [hTk])
            if hook is not None:
                hook(idx, t, h32[b], hkey)

    def moe(i, tiles_all):
        nhalf = 2
        per = len(tiles_all) // nhalf
        for hf in range(nhalf):
            tiles = tiles_all[hf * per:(hf + 1) * per]
            ntk = per * 128
            with contextlib.ExitStack() as st:
                hT = P.sb(st, "m_hT", [128, 8, ntk], BF16)
                Y = P.sb(st, "m_Y", [128, per, D])
                Wt = P.sb(st, "m_Wt", [128, per, NE])
                wr = P.sb(st, "m_wr", [128, 8, 36])
                wgu = [P.sb(st, "m_wgu%d" % j, [128, 8, 2 * FH], BF16) for j in range(2)]
                wdn = [P.sb(st, "m_wdn%d" % j, [128, 4, D], BF16) for j in range(2)]
                sg = [P.sb(st, "m_sg%d" % j, [128, 512], BF16) for j in range(2)]
                aT = [P.sb(st, "m_a%d" % j, [128, 4, 512], BF16) for j in range(2)]
                rt = P.sb(st, "m_rt", [128, 160])

                def loadw(e):
                    b = e % 2
                    P.dma("pool", wgu[b][:], moe_w_gu[i][e * 128:(e + 1) * 128, :].rearrange("p (k n) -> p k n", k=8),
                          writes=["m_wgu%d" % b])
                    P.dma("pool", wdn[b][:], moe_w_dn[i][e * 128:(e + 1) * 128, :].rearrange("p (k n) -> p k n", k=4),
                          writes=["m_wdn%d" % b])

                P.dma("sp", wr[:], moe_wr[i].rearrange("(k p) n -> p k n", p=128), writes=["m_wr"])
                loadw(0)

                def router(idx, t, h32, hkey):
                    lg = ps[5]
                    for k in range(8):
                        P.mm(lg[:, 0:36], h32[:, k, :], wr[:, k, :], k == 0, k == 7, [hkey, "m_wr"], [pk(5)])
                    R = "m_rt"
                    lgs = rt[:, 0:36]
                    P.cp("dve", lgs, lg[:, 0:36], [pk(5)], [R])
                    gmax, ngmax, gsum, gate = rt[:, 36:37], rt[:, 37:38], rt[:, 38:39], rt[:, 39:40]
                    P.red("dve", gmax, rt[:, 0:4], ALU.max, [R], [R])
                    P.ts("dve", ngmax, gmax, -1.0, ALU.mult, [R], [R])
                    P.act(rt[:, 40:44], rt[:, 0:4], AF.Exp, [R], [R], bias=ngmax, scale=1.0, accum_out=gsum)
                    P.recip(gate, gsum, [R], [R])
                    pen = rt[:, 44:48]
                    P.ts("dve", pen, rt[:, 0:4], gmax, ALU.is_ge, [R], [R])
                    P.ts("dve", pen, pen, -1.0, ALU.add, [R], [R], s2=BIG, op1=ALU.mult)
                    le = rt[:, 48:80]
                    P.tt("dve", le.rearrange("p (g j) -> p g j", g=4), rt[:, 4:36].rearrange("p (g j) -> p g j", g=4),
                         pen.unsqueeze(2).broadcast_to([128, 4, 8]), ALU.add, [R], [R])
                    m1, m2 = rt[:, 80:81], rt[:, 81:82]
                    P.red("dve", m1, le, ALU.max, [R], [R])
                    oh1, oh2, le2 = rt[:, 84:116], rt[:, 116:148], rt[:, 4:36]
                    P.ts("dve", oh1, le, m1, ALU.is_ge, [R], [R])
                    P.stt("dve", le2, oh1, -BIG, le, ALU.mult, ALU.add, [R], [R])
                    P.red("dve", m2, le2, ALU.max, [R], [R])
                    P.ts("dve", oh2, le2, m2, ALU.is_ge, [R], [R])
                    dd, ee, p1, p2 = rt[:, 148:149], rt[:, 149:150], rt[:, 150:151], rt[:, 151:152]
                    P.tt("dve", dd, m2, m1, ALU.subtract, [R], [R])
                    P.act(ee, dd, AF.Exp, [R], [R])
                    P.ts("dve", p1, ee, 1.0, ALU.add, [R], [R])
                    P.recip(p1, p1, [R], [R])
                    P.tt("dve", p2, ee, p1, ALU.mult, [R], [R])
                    P.tt("dve", p1, p1, gate, ALU.mult, [R], [R])
                    P.tt("dve", p2, p2, gate, ALU.mult, [R], [R])
                    P.ts("dve", oh1, oh1, p1, ALU.mult, [R], [R])
                    P.stt("dve", Wt[:, idx, :], oh2, p2, oh1, ALU.mult, ALU.add, [R], [("m_Wt", idx)])

                with contextlib.ExitStack() as st2:
                    norm_tiles(st2, tiles, 1, hT, "m_hT", 0, hook=router, tag="mn")
                    S.wait_all("sp", ["mn_xt0", "mn_xt1"])
                    S.wait_all("act", ["mn_xt0", "mn_xt1", "mn_junk", "mn_st0", "mn_st1", "mn_h320", "mn_h321"])
                    S.wait_all("dve", ["mn_xt0", "mn_xt1", "mn_st0", "mn_st1"])
                    S.wait_all("pool", ["mn_h320", "mn_h321"])
                    S.wait_all("pe", ["mn_xt0", "mn_xt1", "mn_h320", "mn_h321"])

                blocks = []
                o = 0
                while o < ntk:
                    n_ = min(512, ntk - o)
                    blocks.append((o, n_))
                    o += n_
                cnt = 0
                dcnt = 0
                for e in range(NE):
                    if e + 1 < NE:
                        loadw(e + 1)
                    b = e % 2
                    gk, dk = "m_wgu%d" % b, "m_wdn%d" % b
                    for (o, n_) in blocks:
                        ab = cnt % 2
                        akey = "m_a%d" % ab
                        for j in range(4):
                            gb, ub = (j % 2), 2 + (j % 2)
                            for k in range(8):
                                P.mm(ps[gb][:, 0:n_], wgu[b][:, k, j * 128:(j + 1) * 128], hT[:, k, o:o + n_],
                                     k == 0, k == 7, [gk, "m_hT"], [pk(gb)])
                            for k in range(8):
                                P.mm(ps[ub][:, 0:n_], wgu[b][:, k, FH + j * 128:FH + (j + 1) * 128],
                                     hT[:, k, o:o + n_], k == 0, k == 7, [gk, "m_hT"], [pk(ub)])
                            sk = "m_sg%d" % (j % 2)
                            P.act(sg[j % 2][:, 0:n_], ps[gb][:, 0:n_], AF.Silu, [pk(gb)], [sk])
                            P.tt("dve", aT[ab][:, j, 0:n_], sg[j % 2][:, 0:n_], ps[ub][:, 0:n_], ALU.mult,
                                 [sk, pk(ub)], [(akey, j)])
                        for tt_ in range(n_ // 128):
                            tidx = o // 128 + tt_
                            for half in range(2):
                                db = 4 + dcnt % 2
                                dcnt += 1
                                for j in range(4):
                                    P.mm(ps[db][:, :], aT[ab][:, j, tt_ * 128:(tt_ + 1) * 128],
                                         wdn[b][:, j, half * 512:(half + 1) * 512], j == 0, j == 3,
                                         [(akey, j), dk], [pk(db)])
                                yk = ("m_Y", tidx, half)
                                ysl = Y[:, tidx, half * 512:(half + 1) * 512]
                                if e == 0:
                                    P.ts("dve", ysl, ps[db][:, :], Wt[:, tidx, e:e + 1], ALU.mult,
                                         [pk(db), ("m_Wt", tidx)], [yk])
                                else:
                                    P.stt("dve", ysl, ps[db][:, :], Wt[:, tidx, e:e + 1], ysl, ALU.mult, ALU.add,
                                          [pk(db), ("m_Wt", tidx), yk], [yk])
                        cnt += 1
                xo = [P.sb(st, "m_xo%d" % j, [128, D]) for j in range(2)]
                for idx, t in enumerate(tiles):
                    b = idx % 2
                    s = 1 if t < 2 else 0
                    ok = "m_xo%d" % b
                    P.dma("sp", xo[b][:], X[t * 128:(t + 1) * 128, :], reads=[xk(t)], writes=[ok], semkey=ok)
                    P.tt("pool", Y[:, idx, :], Y[:, idx, :], gates[:, 1, s, :], ALU.mult,
                         [("m_Y", idx, 0), ("m_Y", idx, 1), ("gates", 1, s)], [("m_Y", idx, 0), ("m_Y", idx, 1)])
                    P.tt("dve", xo[b][:], xo[b][:], Y[:, idx, :], ALU.add,
                         [ok, ("m_Y", idx, 0), ("m_Y", idx, 1)], [ok])
                    P.dma("sp", X[t * 128:(t + 1) * 128, :], xo[b][:], reads=[ok], writes=[xk(t)], semkey=ok)
                keys = ["m_hT", "m_wr", "m_wgu0", "m_wgu1", "m_wdn0", "m_wdn1", "m_sg0", "m_sg1", "m_rt",
                        "m_xo0", "m_xo1"]
                keys += [("m_a%d" % a, j) for a in range(2) for j in range(4)]
                keys += [("m_Y", idx, h) for idx in range(per) for h in range(2)]
                keys += [("m_Wt", idx) for idx in range(per)]
                for e_ in ("sp", "pe", "act", "dve", "pool"):
                    S.wait_all(e_, keys)


    def attention(i, j, ctx_out):
        QD = P.dscratch("QD%d" % i, [64, 16, T], BF16)
        with contextlib.ExitStack() as st:
            KT = P.sb(st, "a_KT", [128, 4, T], BF16)
            Vg = P.sb(st, "a_V", [128, NT, 4, 128], BF16)
            P.memset("dve", Vg[:], 0.0, ["a_V"])
            P.memset("dve", Vg[:, :, :, 64:66], 1.0, ["a_V"])
            with contextlib.ExitStack() as s1:
                hT = P.sb(s1, "a_hT", [128, 8, T], BF16)
                wqkv = P.sb(s1, "a_wqkv", [128, 8, 1536], BF16)
                gqk = P.sb(s1, "a_gqk", [128, 2, 64])
                P.dma("pool", wqkv[:], attn_w_qkv[j].rearrange("(k p) n -> p k n", p=128), writes=["a_wqkv"])
                P.dma("sp", gqk[:, 0, :], attn_qg[j:j + 1, :].broadcast_to([128, 64]), writes=["a_gqk"])
                P.dma("sp", gqk[:, 1, :], attn_kg[j:j + 1, :].broadcast_to([128, 64]), writes=["a_gqk"])
                with contextlib.ExitStack() as s2:
                    norm_tiles(s2, ALL, 0, hT, "a_hT", 0, tag="an")
                    drain(["an_xt0", "an_xt1", "an_junk", "an_st0", "an_st1", "an_h320", "an_h321"])
                qk_ = P.sb(s1, "a_qk0", [128, 20, 64])
                qk = [qk_, qk_]
                T4 = P.sb(s1, "a_T4", [128, 4 * 512])
                tmp = [T4[:, b * 512:(b + 1) * 512].rearrange("p (h i) -> p h i", i=32) for b in range(4)]
                sq = T4[:, 0:1280].rearrange("p (h d) -> p h d", d=64)
                qr = [P.sb(s1, "a_qr%d" % b, [128, 20, 64], BF16) for b in range(2)]
                ss = [P.sb(s1, "a_ss%d" % b, [128, 20]) for b in range(2)]
                cs = [P.sb(s1, "a_cs%d" % b, [128, 64]) for b in range(2)]
                tab = [P.sb(s1, "a_tab%d" % b, [128, 2, 4, 32]) for b in range(2)]
                QTt_ = P.sb(s1, "a_QTt0", [64, 16, 128], BF16)
                QTt = [QTt_, QTt_]
                gq4 = gqk[:].rearrange("p w (i two) -> p w i two", two=2)
                for t in range(NT):
                    b = t % 2
                    qkk, ssk, csk, tabk, qrk, qtk = ("a_qk0", "a_ss%d" % b, "a_cs%d" % b, "a_tab%d" % b,
                                                     "a_qr%d" % b, "a_QTt0")
                    if cfg.get("a1_lvl", 9) < 0.2:
                        continue
                    P.dma("sp", cs[b][:], k_rope[t * 128:(t + 1) * 128, :], writes=[csk])
                    if cfg.get("a1_lvl", 9) < 0.4:
                        continue
                    for nb in range(3):
                        for k in range(8):
                            P.mm(ps[nb][:, :], hT[:, k, t * 128:(t + 1) * 128], wqkv[:, k, nb * 512:(nb + 1) * 512],
                                 k == 0, k == 7, ["a_hT", "a_wqkv"], [pk(nb)])
                    if cfg.get("a1_lvl", 9) < 0.6:
                        continue
                    P.cp("act", qk[b][:, 0:8, :], ps[0][:, :].rearrange("p (h d) -> p h d", d=64), [pk(0)], [qkk])
                    P.cp("act", qk[b][:, 8:16, :], ps[1][:, :].rearrange("p (h d) -> p h d", d=64), [pk(1)], [qkk])
                    P.cp("act", qk[b][:, 16:20, :], ps[2][:, 0:256].rearrange("p (h d) -> p h d", d=64), [pk(2)], [qkk])
                    if cfg.get("a1_lvl", 9) < 0.8:
                        continue
                    P.cp("act", Vg[:, t, :, 0:64], ps[2][:, 256:512].rearrange("p (h d) -> p h d", d=64),
                         [pk(2)], [("a_V", t)])
                    if cfg.get("a1_lvl", 9) < 2:
                        continue
                    P.tt("pool", sq, qk[b][:], qk[b][:], ALU.mult, [qkk], ["a_sq", "a_t0", "a_t1", "a_t2"])
                    P.red("dve", ss[b][:], sq, ALU.add, ["a_sq", "a_t0", "a_t1", "a_t2"], [ssk])
                    P.act(ss[b][:], ss[b][:], AF.Sqrt, [ssk], [ssk], bias=EPS, scale=1.0 / HD)
                    P.recip(ss[b][:], ss[b][:], [ssk], [ssk])
                    P.tt("dve", qk[b][:], qk[b][:], ss[b][:].unsqueeze(2).broadcast_to([128, 20, 64]), ALU.mult,
                         [qkk, ssk], [qkk])
                    if cfg.get("a1_lvl", 9) < 3:
                        continue
                    for w in range(2):
                        P.tt("pool", tab[b][:, w, 0, :], cs[b][:, 0:32], gq4[:, w, :, 0], ALU.mult, [csk, "a_gqk"], [tabk])
                        P.tt("pool", tab[b][:, w, 1, :], cs[b][:, 32:64], gq4[:, w, :, 1], ALU.mult, [csk, "a_gqk"], [tabk])
                        P.tt("pool", tab[b][:, w, 2, :], cs[b][:, 32:64], gq4[:, w, :, 0], ALU.mult, [csk, "a_gqk"], [tabk])
                        P.tt("pool", tab[b][:, w, 3, :], cs[b][:, 0:32], gq4[:, w, :, 1], ALU.mult, [csk, "a_gqk"], [tabk])
                    qk4 = qk[b][:].rearrange("p h (i two) -> p h i two", two=2)
                    qr4 = qr[b][:].rearrange("p h (i two) -> p h i two", two=2)
                    for w, (h0, h1) in enumerate(((0, 16), (16, 20))):
                        nh = h1 - h0
                        x0, x1 = qk4[:, h0:h1, :, 0], qk4[:, h0:h1, :, 1]
                        tb = lambda kind: tab[b][:, w, kind, :].unsqueeze(1).broadcast_to([128, nh, 32])
                        P.tt("pool", tmp[0][:, 0:nh, :], x0, tb(0), ALU.mult, [qkk, tabk, "a_sq"], ["a_t0"])
                        P.tt("dve", tmp[1][:, 0:nh, :], x1, tb(1), ALU.mult, [qkk, tabk, "a_sq"], ["a_t1"])
                        P.tt("pool", tmp[2][:, 0:nh, :], x0, tb(2), ALU.mult, [qkk, tabk, "a_sq"], ["a_t2"])
                        P.tt("dve", tmp[3][:, 0:nh, :], x1, tb(3), ALU.mult, [qkk, tabk], ["a_t3"])
                        P.tt("dve", qr4[:, h0:h1, :, 0], tmp[0][:, 0:nh, :], tmp[1][:, 0:nh, :], ALU.subtract,
                             ["a_t0", "a_t1"], [qrk])
                        P.tt("pool", qr4[:, h0:h1, :, 1], tmp[2][:, 0:nh, :], tmp[3][:, 0:nh, :], ALU.add,
                             ["a_t2", "a_t3"], [qrk])
                    if cfg.get("a1_lvl", 9) < 4:
                        continue
                    for hh in range(20):
                        bank = 3 + hh // 8
                        pv = ps[bank][:, :].bitcast(BF16)
                        P.tr(pv[0:64, (hh % 8) * 128:(hh % 8 + 1) * 128], qr[b][:, hh, :], identb[:],
                             [qrk, "identb"], [pk(bank)])
                    if cfg.get("a1_lvl", 9) < 5:
                        continue
                    P.cp("act", QTt[b][:, 0:8, :], ps[3][:, :].bitcast(BF16)[0:64, :].rearrange("p (h t) -> p h t", t=128),
                         [pk(3)], [qtk])
                    P.cp("act", QTt[b][:, 8:16, :], ps[4][:, :].bitcast(BF16)[0:64, :].rearrange("p (h t) -> p h t", t=128),
                         [pk(4)], [qtk])
                    P.cp("act", KT[0:64, :, t * 128:(t + 1) * 128],
                         ps[5][:, :].bitcast(BF16)[0:64, 0:512].rearrange("p (h t) -> p h t", t=128),
                         [pk(5)], [("a_KT", t)])
                    if cfg.get("a1_lvl", 9) < 6:
                        continue
                    P.dma("sp", QD[:, :, t * 128:(t + 1) * 128], QTt[b][:], reads=[qtk], writes=[("QD", t)], semkey=qtk)
                keys = ["a_hT", "a_wqkv", "a_gqk", "a_sq"] + ["a_t%d" % b for b in range(4)]
                for b in range(2):
                    keys += ["a_qk%d" % b, "a_ss%d" % b, "a_cs%d" % b, "a_tab%d" % b, "a_qr%d" % b, "a_QTt%d" % b]
                drain(keys)
            with contextlib.ExitStack() as s1:
                if cfg.get("skip_a2"):
                    return
                wo = P.sb(s1, "a_wo", [64, 16, D], BF16)
                P.dma("pool", wo[:], attn_w_o[j].rearrange("(h d) n -> d h n", d=64), writes=["a_wo"])
                for kv_ in range(4):
                    P.memset("pool", KT[64:128, kv_, :], 0.0, ["a_KTz"])
                QTb = [P.sb(s1, "a_QTb%d" % b, [128, 16, 512], BF16) for b in range(2)]
                for b_ in range(2):
                    P.memset("pool", QTb[b_][64:128, :, :], 0.0, ["a_QTbz"])
                aT = [P.sb(s1, "a_aT%d" % b, [64, 16, 512], BF16) for b in range(2)]
                Pb = [P.sb(s1, "a_P%d" % b, [128, 512], BF16) for b in range(3)]
                rec = P.sb(s1, "a_rec", [65, 512])
                bcs = P.sb(s1, "a_bcs", [64, 512])
                xo = [P.sb(s1, "a_xo%d" % b, [128, D]) for b in range(2)]
                tm = [P.sb(s1, "a_tm%d" % b, [128, D]) for b in range(2)]
                qblocks = []
                if ctx_out:
                    qblocks.append((0, CTX, [0, 1]))
                for qb in range(SEQ // 512):
                    qblocks.append((CTX + qb * 512, 512, list(range(NT))))
                pcnt = 0
                xcnt = 0
                for bi, (qo, nq, ktiles) in enumerate(qblocks):
                    b = bi % 2
                    qbk, atk = "a_QTb%d" % b, "a_aT%d" % b
                    P.dma("sp", QTb[b][0:64, :, 0:nq], QD[:, :, qo:qo + nq],
                          reads=[("QD", t) for t in range(qo // 128, (qo + nq) // 128)], writes=[qbk], semkey=qbk)
                    items = [(h, idx, kt) for h in range(NH) for idx, kt in enumerate(ktiles)]
                    LOOK = 2

                    def emit_S(n):
                        h_, idx_, kt_ = items[n]
                        P.mm(ps[n % 3][:, 0:nq], KT[:, h_ // 4, kt_ * 128:(kt_ + 1) * 128], QTb[b][:, h_, 0:nq], True, True,
                             [("a_KT", kt_), "a_KTz", "a_QTbz", qbk], [pk(n % 3)])

                    def fin1(h_):
                        ob_ = 3 + h_ % 2
                        P.recip(rec[64:65, 0:nq], ps[ob_][64:65, 0:nq], [pk(ob_)], ["a_rec"])

                    def fin2(h_):
                        ob_ = 3 + h_ % 2
                        P.mm(ps[5][0:64, 0:nq], ones[64:65, 0:64], rec[64:65, 0:nq], True, True, ["ones", "a_rec"], [pk(5)])
                        P.cp("dve", bcs[:, 0:nq], ps[5][0:64, 0:nq], [pk(5)], ["a_bcs"])
                        P.tt("dve", aT[b][:, h_, 0:nq], ps[ob_][0:64, 0:nq], bcs[:, 0:nq], ALU.mult,
                             [pk(ob_), "a_bcs"], [(atk, h_)])

                    for n in range(min(LOOK, len(items))):
                        emit_S(n)
                    pend = None
                    for n, (h, idx, kt) in enumerate(items):
                        if n + LOOK < len(items):
                            emit_S(n + LOOK)
                        kv = h // 4
                        ob = 3 + h % 2
                        pb = n % 3
                        P.act(Pb[pb][:, 0:nq], ps[n % 3][:, 0:nq], AF.Exp, [pk(n % 3)], ["a_P%d" % pb], scale=HD ** -0.5)
                        P.mm(ps[ob][0:65, 0:nq], Vg[:, kt, kv, 0:65], Pb[pb][:, 0:nq], idx == 0, idx == len(ktiles) - 1,
                             [("a_V", kt), "a_V", "a_P%d" % pb], [pk(ob)])
                        if pend is not None and (idx == min(3, len(ktiles) - 1)):
                            fin2(pend)
                            pend = None
                        if idx == len(ktiles) - 1:
                            fin1(h)
                            pend = h
                    if pend is not None:
                        fin2(pend)
                    for tt_ in range(nq // 128):
                        t = qo // 128 + tt_
                        s = 1 if t < 2 else 0
                        xb = xcnt % 2
                        xcnt += 1
                        xok, tmk = "a_xo%d" % xb, "a_tm%d" % xb
                        P.dma("sp", xo[xb][:], X[t * 128:(t + 1) * 128, :], reads=[xk(t)], writes=[xok], semkey=xok)
                        for half in range(2):
                            bank = 6 + half
                            for h in range(NH):
                                P.mm(ps[bank][:, :], aT[b][:, h, tt_ * 128:(tt_ + 1) * 128],
                                     wo[:, h, half * 512:(half + 1) * 512], h == 0, h == NH - 1,
                                     [(atk, h), "a_wo"], [pk(bank)])
                            P.tt("dve", tm[xb][:, half * 512:(half + 1) * 512], ps[bank][:, :],
                                 gates[:, 0, s, half * 512:(half + 1) * 512], ALU.mult,
                                 [pk(bank), ("gates", 0, s)], [tmk])
                        P.tt("pool", xo[xb][:], xo[xb][:], tm[xb][:], ALU.add, [xok, tmk], [xok])
                        P.dma("sp", X[t * 128:(t + 1) * 128, :], xo[xb][:], reads=[xok], writes=[xk(t)], semkey=xok)
                if dbg is not None:
                    dt_ = tm[0]
                    P.cp("dve", dt_[:, 0:512], KT[:, 0, 0:512], ["a_KTz"] + [("a_KT", t) for t in range(4)], ["a_dbgt", "a_tm0"])
                    P.cp("dve", dt_[:, 512:1024], QTb[0][:, 0, 0:512], ["a_QTbz", "a_QTb0"], ["a_dbgt", "a_tm0"])
                    P.dma("sp", dbg, dt_[:], reads=["a_dbgt"], writes=["dbg"])
                    drain(["a_dbgt", "a_tm0"])
                keys = ["a_wo", "a_rec", "a_bcs", "a_V", "a_KTz", "a_QTbz"] + ["a_P%d" % b for b in range(3)]
                for b in range(2):
                    keys += ["a_QTb%d" % b, "a_xo%d" % b, "a_tm%d" % b] + [("a_aT%d" % b, h) for h in range(NH)]
                keys += [("a_KT", t) for t in range(NT)] + [("a_V", t) for t in range(NT)]
                drain(keys)


    def pool_mixer(i):
        with contextlib.ExitStack() as st:
            bcm = P.sb(st, "p_bcm", [128, 2, 2, D])
            gmb = P.sb(st, "p_gmb", [128, 2, D])
            psg = P.sb(st, "p_psg", [128, 2, D])
            band = P.sb(st, "p_band", [128, 4, 5, 128])
            wp = P.sb(st, "p_wp", [128, 4, 2, 256])
            P.dma("sp", band[:], k_band.rearrange("w k p n -> p w k n"), writes=["p_band"])
            P.dma("sp", wp[:], pool_w[0].rearrange("g (cc p) n -> p g cc n", p=128), writes=["p_wp"])
            with contextlib.ExitStack() as s1:
                wblk = [P.sb(s1, "p_wblk%d" % b, [128, 8, 512]) for b in range(2)]
                cbc = P.sb(s1, "p_cbc", [128, 2, 8, 128])
                brow = P.sb(s1, "p_brow", [1, 2 * D])
                gb = P.sb(s1, "p_gb", [128, D])
                psb = P.sb(s1, "p_psb", [128, D])
                P.dma("sp", brow[:], b_ada[i][0:1, 0:2 * D], writes=["p_brow"])
                P.dma("sp", gb[:], norm_mix_g[i:i + 1, :].broadcast_to([128, D]), writes=["p_gb"])
                P.dma("sp", psb[:], pool_scale[0:1, :].broadcast_to([128, D]), writes=["p_psb"])
                for s_ in range(2):
                    P.cp("dve", cbc[:, s_], cT[:, :, s_:s_ + 1].broadcast_to([128, 8, 128]), ["cT"], ["p_cbc"])
                for n in range(4):
                    wb, wkey = wblk[n % 2], "p_wblk%d" % (n % 2)
                    P.dma("sp", wb[:], w_ada[i][:, n * 512:(n + 1) * 512].rearrange("(k p) n -> p k n", p=128),
                          writes=[wkey])
                    for s_ in range(2):
                        bank = 1 + s_
                        for k in range(8):
                            P.mm(ps[bank][:, :], cbc[:, s_, k, :], wb[:, k, :], k == 0, False, [wkey, "p_cbc"], [pk(bank)])
                        P.mm(ps[bank][:, :], ones[0:1, :], brow[0:1, n * 512:(n + 1) * 512], False, True,
                             ["ones", "p_brow"], [pk(bank)])
                        P.cp("act", bcm[:, s_, n // 2, (n % 2) * 512:(n % 2 + 1) * 512], ps[bank][:, :],
                             [pk(bank)], ["p_bcm"])
                for s_ in range(2):
                    P.stt("dve", gmb[:, s_, :], bcm[:, s_, 1, :], 1.0, gb[:], ALU.add, ALU.mult, ["p_bcm", "p_gb"], ["p_gmb"])
                    P.tt("pool", psg[:, s_, :], psb[:], gates[:, 0, s_, :], ALU.mult, ["p_psb", ("gates", 0, s_)], ["p_psg"])
                drain(["p_wblk0", "p_wblk1", "p_cbc", "p_brow", "p_gb", "p_psb"])
            xt = [P.sb(st, "p_x%d" % b, [128, D]) for b in range(4)]
            hh = [P.sb(st, "p_h%d" % b, [128, D]) for b in range(4)]
            dT = [P.sb(st, "p_dT%d" % b, [128, 8, 128]) for b in range(2)]
            tm = [P.sb(st, "p_tm%d" % b, [128, D]) for b in range(2)]
            st8 = [P.sb(st, "p_st%d" % b, [128, 4]) for b in range(4)]
            junk = P.sb(st, "p_junk", [128, D], BF16)

            def compute_h(t):
                b = t % 4
                s_ = 1 if t < 2 else 0
                xkey, hkey, skey = "p_x%d" % b, "p_h%d" % b, "p_st%d" % b
                P.dma("sp", xt[b][:], X[t * 128:(t + 1) * 128, :], reads=[xk(t)], writes=[xkey], semkey=xkey)
                P.act(junk[:], xt[b][:], AF.Square, [xkey], ["p_junk", skey], accum_out=st8[b][:, 0:1])
                P.act(st8[b][:, 1:2], st8[b][:, 0:1], AF.Sqrt, [skey], [skey], bias=EPS, scale=1.0 / D)
                P.recip(st8[b][:, 2:3], st8[b][:, 1:2], [skey], [skey])
                P.stt("dve", hh[b][:], xt[b][:], st8[b][:, 2:3], gmb[:, s_, :], ALU.mult, ALU.mult,
                      [xkey, skey, "p_gmb"], [hkey])
                P.tt("pool", hh[b][:], hh[b][:], bcm[:, s_, 0, :], ALU.add, [hkey, "p_bcm"], [hkey])

            done = set()
            for t in range(NT):
                first = t in (0, 2)
                last = t in (1, NT - 1)
                need = [t] + ([] if first else [t - 1]) + ([] if last else [t + 1])
                for tt_ in sorted(need):
                    if tt_ not in done:
                        compute_h(tt_)
                        done.add(tt_)
                s_ = 1 if t < 2 else 0
                db = t % 2
                dkey, tmk = "p_dT%d" % db, "p_tm%d" % db
                for c in range(8):
                    wi = c // 2
                    bank = c // 4
                    col = (c % 4) * 128
                    srcs = []
                    if not first:
                        srcs.append((t - 1, 0))
                    srcs.append((t, 1 if first else (3 if last else 2)))
                    if not last:
                        srcs.append((t + 1, 4))
                    for si, (tt_, kind) in enumerate(srcs):
                        P.mm(ps[bank][:, col:col + 128], hh[tt_ % 4][:, c * 128:(c + 1) * 128], band[:, wi, kind, :],
                             si == 0, si == len(srcs) - 1, ["p_h%d" % (tt_ % 4), "p_band"], [pk(bank)])
                for bank in range(2):
                    P.cp("act", dT[db][:, bank * 4:(bank + 1) * 4, :], ps[bank][:, :].rearrange("p (c t) -> p c t", t=128),
                         [pk(bank)], [dkey])
                for g in range(4):
                    bank = 2 + g // 2
                    for cc in range(2):
                        P.mm(ps[bank][:, (g % 2) * 256:(g % 2 + 1) * 256], dT[db][:, 2 * g + cc, :], wp[:, g, cc, :],
                             cc == 0, cc == 1, [dkey, "p_wp"], [pk(bank)])
                xb = t % 4
                for half in range(2):
                    P.tt("dve", tm[db][:, half * 512:(half + 1) * 512], ps[2 + half][:, :],
                         psg[:, s_, half * 512:(half + 1) * 512], ALU.mult, [pk(2 + half), "p_psg"], [tmk])
                P.tt("pool", tm[db][:], tm[db][:], xt[xb][:], ALU.add, [tmk, "p_x%d" % xb], [tmk])
                P.dma("sp", X[t * 128:(t + 1) * 128, :], tm[db][:], reads=[tmk], writes=[xk(t)], semkey=tmk)
            keys = ["p_bcm", "p_gmb", "p_psg", "p_band", "p_wp", "p_junk", "p_dT0", "p_dT1", "p_tm0", "p_tm1"]
            for b in range(4):
                keys += ["p_x%d" % b, "p_h%d" % b, "p_st%d" % b]
            drain(keys)


    def ssd_mixer(i):
        XT = P.dscratch("s_XT", [T, SSM_DI], BF16)
        BTK = P.dscratch("s_BTK", [T, 512], BF16)
        BF = P.dscratch("s_BF", [4, 128, T], BF16)
        CF = P.dscratch("s_CF", [4, 128, T], BF16)
        Yd = P.dscratch("s_Yd", [2, T, SSM_DI])
        w_in = ssm_w_in[0]
        NU = T + 3

        def xcol(t):
            return t * 128 if t < 2 else 259 + (t - 2) * 128

        ZG = P.dscratch("s_ZG", [T, SSM_DI], BF16)
        with contextlib.ExitStack() as st:
            with contextlib.ExitStack() as sA:
                dtA = P.sb(sA, "s_dtA", [128, NT, 64])
                LA = P.sb(sA, "s_LA", [128, NT, 64])
                sH = contextlib.ExitStack()
                hT = P.sb(sH, "s_hT", [128, 8, T], BF16)
                with contextlib.ExitStack() as s1:
                    wdt = P.sb(s1, "s_wdt", [128, 8, 64])
                    dtb = P.sb(s1, "s_dtb", [128, 64])
                    aB = P.sb(s1, "s_aB", [128, 64])
                    sp_ = P.sb(s1, "s_sp", [128, 4, 64])
                    P.dma("sp", wdt[:], w_in[:, 5120:5184].rearrange("(k p) n -> p k n", p=128), writes=["s_wdt"])
                    P.dma("sp", dtb[:], ssm_dt_bias[0:1, :].broadcast_to([128, 64]), writes=["s_dtb"])
                    P.dma("sp", aB[:], ssm_a_log[0:1, :].broadcast_to([128, 64]), writes=["s_aB"])
                    P.act(aB[:], aB[:], AF.Exp, ["s_aB"], ["s_aB"])
                    P.ts("dve", aB[:], aB[:], -1.0, ALU.mult, ["s_aB"], ["s_aB"])

                    def dthook(idx, t, h32, hkey):
                        for k in range(8):
                            P.mm(ps[5][:, 0:64], h32[:, k, :], wdt[:, k, :], k == 0, k == 7, [hkey, "s_wdt"], [pk(5)])
                        K_ = "s_sp"
                        xr, ab, ee = sp_[:, 0, :], sp_[:, 1, :], sp_[:, 2, :]
                        P.tt("dve", xr, ps[5][:, 0:64], dtb[:], ALU.add, [pk(5), "s_dtb"], [K_])
                        P.ts("dve", sp_[:, 3, :], xr, -1.0, ALU.mult, [K_], [K_])
                        P.tt("dve", ab, xr, sp_[:, 3, :], ALU.max, [K_], [K_])
                        P.act(ee, ab, AF.Exp, [K_], [K_], scale=-1.0)
                        P.act(ee, ee, AF.Ln, [K_], [K_], bias=1.0, scale=1.0)
                        P.stt("dve", dtA[:, t, :], xr, 0.0, ee, ALU.max, ALU.add, [K_], [("s_dtA", t)])
                        P.tt("pool", LA[:, t, :], dtA[:, t, :], aB[:], ALU.mult, [("s_dtA", t), "s_aB"], [("s_LA", t)])

                    with contextlib.ExitStack() as s2:
                        norm_tiles(s2, ALL, 0, hT, "s_hT", 0, hook=dthook, tag="sn")
                        drain(["sn_xt0", "sn_xt1", "sn_junk", "sn_st0", "sn_st1", "sn_h320", "sn_h321"])
                    drain(["s_wdt", "s_dtb", "s_sp"])
                with contextlib.ExitStack() as s1:
                    cwA = P.sb(s1, "s_cwA", [128, 24, 4])
                    cbA = P.sb(s1, "s_cbA", [128, 24])
                    U2 = [P.sb(s1, "s_U%d" % b, [128, T + 8]) for b in range(2)]
                    acc = P.sb(s1, "s_acc", [128, NU])
                    xc = [P.sb(s1, "s_xc%d" % b, [128, NU], BF16) for b in range(2)]
                    wc = [P.sb(s1, "s_wc%d" % b, [128, 8, 128], BF16) for b in range(2)]
                    stg = [P.sb(s1, "s_stg%d" % b, [128, NT, 128], BF16) for b in range(2)]
                    for k in range(4):
                        P.dma("sp", cwA[:, :, k], ssm_conv_w[0, k].rearrange("(c p) -> p c", p=128), writes=["s_cwA"],
                              allow_slow_non_contiguous=True)
                    P.dma("sp", cbA[:], ssm_conv_b[0].rearrange("(c p) -> p c", p=128), writes=["s_cbA"],
                          allow_slow_non_contiguous=True)
                    for b_ in range(2):
                        P.memset("dve" if b_ == 0 else "pool", U2[b_][:], 0.0, ["s_U%d" % b_])
                    blocks = [(0, 256, 2)] + [(256 + b * 512, 512, 261 + b * 512) for b in range(8)]
                    mcnt = [0]

                    def inproj(cc):
                        b = cc % 2
                        wck = "s_wc%d" % b
                        U, uk = U2[b], "s_U%d" % b
                        P.dma("pool", wc[b][:], w_in[:, 2048 + cc * 128:2048 + (cc + 1) * 128].rearrange("(k p) n -> p k n", p=128),
                              writes=[wck])
                        for (t0, n_, uo) in blocks:
                            bank = mcnt[0] % 2
                            mcnt[0] += 1
                            for k in range(8):
                                P.mm(ps[bank][:, 0:n_], wc[b][:, k, :], hT[:, k, t0:t0 + n_], k == 0, k == 7,
                                     [wck, "s_hT"], [pk(bank)])
                            P.cp("act", U[:, uo:uo + n_], ps[bank][:, 0:n_], [pk(bank)], [uk])

                    inproj(0)
                    for cc in range(24):
                        b = cc % 2
                        wck, xck, stk = "s_wc%d" % b, "s_xc%d" % b, "s_stg%d" % b
                        U, uk = U2[b], "s_U%d" % b
                        if cc + 1 < 24:
                            inproj(cc + 1)
                        ce = "dve"
                        P.ts(ce, acc[:], U[:, 0:NU], cwA[:, cc, 0:1], ALU.mult, [uk, "s_cwA"], ["s_acc"])
                        for k in range(1, 4):
                            P.stt(ce, acc[:], U[:, k:k + NU], cwA[:, cc, k:k + 1], acc[:], ALU.mult, ALU.add,
                                  [uk, "s_cwA", "s_acc"], ["s_acc"])
                        P.act(xc[b][:], acc[:], AF.Silu, ["s_acc", "s_cbA"], [xck], bias=cbA[:, cc:cc + 1], scale=1.0)
                        if cc >= 16:
                            g = (cc - 16) % 4
                            dst = BF if cc < 20 else CF
                            dk = "BF" if cc < 20 else "CF"
                            P.dma("sp", dst[g, :, 0:CTX], xc[b][:, 0:CTX], reads=[xck], writes=[(dk, g)], semkey=xck)
                            P.dma("sp", dst[g, :, CTX:T], xc[b][:, 259:259 + SEQ], reads=[xck], writes=[(dk, g)], semkey=xck)
                        if cc < 20:
                            for t in range(NT):
                                bank = 2 + (t // 8) % 4
                                pv = ps[bank][:, :].bitcast(BF16)
                                P.tr(pv[:, (t % 8) * 128:(t % 8 + 1) * 128], xc[b][:, xcol(t):xcol(t) + 128], identb[:],
                                     [xck, "identb"], [pk(bank)])
                                if t % 8 == 7 or t == NT - 1:
                                    t0 = (t // 8) * 8
                                    nt_ = t - t0 + 1
                                    P.cp("act", stg[b][:, t0:t0 + nt_, :],
                                         pv[:, 0:nt_ * 128].rearrange("p (t c) -> p t c", c=128), [pk(bank)], [stk])
                            if cc < 16:
                                dv = XT.rearrange("(t p) c -> p t c", p=128)[:, :, cc * 128:(cc + 1) * 128]
                                wk = ("XT", cc)
                            else:
                                dv = BTK.rearrange("(t p) c -> p t c", p=128)[:, :, (cc - 16) * 128:(cc - 15) * 128]
                                wk = ("BTK", cc - 16)
                            P.dma("sp", dv[:, 0:17, :], stg[b][:, 0:17, :], reads=[stk], writes=[wk], semkey=stk)
                            P.dma("sp", dv[:, 17:NT, :], stg[b][:, 17:NT, :], reads=[stk], writes=[wk], semkey=stk)
                    drain(["s_cwA", "s_cbA", "s_U0", "s_U1", "s_acc", "s_xc0", "s_xc1", "s_wc0", "s_wc1", "s_stg0", "s_stg1"])
                with contextlib.ExitStack() as s1:
                    wz = P.sb(s1, "s_wz", [128, 8, SSM_DI], BF16)
                    szb = [P.sb(s1, "s_szb%d" % b, [128, SSM_DI], BF16) for b in range(2)]
                    P.dma("pool", wz[:], w_in[:, 0:SSM_DI].rearrange("(k p) n -> p k n", p=128), writes=["s_wz"])
                    for t in range(NT):
                        b = t % 2
                        for nb in range(4):
                            bank = (t * 4 + nb) % 8
                            for k in range(8):
                                P.mm(ps[bank][:, :], hT[:, k, t * 128:(t + 1) * 128], wz[:, k, nb * 512:(nb + 1) * 512],
                                     k == 0, k == 7, ["s_hT", "s_wz"], [pk(bank)])
                            P.act(szb[b][:, nb * 512:(nb + 1) * 512], ps[bank][:, :], AF.Silu, [pk(bank)], ["s_szb%d" % b])
                        P.dma("sp", ZG[t * 128:(t + 1) * 128, :], szb[b][:], reads=["s_szb%d" % b], writes=[("ZG", t)],
                              semkey="s_szb%d" % b)
                    drain(["s_wz", "s_szb0", "s_szb1", "s_hT"])
                sH.close()
                with contextlib.ExitStack() as s1:
                    tri = P.sb(s1, "s_tri", [128, 4, 128])
                    state = P.sb(s1, "s_state", [128, 32, 64])
                    stbf = P.sb(s1, "s_stbf", [128, 32, 64], BF16)
                    xk_ = [P.sb(s1, "s_xk%d" % b, [128, 32, 64], BF16) for b in range(2)]
                    btk = [P.sb(s1, "s_btk%d" % b, [128, 512], BF16) for b in range(2)]
                    bfc = [P.sb(s1, "s_bfc%d" % b, [128, 4, 128], BF16) for b in range(2)]
                    cfc = [P.sb(s1, "s_cfc%d" % b, [128, 4, 128], BF16) for b in range(2)]
                    xdt = P.sb(s1, "s_xdt", [128, 32, 64], BF16)
                    xsd = P.sb(s1, "s_xsd", [128, 32, 64], BF16)
                    laB = P.sb(s1, "s_laB", [128, 32, 128])
                    CM = P.sb(s1, "s_CM", [128, 32, 128])
                    sm = P.sb(s1, "s_sm", [128, 6, 32])
                    GTs = [P.sb(s1, "s_GTs%d" % b, [128, 128]) for b in range(2)]
                    Lg = [P.sb(s1, "s_Lg%d" % b, [128, 4, 128]) for b in range(2)]
                    WT = [P.sb(s1, "s_WT%d" % b, [128, 4, 128], BF16) for b in range(2)]
                    yoff = [P.sb(s1, "s_yoff%d" % b, [128, 8, 64]) for b in range(2)]
                    ybuf = [P.sb(s1, "s_ybuf%d" % b, [128, 32, 64]) for b in range(2)]
                    P.dma("sp", tri[:], k_tri.rearrange("w p n -> p w n"), writes=["s_tri"])
                    ccount = 0
                    for d in range(2):
                        order = list(range(NT)) if d == 0 else [1, 0] + list(range(NT - 1, 1, -1))
                        Ut, Mneg = tri[:, 2 * d, :], tri[:, 2 * d + 1, :]
                        P.memset("pool", state[:], 0.0, [("s_state", g_) for g_ in range(4)])
                        for c in order:
                            b = ccount % 2
                            ccount += 1
                            xkk, btkk, bfk, cfk, ybk = "s_xk%d" % b, "s_btk%d" % b, "s_bfc%d" % b, "s_cfc%d" % b, "s_ybuf%d" % b
                            P.dma("sp", xk_[b][:], XT[c * 128:(c + 1) * 128, :].rearrange("p (h d) -> p h d", d=64),
                                  reads=[("XT", q) for q in range(16)], writes=[xkk], semkey=xkk)
                            P.dma("sp", btk[b][:], BTK[c * 128:(c + 1) * 128, :], reads=[("BTK", q) for q in range(4)],
                                  writes=[btkk], semkey=btkk)
                            P.dma("sp", bfc[b][:], BF[:, :, c * 128:(c + 1) * 128].rearrange("g n t -> n g t"),
                                  reads=[("BF", q) for q in range(4)], writes=[bfk], semkey=bfk)
                            P.dma("sp", cfc[b][:], CF[:, :, c * 128:(c + 1) * 128].rearrange("g n t -> n g t"),
                                  reads=[("CF", q) for q in range(4)], writes=[cfk], semkey=cfk)
                            la_c = LA[:, c, d * 32:(d + 1) * 32]
                            dt_c = dtA[:, c, d * 32:(d + 1) * 32]
                            lak, dtk = ("s_LA", c), ("s_dtA", c)
                            SM = "s_sm"
                            csc, tot, ff, fx, ecs, dec = (sm[:, q, :] for q in range(6))
                            P.mm(ps[0][:, 0:32], Ut, la_c, True, True, ["s_tri", lak], [pk(0)])
                            P.mm(ps[0][:, 32:64], ones[:], la_c, True, True, ["ones", lak], [pk(0)])
                            P.cp("dve", sm[:, 0:2, :], ps[0][:, 0:64].rearrange("p (a h) -> p a h", h=32), [pk(0)], [SM])
                            P.tt("dve", ff, tot, csc, ALU.subtract, [SM], [SM])
                            P.act(ff, ff, AF.Exp, [SM], [SM])
                            P.act(ecs, csc, AF.Exp, [SM], [SM])
                            P.act(dec, tot, AF.Exp, [SM], [SM])
                            P.tt("dve", fx, ff, dt_c, ALU.mult, [SM, dtk], [SM])
                            P.tt("pool", xdt[:], xk_[b][:], dt_c.unsqueeze(2).broadcast_to([128, 32, 64]), ALU.mult,
                                 [xkk, dtk], ["s_xdt"])
                            P.tt("pool", xsd[:], xk_[b][:], fx.unsqueeze(2).broadcast_to([128, 32, 64]), ALU.mult,
                                 [xkk, SM], ["s_xsd"])
                            P.cp("dve", laB[:], la_c.unsqueeze(2).broadcast_to([128, 32, 128]), [lak], ["s_laB"])
                            P.cp("act", stbf[:], state[:], [("s_state", g_) for g_ in range(4)], ["s_stbf"])
                            P.tt("dve", CM[:], csc.unsqueeze(2).broadcast_to([128, 32, 128]),
                                 Mneg.unsqueeze(1).broadcast_to([128, 32, 128]), ALU.subtract, [SM, "s_tri"], ["s_CM"])
                            def grp_begin(g):
                                P.mm(ps[1][:, 0:128], bfc[b][:, g, :], cfc[b][:, g, :], True, True, [bfk, cfk], [pk(1)])
                                P.cp("act", GTs[g % 2][:], ps[1][:, 0:128], [pk(1)], ["s_GTs%d" % (g % 2)])
                                P.mm(ps[2][:, :], cfc[b][:, g, :], stbf[:, g * 8:(g + 1) * 8, :].rearrange("p h d -> p (h d)"),
                                     True, True, [cfk, "s_stbf"], [pk(2)])
                                P.cp("act", yoff[g % 2][:], ps[2][:, :].rearrange("p (h d) -> p h d", d=64), [pk(2)],
                                     ["s_yoff%d" % (g % 2)])

                            def quad_cs(n):
                                g, q = n // 2, n % 2
                                if q == 0:
                                    grp_begin(g)
                                cb_ = 3 + n % 2
                                for j in range(4):
                                    P.mm(ps[cb_][:, j * 128:(j + 1) * 128], laB[:, g * 8 + q * 4 + j, :], Ut, True, True,
                                         ["s_laB", "s_tri"], [pk(cb_)])

                            def grp_end(g):
                                yb_ = 5 + g % 2
                                yo, yok = yoff[g % 2], "s_yoff%d" % (g % 2)
                                P.tt("pool", yo[:], yo[:], ecs[:, g * 8:(g + 1) * 8].unsqueeze(2).broadcast_to([128, 8, 64]),
                                     ALU.mult, [yok, SM], [yok])
                                P.tt("dve", ybuf[b][:, g * 8:(g + 1) * 8, :], yo[:],
                                     ps[yb_][:, :].rearrange("p (h d) -> p h d", d=64), ALU.add, [yok, pk(yb_)], [ybk])
                                P.mm(ps[7][:, :], btk[b][:, g * 128:(g + 1) * 128],
                                     xsd[:, g * 8:(g + 1) * 8, :].rearrange("p h d -> p (h d)"), True, True,
                                     [btkk, "s_xsd"], [pk(7)])
                                P.tt("pool", state[:, g * 8:(g + 1) * 8, :], state[:, g * 8:(g + 1) * 8, :],
                                     dec[:, g * 8:(g + 1) * 8].unsqueeze(2).broadcast_to([128, 8, 64]), ALU.mult,
                                     [("s_state", g), SM], [("s_state", g)])
                                P.tt("dve", state[:, g * 8:(g + 1) * 8, :], state[:, g * 8:(g + 1) * 8, :],
                                     ps[7][:, :].rearrange("p (h d) -> p h d", d=64), ALU.add, [("s_state", g), pk(7)],
                                     [("s_state", g)])

                            quad_cs(0)
                            for n in range(8):
                                g, q = n // 2, n % 2
                                h0 = g * 8 + q * 4
                                if n + 1 < 8:
                                    quad_cs(n + 1)
                                cb_ = 3 + n % 2
                                lb = n % 2
                                lgk, wtk = "s_Lg%d" % lb, "s_WT%d" % lb
                                csv = ps[cb_][:, :].rearrange("p (j l) -> p j l", l=128)
                                P.tt("dve", Lg[lb][:], csv, CM[:, h0:h0 + 4, :], ALU.subtract, [pk(cb_), "s_CM"], [lgk])
                                P.act(Lg[lb][:], Lg[lb][:], AF.Exp, [lgk], [lgk])
                                P.tt("dve", WT[lb][:], Lg[lb][:], GTs[g % 2][:].unsqueeze(1).broadcast_to([128, 4, 128]),
                                     ALU.mult, [lgk, "s_GTs%d" % (g % 2)], [wtk])
                                yb_ = 5 + g % 2
                                for j in range(4):
                                    P.mm(ps[yb_][:, (q * 4 + j) * 64:(q * 4 + j + 1) * 64], WT[lb][:, j, :], xdt[:, h0 + j, :],
                                         True, True, [wtk, "s_xdt"], [pk(yb_)])
                                if q == 1:
                                    grp_end(g)
                            P.dma("sp", Yd[d, c * 128:(c + 1) * 128, :], ybuf[b][:].rearrange("p h d -> p (h d)"),
                                  reads=[ybk], writes=[("Yd", d, c)], semkey=ybk)
                    keys = ["s_tri", "s_stbf", "s_xdt", "s_xsd", "s_laB", "s_CM", "s_sm", "s_GTs0", "s_GTs1", "s_yoff0", "s_yoff1"]
                    keys += [("s_state", g_) for g_ in range(4)]
                    for b in range(2):
                        keys += ["s_xk%d" % b, "s_btk%d" % b, "s_bfc%d" % b, "s_cfc%d" % b, "s_Lg%d" % b, "s_WT%d" % b,
                                 "s_ybuf%d" % b]
                    keys += [("s_dtA", t) for t in range(NT)] + [("s_LA", t) for t in range(NT)] + ["s_aB"]
                    drain(keys)
            with contextlib.ExitStack() as s1:
                wo = P.sb(s1, "s_wo", [128, 16, D], BF16)
                ng = P.sb(s1, "s_ng", [128, SSM_DI])
                dsk = P.sb(s1, "s_dsk", [128, 64])
                yf = [P.sb(s1, "s_yf%d" % b, [128, 32, 64]) for b in range(2)]
                yb2 = [P.sb(s1, "s_yb2%d" % b, [128, 32, 64]) for b in range(2)]
                xk3 = [P.sb(s1, "s_xk3%d" % b, [128, 32, 64], BF16) for b in range(2)]
                zg = [P.sb(s1, "s_zg%d" % b, [128, SSM_DI], BF16) for b in range(2)]
                xo = [P.sb(s1, "s_xo%d" % b, [128, D]) for b in range(2)]
                sz_ = [P.sb(s1, "s_sz%d" % b, [128, SSM_DI]) for b in range(2)]
                gbf_ = [P.sb(s1, "s_gbf%d" % b, [128, SSM_DI], BF16) for b in range(2)]
                gT = [P.sb(s1, "s_gT%d" % b, [128, 16, 128], BF16) for b in range(2)]
                g4_ = [P.sb(s1, "s_g4%d" % b, [128, 12]) for b in range(2)]
                junk = P.sb(s1, "s_junk3", [128, 512], BF16)
                tm = [P.sb(s1, "s_tm%d" % b, [128, D]) for b in range(2)]
                P.dma("pool", wo[:], ssm_w_out[0].rearrange("(k p) n -> p k n", p=128), writes=["s_wo"])
                P.dma("sp", ng[:], ssm_norm_g[0:1, :].broadcast_to([128, SSM_DI]), writes=["s_ng"])
                P.dma("sp", dsk[:], ssm_d[0:1, :].broadcast_to([128, 64]), writes=["s_dsk"])
                P.tt("dve", dsk[:, 0:32], dsk[:, 0:32], dsk[:, 32:64], ALU.add, ["s_dsk"], ["s_dsk"])

                def s3load(t):
                    b = t % 2
                    P.dma("sp", yf[b][:], Yd[0, t * 128:(t + 1) * 128, :].rearrange("p (h d) -> p h d", d=64),
                          reads=[("Yd", 0, t)], writes=["s_yf%d" % b], semkey="s_yf%d" % b)
                    P.dma("sp", yb2[b][:], Yd[1, t * 128:(t + 1) * 128, :].rearrange("p (h d) -> p h d", d=64),
                          reads=[("Yd", 1, t)], writes=["s_yb2%d" % b], semkey="s_yb2%d" % b)
                    P.dma("sp", xk3[b][:], XT[t * 128:(t + 1) * 128, :].rearrange("p (h d) -> p h d", d=64),
                          reads=[("XT", q) for q in range(16)], writes=["s_xk3%d" % b], semkey="s_xk3%d" % b)
                    P.dma("sp", zg[b][:], ZG[t * 128:(t + 1) * 128, :], reads=[("ZG", t)], writes=["s_zg%d" % b],
                          semkey="s_zg%d" % b)
                    P.dma("sp", xo[b][:], X[t * 128:(t + 1) * 128, :], reads=[xk(t)], writes=["s_xo%d" % b], semkey="s_xo%d" % b)

                s3load(0)
                for t in range(NT):
                    if t + 1 < NT:
                        s3load(t + 1)
                    b = t % 2
                    s_ = 1 if t < 2 else 0
                    yfk, ybk, xkk, zgk, xok, gtk, tmk = ("s_yf%d" % b, "s_yb2%d" % b, "s_xk3%d" % b, "s_zg%d" % b, "s_xo%d" % b,
                                                         "s_gT%d" % b, "s_tm%d" % b)
                    P.tt("dve", yf[b][:], yf[b][:], yb2[b][:], ALU.add, [yfk, ybk], [yfk])
                    P.tt("pool", yb2[b][:], xk3[b][:], dsk[:, 0:32].unsqueeze(2).broadcast_to([128, 32, 64]), ALU.mult,
                         [xkk, "s_dsk"], [ybk])
                    P.tt("pool", yf[b][:], yf[b][:], yb2[b][:], ALU.add, [yfk, ybk], [yfk])
                    sz, gbf, g4 = sz_[b], gbf_[b], g4_[b]
                    szk, gbk, g4k = "s_sz%d" % b, "s_gbf%d" % b, "s_g4%d" % b
                    yfl = yf[b][:].rearrange("p h d -> p (h d)")
                    P.tt("dve", sz[:], zg[b][:], yfl, ALU.mult, [zgk, yfk], [szk])
                    for q in range(4):
                        P.act(junk[:], sz[:, q * 512:(q + 1) * 512], AF.Square, [szk], ["s_junk3", g4k],
                              accum_out=g4[:, q:q + 1])
                    P.act(g4[:, 4:8], g4[:, 0:4], AF.Sqrt, [g4k], [g4k], bias=EPS, scale=1.0 / 512)
                    P.recip(g4[:, 8:12], g4[:, 4:8], [g4k], [g4k])
                    for q in range(4):
                        P.stt("dve", gbf[:, q * 512:(q + 1) * 512], sz[:, q * 512:(q + 1) * 512], g4[:, 8 + q:9 + q],
                              ng[:, q * 512:(q + 1) * 512], ALU.mult, ALU.mult, [szk, g4k, "s_ng"], [gbk])
                    for k in range(16):
                        bank = (0 if b == 0 else 2) + k // 8
                        pv = ps[bank][:, :].bitcast(BF16)
                        P.tr(pv[:, (k % 8) * 128:(k % 8 + 1) * 128], gbf[:, k * 128:(k + 1) * 128], identb[:],
                             [gbk, "identb"], [pk(bank)])
                    for q in range(2):
                        bank = (0 if b == 0 else 2) + q
                        P.cp("act", gT[b][:, q * 8:(q + 1) * 8, :],
                             ps[bank][:, :].bitcast(BF16).rearrange("p (k t) -> p k t", t=128), [pk(bank)], [gtk])
                    for half in range(2):
                        bank = 4 + 2 * b + half
                        for k in range(16):
                            P.mm(ps[bank][:, :], gT[b][:, k, :], wo[:, k, half * 512:(half + 1) * 512], k == 0, k == 15,
                                 [gtk, "s_wo"], [pk(bank)])
                        P.tt("dve", tm[b][:, half * 512:(half + 1) * 512], ps[bank][:, :],
                             gates[:, 0, s_, half * 512:(half + 1) * 512], ALU.mult, [pk(bank), ("gates", 0, s_)], [tmk])
                    P.tt("pool", xo[b][:], xo[b][:], tm[b][:], ALU.add, [xok, tmk], [xok])
                    P.dma("sp", X[t * 128:(t + 1) * 128, :], xo[b][:], reads=[xok], writes=[xk(t)], semkey=xok)
                keys = ["s_wo", "s_ng", "s_dsk", "s_junk3"]
                for b in range(2):
                    keys += ["s_sz%d" % b, "s_gbf%d" % b, "s_g4%d" % b]
                    keys += ["s_yf%d" % b, "s_yb2%d" % b, "s_xk3%d" % b, "s_zg%d" % b, "s_xo%d" % b, "s_gT%d" % b, "s_tm%d" % b]
                drain(keys)

    def moe_sparse(i, tiles):
        ntl = len(tiles)
        Hs = P.dscratch("ms_Hs%d" % i, [NSLOT, D], BF16)
        Z = P.dscratch("ms_Z%d" % i, [NSLOT, D])
        wgu_rows = moe_w_gu[i]
        wdn_rows = moe_w_dn[i]
        IOA = bass.IndirectOffsetOnAxis
        with contextlib.ExitStack() as st:
            WW = P.sb(st, "q_WW", [128, ntl, 2])
            POSI = P.sb(st, "q_POSI", [128, ntl, 2], I32)
            OFFGU = P.sb(st, "q_OFFGU", [128, NBLK], I32)
            pst = P.sb(st, "q_pst", [128, NE])
            km = P.sb(st, "q_km", [128, 256])
            P.dma("sp", km[:], k_moe, writes=["q_km"])
            with contextlib.ExitStack() as s1:
                HTOK = P.sb(s1, "q_HTOK", [128, ntl, D], BF16)
                OH = P.sb(s1, "q_OH", [128, ntl, 3, NE])
                wr = P.sb(s1, "q_wr", [128, 8, 36])
                rt = P.sb(s1, "q_rt", [128, 160])
                hT2 = P.sb(s1, "q_hT2", [128, 8, 256], BF16)
                P.dma("sp", wr[:], moe_wr[i].rearrange("(k p) n -> p k n", p=128), writes=["q_wr"])

                LG = P.sb(s1, "q_LG", [128, ntl, 36])

                def router(idx, t, h32, hkey):
                    lg = ps[5]
                    for k in range(8):
                        P.mm(lg[:, 0:36], h32[:, k, :], wr[:, k, :], k == 0, k == 7, [hkey, "q_wr"], [pk(5)])
                    P.cp("dve", LG[:, idx, :], lg[:, 0:36], [pk(5)], [("q_LG", idx)])
                    c0 = (idx % 2) * 128
                    pv = ps[3][:, :].bitcast(BF16)
                    for c in range(8):
                        P.tr(pv[:, c * 128:(c + 1) * 128], hT2[:, c, c0:c0 + 128], identb[:],
                             [("q_hT2", idx % 2), "identb"], [pk(3)])
                    P.cp("act", HTOK[:, idx, :], pv[:, :], [pk(3)], [("q_HTOK", idx)])

                with contextlib.ExitStack() as s2:
                    norm_tiles(s2, tiles, 1, hT2, "q_hT2", 0, hook=router, tag="qn", colfn=lambda idx: (idx % 2) * 128)
                    drain(["qn_xt0", "qn_xt1", "qn_junk", "qn_st0", "qn_st1", "qn_h320", "qn_h321"])
                R = "q_rt"
                rb = P.sb(s1, "q_rb", [128, 8, ntl])
                g4 = P.sb(s1, "q_g4", [128, 2, ntl, 4])
                le = P.sb(s1, "q_le", [128, 2, ntl, NE])
                lgk = [("q_LG", idx) for idx in range(ntl)]
                ohk = [("q_OH", idx) for idx in range(ntl)]
                wwk = [("q_WW", idx) for idx in range(ntl)]
                LGg, LGe = LG[:, :, 0:4], LG[:, :, 4:36]
                gmax, gate, m1, m2, dd, p1, p2 = (rb[:, q, :] for q in range(7))
                bc4 = lambda v: v.unsqueeze(2).broadcast_to([128, ntl, 4])
                bc32 = lambda v: v.unsqueeze(2).broadcast_to([128, ntl, NE])
                P.red("dve", gmax, LGg, ALU.max, lgk, [R])
                P.tt("dve", g4[:, 0], LGg, bc4(gmax), ALU.subtract, lgk + [R], [R])
                P.act(g4[:, 0], g4[:, 0], AF.Exp, [R], [R])
                P.red("dve", gate, g4[:, 0], ALU.add, [R], [R])
                P.recip(gate, gate, [R], [R])
                P.tt("dve", g4[:, 1], LGg, bc4(gmax), ALU.is_ge, lgk + [R], [R])
                P.ts("dve", g4[:, 1], g4[:, 1], -1.0, ALU.add, [R], [R], s2=BIG, op1=ALU.mult)
                P.tt("dve", le[:, 0].rearrange("p t (g j) -> p t g j", g=4), LGe.rearrange("p t (g j) -> p t g j", g=4),
                     g4[:, 1].unsqueeze(3).broadcast_to([128, ntl, 4, 8]), ALU.add, lgk + [R], [R])
                P.red("dve", m1, le[:, 0], ALU.max, [R], [R])
                oh1, oh2, oha = OH[:, :, 0, :], OH[:, :, 1, :], OH[:, :, 2, :]
                P.tt("dve", oh1, le[:, 0], bc32(m1), ALU.is_ge, [R], ohk)
                P.stt("dve", le[:, 1], oh1, -BIG, le[:, 0], ALU.mult, ALU.add, [R] + ohk, [R])
                P.red("dve", m2, le[:, 1], ALU.max, [R], [R])
                P.tt("dve", oh2, le[:, 1], bc32(m2), ALU.is_ge, [R], ohk)
                P.tt("pool", oha, oh1, oh2, ALU.add, ohk, ohk)
                P.tt("dve", dd, m2, m1, ALU.subtract, [R], [R])
                P.act(dd, dd, AF.Exp, [R], [R])
                P.ts("dve", p1, dd, 1.0, ALU.add, [R], [R])
                P.recip(p1, p1, [R], [R])
                P.tt("dve", p2, dd, p1, ALU.mult, [R], [R])
                P.tt("dve", WW[:, :, 0], p1, gate, ALU.mult, [R], wwk)
                P.tt("dve", WW[:, :, 1], p2, gate, ALU.mult, [R], wwk)
                for idx in range(ntl):
                    P.mm(ps[4][:, 0:NE], ones[:], OH[:, idx, 2, :], idx == 0, idx == ntl - 1, ["ones", ("q_OH", idx)], [pk(4)])
                ob = P.sb(s1, "q_ob", [128, 8, NE])
                c3 = P.sb(s1, "q_c3", [128, NBLK, NE])
                be = P.sb(s1, "q_be", [128, NBLK])
                O_ = "q_ob"
                cnt, nb_, pend, tmpa = ob[:, 0, :], ob[:, 1, :], ob[:, 2, :], ob[:, 3, :]
                P.cp("dve", cnt, ps[4][:, 0:NE], [pk(4)], [O_])
                P.tt("dve", c3[:, 0:NTH, :],
                     cnt.unsqueeze(1).broadcast_to([128, NTH, NE]), km[:, 128:128 + NTH].unsqueeze(2).broadcast_to([128, NTH, NE]),
                     ALU.is_gt, [O_, "q_km"], ["q_c3"])
                P.red("dve", nb_, c3[:, 0:NTH, :].rearrange("p m e -> p e m"), ALU.add, ["q_c3"], [O_])
                P.ts("dve", nb_, nb_, float(BLK), ALU.mult, [O_], [O_])
                P.cp("dve", pend, nb_, [O_], [O_])
                src, dst = pend, tmpa
                for sft in (1, 2, 4, 8, 16):
                    P.cp("dve", dst[:, 0:sft], src[:, 0:sft], [O_], [O_])
                    P.tt("dve", dst[:, sft:NE], src[:, sft:NE], src[:, 0:NE - sft], ALU.add, [O_], [O_])
                    src, dst = dst, src
                pend_f = src
                P.tt("dve", pst[:], pend_f, nb_, ALU.subtract, [O_], ["q_pst"])
                P.tt("dve", c3[:], pend_f.unsqueeze(1).broadcast_to([128, NBLK, NE]),
                     km[:, 0:NBLK].unsqueeze(2).broadcast_to([128, NBLK, NE]), ALU.is_le, [O_, "q_km"], ["q_c3"])
                P.red("dve", be[:], c3[:], ALU.add, ["q_c3"], ["q_be"])
                P.ts("dve", be[:], be[:], float(NE - 1), ALU.min, ["q_be"], ["q_be"])
                P.ts("dve", be[:], be[:], 128.0, ALU.mult, ["q_be"], ["q_be"])
                P.tt("dve", be[:], be[:], km[:, 200:201].broadcast_to([128, NBLK]), ALU.add, ["q_be", "q_km"], ["q_be"])
                sk_ = c3[:, 0:4, :].rearrange("p a e -> p (a e)")[:, 0:NBLK - 1]
                P.tt("dve", sk_, be[:, 1:NBLK], be[:, 0:NBLK - 1], ALU.is_equal, ["q_be"], ["q_c3"])
                P.stt("dve", be[:, 1:NBLK], sk_, 1.0e6, be[:, 1:NBLK], ALU.mult, ALU.add, ["q_c3", "q_be"], ["q_be"])
                P.cp("dve", OFFGU[:], be[:], ["q_be"], ["q_OFFGU"])
                ustr = P.sb(s1, "q_ustr", [128, 128])
                trl = P.sb(s1, "q_trl", [128, 128])
                P.dma("sp", trl[:], k_tri[0], writes=["q_trl"])
                P.tt("dve", ustr[:], trl[:], ident[:], ALU.subtract, ["q_trl", "ident"], ["q_ustr"])
                rk = P.sb(s1, "q_rk", [128, 3, ntl, NE])
                posf = P.sb(s1, "q_posf", [128, ntl, 2])
                ohk2 = [("q_OH", idx) for idx in range(ntl)]
                for idx in range(ntl):
                    bank, col = idx // 16, (idx % 16) * NE
                    P.mm(ps[bank][:, col:col + NE], ustr[:], OH[:, idx, 2, :], True, True, ["q_ustr", ("q_OH", idx)], [pk(bank)])
                    P.mm(ps[3 + bank][:, col:col + NE], ones[:], OH[:, idx, 2, :], True, True, ["ones", ("q_OH", idx)], [pk(3 + bank)])
                for bank in range((ntl + 15) // 16):
                    n_ = min(16, ntl - bank * 16)
                    P.cp("act", rk[:, 0, bank * 16:bank * 16 + n_, :], ps[bank][:, 0:n_ * NE].rearrange("p (t e) -> p t e", e=NE),
                         [pk(bank)], ["q_rk0"])
                    P.cp("dve", rk[:, 1, bank * 16:bank * 16 + n_, :], ps[3 + bank][:, 0:n_ * NE].rearrange("p (t e) -> p t e", e=NE),
                         [pk(3 + bank)], ["q_rk1"])
                src, dst, sk1, dk1 = 1, 2, "q_rk1", "q_rk2"
                sft = 1
                while sft < ntl:
                    P.cp("dve", rk[:, dst, 0:sft, :], rk[:, src, 0:sft, :], [sk1], [dk1])
                    P.tt("dve", rk[:, dst, sft:ntl, :], rk[:, src, sft:ntl, :], rk[:, src, 0:ntl - sft, :], ALU.add, [sk1], [dk1])
                    src, dst, sk1, dk1 = dst, src, dk1, sk1
                    sft *= 2
                for bank in range((ntl + 15) // 16):
                    n_ = min(16, ntl - bank * 16)
                    P.tt("dve", rk[:, src, bank * 16:bank * 16 + n_, :], rk[:, src, bank * 16:bank * 16 + n_, :],
                         ps[3 + bank][:, 0:n_ * NE].rearrange("p (t e) -> p t e", e=NE), ALU.subtract, [sk1, pk(3 + bank)], [sk1])
                P.tt("dve", rk[:, 0], rk[:, 0], rk[:, src], ALU.add, ["q_rk0", sk1], ["q_rk0"])
                P.tt("dve", rk[:, 0], rk[:, 0], pst[:].unsqueeze(1).broadcast_to([128, ntl, NE]), ALU.add, ["q_rk0", "q_pst"], ["q_rk0"])
                for k2 in range(2):
                    P.tt("dve", rk[:, dst], rk[:, 0], OH[:, :, k2, :], ALU.mult, ["q_rk0", dk1] + ohk2, [dk1])
                    P.red("dve", posf[:, :, k2], rk[:, dst], ALU.add, [dk1], ["q_posf"])
                P.cp("dve", POSI[:], posf[:], ["q_posf"], ["q_POSI"])
                for idx in range(ntl):
                    for k2 in range(2):
                        S.idma(Hs[:, :], IOA(ap=POSI[:, idx, k2:k2 + 1], axis=0), HTOK[:, idx, :], None, NSLOT - 1,
                               reads=[("q_HTOK", idx), "q_POSI"], writes=["Hs"], semkey="q_scat")
                drain(["q_wr", "q_rt", "q_rb", "q_g4", "q_le"] + [("q_LG", idx) for idx in range(ntl)] + ["q_hT2", ("q_hT2", 0), ("q_hT2", 1), "q_ob", "q_c3", "q_be", "q_ustr", "q_trl",
                       "q_rk0", "q_rk1", "q_rk2", "q_posf"] + [("q_HTOK", idx) for idx in range(ntl)] + [("q_OH", idx) for idx in range(ntl)])
            with contextlib.ExitStack() as s1:
                w32g = P.sb(s1, "q_w32g", [128, 8, 2 * FH])
                w32d = P.sb(s1, "q_w32d", [128, 4, D])
                wgu = [P.sb(s1, "q_wgu%d" % b, [128, 8, 2 * FH], BF16) for b in range(2)]
                wdn = [P.sb(s1, "q_wdn%d" % b, [128, 4, D], BF16) for b in range(2)]
                hs = [P.sb(s1, "q_hs%d" % b, [128, NSB, D], BF16) for b in range(2)]
                hTs = P.sb(s1, "q_hTs", [128, 8, BLK], BF16)
                sg = [P.sb(s1, "q_sg%d" % b, [128, BLK], BF16) for b in range(2)]
                aT = P.sb(s1, "q_aT", [128, 4, BLK], BF16)
                zt = [P.sb(s1, "q_zt%d" % b, [128, D]) for b in range(2)]

                def gatherw(j):
                    b = j % 2
                    S.idma(w32g[:].rearrange("p k n -> p (k n)"), None, wgu_rows, IOA(ap=OFFGU[:, j:j + 1], axis=0), NE * 128 - 1,
                           reads=["q_OFFGU"], writes=[("q_w32g", k) for k in range(8)], semkey="q_w32g")
                    S.idma(w32d[:].rearrange("p k n -> p (k n)"), None, wdn_rows, IOA(ap=OFFGU[:, j:j + 1], axis=0), NE * 128 - 1,
                           reads=["q_OFFGU"], writes=[("q_w32d", k) for k in range(4)], semkey="q_w32d")
                    P.dma("sp", hs[b][:], Hs[j * BLK:(j + 1) * BLK, :].rearrange("(s p) d -> p s d", p=128),
                          reads=["Hs"], writes=["q_hs%d" % b], semkey="q_hs%d" % b)

                def castw(j):
                    b = j % 2
                    for k in range(8):
                        eng_ = "act" if k % 2 == 0 else "dve"
                        P.cp(eng_, wgu[b][:, k, :], w32g[:, k, :], [("q_w32g", k)], [("q_wgu%d" % b, k)])
                    for k in range(4):
                        P.cp("act" if k % 2 == 0 else "dve", wdn[b][:, k, :], w32d[:, k, :], [("q_w32d", k)],
                             [("q_wdn%d" % b, k)])

                def slots_T(j):
                    b = j % 2
                    for s_ in range(NSB):
                        bank = 4 + s_
                        pv = ps[bank][:, :].bitcast(BF16)
                        for c in range(8):
                            P.tr(pv[:, c * 128:(c + 1) * 128], hs[b][:, s_, c * 128:(c + 1) * 128], identb[:],
                                 ["q_hs%d" % b, "identb"], [pk(bank)])
                        P.cp("act" if s_ % 2 == 0 else "dve", hTs[:, :, s_ * 128:(s_ + 1) * 128],
                             pv[:, :].rearrange("p (c t) -> p c t", t=128), [pk(bank)], [("q_hTs", s_)])

                gatherw(0)
                castw(0)
                slots_T(0)
                zc = 0
                for j in range(NBLK):
                    b = j % 2
                    if j + 1 < NBLK:
                        gatherw(j + 1)
                    gkeys = [("q_wgu%d" % b, k) for k in range(8)]
                    dkeys = [("q_wdn%d" % b, k) for k in range(4)]
                    hkeys = [("q_hTs", s_) for s_ in range(NSB)]
                    for jj in range(4):
                        gb, ub = (jj % 2), 2 + (jj % 2)
                        for k in range(8):
                            P.mm(ps[gb][:, 0:BLK], wgu[b][:, k, jj * 128:(jj + 1) * 128], hTs[:, k, :], k == 0, k == 7,
                                 [gkeys[k]] + hkeys, [pk(gb)])
                        for k in range(8):
                            P.mm(ps[ub][:, 0:BLK], wgu[b][:, k, FH + jj * 128:FH + (jj + 1) * 128], hTs[:, k, :], k == 0, k == 7,
                                 [gkeys[k]] + hkeys, [pk(ub)])
                        sk = "q_sg%d" % (jj % 2)
                        P.act(sg[jj % 2][:], ps[gb][:, 0:BLK], AF.Silu, [pk(gb)], [sk])
                        P.tt("dve", aT[:, jj, :], sg[jj % 2][:], ps[ub][:, 0:BLK], ALU.mult, [sk, pk(ub)], [("q_aT", jj)])
                    for tt_ in range(NSB):
                        zb = zc % 2
                        zc += 1
                        zk = "q_zt%d" % zb
                        for half in range(2):
                            db = 4 + half
                            for jj in range(4):
                                P.mm(ps[db][:, :], aT[:, jj, tt_ * 128:(tt_ + 1) * 128], wdn[b][:, jj, half * 512:(half + 1) * 512],
                                     jj == 0, jj == 3, [("q_aT", jj), dkeys[jj]], [pk(db)])
                            P.cp("act" if half == 0 else "dve", zt[zb][:, half * 512:(half + 1) * 512], ps[db][:, :],
                                 [pk(db)], [zk])
                        r0 = j * BLK + tt_ * 128
                        P.dma("sp", Z[r0:r0 + 128, :], zt[zb][:], reads=[zk], writes=["Z"], semkey=zk)
                    if j + 1 < NBLK:
                        slots_T(j + 1)
                        castw(j + 1)
                keys = ["q_hTs", "q_sg0", "q_sg1", "q_zt0", "q_zt1", "q_hs0", "q_hs1"]
                keys += [("q_w32g", k) for k in range(8)] + [("q_w32d", k) for k in range(4)]
                keys += [("q_wgu%d" % b, k) for b in range(2) for k in range(8)]
                keys += [("q_wdn%d" % b, k) for b in range(2) for k in range(4)]
                keys += [("q_aT", jj) for jj in range(4)] + [("q_hTs", s_) for s_ in range(NSB)]
                drain(keys)
            with contextlib.ExitStack() as s1:
                NBUF = 4
                z1 = [P.sb(s1, "q_z1%d" % b, [128, D]) for b in range(NBUF)]
                z2 = [P.sb(s1, "q_z2%d" % b, [128, D]) for b in range(NBUF)]
                xo = [P.sb(s1, "q_xo%d" % b, [128, D]) for b in range(NBUF)]

                def cload(idx):
                    b = idx % NBUF
                    t = tiles[idx]
                    S.idma(z1[b][:], None, Z[:, :], IOA(ap=POSI[:, idx, 0:1], axis=0), NSLOT - 1,
                           reads=["Z", "q_POSI"], writes=["q_z1%d" % b], semkey="q_z1%d" % b)
                    S.idma(z2[b][:], None, Z[:, :], IOA(ap=POSI[:, idx, 1:2], axis=0), NSLOT - 1,
                           reads=["Z", "q_POSI"], writes=["q_z2%d" % b], semkey="q_z2%d" % b)
                    P.dma("sp", xo[b][:], X[t * 128:(t + 1) * 128, :], reads=[xk(t)], writes=["q_xo%d" % b], semkey="q_xo%d" % b)

                for idx in range(min(NBUF - 1, ntl)):
                    cload(idx)
                for idx, t in enumerate(tiles):
                    if idx + NBUF - 1 < ntl:
                        cload(idx + NBUF - 1)
                    b = idx % NBUF
                    s_ = 1 if t < 2 else 0
                    k1, k2_, ok = "q_z1%d" % b, "q_z2%d" % b, "q_xo%d" % b
                    P.ts("dve", z1[b][:], z1[b][:], WW[:, idx, 0:1], ALU.mult, [k1, ("q_WW", idx)], [k1])
                    P.stt("dve", z1[b][:], z2[b][:], WW[:, idx, 1:2], z1[b][:], ALU.mult, ALU.add, [k1, k2_, ("q_WW", idx)], [k1])
                    P.tt("pool", z1[b][:], z1[b][:], gates[:, 1, s_, :], ALU.mult, [k1, ("gates", 1, s_)], [k1])
                    P.tt("dve", xo[b][:], xo[b][:], z1[b][:], ALU.add, [ok, k1], [ok])
                    P.dma("sp", X[t * 128:(t + 1) * 128, :], xo[b][:], reads=[ok], writes=[xk(t)], semkey=ok)
                drain(["q_z1%d" % b for b in range(4)] + ["q_z2%d" % b for b in range(4)] + ["q_xo%d" % b for b in range(4)] + ["q_POSI", "q_OFFGU", "q_pst", "q_km",
                       "Hs", "Z"] + [("q_WW", idx) for idx in range(ntl)])

    def final_norm():
        with contextlib.ExitStack() as st:
            gb = P.sb(st, "f_g", [128, D])
            xt = [P.sb(st, "f_x%d" % j, [128, D]) for j in range(2)]
            junk = P.sb(st, "f_junk", [128, D])
            st8 = [P.sb(st, "f_st%d" % j, [128, 4]) for j in range(2)]
            P.dma("sp", gb[:], final_g.partition_broadcast(128) if False else final_g[0:1, :].broadcast_to([128, D]),
                  writes=["f_g"])
            for idx in range(SEQ // 128):
                t = idx + 2
                b = idx % 2
                xkey, skey = "f_x%d" % b, "f_st%d" % b
                P.dma("sp", xt[b][:], X[t * 128:(t + 1) * 128, :], reads=[xk(t)], writes=[xkey], semkey=xkey)
                P.act(junk[:], xt[b][:], AF.Square, [xkey], ["f_junk", skey], accum_out=st8[b][:, 0:1])
                P.act(st8[b][:, 1:2], st8[b][:, 0:1], AF.Sqrt, [skey], [skey], bias=EPS, scale=1.0 / D)
                P.recip(st8[b][:, 2:3], st8[b][:, 1:2], [skey], [skey])
                P.stt("dve", xt[b][:], xt[b][:], st8[b][:, 2:3], gb[:], ALU.mult, ALU.mult, [xkey, skey, "f_g"], [xkey])
                P.dma("sp", out[idx * 128:(idx + 1) * 128, :], xt[b][:], reads=[xkey], writes=[("out", idx)],
                      semkey=xkey)
            for e_ in ("sp", "act", "dve"):
                S.wait_all(e_, ["f_g", "f_x0", "f_x1", "f_junk", "f_st0", "f_st1"])

    ALL = list(range(NT))
    LAT = list(range(2, NT))
    for i in layers:
        kind = i % 3
        ctx_out = i < DEPTH - 1
        adaln(i, kind == 1)
        S.recycle()
        if mixers_on:
            if kind == 0:
                attention(i, i // 3, ctx_out)
            elif kind == 1:
                pool_mixer(i)
            else:
                ssd_mixer(i)
            S.recycle()
        if moe_on:
            if cfg.get("dense_moe"):
                moe(i, ALL if ctx_out else LAT)
            else:
                moe_sparse(i, ALL if ctx_out else LAT)
            S.recycle()
    final_norm()
    for e_ in ("sp", "pe", "act", "dve", "pool"):
        S.wait_all(e_, list(S.wr.keys()))
    P.es.close()
    return P


def host_consts():
    ident = np.eye(128, dtype=np.float32)
    n_freq = HD // 4
    inv = (10000.0 ** (-np.arange(n_freq, dtype=np.float32) / n_freq)).astype(np.float32)
    tok = np.arange(SEQ)
    row = (tok // GRID_W).astype(np.float32)
    col = (tok % GRID_W).astype(np.float32)
    ang = np.concatenate([row[:, None] * inv[None, :], col[:, None] * inv[None, :]], axis=-1).astype(np.float32)
    rope = np.zeros((T, 64), np.float32)
    rope[:CTX, :32] = 1.0
    rope[CTX:, :32] = np.cos(ang)
    rope[CTX:, 32:] = np.sin(ang)
    band = np.zeros((4, 5, 128, 128), np.float32)
    n = 128 * 4
    for wi, w in enumerate(POOL_WINDOWS):
        M = np.zeros((n, n), np.float64)
        for t in range(n):
            lo = max(t - w // 2, 0)
            hi = min(t + w // 2, n)
            M[lo:hi, t] = 1.0 / (hi - lo)
            M[t, t] -= 1.0
        band[wi, 0] = M[0:128, 128:256]
        band[wi, 1] = M[0:128, 0:128]
        band[wi, 2] = M[128:256, 128:256]
        band[wi, 3] = M[384:512, 384:512]
        band[wi, 4] = M[256:384, 128:256]
    tri = np.zeros((4, 128, 128), np.float32)
    s = np.arange(128)[:, None]
    l = np.arange(128)[None, :]
    tri[0] = (s <= l)
    tri[1] = np.where(s <= l, 0.0, -1.0e4)
    tri[2] = (s >= l)
    tri[3] = np.where(s >= l, 0.0, -1.0e4)
    kmoe = np.zeros((128, 256), np.float32)
    kmoe[:, 0:NBLK] = (np.arange(NBLK) * BLK)[None, :]
    kmoe[:, 128:128 + NTH] = (np.arange(NTH) * BLK)[None, :]
    kmoe[:, 200:208] = np.arange(8)[None, :] * 128 + np.arange(128)[:, None]
    return {"k_ident": ident, "k_rope": rope, "k_band": band, "k_tri": tri, "k_moe": kmoe}


_CACHE = {}


def kernel(**inputs):
    cfg = inputs.pop("_cfg", {})
    key = repr(sorted(cfg.items()))
    if key not in _CACHE:
        _CACHE[key] = build(cfg)
    P = _CACHE[key]
    f = lambda a: np.ascontiguousarray(np.asarray(a, dtype=np.float32))
    consts = host_consts()
    shared = {}
    for name in ("norm_mix_g", "norm_ffn_g",
                 "attn_q_norm_g", "attn_k_norm_g", "pool_w", "pool_scale", "ssm_w_in", "ssm_conv_w",
                 "ssm_conv_b", "ssm_norm_g", "ssm_w_out"):
        shared[name] = f(inputs[name])
    wrc = np.concatenate([f(inputs["moe_w_router_group"]), f(inputs["moe_w_router_expert"])], axis=-1)
    for i in range(DEPTH):
        if "w_ada_%d" % i in P.inp:
            shared["w_ada_%d" % i] = f(inputs["w_ada"][i])
            shared["b_ada_%d" % i] = f(inputs["b_ada"][i]).reshape(1, 6 * D)
        if "moe_w_gu_%d" % i in P.inp:
            shared["moe_w_gu_%d" % i] = np.ascontiguousarray(
                f(inputs["moe_w_gate_up"][i]).reshape(NE, 8, 128, 2 * FH).transpose(0, 2, 1, 3)).reshape(NE * 128, 8 * 2 * FH)
            shared["moe_w_dn_%d" % i] = np.ascontiguousarray(
                f(inputs["moe_w_down"][i]).reshape(NE, 4, 128, D).transpose(0, 2, 1, 3)).reshape(NE * 128, 4 * D)
            shared["moe_wr_%d" % i] = np.ascontiguousarray(wrc[i])
    for j in range(2):
        if "attn_w_qkv_%d" % j in P.inp:
            shared["attn_w_qkv_%d" % j] = f(inputs["attn_w_qkv"][j])
            shared["attn_w_o_%d" % j] = f(inputs["attn_w_o"][j])
    shared["final_norm_g"] = f(inputs["final_norm_g"]).reshape(1, D)
    shared["c_ctx"] = f(inputs["c_ctx"]).reshape(1, D)
    for name in ("ssm_dt_bias", "ssm_a_log", "ssm_d"):
        shared[name] = f(inputs[name]).reshape(1, 2 * SSM_H)
    shared.update(consts)
    x = f(inputs["x"])
    ctx = f(inputs["ctx"])
    c = f(inputs["c"])
    in_maps = []
    ncores = cfg.get("cores", 8)
    for core in range(ncores):
        b = core % NB
        m = dict(shared)
        m["x"] = x[b]
        m["ctx"] = ctx[b]
        m["c"] = c[b:b + 1]
        in_maps.append({k: v for k, v in m.items() if k in P.inp})
    if cfg.get("trace"):
        res = run_bass_kernel_spmd(P.nc, in_maps, core_ids=list(range(ncores)), trace=True)
        kernel.exec_ns = res.exec_time_ns
    else:
        res = run_bass_kernel_spmd(P.nc, in_maps, core_ids=list(range(ncores)))
    nb = min(NB, ncores)
    outs = np.stack([np.asarray(res.results[b]["out"], dtype=np.float32) for b in range(nb)], axis=0)
    if cfg.get("dbg"):
        kernel.dbg = [np.asarray(res.results[b]["dbg"]) for b in range(nb)]
    return outs
```

```python
import contextlib
import numpy as np
import concourse.bass as bass
import concourse.mybir as mybir
from concourse.bass_utils import run_bass_kernel_spmd

F32 = mybir.dt.float32
BF16 = mybir.dt.bfloat16
I32 = mybir.dt.int32
ALU = mybir.AluOpType
AF = mybir.ActivationFunctionType
AX = mybir.AxisListType

D = 1024
NB = 4
SEQ = 4096
CTX = 256
T = SEQ + CTX
NT = T // 128
DEPTH = 4
EPS = 1e-6
GRID_W = 64
NH, NKV, HD = 16, 4, 64
POOL_WINDOWS = (2, 4, 8, 16)
SSM_DI, SSM_H, SSM_P, SSM_G, SSM_N = 2048, 32, 64, 4, 128
SSM_CONV_DIM = SSM_DI + 2 * SSM_G * SSM_N
SSM_IN = SSM_DI + SSM_CONV_DIM + 2 * SSM_H
NE, EPG, FH = 32, 8, 512
BIG = 1.0e30
BLK = 256
NSB = BLK // 128
NTH = (2 * T + BLK - 1) // BLK + 1
NBLK = (2 * T + BLK - 1) // BLK + NE
NSLOT = NBLK * BLK

SAME_ENGINE_SYNC = ("act", "dve", "pool")


class Sched:
    def __init__(self, nc):
        self.nc = nc
        self.eng = {"pe": nc.tensor, "act": nc.scalar, "dve": nc.vector,
                    "pool": nc.gpsimd, "sp": nc.sync}
        self.esem = {}
        self.ecnt = {}
        for e in ("pe", "act", "dve", "pool"):
            self.esem[e] = nc.alloc_semaphore("s_" + e)
            self.ecnt[e] = 0
        self.seen = {e: {} for e in self.eng}
        self.wr = {}
        self.rd = {}
        self.dsem = {}
        self.dfree = []
        self.nsem = 0
        self.bregs = {}
        self.n_inst = 0

    def _wait(self, e, toks):
        eng = self.eng[e]
        for name, (sem, val, src) in toks.items():
            if src == e and e not in SAME_ENGINE_SYNC:
                continue
            if self.seen[e].get(name, 0) >= val:
                continue
            eng.wait_ge(sem, val)
            self.seen[e][name] = val

    def _deps(self, e, reads, writes):
        for k in reads:
            self._wait(e, self.wr.get(k, {}))
        for k in writes:
            self._wait(e, self.wr.get(k, {}))
            self._wait(e, self.rd.get(k, {}))

    def _commit(self, name, tok, reads, writes):
        for k in reads:
            self.rd.setdefault(k, {})[name] = tok
        for k in writes:
            self.wr[k] = {name: tok}
            self.rd[k] = {}

    def op(self, e, fn, reads=(), writes=()):
        self._deps(e, reads, writes)
        ins = fn(self.eng[e])
        self.ecnt[e] += 1
        ins.then_inc(self.esem[e], 1)
        self._commit("s_" + e, (self.esem[e], self.ecnt[e], e), reads, writes)
        self.n_inst += 1
        return ins

    def dma(self, q, out, in_, reads=(), writes=(), semkey=None, **kw):
        if semkey is None:
            semkey = tuple(writes) + tuple(reads)
        ent = self._dsem_get(semkey)
        self._deps(q, reads, writes)
        ins = self.eng[q].dma_start(out=out, in_=in_, **kw)
        ent[1] += 16
        ins.then_inc(ent[0], 16)
        self._commit(ent[2], (ent[0], ent[1], "dma"), reads, writes)
        self.n_inst += 1

    def idma(self, out, out_off, in_, in_off, bounds, reads=(), writes=(), semkey=None):
        q = "pool"
        ent = self._dsem_get(semkey)
        self._deps(q, reads, writes)
        if bounds not in self.bregs:
            self.bregs[bounds] = self.eng[q].to_reg(bounds)
        ins = self.eng[q].indirect_dma_start(out=out, out_offset=out_off, in_=in_, in_offset=in_off,
                                             bounds_check=self.bregs[bounds], oob_is_err=False)
        ent[1] += 16
        ins.then_inc(ent[0], 16)
        self._commit(ent[2], (ent[0], ent[1], "dma"), reads, writes)
        self.n_inst += 1

    def recycle(self):
        for key, ent in list(self.dsem.items()):
            for e in self.eng:
                if self.seen[e].get(ent[2], 0) < ent[1]:
                    self.eng[e].wait_ge(ent[0], ent[1])
                    self.seen[e][ent[2]] = ent[1]
            self.dfree.append(ent)
            del self.dsem[key]

    def _dsem_get(self, semkey):
        if semkey not in self.dsem:
            if self.dfree:
                self.dsem[semkey] = self.dfree.pop()
            else:
                nm = "d%d" % self.nsem
                self.nsem += 1
                self.dsem[semkey] = [self.nc.alloc_semaphore(nm), 0, nm]
        return self.dsem[semkey]

    def wait_all(self, e, keys):
        for k in keys:
            self._wait(e, self.wr.get(k, {}))
            self._wait(e, self.rd.get(k, {}))


class Prog:
    def __init__(self, cfg):
        self.cfg = cfg
        self.nc = nc = bass.Bass("TRN2", target_bir_lowering=False)
        self.S = Sched(nc)
        self.es = contextlib.ExitStack()
        self.inp = {}
        self.uid = 0

    def din(self, name, shape, dt=F32):
        t = self.nc.dram_tensor(name, list(shape), dt, kind="ExternalInput").ap()
        self.inp[name] = t
        return t

    def dscratch(self, name, shape, dt=F32):
        return self.nc.dram_tensor(name, list(shape), dt, kind="Internal").ap()

    def sb(self, stack, name, shape, dt=F32):
        self.uid += 1
        return stack.enter_context(self.nc.sbuf_tensor("%s_u%d" % (name, self.uid), list(shape), dt))

    def mm(self, out, lhsT, rhs, start, stop, reads, writes):
        self.S.op("pe", lambda e: e.matmul(out, lhsT=lhsT, rhs=rhs, start=start, stop=stop),
                  reads=reads, writes=writes)

    def tr(self, out, in_, ident, reads, writes):
        self.S.op("pe", lambda e: e.transpose(out, in_, ident), reads=reads, writes=writes)

    def act(self, out, in_, func, reads, writes, bias=None, scale=None, accum_out=None):
        kw = {}
        if bias is not None:
            kw["bias"] = bias
        if scale is not None:
            kw["scale"] = scale
        if accum_out is not None:
            kw["accum_out"] = accum_out
        self.S.op("act", lambda e: e.activation(out=out, in_=in_, func=func, **kw),
                  reads=reads, writes=writes)

    def tt(self, e, out, in0, in1, op, reads, writes):
        self.S.op(e, lambda g: g.tensor_tensor(out=out, in0=in0, in1=in1, op=op),
                  reads=reads, writes=writes)

    def ts(self, e, out, in0, s1, op0, reads, writes, s2=None, op1=None, accum_out=None):
        kw = {}
        if op1 is not None:
            kw["op1"] = op1
        if accum_out is not None:
            kw["accum_out"] = accum_out
        self.S.op(e, lambda g: g.tensor_scalar(out=out, in0=in0, scalar1=s1, scalar2=s2, op0=op0, **kw),
                  reads=reads, writes=writes)

    def stt(self, e, out, in0, scalar, in1, op0, op1, reads, writes):
        self.S.op(e, lambda g: g.scalar_tensor_tensor(out=out, in0=in0, scalar=scalar, in1=in1,
                                                      op0=op0, op1=op1),
                  reads=reads, writes=writes)

    def cp(self, e, out, in_, reads, writes):
        if e == "act":
            self.S.op("act", lambda g: g.copy(out=out, in_=in_), reads=reads, writes=writes)
        else:
            self.S.op(e, lambda g: g.tensor_copy(out=out, in_=in_), reads=reads, writes=writes)

    def red(self, e, out, in_, op, reads, writes, axis=AX.X):
        self.S.op(e, lambda g: g.tensor_reduce(out=out, in_=in_, axis=axis, op=op),
                  reads=reads, writes=writes)

    def recip(self, out, in_, reads, writes):
        self.S.op("dve", lambda g: g.reciprocal(out=out, in_=in_), reads=reads, writes=writes)

    def memset(self, e, ap, val, writes):
        self.S.op(e, lambda g: g.memset(ap, val), writes=writes)

    def dma(self, q, out, in_, reads=(), writes=(), semkey=None, **kw):
        self.S.dma(q, out, in_, reads=reads, writes=writes, semkey=semkey, **kw)


def build(cfg):
    P = Prog(cfg)
    nc, S = P.nc, P.S
    layers = cfg.get("layers", list(range(DEPTH)))
    mixers_on = cfg.get("mixers", True)
    moe_on = cfg.get("moe", True)

    x_in = P.din("x", [SEQ, D])
    ctx_in = P.din("ctx", [CTX, D])
    c_in = P.din("c", [1, D])
    cctx_in = P.din("c_ctx", [1, D])
    w_ada = {i: P.din("w_ada_%d" % i, [D, 6 * D]) for i in layers}
    b_ada = {i: P.din("b_ada_%d" % i, [1, 6 * D]) for i in layers}
    norm_mix_g = P.din("norm_mix_g", [DEPTH, D])
    norm_ffn_g = P.din("norm_ffn_g", [DEPTH, D])
    final_g = P.din("final_norm_g", [1, D])
    attn_w_qkv = {j: P.din("attn_w_qkv_%d" % j, [D, 1536]) for j in range(2) if 3 * j in layers and mixers_on}
    attn_w_o = {j: P.din("attn_w_o_%d" % j, [D, D]) for j in range(2) if 3 * j in layers and mixers_on}
    attn_qg = P.din("attn_q_norm_g", [2, HD])
    attn_kg = P.din("attn_k_norm_g", [2, HD])
    pool_w = P.din("pool_w", [1, 4, 256, 256])
    pool_scale = P.din("pool_scale", [1, D])
    ssm_on = 2 in layers and mixers_on
    ssm_w_in = P.din("ssm_w_in", [1, D, SSM_IN]) if ssm_on else None
    ssm_conv_w = P.din("ssm_conv_w", [1, 4, SSM_CONV_DIM])
    ssm_conv_b = P.din("ssm_conv_b", [1, SSM_CONV_DIM])
    ssm_dt_bias = P.din("ssm_dt_bias", [1, 2 * SSM_H])
    ssm_a_log = P.din("ssm_a_log", [1, 2 * SSM_H])
    ssm_d = P.din("ssm_d", [1, 2 * SSM_H])
    ssm_norm_g = P.din("ssm_norm_g", [1, SSM_DI])
    ssm_w_out = P.din("ssm_w_out", [1, SSM_DI, D]) if ssm_on else None
    moe_l = [i for i in layers if moe_on]
    moe_wr = {i: P.din("moe_wr_%d" % i, [D, 36]) for i in moe_l}
    moe_w_gu = {i: P.din("moe_w_gu_%d" % i, [NE * 128, 8 * 2 * FH]) for i in moe_l}
    moe_w_dn = {i: P.din("moe_w_dn_%d" % i, [NE * 128, 4 * D]) for i in moe_l}
    k_ident = P.din("k_ident", [128, 128])
    k_rope = P.din("k_rope", [T, 64])
    k_band = P.din("k_band", [4, 5, 128, 128])
    k_tri = P.din("k_tri", [4, 128, 128])
    k_moe = P.din("k_moe", [128, 256])
    out = nc.dram_tensor("out", [SEQ, D], F32, kind="ExternalOutput").ap()
    dbg = None
    if cfg.get("dbg"):
        dbg = nc.dram_tensor("dbg", list(cfg["dbg"]), F32, kind="ExternalOutput").ap()

    X = P.dscratch("X", [T, D])

    def xk(t):
        return ("X", t)

    top = P.es
    ident = P.sb(top, "ident", [128, 128])
    identb = P.sb(top, "identb", [128, 128], BF16)
    ones = P.sb(top, "ones", [128, 128])
    cT = P.sb(top, "cT", [128, 8, 2])
    modL = P.sb(top, "modL", [128, 48])
    modC = P.sb(top, "modC", [128, 48])
    vec = P.sb(top, "vec", [128, 2, 2, 2, 8])
    gates = P.sb(top, "gates", [128, 2, 2, D])
    ps = [top.enter_context(nc.psum_tensor("ps%d" % i, [128, 512], F32)) for i in range(8)]

    def pk(i):
        return "ps%d" % i

    P.dma("sp", ident[:], k_ident, writes=["ident"])
    P.cp("dve", identb[:], ident[:], ["ident"], ["identb"])
    P.memset("pool", ones[:], 1.0, ["ones"])
    P.dma("sp", X[0:CTX, :], ctx_in, writes=[xk(0), xk(1)], semkey="xinit")
    P.dma("sp", X[CTX:T, :], x_in, writes=[xk(t) for t in range(2, NT)], semkey="xinit")

    with contextlib.ExitStack() as st:
        craw = P.sb(st, "craw", [128, 8, 2])
        P.dma("sp", craw[:, :, 0], c_in[0].rearrange("(k p) -> p k", p=128), writes=["craw"],
              allow_slow_non_contiguous=True)
        P.dma("sp", craw[:, :, 1], cctx_in[0].rearrange("(k p) -> p k", p=128), writes=["craw"],
              allow_slow_non_contiguous=True)
        P.act(cT[:], craw[:], AF.Silu, ["craw"], ["cT"])
        S.wait_all("sp", ["craw"])
        S.wait_all("act", ["craw"])


    def drain(keys):
        for e_ in ("sp", "pe", "act", "dve", "pool"):
            S.wait_all(e_, keys)

    def adaln(i, need_bc_all):
        with contextlib.ExitStack() as st:
            wblk = [P.sb(st, "wblk%d" % j, [128, 8, 512]) for j in range(2)]
            brow = P.sb(st, "brow", [1, 6 * D])
            bT = P.sb(st, "bT", [128, 48])
            g2 = P.sb(st, "g2", [128, 2, 8])
            cbc = P.sb(st, "cbc", [128, 2, 8, 128])
            for s in range(2):
                P.cp("dve", cbc[:, s], cT[:, :, s:s + 1].broadcast_to([128, 8, 128]), ["cT"], ["cbc"])
            P.dma("sp", brow[:], b_ada[i][0:1, :], writes=["brow"])
            P.dma("sp", bT[:], b_ada[i][0].rearrange("(j p) -> p j", p=128), writes=["bT"],
                  allow_slow_non_contiguous=True)
            P.dma("sp", g2[:, 0, :], norm_mix_g[i].rearrange("(j p) -> p j", p=128), writes=["g2"],
                  allow_slow_non_contiguous=True)
            P.dma("sp", g2[:, 1, :], norm_ffn_g[i].rearrange("(j p) -> p j", p=128), writes=["g2"],
                  allow_slow_non_contiguous=True)
            for n in range(12):
                wb = wblk[n % 2]
                wkey = "wblk%d" % (n % 2)
                P.dma("sp", wb[:], w_ada[i][:, n * 512:(n + 1) * 512].rearrange("(k p) n -> p k n", p=128),
                      writes=[wkey])
                for q in range(4):
                    j = n * 4 + q
                    for k in range(8):
                        P.mm(ps[0][:, 2 * j:2 * j + 2], wb[:, k, q * 128:(q + 1) * 128], cT[:, k, :],
                             k == 0, k == 7, [wkey, "cT"], [pk(0)])
                split = n // 2
                if split in (2, 5):
                    which = 0 if split == 2 else 1
                    half = n % 2
                    for s in range(2):
                        bank = 1 + s
                        for k in range(8):
                            P.mm(ps[bank][:, :], cbc[:, s, k, :], wb[:, k, :], k == 0, False,
                                 [wkey, "cbc"], [pk(bank)])
                        P.mm(ps[bank][:, :], ones[0:1, :], brow[0:1, n * 512:(n + 1) * 512], False, True,
                             ["ones", "brow"], [pk(bank)])
                        P.cp("act", gates[:, which, s, half * 512:(half + 1) * 512], ps[bank][:, :],
                             [pk(bank)], [("gates", which, s)])
            psv = ps[0][:, 0:96].rearrange("p (j s) -> p j s", s=2)
            P.tt("dve", modL[:], psv[:, :, 0], bT[:], ALU.add, [pk(0), "bT"], ["modL"])
            P.tt("dve", modC[:], psv[:, :, 1], bT[:], ALU.add, [pk(0), "bT"], ["modC"])
            for which in range(2):
                for s, m in enumerate((modL, modC)):
                    mk = "modL" if s == 0 else "modC"
                    base = which * 24
                    P.stt("dve", vec[:, which, s, 0, :], m[:, base + 8:base + 16], 1.0, g2[:, which, :],
                          ALU.add, ALU.mult, [mk, "g2"], [("vec", which, s)])
                    P.cp("dve", vec[:, which, s, 1, :], m[:, base:base + 8], [mk], [("vec", which, s)])
            S.wait_all("sp", ["wblk0", "wblk1", "brow", "bT", "g2"])
            S.wait_all("pe", ["wblk0", "wblk1", "brow", "cbc"])
            S.wait_all("dve", ["bT", "g2"])

    def norm_tiles(st, tiles, which, hT, hT_key, col0, hook=None, tag="n", colfn=None):
        NX = 3
        n = len(tiles)
        xt = [P.sb(st, "%s_xt%d" % (tag, j), [128, D]) for j in range(NX)]
        h32 = [P.sb(st, "%s_h32%d" % (tag, j), [128, 8, 128]) for j in range(2)]
        st8 = [P.sb(st, "%s_st%d" % (tag, j), [128, 4]) for j in range(NX)]
        junk = P.sb(st, "%s_junk" % tag, [128, D], BF16)

        def load(idx):
            t = tiles[idx]
            P.dma("sp", xt[idx % NX][:], X[t * 128:(t + 1) * 128, :], reads=[xk(t)],
                  writes=["%s_xt%d" % (tag, idx % NX)], semkey="%s_xt%d" % (tag, idx % NX))

        def prep(idx):
            b = idx % NX
            xkey, skey = "%s_xt%d" % (tag, b), "%s_st%d" % (tag, b)
            P.act(junk[:], xt[b][:], AF.Square, [xkey], ["%s_junk" % tag, skey], accum_out=st8[b][:, 0:1])
            P.act(st8[b][:, 1:2], st8[b][:, 0:1], AF.Sqrt, [skey], [skey], bias=EPS, scale=1.0 / D)
            P.recip(st8[b][:, 2:3], st8[b][:, 1:2], [skey], [skey])
            P.ts("dve", xt[b][:], xt[b][:], st8[b][:, 2:3], ALU.mult, [xkey, skey], [xkey])

        def fin(idx):
            t = tiles[idx]
            b = idx % NX
            hb = idx % 2
            xkey, hkey = "%s_xt%d" % (tag, b), "%s_h32%d" % (tag, hb)
            s = 1 if t < 2 else 0
            for c in range(8):
                bank = 6 + c // 4
                P.tr(ps[bank][:, (c % 4) * 128:(c % 4 + 1) * 128], xt[b][:, c * 128:(c + 1) * 128], ident[:],
                     [xkey, "ident"], [pk(bank)])
            c0 = col0 + idx * 128 if colfn is None else colfn(idx)
            hTk = hT_key if colfn is None else (hT_key, idx % 2)
            if hook is None:
                for c in range(8):
                    bank = 6 + c // 4
                    P.act(hT[:, c, c0:c0 + 128], ps[bank][:, (c % 4) * 128:(c % 4 + 1) * 128], AF.Identity,
                          [pk(bank), ("vec", which, s)], [hTk],
                          bias=vec[:, which, s, 1, c:c + 1], scale=vec[:, which, s, 0, c:c + 1])
            else:
                for c in range(8):
                    bank = 6 + c // 4
                    P.act(h32[hb][:, c, :], ps[bank][:, (c % 4) * 128:(c % 4 + 1) * 128], AF.Identity,
                          [pk(bank), ("vec", which, s)], [hkey],
                          bias=vec[:, which, s, 1, c:c + 1], scale=vec[:, which, s, 0, c:c + 1])
                P.cp("dve", hT[:, :, c0:c0 + 128], h32[hb][:], [hkey], [hTk])
                hook(idx, t, h32[hb], hkey)

        load(0)
        if n > 1:
            load(1)
        prep(0)
        for idx in range(n):
            if idx + 2 < n:
                load(idx + 2)
            if idx + 1 < n:
                prep(idx + 1)
            fin(idx)
        drain(["%s_xt%d" % (tag, j) for j in range(NX)] + ["%s_st%d" % (tag, j) for j in range(NX)]
              + ["%s_h32%d" % (tag, j) for j in range(2)] + ["%s_junk" % tag])

    def moe(i, tiles_all):
        nhalf = 2
        per = len(tiles_all) // nhalf
        for hf in range(nhalf):
            tiles = tiles_all[hf * per:(hf + 1) * per]
            ntk = per * 128
            with contextlib.ExitStack() as st:
                hT = P.sb(st, "m_hT", [128, 8, ntk], BF16)
                Y = P.sb(st, "m_Y", [128, per, D])
                Wt = P.sb(st, "m_Wt", [128, per, NE])
                wr = P.sb(st, "m_wr", [128, 8, 36])
                wgu = [P.sb(st, "m_wgu%d" % j, [128, 8, 2 * FH], BF16) for j in range(2)]
                wdn = [P.sb(st, "m_wdn%d" % j, [128, 4, D], BF16) for j in range(2)]
                sg = [P.sb(st, "m_sg%d" % j, [128, 512], BF16) for j in range(2)]
                aT = [P.sb(st, "m_a%d" % j, [128, 4, 512], BF16) for j in range(2)]
                rt = P.sb(st, "m_rt", [128, 160])

                def loadw(e):
                    b = e % 2
                    P.dma("pool", wgu[b][:], moe_w_gu[i][e * 128:(e + 1) * 128, :].rearrange("p (k n) -> p k n", k=8),
                          writes=["m_wgu%d" % b])
                    P.dma("pool", wdn[b][:], moe_w_dn[i][e * 128:(e + 1) * 128, :].rearrange("p (k n) -> p k n", k=4),
                          writes=["m_wdn%d" % b])

                P.dma("sp", wr[:], moe_wr[i].rearrange("(k p) n -> p k n", p=128), writes=["m_wr"])
                loadw(0)

                def router(idx, t, h32, hkey):
                    lg = ps[5]
                    for k in range(8):
                        P.mm(lg[:, 0:36], h32[:, k, :], wr[:, k, :], k == 0, k == 7, [hkey, "m_wr"], [pk(5)])
                    R = "m_rt"
                    lgs = rt[:, 0:36]
                    P.cp("dve", lgs, lg[:, 0:36], [pk(5)], [R])
                    gmax, ngmax, gsum, gate = rt[:, 36:37], rt[:, 37:38], rt[:, 38:39], rt[:, 39:40]
                    P.red("dve", gmax, rt[:, 0:4], ALU.max, [R], [R])
                    P.ts("dve", ngmax, gmax, -1.0, ALU.mult, [R], [R])
                    P.act(rt[:, 40:44], rt[:, 0:4], AF.Exp, [R], [R], bias=ngmax, scale=1.0, accum_out=gsum)
                    P.recip(gate, gsum, [R], [R])
                    pen = rt[:, 44:48]
                    P.ts("dve", pen, rt[:, 0:4], gmax, ALU.is_ge, [R], [R])
                    P.ts("dve", pen, pen, -1.0, ALU.add, [R], [R], s2=BIG, op1=ALU.mult)
                    le = rt[:, 48:80]
                    P.tt("dve", le.rearrange("p (g j) -> p g j", g=4), rt[:, 4:36].rearrange("p (g j) -> p g j", g=4),
                         pen.unsqueeze(2).broadcast_to([128, 4, 8]), ALU.add, [R], [R])
                    m1, m2 = rt[:, 80:81], rt[:, 81:82]
                    P.red("dve", m1, le, ALU.max, [R], [R])
                    oh1, oh2, le2 = rt[:, 84:116], rt[:, 116:148], rt[:, 4:36]
                    P.ts("dve", oh1, le, m1, ALU.is_ge, [R], [R])
                    P.stt("dve", le2, oh1, -BIG, le, ALU.mult, ALU.add, [R], [R])
                    P.red("dve", m2, le2, ALU.max, [R], [R])
                    P.ts("dve", oh2, le2, m2, ALU.is_ge, [R], [R])
                    dd, ee, p1, p2 = rt[:, 148:149], rt[:, 149:150], rt[:, 150:151], rt[:, 151:152]
                    P.tt("dve", dd, m2, m1, ALU.subtract, [R], [R])
                    P.act(ee, dd, AF.Exp, [R], [R])
                    P.ts("dve", p1, ee, 1.0, ALU.add, [R], [R])
                    P.recip(p1, p1, [R], [R])
                    P.tt("dve", p2, ee, p1, ALU.mult, [R], [R])
                    P.tt("dve", p1, p1, gate, ALU.mult, [R], [R])
                    P.tt("dve", p2, p2, gate, ALU.mult, [R], [R])
                    P.ts("dve", oh1, oh1, p1, ALU.mult, [R], [R])
                    P.stt("dve", Wt[:, idx, :], oh2, p2, oh1, ALU.mult, ALU.add, [R], [("m_Wt", idx)])

                with contextlib.ExitStack() as st2:
                    norm_tiles(st2, tiles, 1, hT, "m_hT", 0, hook=router, tag="mn")
                    S.wait_all("sp", ["mn_xt0", "mn_xt1"])
                    S.wait_all("act", ["mn_xt0", "mn_xt1", "mn_junk", "mn_st0", "mn_st1", "mn_h320", "mn_h321"])
                    S.wait_all("dve", ["mn_xt0", "mn_xt1", "mn_st0", "mn_st1"])
                    S.wait_all("pool", ["mn_h320", "mn_h321"])
                    S.wait_all("pe", ["mn_xt0", "mn_xt1", "mn_h320", "mn_h321"])

                blocks = []
                o = 0
                while o < ntk:
                    n_ = min(512, ntk - o)
                    blocks.append((o, n_))
                    o += n_
                cnt = 0
                dcnt = 0
                for e in range(NE):
                    if e + 1 < NE:
                        loadw(e + 1)
                    b = e % 2
                    gk, dk = "m_wgu%d" % b, "m_wdn%d" % b
                    for (o, n_) in blocks:
                        ab = cnt % 2
                        akey = "m_a%d" % ab
                        for j in range(4):
                            gb, ub = (j % 2), 2 + (j % 2)
                            for k in range(8):
                                P.mm(ps[gb][:, 0:n_], wgu[b][:, k, j * 128:(j + 1) * 128], hT[:, k, o:o + n_],
                                     k == 0, k == 7, [gk, "m_hT"], [pk(gb)])
                            for k in range(8):
                                P.mm(ps[ub][:, 0:n_], wgu[b][:, k, FH + j * 128:FH + (j + 1) * 128],
                                     hT[:, k, o:o + n_], k == 0, k == 7, [gk, "m_hT"], [pk(ub)])
                            sk = "m_sg%d" % (j % 2)
                            P.act(sg[j % 2][:, 0:n_], ps[gb][:, 0:n_], AF.Silu, [pk(gb)], [sk])
                            P.tt("dve", aT[ab][:, j, 0:n_], sg[j % 2][:, 0:n_], ps[ub][:, 0:n_], ALU.mult,
                                 [sk, pk(ub)], [(akey, j)])
                        for tt_ in range(n_ // 128):
                            tidx = o // 128 + tt_
                            for half in range(2):
                                db = 4 + dcnt % 2
                                dcnt += 1
                                for j in range(4):
                                    P.mm(ps[db][:, :], aT[ab][:, j, tt_ * 128:(tt_ + 1) * 128],
                                         wdn[b][:, j, half * 512:(half + 1) * 512], j == 0, j == 3,
                                         [(akey, j), dk], [pk(db)])
                                yk = ("m_Y", tidx, half)
                                ysl = Y[:, tidx, half * 512:(half + 1) * 512]
                                if e == 0:
                                    P.ts("dve", ysl, ps[db][:, :], Wt[:, tidx, e:e + 1], ALU.mult,
                                         [pk(db), ("m_Wt", tidx)], [yk])
                                else:
                                    P.stt("dve", ysl, ps[db][:, :], Wt[:, tidx, e:e + 1], ysl, ALU.mult, ALU.add,
                                          [pk(db), ("m_Wt", tidx), yk], [yk])
                        cnt += 1
                xo = [P.sb(st, "m_xo%d" % j, [128, D]) for j in range(2)]
                for idx, t in enumerate(tiles):
                    b = idx % 2
                    s = 1 if t < 2 else 0
                    ok = "m_xo%d" % b
                    P.dma("sp", xo[b][:], X[t * 128:(t + 1) * 128, :], reads=[xk(t)], writes=[ok], semkey=ok)
                    P.tt("pool", Y[:, idx, :], Y[:, idx, :], gates[:, 1, s, :], ALU.mult,
                         [("m_Y", idx, 0), ("m_Y", idx, 1), ("gates", 1, s)], [("m_Y", idx, 0), ("m_Y", idx, 1)])
                    P.tt("dve", xo[b][:], xo[b][:], Y[:, idx, :], ALU.add,
                         [ok, ("m_Y", idx, 0), ("m_Y", idx, 1)], [ok])
                    P.dma("sp", X[t * 128:(t + 1) * 128, :], xo[b][:], reads=[ok], writes=[xk(t)], semkey=ok)
                keys = ["m_hT", "m_wr", "m_wgu0", "m_wgu1", "m_wdn0", "m_wdn1", "m_sg0", "m_sg1", "m_rt",
                        "m_xo0", "m_xo1"]
                keys += [("m_a%d" % a, j) for a in range(2) for j in range(4)]
                keys += [("m_Y", idx, h) for idx in range(per) for h in range(2)]
                keys += [("m_Wt", idx) for idx in range(per)]
                for e_ in ("sp", "pe", "act", "dve", "pool"):
                    S.wait_all(e_, keys)


    def attention(i, j, ctx_out):
        QD = P.dscratch("QD%d" % i, [64, 16, T], BF16)
        with contextlib.ExitStack() as st:
            KT = P.sb(st, "a_KT", [128, 4, T], BF16)
            Vg = P.sb(st, "a_V", [128, NT, 4, 128], BF16)
            P.memset("dve", Vg[:], 0.0, ["a_V"])
            P.memset("dve", Vg[:, :, :, 64:66], 1.0, ["a_V"])
            with contextlib.ExitStack() as s1:
                hT = P.sb(s1, "a_hT", [128, 8, T], BF16)
                wqkv = P.sb(s1, "a_wqkv", [128, 8, 1536], BF16)
                gqk = P.sb(s1, "a_gqk", [128, 2, 64])
                P.dma("pool", wqkv[:], attn_w_qkv[j].rearrange("(k p) n -> p k n", p=128), writes=["a_wqkv"])
                P.dma("sp", gqk[:, 0, :], attn_qg[j:j + 1, :].broadcast_to([128, 64]), writes=["a_gqk"])
                P.dma("sp", gqk[:, 1, :], attn_kg[j:j + 1, :].broadcast_to([128, 64]), writes=["a_gqk"])
                with contextlib.ExitStack() as s2:
                    norm_tiles(s2, ALL, 0, hT, "a_hT", 0, tag="an")
                    drain(["an_xt0", "an_xt1", "an_junk", "an_st0", "an_st1", "an_h320", "an_h321"])
                qk_ = P.sb(s1, "a_qk0", [128, 20, 64])
                qk = [qk_, qk_]
                T4 = P.sb(s1, "a_T4", [128, 4 * 512])
                tmp = [T4[:, b * 512:(b + 1) * 512].rearrange("p (h i) -> p h i", i=32) for b in range(4)]
                sq = T4[:, 0:1280].rearrange("p (h d) -> p h d", d=64)
                qr = [P.sb(s1, "a_qr%d" % b, [128, 20, 64], BF16) for b in range(2)]
                ss = [P.sb(s1, "a_ss%d" % b, [128, 20]) for b in range(2)]
                cs = [P.sb(s1, "a_cs%d" % b, [128, 64]) for b in range(2)]
                tab = [P.sb(s1, "a_tab%d" % b, [128, 2, 4, 32]) for b in range(2)]
                QTt_ = P.sb(s1, "a_QTt0", [64, 16, 128], BF16)
                QTt = [QTt_, QTt_]
                gq4 = gqk[:].rearrange("p w (i two) -> p w i two", two=2)
                for t in range(NT):
                    b = t % 2
                    qkk, ssk, csk, tabk, qrk, qtk = ("a_qk0", "a_ss%d" % b, "a_cs%d" % b, "a_tab%d" % b,
                                                     "a_qr%d" % b, "a_QTt0")
                    if cfg.get("a1_lvl", 9) < 0.2:
                        continue
                    P.dma("sp", cs[b][:], k_rope[t * 128:(t + 1) * 128, :], writes=[csk])
                    if cfg.get("a1_lvl", 9) < 0.4:
                        continue
                    for nb in range(3):
                        for k in range(8):
                            P.mm(ps[nb][:, :], hT[:, k, t * 128:(t + 1) * 128], wqkv[:, k, nb * 512:(nb + 1) * 512],
                                 k == 0, k == 7, ["a_hT", "a_wqkv"], [pk(nb)])
                    if cfg.get("a1_lvl", 9) < 0.6:
                        continue
                    P.cp("act", qk[b][:, 0:8, :], ps[0][:, :].rearrange("p (h d) -> p h d", d=64), [pk(0)], [qkk])
                    P.cp("act", qk[b][:, 8:16, :], ps[1][:, :].rearrange("p (h d) -> p h d", d=64), [pk(1)], [qkk])
                    P.cp("act", qk[b][:, 16:20, :], ps[2][:, 0:256].rearrange("p (h d) -> p h d", d=64), [pk(2)], [qkk])
                    if cfg.get("a1_lvl", 9) < 0.8:
                        continue
                    P.cp("act", Vg[:, t, :, 0:64], ps[2][:, 256:512].rearrange("p (h d) -> p h d", d=64),
                         [pk(2)], [("a_V", t)])
                    if cfg.get("a1_lvl", 9) < 2:
                        continue
                    P.tt("pool", sq, qk[b][:], qk[b][:], ALU.mult, [qkk], ["a_sq", "a_t0", "a_t1", "a_t2"])
                    P.red("dve", ss[b][:], sq, ALU.add, ["a_sq", "a_t0", "a_t1", "a_t2"], [ssk])
                    P.act(ss[b][:], ss[b][:], AF.Sqrt, [ssk], [ssk], bias=EPS, scale=1.0 / HD)
                    P.recip(ss[b][:], ss[b][:], [ssk], [ssk])
                    P.tt("dve", qk[b][:], qk[b][:], ss[b][:].unsqueeze(2).broadcast_to([128, 20, 64]), ALU.mult,
                         [qkk, ssk], [qkk])
                    if cfg.get("a1_lvl", 9) < 3:
                        continue
                    for w in range(2):
                        P.tt("pool", tab[b][:, w, 0, :], cs[b][:, 0:32], gq4[:, w, :, 0], ALU.mult, [csk, "a_gqk"], [tabk])
                        P.tt("pool", tab[b][:, w, 1, :], cs[b][:, 32:64], gq4[:, w, :, 1], ALU.mult, [csk, "a_gqk"], [tabk])
                        P.tt("pool", tab[b][:, w, 2, :], cs[b][:, 32:64], gq4[:, w, :, 0], ALU.mult, [csk, "a_gqk"], [tabk])
                        P.tt("pool", tab[b][:, w, 3, :], cs[b][:, 0:32], gq4[:, w, :, 1], ALU.mult, [csk, "a_gqk"], [tabk])
                    qk4 = qk[b][:].rearrange("p h (i two) -> p h i two", two=2)
                    qr4 = qr[b][:].rearrange("p h (i two) -> p h i two", two=2)
                    for w, (h0, h1) in enumerate(((0, 16), (16, 20))):
                        nh = h1 - h0
                        x0, x1 = qk4[:, h0:h1, :, 0], qk4[:, h0:h1, :, 1]
                        tb = lambda kind: tab[b][:, w, kind, :].unsqueeze(1).broadcast_to([128, nh, 32])
                        P.tt("pool", tmp[0][:, 0:nh, :], x0, tb(0), ALU.mult, [qkk, tabk, "a_sq"], ["a_t0"])
                        P.tt("dve", tmp[1][:, 0:nh, :], x1, tb(1), ALU.mult, [qkk, tabk, "a_sq"], ["a_t1"])
                        P.tt("pool", tmp[2][:, 0:nh, :], x0, tb(2), ALU.mult, [qkk, tabk, "a_sq"], ["a_t2"])
                        P.tt("dve", tmp[3][:, 0:nh, :], x1, tb(3), ALU.mult, [qkk, tabk], ["a_t3"])
                        P.tt("dve", qr4[:, h0:h1, :, 0], tmp[0][:, 0:nh, :], tmp[1][:, 0:nh, :], ALU.subtract,
                             ["a_t0", "a_t1"], [qrk])
                        P.tt("pool", qr4[:, h0:h1, :, 1], tmp[2][:, 0:nh, :], tmp[3][:, 0:nh, :], ALU.add,
                             ["a_t2", "a_t3"], [qrk])
                    if cfg.get("a1_lvl", 9) < 4:
                        continue
                    for hh in range(20):
                        bank = 3 + hh // 8
                        pv = ps[bank][:, :].bitcast(BF16)
                        P.tr(pv[0:64, (hh % 8) * 128:(hh % 8 + 1) * 128], qr[b][:, hh, :], identb[:],
                             [qrk, "identb"], [pk(bank)])
                    if cfg.get("a1_lvl", 9) < 5:
                        continue
                    P.cp("act", QTt[b][:, 0:8, :], ps[3][:, :].bitcast(BF16)[0:64, :].rearrange("p (h t) -> p h t", t=128),
                         [pk(3)], [qtk])
                    P.cp("act", QTt[b][:, 8:16, :], ps[4][:, :].bitcast(BF16)[0:64, :].rearrange("p (h t) -> p h t", t=128),
                         [pk(4)], [qtk])
                    P.cp("act", KT[0:64, :, t * 128:(t + 1) * 128],
                         ps[5][:, :].bitcast(BF16)[0:64, 0:512].rearrange("p (h t) -> p h t", t=128),
                         [pk(5)], [("a_KT", t)])
                    if cfg.get("a1_lvl", 9) < 6:
                        continue
                    P.dma("sp", QD[:, :, t * 128:(t + 1) * 128], QTt[b][:], reads=[qtk], writes=[("QD", t)], semkey=qtk)
                keys = ["a_hT", "a_wqkv", "a_gqk", "a_sq"] + ["a_t%d" % b for b in range(4)]
                for b in range(2):
                    keys += ["a_qk%d" % b, "a_ss%d" % b, "a_cs%d" % b, "a_tab%d" % b, "a_qr%d" % b, "a_QTt%d" % b]
                drain(keys)
            with contextlib.ExitStack() as s1:
                if cfg.get("skip_a2"):
                    return
                wo = P.sb(s1, "a_wo", [64, 16, D], BF16)
                P.dma("pool", wo[:], attn_w_o[j].rearrange("(h d) n -> d h n", d=64), writes=["a_wo"])
                for kv_ in range(4):
                    P.memset("pool", KT[64:128, kv_, :], 0.0, ["a_KTz"])
                QTb = [P.sb(s1, "a_QTb%d" % b, [128, 16, 512], BF16) for b in range(2)]
                for b_ in range(2):
                    P.memset("pool", QTb[b_][64:128, :, :], 0.0, ["a_QTbz"])
                aT = [P.sb(s1, "a_aT%d" % b, [64, 16, 512], BF16) for b in range(2)]
                Pb = [P.sb(s1, "a_P%d" % b, [128, 512], BF16) for b in range(3)]
                rec = P.sb(s1, "a_rec", [65, 512])
                bcs = P.sb(s1, "a_bcs", [64, 512])
                xo = [P.sb(s1, "a_xo%d" % b, [128, D]) for b in range(2)]
                tm = [P.sb(s1, "a_tm%d" % b, [128, D]) for b in range(2)]
                qblocks = []
                if ctx_out:
                    qblocks.append((0, CTX, [0, 1]))
                for qb in range(SEQ // 512):
                    qblocks.append((CTX + qb * 512, 512, list(range(NT))))
                pcnt = 0
                xcnt = 0
                for bi, (qo, nq, ktiles) in enumerate(qblocks):
                    b = bi % 2
                    qbk, atk = "a_QTb%d" % b, "a_aT%d" % b
                    P.dma("sp", QTb[b][0:64, :, 0:nq], QD[:, :, qo:qo + nq],
                          reads=[("QD", t) for t in range(qo // 128, (qo + nq) // 128)], writes=[qbk], semkey=qbk)
                    items = [(h, idx, kt) for h in range(NH) for idx, kt in enumerate(ktiles)]
                    LOOK = 2

                    def emit_S(n):
                        h_, idx_, kt_ = items[n]
                        P.mm(ps[n % 3][:, 0:nq], KT[:, h_ // 4, kt_ * 128:(kt_ + 1) * 128], QTb[b][:, h_, 0:nq], True, True,
                             [("a_KT", kt_), "a_KTz", "a_QTbz", qbk], [pk(n % 3)])

                    def fin1(h_):
                        ob_ = 3 + h_ % 2
                        P.recip(rec[64:65, 0:nq], ps[ob_][64:65, 0:nq], [pk(ob_)], ["a_rec"])

                    def fin2(h_):
                        ob_ = 3 + h_ % 2
                        P.mm(ps[5][0:64, 0:nq], ones[64:65, 0:64], rec[64:65, 0:nq], True, True, ["ones", "a_rec"], [pk(5)])
                        P.cp("dve", bcs[:, 0:nq], ps[5][0:64, 0:nq], [pk(5)], ["a_bcs"])
                        P.tt("dve", aT[b][:, h_, 0:nq], ps[ob_][0:64, 0:nq], bcs[:, 0:nq], ALU.mult,
                             [pk(ob_), "a_bcs"], [(atk, h_)])

                    for n in range(min(LOOK, len(items))):
                        emit_S(n)
                    pend = None
                    for n, (h, idx, kt) in enumerate(items):
                        if n + LOOK < len(items):
                            emit_S(n + LOOK)
                        kv = h // 4
                        ob = 3 + h % 2
                        pb = n % 3
                        P.act(Pb[pb][:, 0:nq], ps[n % 3][:, 0:nq], AF.Exp, [pk(n % 3)], ["a_P%d" % pb], scale=HD ** -0.5)
                        P.mm(ps[ob][0:65, 0:nq], Vg[:, kt, kv, 0:65], Pb[pb][:, 0:nq], idx == 0, idx == len(ktiles) - 1,
                             [("a_V", kt), "a_V", "a_P%d" % pb], [pk(ob)])
                        if pend is not None and (idx == min(3, len(ktiles) - 1)):
                            fin2(pend)
                            pend = None
                        if idx == len(ktiles) - 1:
                            fin1(h)
                            pend = h
                    if pend is not None:
                        fin2(pend)
                    for tt_ in range(nq // 128):
                        t = qo // 128 + tt_
                        s = 1 if t < 2 else 0
                        xb = xcnt % 2
                        xcnt += 1
                        xok, tmk = "a_xo%d" % xb, "a_tm%d" % xb
                        P.dma("sp", xo[xb][:], X[t * 128:(t + 1) * 128, :], reads=[xk(t)], writes=[xok], semkey=xok)
                        for half in range(2):
                            bank = 6 + half
                            for h in range(NH):
                                P.mm(ps[bank][:, :], aT[b][:, h, tt_ * 128:(tt_ + 1) * 128],
                                     wo[:, h, half * 512:(half + 1) * 512], h == 0, h == NH - 1,
                                     [(atk, h), "a_wo"], [pk(bank)])
                            P.tt("dve", tm[xb][:, half * 512:(half + 1) * 512], ps[bank][:, :],
                                 gates[:, 0, s, half * 512:(half + 1) * 512], ALU.mult,
                                 [pk(bank), ("gates", 0, s)], [tmk])
                        P.tt("pool", xo[xb][:], xo[xb][:], tm[xb][:], ALU.add, [xok, tmk], [xok])
                        P.dma("sp", X[t * 128:(t + 1) * 128, :], xo[xb][:], reads=[xok], writes=[xk(t)], semkey=xok)
                if dbg is not None:
                    dt_ = tm[0]
                    P.cp("dve", dt_[:, 0:512], KT[:, 0, 0:512], ["a_KTz"] + [("a_KT", t) for t in range(4)], ["a_dbgt", "a_tm0"])
                    P.cp("dve", dt_[:, 512:1024], QTb[0][:, 0, 0:512], ["a_QTbz", "a_QTb0"], ["a_dbgt", "a_tm0"])
                    P.dma("sp", dbg, dt_[:], reads=["a_dbgt"], writes=["dbg"])
                    drain(["a_dbgt", "a_tm0"])
                keys = ["a_wo", "a_rec", "a_bcs", "a_V", "a_KTz", "a_QTbz"] + ["a_P%d" % b for b in range(3)]
                for b in range(2):
                    keys += ["a_QTb%d" % b, "a_xo%d" % b, "a_tm%d" % b] + [("a_aT%d" % b, h) for h in range(NH)]
                keys += [("a_KT", t) for t in range(NT)] + [("a_V", t) for t in range(NT)]
                drain(keys)


    def pool_mixer(i):
        with contextlib.ExitStack() as st:
            bcm = P.sb(st, "p_bcm", [128, 2, 2, D])
            gmb = P.sb(st, "p_gmb", [128, 2, D])
            psg = P.sb(st, "p_psg", [128, 2, D])
            band = P.sb(st, "p_band", [128, 4, 5, 128])
            wp = P.sb(st, "p_wp", [128, 4, 2, 256])
            P.dma("sp", band[:], k_band.rearrange("w k p n -> p w k n"), writes=["p_band"])
            P.dma("sp", wp[:], pool_w[0].rearrange("g (cc p) n -> p g cc n", p=128), writes=["p_wp"])
            with contextlib.ExitStack() as s1:
                wblk = [P.sb(s1, "p_wblk%d" % b, [128, 8, 512]) for b in range(2)]
                cbc = P.sb(s1, "p_cbc", [128, 2, 8, 128])
                brow = P.sb(s1, "p_brow", [1, 2 * D])
                gb = P.sb(s1, "p_gb", [128, D])
                psb = P.sb(s1, "p_psb", [128, D])
                P.dma("sp", brow[:], b_ada[i][0:1, 0:2 * D], writes=["p_brow"])
                P.dma("sp", gb[:], norm_mix_g[i:i + 1, :].broadcast_to([128, D]), writes=["p_gb"])
                P.dma("sp", psb[:], pool_scale[0:1, :].broadcast_to([128, D]), writes=["p_psb"])
                for s_ in range(2):
                    P.cp("dve", cbc[:, s_], cT[:, :, s_:s_ + 1].broadcast_to([128, 8, 128]), ["cT"], ["p_cbc"])
                for n in range(4):
                    wb, wkey = wblk[n % 2], "p_wblk%d" % (n % 2)
                    P.dma("sp", wb[:], w_ada[i][:, n * 512:(n + 1) * 512].rearrange("(k p) n -> p k n", p=128),
                          writes=[wkey])
                    for s_ in range(2):
                        bank = 1 + s_
                        for k in range(8):
                            P.mm(ps[bank][:, :], cbc[:, s_, k, :], wb[:, k, :], k == 0, False, [wkey, "p_cbc"], [pk(bank)])
                        P.mm(ps[bank][:, :], ones[0:1, :], brow[0:1, n * 512:(n + 1) * 512], False, True,
                             ["ones", "p_brow"], [pk(bank)])
                        P.cp("act", bcm[:, s_, n // 2, (n % 2) * 512:(n % 2 + 1) * 512], ps[bank][:, :],
                             [pk(bank)], ["p_bcm"])
                for s_ in range(2):
                    P.stt("dve", gmb[:, s_, :], bcm[:, s_, 1, :], 1.0, gb[:], ALU.add, ALU.mult, ["p_bcm", "p_gb"], ["p_gmb"])
                    P.tt("pool", psg[:, s_, :], psb[:], gates[:, 0, s_, :], ALU.mult, ["p_psb", ("gates", 0, s_)], ["p_psg"])
                drain(["p_wblk0", "p_wblk1", "p_cbc", "p_brow", "p_gb", "p_psb"])
            xt = [P.sb(st, "p_x%d" % b, [128, D]) for b in range(4)]
            hh = [P.sb(st, "p_h%d" % b, [128, D]) for b in range(4)]
            dT = [P.sb(st, "p_dT%d" % b, [128, 8, 128]) for b in range(2)]
            tm = [P.sb(st, "p_tm%d" % b, [128, D]) for b in range(2)]
            st8 = [P.sb(st, "p_st%d" % b, [128, 4]) for b in range(4)]
            junk = P.sb(st, "p_junk", [128, D], BF16)

            def compute_h(t):
                b = t % 4
                s_ = 1 if t < 2 else 0
                xkey, hkey, skey = "p_x%d" % b, "p_h%d" % b, "p_st%d" % b
                P.dma("sp", xt[b][:], X[t * 128:(t + 1) * 128, :], reads=[xk(t)], writes=[xkey], semkey=xkey)
                P.act(junk[:], xt[b][:], AF.Square, [xkey], ["p_junk", skey], accum_out=st8[b][:, 0:1])
                P.act(st8[b][:, 1:2], st8[b][:, 0:1], AF.Sqrt, [skey], [skey], bias=EPS, scale=1.0 / D)
                P.recip(st8[b][:, 2:3], st8[b][:, 1:2], [skey], [skey])
                P.stt("dve", hh[b][:], xt[b][:], st8[b][:, 2:3], gmb[:, s_, :], ALU.mult, ALU.mult,
                      [xkey, skey, "p_gmb"], [hkey])
                P.tt("pool", hh[b][:], hh[b][:], bcm[:, s_, 0, :], ALU.add, [hkey, "p_bcm"], [hkey])

            done = set()
            for t in range(NT):
                first = t in (0, 2)
                last = t in (1, NT - 1)
                need = [t] + ([] if first else [t - 1]) + ([] if last else [t + 1])
                for tt_ in sorted(need):
                    if tt_ not in done:
                        compute_h(tt_)
                        done.add(tt_)
                s_ = 1 if t < 2 else 0
                db = t % 2
                dkey, tmk = "p_dT%d" % db, "p_tm%d" % db
                for c in range(8):
                    wi = c // 2
                    bank = c // 4
                    col = (c % 4) * 128
                    srcs = []
                    if not first:
                        srcs.append((t - 1, 0))
                    srcs.append((t, 1 if first else (3 if last else 2)))
                    if not last:
                        srcs.append((t + 1, 4))
                    for si, (tt_, kind) in enumerate(srcs):
                        P.mm(ps[bank][:, col:col + 128], hh[tt_ % 4][:, c * 128:(c + 1) * 128], band[:, wi, kind, :],
                             si == 0, si == len(srcs) - 1, ["p_h%d" % (tt_ % 4), "p_band"], [pk(bank)])
                for bank in range(2):
                    P.cp("act", dT[db][:, bank * 4:(bank + 1) * 4, :], ps[bank][:, :].rearrange("p (c t) -> p c t", t=128),
                         [pk(bank)], [dkey])
                for g in range(4):
                    bank = 2 + g // 2
                    for cc in range(2):
                        P.mm(ps[bank][:, (g % 2) * 256:(g % 2 + 1) * 256], dT[db][:, 2 * g + cc, :], wp[:, g, cc, :],
                             cc == 0, cc == 1, [dkey, "p_wp"], [pk(bank)])
                xb = t % 4
                for half in range(2):
                    P.tt("dve", tm[db][:, half * 512:(half + 1) * 512], ps[2 + half][:, :],
                         psg[:, s_, half * 512:(half + 1) * 512], ALU.mult, [pk(2 + half), "p_psg"], [tmk])
                P.tt("pool", tm[db][:], tm[db][:], xt[xb][:], ALU.add, [tmk, "p_x%d" % xb], [tmk])
                P.dma("sp", X[t * 128:(t + 1) * 128, :], tm[db][:], reads=[tmk], writes=[xk(t)], semkey=tmk)
            keys = ["p_bcm", "p_gmb", "p_psg", "p_band", "p_wp", "p_junk", "p_dT0", "p_dT1", "p_tm0", "p_tm1"]
            for b in range(4):
                keys += ["p_x%d" % b, "p_h%d" % b, "p_st%d" % b]
            drain(keys)


    def ssd_mixer(i):
        XT = P.dscratch("s_XT", [T, SSM_DI], BF16)
        BTK = P.dscratch("s_BTK", [T, 512], BF16)
        BF = P.dscratch("s_BF", [4, 128, T], BF16)
        CF = P.dscratch("s_CF", [4, 128, T], BF16)
        Yd = P.dscratch("s_Yd", [2, T, SSM_DI])
        w_in = ssm_w_in[0]
        NU = T + 3

        def xcol(t):
            return t * 128 if t < 2 else 259 + (t - 2) * 128

        ZG = P.dscratch("s_ZG", [T, SSM_DI], BF16)
        with contextlib.ExitStack() as st:
            with contextlib.ExitStack() as sA:
                dtA = P.sb(sA, "s_dtA", [128, NT, 64])
                LA = P.sb(sA, "s_LA", [128, NT, 64])
                sH = contextlib.ExitStack()
                hT = P.sb(sH, "s_hT", [128, 8, T], BF16)
                with contextlib.ExitStack() as s1:
                    wdt = P.sb(s1, "s_wdt", [128, 8, 64])
                    dtb = P.sb(s1, "s_dtb", [128, 64])
                    aB = P.sb(s1, "s_aB", [128, 64])
                    sp_ = P.sb(s1, "s_sp", [128, 4, 64])
                    P.dma("sp", wdt[:], w_in[:, 5120:5184].rearrange("(k p) n -> p k n", p=128), writes=["s_wdt"])
                    P.dma("sp", dtb[:], ssm_dt_bias[0:1, :].broadcast_to([128, 64]), writes=["s_dtb"])
                    P.dma("sp", aB[:], ssm_a_log[0:1, :].broadcast_to([128, 64]), writes=["s_aB"])
                    P.act(aB[:], aB[:], AF.Exp, ["s_aB"], ["s_aB"])
                    P.ts("dve", aB[:], aB[:], -1.0, ALU.mult, ["s_aB"], ["s_aB"])

                    def dthook(idx, t, h32, hkey):
                        for k in range(8):
                            P.mm(ps[5][:, 0:64], h32[:, k, :], wdt[:, k, :], k == 0, k == 7, [hkey, "s_wdt"], [pk(5)])
                        K_ = "s_sp"
                        xr, ab, ee = sp_[:, 0, :], sp_[:, 1, :], sp_[:, 2, :]
                        P.tt("dve", xr, ps[5][:, 0:64], dtb[:], ALU.add, [pk(5), "s_dtb"], [K_])
                        P.ts("dve", sp_[:, 3, :], xr, -1.0, ALU.mult, [K_], [K_])
                        P.tt("dve", ab, xr, sp_[:, 3, :], ALU.max, [K_], [K_])
                        P.act(ee, ab, AF.Exp, [K_], [K_], scale=-1.0)
                        P.act(ee, ee, AF.Ln, [K_], [K_], bias=1.0, scale=1.0)
                        P.stt("dve", dtA[:, t, :], xr, 0.0, ee, ALU.max, ALU.add, [K_], [("s_dtA", t)])
                        P.tt("pool", LA[:, t, :], dtA[:, t, :], aB[:], ALU.mult, [("s_dtA", t), "s_aB"], [("s_LA", t)])

                    with contextlib.ExitStack() as s2:
                        norm_tiles(s2, ALL, 0, hT, "s_hT", 0, hook=dthook, tag="sn")
                        drain(["sn_xt0", "sn_xt1", "sn_junk", "sn_st0", "sn_st1", "sn_h320", "sn_h321"])
                    drain(["s_wdt", "s_dtb", "s_sp"])
                with contextlib.ExitStack() as s1:
                    cwA = P.sb(s1, "s_cwA", [128, 24, 4])
                    cbA = P.sb(s1, "s_cbA", [128, 24])
                    U2 = [P.sb(s1, "s_U%d" % b, [128, T + 8]) for b in range(2)]
                    acc = P.sb(s1, "s_acc", [128, NU])
                    xc = [P.sb(s1, "s_xc%d" % b, [128, NU], BF16) for b in range(2)]
                    wc = [P.sb(s1, "s_wc%d" % b, [128, 8, 128], BF16) for b in range(2)]
                    stg = [P.sb(s1, "s_stg%d" % b, [128, NT, 128], BF16) for b in range(2)]
                    for k in range(4):
                        P.dma("sp", cwA[:, :, k], ssm_conv_w[0, k].rearrange("(c p) -> p c", p=128), writes=["s_cwA"],
                              allow_slow_non_contiguous=True)
                    P.dma("sp", cbA[:], ssm_conv_b[0].rearrange("(c p) -> p c", p=128), writes=["s_cbA"],
                          allow_slow_non_contiguous=True)
                    for b_ in range(2):
                        P.memset("dve" if b_ == 0 else "pool", U2[b_][:], 0.0, ["s_U%d" % b_])
                    blocks = [(0, 256, 2)] + [(256 + b * 512, 512, 261 + b * 512) for b in range(8)]
                    mcnt = [0]

                    def inproj(cc):
                        b = cc % 2
                        wck = "s_wc%d" % b
                        U, uk = U2[b], "s_U%d" % b
                        P.dma("pool", wc[b][:], w_in[:, 2048 + cc * 128:2048 + (cc + 1) * 128].rearrange("(k p) n -> p k n", p=128),
                              writes=[wck])
                        for (t0, n_, uo) in blocks:
                            bank = mcnt[0] % 2
                            mcnt[0] += 1
                            for k in range(8):
                                P.mm(ps[bank][:, 0:n_], wc[b][:, k, :], hT[:, k, t0:t0 + n_], k == 0, k == 7,
                                     [wck, "s_hT"], [pk(bank)])
                            P.cp("act", U[:, uo:uo + n_], ps[bank][:, 0:n_], [pk(bank)], [uk])

                    inproj(0)
                    for cc in range(24):
                        b = cc % 2
                        wck, xck, stk = "s_wc%d" % b, "s_xc%d" % b, "s_stg%d" % b
                        U, uk = U2[b], "s_U%d" % b
                        if cc + 1 < 24:
                            inproj(cc + 1)
                        ce = "dve"
                        P.ts(ce, acc[:], U[:, 0:NU], cwA[:, cc, 0:1], ALU.mult, [uk, "s_cwA"], ["s_acc"])
                        for k in range(1, 4):
                            P.stt(ce, acc[:], U[:, k:k + NU], cwA[:, cc, k:k + 1], acc[:], ALU.mult, ALU.add,
                                  [uk, "s_cwA", "s_acc"], ["s_acc"])
                        P.act(xc[b][:], acc[:], AF.Silu, ["s_acc", "s_cbA"], [xck], bias=cbA[:, cc:cc + 1], scale=1.0)
                        if cc >= 16:
                            g = (cc - 16) % 4
                            dst = BF if cc < 20 else CF
                            dk = "BF" if cc < 20 else "CF"
                            P.dma("sp", dst[g, :, 0:CTX], xc[b][:, 0:CTX], reads=[xck], writes=[(dk, g)], semkey=xck)
                            P.dma("sp", dst[g, :, CTX:T], xc[b][:, 259:259 + SEQ], reads=[xck], writes=[(dk, g)], semkey=xck)
                        if cc < 20:
                            for t in range(NT):
                                bank = 2 + (t // 8) % 4
                                pv = ps[bank][:, :].bitcast(BF16)
                                P.tr(pv[:, (t % 8) * 128:(t % 8 + 1) * 128], xc[b][:, xcol(t):xcol(t) + 128], identb[:],
                                     [xck, "identb"], [pk(bank)])
                                if t % 8 == 7 or t == NT - 1:
                                    t0 = (t // 8) * 8
                                    nt_ = t - t0 + 1
                                    P.cp("act", stg[b][:, t0:t0 + nt_, :],
                                         pv[:, 0:nt_ * 128].rearrange("p (t c) -> p t c", c=128), [pk(bank)], [stk])
                            if cc < 16:
                                dv = XT.rearrange("(t p) c -> p t c", p=128)[:, :, cc * 128:(cc + 1) * 128]
                                wk = ("XT", cc)
                            else:
                                dv = BTK.rearrange("(t p) c -> p t c", p=128)[:, :, (cc - 16) * 128:(cc - 15) * 128]
                                wk = ("BTK", cc - 16)
                            P.dma("sp", dv[:, 0:17, :], stg[b][:, 0:17, :], reads=[stk], writes=[wk], semkey=stk)
                            P.dma("sp", dv[:, 17:NT, :], stg[b][:, 17:NT, :], reads=[stk], writes=[wk], semkey=stk)
                    drain(["s_cwA", "s_cbA", "s_U0", "s_U1", "s_acc", "s_xc0", "s_xc1", "s_wc0", "s_wc1", "s_stg0", "s_stg1"])
                with contextlib.ExitStack() as s1:
                    wz = P.sb(s1, "s_wz", [128, 8, SSM_DI], BF16)
                    szb = [P.sb(s1, "s_szb%d" % b, [128, SSM_DI], BF16) for b in range(2)]
                    P.dma("pool", wz[:], w_in[:, 0:SSM_DI].rearrange("(k p) n -> p k n", p=128), writes=["s_wz"])
                    for t in range(NT):
                        b = t % 2
                        for nb in range(4):
                            bank = (t * 4 + nb) % 8
                            for k in range(8):
                                P.mm(ps[bank][:, :], hT[:, k, t * 128:(t + 1) * 128], wz[:, k, nb * 512:(nb + 1) * 512],
                                     k == 0, k == 7, ["s_hT", "s_wz"], [pk(bank)])
                            P.act(szb[b][:, nb * 512:(nb + 1) * 512], ps[bank][:, :], AF.Silu, [pk(bank)], ["s_szb%d" % b])
                        P.dma("sp", ZG[t * 128:(t + 1) * 128, :], szb[b][:], reads=["s_szb%d" % b], writes=[("ZG", t)],
                              semkey="s_szb%d" % b)
                    drain(["s_wz", "s_szb0", "s_szb1", "s_hT"])
                sH.close()
                with contextlib.ExitStack() as s1:
                    tri = P.sb(s1, "s_tri", [128, 4, 128])
                    state = P.sb(s1, "s_state", [128, 32, 64])
                    stbf = P.sb(s1, "s_stbf", [128, 32, 64], BF16)
                    xk_ = [P.sb(s1, "s_xk%d" % b, [128, 32, 64], BF16) for b in range(2)]
                    btk = [P.sb(s1, "s_btk%d" % b, [128, 512], BF16) for b in range(2)]
                    bfc = [P.sb(s1, "s_bfc%d" % b, [128, 4, 128], BF16) for b in range(2)]
                    cfc = [P.sb(s1, "s_cfc%d" % b, [128, 4, 128], BF16) for b in range(2)]
                    xdt = P.sb(s1, "s_xdt", [128, 32, 64], BF16)
                    xsd = P.sb(s1, "s_xsd", [128, 32, 64], BF16)
                    laB = P.sb(s1, "s_laB", [128, 32, 128])
                    CM = P.sb(s1, "s_CM", [128, 32, 128])
                    sm = P.sb(s1, "s_sm", [128, 6, 32])
                    GTs = [P.sb(s1, "s_GTs%d" % b, [128, 128]) for b in range(2)]
                    Lg = [P.sb(s1, "s_Lg%d" % b, [128, 4, 128]) for b in range(2)]
                    WT = [P.sb(s1, "s_WT%d" % b, [128, 4, 128], BF16) for b in range(2)]
                    yoff = [P.sb(s1, "s_yoff%d" % b, [128, 8, 64]) for b in range(2)]
                    ybuf = [P.sb(s1, "s_ybuf%d" % b, [128, 32, 64]) for b in range(2)]
                    P.dma("sp", tri[:], k_tri.rearrange("w p n -> p w n"), writes=["s_tri"])
                    ccount = 0
                    for d in range(2):
                        order = list(range(NT)) if d == 0 else [1, 0] + list(range(NT - 1, 1, -1))
                        Ut, Mneg = tri[:, 2 * d, :], tri[:, 2 * d + 1, :]
                        P.memset("pool", state[:], 0.0, [("s_state", g_) for g_ in range(4)])
                        for c in order:
                            b = ccount % 2
                            ccount += 1
                            xkk, btkk, bfk, cfk, ybk = "s_xk%d" % b, "s_btk%d" % b, "s_bfc%d" % b, "s_cfc%d" % b, "s_ybuf%d" % b
                            P.dma("sp", xk_[b][:], XT[c * 128:(c + 1) * 128, :].rearrange("p (h d) -> p h d", d=64),
                                  reads=[("XT", q) for q in range(16)], writes=[xkk], semkey=xkk)
                            P.dma("sp", btk[b][:], BTK[c * 128:(c + 1) * 128, :], reads=[("BTK", q) for q in range(4)],
                                  writes=[btkk], semkey=btkk)
                            P.dma("sp", bfc[b][:], BF[:, :, c * 128:(c + 1) * 128].rearrange("g n t -> n g t"),
                                  reads=[("BF", q) for q in range(4)], writes=[bfk], semkey=bfk)
                            P.dma("sp", cfc[b][:], CF[:, :, c * 128:(c + 1) * 128].rearrange("g n t -> n g t"),
                                  reads=[("CF", q) for q in range(4)], writes=[cfk], semkey=cfk)
                            la_c = LA[:, c, d * 32:(d + 1) * 32]
                            dt_c = dtA[:, c, d * 32:(d + 1) * 32]
                            lak, dtk = ("s_LA", c), ("s_dtA", c)
                            SM = "s_sm"
                            csc, tot, ff, fx, ecs, dec = (sm[:, q, :] for q in range(6))
                            P.mm(ps[0][:, 0:32], Ut, la_c, True, True, ["s_tri", lak], [pk(0)])
                            P.mm(ps[0][:, 32:64], ones[:], la_c, True, True, ["ones", lak], [pk(0)])
                            P.cp("dve", sm[:, 0:2, :], ps[0][:, 0:64].rearrange("p (a h) -> p a h", h=32), [pk(0)], [SM])
                            P.tt("dve", ff, tot, csc, ALU.subtract, [SM], [SM])
                            P.act(ff, ff, AF.Exp, [SM], [SM])
                            P.act(ecs, csc, AF.Exp, [SM], [SM])
                            P.act(dec, tot, AF.Exp, [SM], [SM])
                            P.tt("dve", fx, ff, dt_c, ALU.mult, [SM, dtk], [SM])
                            P.tt("pool", xdt[:], xk_[b][:], dt_c.unsqueeze(2).broadcast_to([128, 32, 64]), ALU.mult,
                                 [xkk, dtk], ["s_xdt"])
                            P.tt("pool", xsd[:], xk_[b][:], fx.unsqueeze(2).broadcast_to([128, 32, 64]), ALU.mult,
                                 [xkk, SM], ["s_xsd"])
                            P.cp("dve", laB[:], la_c.unsqueeze(2).broadcast_to([128, 32, 128]), [lak], ["s_laB"])
                            P.cp("act", stbf[:], state[:], [("s_state", g_) for g_ in range(4)], ["s_stbf"])
                            P.tt("dve", CM[:], csc.unsqueeze(2).broadcast_to([128, 32, 128]),
                                 Mneg.unsqueeze(1).broadcast_to([128, 32, 128]), ALU.subtract, [SM, "s_tri"], ["s_CM"])
                            def grp_begin(g):
                                P.mm(ps[1][:, 0:128], bfc[b][:, g, :], cfc[b][:, g, :], True, True, [bfk, cfk], [pk(1)])
                                P.cp("act", GTs[g % 2][:], ps[1][:, 0:128], [pk(1)], ["s_GTs%d" % (g % 2)])
                                P.mm(ps[2][:, :], cfc[b][:, g, :], stbf[:, g * 8:(g + 1) * 8, :].rearrange("p h d -> p (h d)"),
                                     True, True, [cfk, "s_stbf"], [pk(2)])
                                P.cp("act", yoff[g % 2][:], ps[2][:, :].rearrange("p (h d) -> p h d", d=64), [pk(2)],
                                     ["s_yoff%d" % (g % 2)])

                            def quad_cs(n):
                                g, q = n // 2, n % 2
                                if q == 0:
                                    grp_begin(g)
                                cb_ = 3 + n % 2
                                for j in range(4):
                                    P.mm(ps[cb_][:, j * 128:(j + 1) * 128], laB[:, g * 8 + q * 4 + j, :], Ut, True, True,
                                         ["s_laB", "s_tri"], [pk(cb_)])

                            def grp_end(g):
                                yb_ = 5 + g % 2
                                yo, yok = yoff[g % 2], "s_yoff%d" % (g % 2)
                                P.tt("pool", yo[:], yo[:], ecs[:, g * 8:(g + 1) * 8].unsqueeze(2).broadcast_to([128, 8, 64]),
                                     ALU.mult, [yok, SM], [yok])
                                P.tt("dve", ybuf[b][:, g * 8:(g + 1) * 8, :], yo[:],
                                     ps[yb_][:, :].rearrange("p (h d) -> p h d", d=64), ALU.add, [yok, pk(yb_)], [ybk])
                                P.mm(ps[7][:, :], btk[b][:, g * 128:(g + 1) * 128],
                                     xsd[:, g * 8:(g + 1) * 8, :].rearrange("p h d -> p (h d)"), True, True,
                                     [btkk, "s_xsd"], [pk(7)])
                                P.tt("pool", state[:, g * 8:(g + 1) * 8, :], state[:, g * 8:(g + 1) * 8, :],
                                     dec[:, g * 8:(g + 1) * 8].unsqueeze(2).broadcast_to([128, 8, 64]), ALU.mult,
                                     [("s_state", g), SM], [("s_state", g)])
                                P.tt("dve", state[:, g * 8:(g + 1) * 8, :], state[:, g * 8:(g + 1) * 8, :],
                                     ps[7][:, :].rearrange("p (h d) -> p h d", d=64), ALU.add, [("s_state", g), pk(7)],
                                     [("s_state", g)])

                            quad_cs(0)
                            for n in range(8):
                                g, q = n // 2, n % 2
                                h0 = g * 8 + q * 4
                                if n + 1 < 8:
                                    quad_cs(n + 1)
                                cb_ = 3 + n % 2
                                lb = n % 2
                                lgk, wtk = "s_Lg%d" % lb, "s_WT%d" % lb
                                csv = ps[cb_][:, :].rearrange("p (j l) -> p j l", l=128)
                                P.tt("dve", Lg[lb][:], csv, CM[:, h0:h0 + 4, :], ALU.subtract, [pk(cb_), "s_CM"], [lgk])
                                P.act(Lg[lb][:], Lg[lb][:], AF.Exp, [lgk], [lgk])
                                P.tt("dve", WT[lb][:], Lg[lb][:], GTs[g % 2][:].unsqueeze(1).broadcast_to([128, 4, 128]),
                                     ALU.mult, [lgk, "s_GTs%d" % (g % 2)], [wtk])
                                yb_ = 5 + g % 2
                                for j in range(4):
                                    P.mm(ps[yb_][:, (q * 4 + j) * 64:(q * 4 + j + 1) * 64], WT[lb][:, j, :], xdt[:, h0 + j, :],
                                         True, True, [wtk, "s_xdt"], [pk(yb_)])
                                if q == 1:
                                    grp_end(g)
                            P.dma("sp", Yd[d, c * 128:(c + 1) * 128, :], ybuf[b][:].rearrange("p h d -> p (h d)"),
                                  reads=[ybk], writes=[("Yd", d, c)], semkey=ybk)
                    keys = ["s_tri", "s_stbf", "s_xdt", "s_xsd", "s_laB", "s_CM", "s_sm", "s_GTs0", "s_GTs1", "s_yoff0", "s_yoff1"]
                    keys += [("s_state", g_) for g_ in range(4)]
                    for b in range(2):
                        keys += ["s_xk%d" % b, "s_btk%d" % b, "s_bfc%d" % b, "s_cfc%d" % b, "s_Lg%d" % b, "s_WT%d" % b,
                                 "s_ybuf%d" % b]
                    keys += [("s_dtA", t) for t in range(NT)] + [("s_LA", t) for t in range(NT)] + ["s_aB"]
                    drain(keys)
            with contextlib.ExitStack() as s1:
                wo = P.sb(s1, "s_wo", [128, 16, D], BF16)
                ng = P.sb(s1, "s_ng", [128, SSM_DI])
                dsk = P.sb(s1, "s_dsk", [128, 64])
                yf = [P.sb(s1, "s_yf%d" % b, [128, 32, 64]) for b in range(2)]
                yb2 = [P.sb(s1, "s_yb2%d" % b, [128, 32, 64]) for b in range(2)]
                xk3 = [P.sb(s1, "s_xk3%d" % b, [128, 32, 64], BF16) for b in range(2)]
                zg = [P.sb(s1, "s_zg%d" % b, [128, SSM_DI], BF16) for b in range(2)]
                xo = [P.sb(s1, "s_xo%d" % b, [128, D]) for b in range(2)]
                sz_ = [P.sb(s1, "s_sz%d" % b, [128, SSM_DI]) for b in range(2)]
                gbf_ = [P.sb(s1, "s_gbf%d" % b, [128, SSM_DI], BF16) for b in range(2)]
                gT = [P.sb(s1, "s_gT%d" % b, [128, 16, 128], BF16) for b in range(2)]
                g4_ = [P.sb(s1, "s_g4%d" % b, [128, 12]) for b in range(2)]
                junk = P.sb(s1, "s_junk3", [128, 512], BF16)
                tm = [P.sb(s1, "s_tm%d" % b, [128, D]) for b in range(2)]
                P.dma("pool", wo[:], ssm_w_out[0].rearrange("(k p) n -> p k n", p=128), writes=["s_wo"])
                P.dma("sp", ng[:], ssm_norm_g[0:1, :].broadcast_to([128, SSM_DI]), writes=["s_ng"])
                P.dma("sp", dsk[:], ssm_d[0:1, :].broadcast_to([128, 64]), writes=["s_dsk"])
                P.tt("dve", dsk[:, 0:32], dsk[:, 0:32], dsk[:, 32:64], ALU.add, ["s_dsk"], ["s_dsk"])

                def s3load(t):
                    b = t % 2
                    P.dma("sp", yf[b][:], Yd[0, t * 128:(t + 1) * 128, :].rearrange("p (h d) -> p h d", d=64),
                          reads=[("Yd", 0, t)], writes=["s_yf%d" % b], semkey="s_yf%d" % b)
                    P.dma("sp", yb2[b][:], Yd[1, t * 128:(t + 1) * 128, :].rearrange("p (h d) -> p h d", d=64),
                          reads=[("Yd", 1, t)], writes=["s_yb2%d" % b], semkey="s_yb2%d" % b)
                    P.dma("sp", xk3[b][:], XT[t * 128:(t + 1) * 128, :].rearrange("p (h d) -> p h d", d=64),
                          reads=[("XT", q) for q in range(16)], writes=["s_xk3%d" % b], semkey="s_xk3%d" % b)
                    P.dma("sp", zg[b][:], ZG[t * 128:(t + 1) * 128, :], reads=[("ZG", t)], writes=["s_zg%d" % b],
                          semkey="s_zg%d" % b)
                    P.dma("sp", xo[b][:], X[t * 128:(t + 1) * 128, :], reads=[xk(t)], writes=["s_xo%d" % b], semkey="s_xo%d" % b)

                s3load(0)
                for t in range(NT):
                    if t + 1 < NT:
                        s3load(t + 1)
                    b = t % 2
                    s_ = 1 if t < 2 else 0
                    yfk, ybk, xkk, zgk, xok, gtk, tmk = ("s_yf%d" % b, "s_yb2%d" % b, "s_xk3%d" % b, "s_zg%d" % b, "s_xo%d" % b,
                                                         "s_gT%d" % b, "s_tm%d" % b)
                    P.tt("dve", yf[b][:], yf[b][:], yb2[b][:], ALU.add, [yfk, ybk], [yfk])
                    P.tt("pool", yb2[b][:], xk3[b][:], dsk[:, 0:32].unsqueeze(2).broadcast_to([128, 32, 64]), ALU.mult,
                         [xkk, "s_dsk"], [ybk])
                    P.tt("pool", yf[b][:], yf[b][:], yb2[b][:], ALU.add, [yfk, ybk], [yfk])
                    sz, gbf, g4 = sz_[b], gbf_[b], g4_[b]
                    szk, gbk, g4k = "s_sz%d" % b, "s_gbf%d" % b, "s_g4%d" % b
                    yfl = yf[b][:].rearrange("p h d -> p (h d)")
                    P.tt("dve", sz[:], zg[b][:], yfl, ALU.mult, [zgk, yfk], [szk])
                    for q in range(4):
                        P.act(junk[:], sz[:, q * 512:(q + 1) * 512], AF.Square, [szk], ["s_junk3", g4k],
                              accum_out=g4[:, q:q + 1])
                    P.act(g4[:, 4:8], g4[:, 0:4], AF.Sqrt, [g4k], [g4k], bias=EPS, scale=1.0 / 512)
                    P.recip(g4[:, 8:12], g4[:, 4:8], [g4k], [g4k])
                    for q in range(4):
                        P.stt("dve", gbf[:, q * 512:(q + 1) * 512], sz[:, q * 512:(q + 1) * 512], g4[:, 8 + q:9 + q],
                              ng[:, q * 512:(q + 1) * 512], ALU.mult, ALU.mult, [szk, g4k, "s_ng"], [gbk])
                    for k in range(16):
                        bank = (0 if b == 0 else 2) + k // 8
                        pv = ps[bank][:, :].bitcast(BF16)
                        P.tr(pv[:, (k % 8) * 128:(k % 8 + 1) * 128], gbf[:, k * 128:(k + 1) * 128], identb[:],
                             [gbk, "identb"], [pk(bank)])
                    for q in range(2):
                        bank = (0 if b == 0 else 2) + q
                        P.cp("act", gT[b][:, q * 8:(q + 1) * 8, :],
                             ps[bank][:, :].bitcast(BF16).rearrange("p (k t) -> p k t", t=128), [pk(bank)], [gtk])
                    for half in range(2):
                        bank = 4 + 2 * b + half
                        for k in range(16):
                            P.mm(ps[bank][:, :], gT[b][:, k, :], wo[:, k, half * 512:(half + 1) * 512], k == 0, k == 15,
                                 [gtk, "s_wo"], [pk(bank)])
                        P.tt("dve", tm[b][:, half * 512:(half + 1) * 512], ps[bank][:, :],
                             gates[:, 0, s_, half * 512:(half + 1) * 512], ALU.mult, [pk(bank), ("gates", 0, s_)], [tmk])
                    P.tt("pool", xo[b][:], xo[b][:], tm[b][:], ALU.add, [xok, tmk], [xok])
                    P.dma("sp", X[t * 128:(t + 1) * 128, :], xo[b][:], reads=[xok], writes=[xk(t)], semkey=xok)
                keys = ["s_wo", "s_ng", "s_dsk", "s_junk3"]
                for b in range(2):
                    keys += ["s_sz%d" % b, "s_gbf%d" % b, "s_g4%d" % b]
                    keys += ["s_yf%d" % b, "s_yb2%d" % b, "s_xk3%d" % b, "s_zg%d" % b, "s_xo%d" % b, "s_gT%d" % b, "s_tm%d" % b]
                drain(keys)

    def moe_sparse(i, tiles):
        ntl = len(tiles)
        Hs = P.dscratch("ms_Hs%d" % i, [NSLOT, D], BF16)
        Z = P.dscratch("ms_Z%d" % i, [NSLOT, D])
        wgu_rows = moe_w_gu[i]
        wdn_rows = moe_w_dn[i]
        IOA = bass.IndirectOffsetOnAxis
        with contextlib.ExitStack() as st:
            WW = P.sb(st, "q_WW", [128, ntl, 2])
            POSI = P.sb(st, "q_POSI", [128, ntl, 2], I32)
            OFFGU = P.sb(st, "q_OFFGU", [128, NBLK], I32)
            pst = P.sb(st, "q_pst", [128, NE])
            km = P.sb(st, "q_km", [128, 256])
            P.dma("sp", km[:], k_moe, writes=["q_km"])
            with contextlib.ExitStack() as s1:
                HTOK = P.sb(s1, "q_HTOK", [128, ntl, D], BF16)
                OH = P.sb(s1, "q_OH", [128, ntl, 3, NE])
                wr = P.sb(s1, "q_wr", [128, 8, 36])
                rt = P.sb(s1, "q_rt", [128, 160])
                hT2 = P.sb(s1, "q_hT2", [128, 8, 256], BF16)
                P.dma("sp", wr[:], moe_wr[i].rearrange("(k p) n -> p k n", p=128), writes=["q_wr"])

                LG = P.sb(s1, "q_LG", [128, ntl, 36])

                def router(idx, t, h32, hkey):
                    lg = ps[5]
                    for k in range(8):
                        P.mm(lg[:, 0:36], h32[:, k, :], wr[:, k, :], k == 0, k == 7, [hkey, "q_wr"], [pk(5)])
                    P.cp("dve", LG[:, idx, :], lg[:, 0:36], [pk(5)], [("q_LG", idx)])
                    c0 = (idx % 2) * 128
                    pv = ps[3][:, :].bitcast(BF16)
                    for c in range(8):
                        P.tr(pv[:, c * 128:(c + 1) * 128], hT2[:, c, c0:c0 + 128], identb[:],
                             [("q_hT2", idx % 2), "identb"], [pk(3)])
                    P.cp("act", HTOK[:, idx, :], pv[:, :], [pk(3)], [("q_HTOK", idx)])

                with contextlib.ExitStack() as s2:
                    norm_tiles(s2, tiles, 1, hT2, "q_hT2", 0, hook=router, tag="qn", colfn=lambda idx: (idx % 2) * 128)
                    drain(["qn_xt0", "qn_xt1", "qn_junk", "qn_st0", "qn_st1", "qn_h320", "qn_h321"])
                R = "q_rt"
                rb = P.sb(s1, "q_rb", [128, 8, ntl])
                g4 = P.sb(s1, "q_g4", [128, 2, ntl, 4])
                le = P.sb(s1, "q_le", [128, 2, ntl, NE])
                lgk = [("q_LG", idx) for idx in range(ntl)]
                ohk = [("q_OH", idx) for idx in range(ntl)]
                wwk = [("q_WW", idx) for idx in range(ntl)]
                LGg, LGe = LG[:, :, 0:4], LG[:, :, 4:36]
                gmax, gate, m1, m2, dd, p1, p2 = (rb[:, q, :] for q in range(7))
                bc4 = lambda v: v.unsqueeze(2).broadcast_to([128, ntl, 4])
                bc32 = lambda v: v.unsqueeze(2).broadcast_to([128, ntl, NE])
                P.red("dve", gmax, LGg, ALU.max, lgk, [R])
                P.tt("dve", g4[:, 0], LGg, bc4(gmax), ALU.subtract, lgk + [R], [R])
                P.act(g4[:, 0], g4[:, 0], AF.Exp, [R], [R])
                P.red("dve", gate, g4[:, 0], ALU.add, [R], [R])
                P.recip(gate, gate, [R], [R])
                P.tt("dve", g4[:, 1], LGg, bc4(gmax), ALU.is_ge, lgk + [R], [R])
                P.ts("dve", g4[:, 1], g4[:, 1], -1.0, ALU.add, [R], [R], s2=BIG, op1=ALU.mult)
                P.tt("dve", le[:, 0].rearrange("p t (g j) -> p t g j", g=4), LGe.rearrange("p t (g j) -> p t g j", g=4),
                     g4[:, 1].unsqueeze(3).broadcast_to([128, ntl, 4, 8]), ALU.add, lgk + [R], [R])
                P.red("dve", m1, le[:, 0], ALU.max, [R], [R])
                oh1, oh2, oha = OH[:, :, 0, :], OH[:, :, 1, :], OH[:, :, 2, :]
                P.tt("dve", oh1, le[:, 0], bc32(m1), ALU.is_ge, [R], ohk)
                P.stt("dve", le[:, 1], oh1, -BIG, le[:, 0], ALU.mult, ALU.add, [R] + ohk, [R])
                P.red("dve", m2, le[:, 1], ALU.max, [R], [R])
                P.tt("dve", oh2, le[:, 1], bc32(m2), ALU.is_ge, [R], ohk)
                P.tt("pool", oha, oh1, oh2, ALU.add, ohk, ohk)
                P.tt("dve", dd, m2, m1, ALU.subtract, [R], [R])
                P.act(dd, dd, AF.Exp, [R], [R])
                P.ts("dve", p1, dd, 1.0, ALU.add, [R], [R])
                P.recip(p1, p1, [R], [R])
                P.tt("dve", p2, dd, p1, ALU.mult, [R], [R])
                P.tt("dve", WW[:, :, 0], p1, gate, ALU.mult, [R], wwk)
                P.tt("dve", WW[:, :, 1], p2, gate, ALU.mult, [R], wwk)
                for idx in range(ntl):
                    P.mm(ps[4][:, 0:NE], ones[:], OH[:, idx, 2, :], idx == 0, idx == ntl - 1, ["ones", ("q_OH", idx)], [pk(4)])
                ob = P.sb(s1, "q_ob", [128, 8, NE])
                c3 = P.sb(s1, "q_c3", [128, NBLK, NE])
                be = P.sb(s1, "q_be", [128, NBLK])
                O_ = "q_ob"
                cnt, nb_, pend, tmpa = ob[:, 0, :], ob[:, 1, :], ob[:, 2, :], ob[:, 3, :]
                P.cp("dve", cnt, ps[4][:, 0:NE], [pk(4)], [O_])
                P.tt("dve", c3[:, 0:NTH, :],
                     cnt.unsqueeze(1).broadcast_to([128, NTH, NE]), km[:, 128:128 + NTH].unsqueeze(2).broadcast_to([128, NTH, NE]),
                     ALU.is_gt, [O_, "q_km"], ["q_c3"])
                P.red("dve", nb_, c3[:, 0:NTH, :].rearrange("p m e -> p e m"), ALU.add, ["q_c3"], [O_])
                P.ts("dve", nb_, nb_, float(BLK), ALU.mult, [O_], [O_])
                P.cp("dve", pend, nb_, [O_], [O_])
                src, dst = pend, tmpa
                for sft in (1, 2, 4, 8, 16):
                    P.cp("dve", dst[:, 0:sft], src[:, 0:sft], [O_], [O_])
                    P.tt("dve", dst[:, sft:NE], src[:, sft:NE], src[:, 0:NE - sft], ALU.add, [O_], [O_])
                    src, dst = dst, src
                pend_f = src
                P.tt("dve", pst[:], pend_f, nb_, ALU.subtract, [O_], ["q_pst"])
                P.tt("dve", c3[:], pend_f.unsqueeze(1).broadcast_to([128, NBLK, NE]),
                     km[:, 0:NBLK].unsqueeze(2).broadcast_to([128, NBLK, NE]), ALU.is_le, [O_, "q_km"], ["q_c3"])
                P.red("dve", be[:], c3[:], ALU.add, ["q_c3"], ["q_be"])
                P.ts("dve", be[:], be[:], float(NE - 1), ALU.min, ["q_be"], ["q_be"])
                P.ts("dve", be[:], be[:], 128.0, ALU.mult, ["q_be"], ["q_be"])
                P.tt("dve", be[:], be[:], km[:, 200:201].broadcast_to([128, NBLK]), ALU.add, ["q_be", "q_km"], ["q_be"])
                sk_ = c3[:, 0:4, :].rearrange("p a e -> p (a e)")[:, 0:NBLK - 1]
                P.tt("dve", sk_, be[:, 1:NBLK], be[:, 0:NBLK - 1], ALU.is_equal, ["q_be"], ["q_c3"])
                P.stt("dve", be[:, 1:NBLK], sk_, 1.0e6, be[:, 1:NBLK], ALU.mult, ALU.add, ["q_c3", "q_be"], ["q_be"])
                P.cp("dve", OFFGU[:], be[:], ["q_be"], ["q_OFFGU"])
                ustr = P.sb(s1, "q_ustr", [128, 128])
                trl = P.sb(s1, "q_trl", [128, 128])
                P.dma("sp", trl[:], k_tri[0], writes=["q_trl"])
                P.tt("dve", ustr[:], trl[:], ident[:], ALU.subtract, ["q_trl", "ident"], ["q_ustr"])
                rk = P.sb(s1, "q_rk", [128, 3, ntl, NE])
                posf = P.sb(s1, "q_posf", [128, ntl, 2])
                ohk2 = [("q_OH", idx) for idx in range(ntl)]
                for idx in range(ntl):
                    bank, col = idx // 16, (idx % 16) * NE
                    P.mm(ps[bank][:, col:col + NE], ustr[:], OH[:, idx, 2, :], True, True, ["q_ustr", ("q_OH", idx)], [pk(bank)])
                    P.mm(ps[3 + bank][:, col:col + NE], ones[:], OH[:, idx, 2, :], True, True, ["ones", ("q_OH", idx)], [pk(3 + bank)])
                for bank in range((ntl + 15) // 16):
                    n_ = min(16, ntl - bank * 16)
                    P.cp("act", rk[:, 0, bank * 16:bank * 16 + n_, :], ps[bank][:, 0:n_ * NE].rearrange("p (t e) -> p t e", e=NE),
                         [pk(bank)], ["q_rk0"])
                    P.cp("dve", rk[:, 1, bank * 16:bank * 16 + n_, :], ps[3 + bank][:, 0:n_ * NE].rearrange("p (t e) -> p t e", e=NE),
                         [pk(3 + bank)], ["q_rk1"])
                src, dst, sk1, dk1 = 1, 2, "q_rk1", "q_rk2"
                sft = 1
                while sft < ntl:
                    P.cp("dve", rk[:, dst, 0:sft, :], rk[:, src, 0:sft, :], [sk1], [dk1])
                    P.tt("dve", rk[:, dst, sft:ntl, :], rk[:, src, sft:ntl, :], rk[:, src, 0:ntl - sft, :], ALU.add, [sk1], [dk1])
                    src, dst, sk1, dk1 = dst, src, dk1, sk1
                    sft *= 2
                for bank in range((ntl + 15) // 16):
                    n_ = min(16, ntl - bank * 16)
                    P.tt("dve", rk[:, src, bank * 16:bank * 16 + n_, :], rk[:, src, bank * 16:bank * 16 + n_, :],
                         ps[3 + bank][:, 0:n_ * NE].rearrange("p (t e) -> p t e", e=NE), ALU.subtract, [sk1, pk(3 + bank)], [sk1])
                P.tt("dve", rk[:, 0], rk[:, 0], rk[:, src], ALU.add, ["q_rk0", sk1], ["q_rk0"])
                P.tt("dve", rk[:, 0], rk[:, 0], pst[:].unsqueeze(1).broadcast_to([128, ntl, NE]), ALU.add, ["q_rk0", "q_pst"], ["q_rk0"])
                for k2 in range(2):
                    P.tt("dve", rk[:, dst], rk[:, 0], OH[:, :, k2, :], ALU.mult, ["q_rk0", dk1] + ohk2, [dk1])
                    P.red("dve", posf[:, :, k2], rk[:, dst], ALU.add, [dk1], ["q_posf"])
                P.cp("dve", POSI[:], posf[:], ["q_posf"], ["q_POSI"])
                for idx in range(ntl):
                    for k2 in range(2):
                        S.idma(Hs[:, :], IOA(ap=POSI[:, idx, k2:k2 + 1], axis=0), HTOK[:, idx, :], None, NSLOT - 1,
                               reads=[("q_HTOK", idx), "q_POSI"], writes=["Hs"], semkey="q_scat")
                drain(["q_wr", "q_rt", "q_rb", "q_g4", "q_le"] + [("q_LG", idx) for idx in range(ntl)] + ["q_hT2", ("q_hT2", 0), ("q_hT2", 1), "q_ob", "q_c3", "q_be", "q_ustr", "q_trl",
                       "q_rk0", "q_rk1", "q_rk2", "q_posf"] + [("q_HTOK", idx) for idx in range(ntl)] + [("q_OH", idx) for idx in range(ntl)])
            with contextlib.ExitStack() as s1:
                w32g = P.sb(s1, "q_w32g", [128, 8, 2 * FH])
                w32d = P.sb(s1, "q_w32d", [128, 4, D])
                wgu = [P.sb(s1, "q_wgu%d" % b, [128, 8, 2 * FH], BF16) for b in range(2)]
                wdn = [P.sb(s1, "q_wdn%d" % b, [128, 4, D], BF16) for b in range(2)]
                hs = [P.sb(s1, "q_hs%d" % b, [128, NSB, D], BF16) for b in range(2)]
                hTs = P.sb(s1, "q_hTs", [128, 8, BLK], BF16)
                sg = [P.sb(s1, "q_sg%d" % b, [128, BLK], BF16) for b in range(2)]
                aT = P.sb(s1, "q_aT", [128, 4, BLK], BF16)
                zt = [P.sb(s1, "q_zt%d" % b, [128, D]) for b in range(2)]

                def gatherw(j):
                    b = j % 2
                    S.idma(w32g[:].rearrange("p k n -> p (k n)"), None, wgu_rows, IOA(ap=OFFGU[:, j:j + 1], axis=0), NE * 128 - 1,
                           reads=["q_OFFGU"], writes=[("q_w32g", k) for k in range(8)], semkey="q_w32g")
                    S.idma(w32d[:].rearrange("p k n -> p (k n)"), None, wdn_rows, IOA(ap=OFFGU[:, j:j + 1], axis=0), NE * 128 - 1,
                           reads=["q_OFFGU"], writes=[("q_w32d", k) for k in range(4)], semkey="q_w32d")
                    P.dma("sp", hs[b][:], Hs[j * BLK:(j + 1) * BLK, :].rearrange("(s p) d -> p s d", p=128),
                          reads=["Hs"], writes=["q_hs%d" % b], semkey="q_hs%d" % b)

                def castw(j):
                    b = j % 2
                    for k in range(8):
                        eng_ = "act" if k % 2 == 0 else "dve"
                        P.cp(eng_, wgu[b][:, k, :], w32g[:, k, :], [("q_w32g", k)], [("q_wgu%d" % b, k)])
                    for k in range(4):
                        P.cp("act" if k % 2 == 0 else "dve", wdn[b][:, k, :], w32d[:, k, :], [("q_w32d", k)],
                             [("q_wdn%d" % b, k)])

                def slots_T(j):
                    b = j % 2
                    for s_ in range(NSB):
                        bank = 4 + s_
                        pv = ps[bank][:, :].bitcast(BF16)
                        for c in range(8):
                            P.tr(pv[:, c * 128:(c + 1) * 128], hs[b][:, s_, c * 128:(c + 1) * 128], identb[:],
                                 ["q_hs%d" % b, "identb"], [pk(bank)])
                        P.cp("act" if s_ % 2 == 0 else "dve", hTs[:, :, s_ * 128:(s_ + 1) * 128],
                             pv[:, :].rearrange("p (c t) -> p c t", t=128), [pk(bank)], [("q_hTs", s_)])

                gatherw(0)
                castw(0)
                slots_T(0)
                zc = 0
                for j in range(NBLK):
                    b = j % 2
                    if j + 1 < NBLK:
                        gatherw(j + 1)
                    gkeys = [("q_wgu%d" % b, k) for k in range(8)]
                    dkeys = [("q_wdn%d" % b, k) for k in range(4)]
                    hkeys = [("q_hTs", s_) for s_ in range(NSB)]
                    for jj in range(4):
                        gb, ub = (jj % 2), 2 + (jj % 2)
                        for k in range(8):
                            P.mm(ps[gb][:, 0:BLK], wgu[b][:, k, jj * 128:(jj + 1) * 128], hTs[:, k, :], k == 0, k == 7,
                                 [gkeys[k]] + hkeys, [pk(gb)])
                        for k in range(8):
                            P.mm(ps[ub][:, 0:BLK], wgu[b][:, k, FH + jj * 128:FH + (jj + 1) * 128], hTs[:, k, :], k == 0, k == 7,
                                 [gkeys[k]] + hkeys, [pk(ub)])
                        sk = "q_sg%d" % (jj % 2)
                        P.act(sg[jj % 2][:], ps[gb][:, 0:BLK], AF.Silu, [pk(gb)], [sk])
                        P.tt("dve", aT[:, jj, :], sg[jj % 2][:], ps[ub][:, 0:BLK], ALU.mult, [sk, pk(ub)], [("q_aT", jj)])
                    for tt_ in range(NSB):
                        zb = zc % 2
                        zc += 1
                        zk = "q_zt%d" % zb
                        for half in range(2):
                            db = 4 + half
                            for jj in range(4):
                                P.mm(ps[db][:, :], aT[:, jj, tt_ * 128:(tt_ + 1) * 128], wdn[b][:, jj, half * 512:(half + 1) * 512],
                                     jj == 0, jj == 3, [("q_aT", jj), dkeys[jj]], [pk(db)])
                            P.cp("act" if half == 0 else "dve", zt[zb][:, half * 512:(half + 1) * 512], ps[db][:, :],
                                 [pk(db)], [zk])
                        r0 = j * BLK + tt_ * 128
                        P.dma("sp", Z[r0:r0 + 128, :], zt[zb][:], reads=[zk], writes=["Z"], semkey=zk)
                    if j + 1 < NBLK:
                        slots_T(j + 1)
                        castw(j + 1)
                keys = ["q_hTs", "q_sg0", "q_sg1", "q_zt0", "q_zt1", "q_hs0", "q_hs1"]
                keys += [("q_w32g", k) for k in range(8)] + [("q_w32d", k) for k in range(4)]
                keys += [("q_wgu%d" % b, k) for b in range(2) for k in range(8)]
                keys += [("q_wdn%d" % b, k) for b in range(2) for k in range(4)]
                keys += [("q_aT", jj) for jj in range(4)] + [("q_hTs", s_) for s_ in range(NSB)]
                drain(keys)
            with contextlib.ExitStack() as s1:
                NBUF = 4
                z1 = [P.sb(s1, "q_z1%d" % b, [128, D]) for b in range(NBUF)]
                z2 = [P.sb(s1, "q_z2%d" % b, [128, D]) for b in range(NBUF)]
                xo = [P.sb(s1, "q_xo%d" % b, [128, D]) for b in range(NBUF)]

                def cload(idx):
                    b = idx % NBUF
                    t = tiles[idx]
                    S.idma(z1[b][:], None, Z[:, :], IOA(ap=POSI[:, idx, 0:1], axis=0), NSLOT - 1,
                           reads=["Z", "q_POSI"], writes=["q_z1%d" % b], semkey="q_z1%d" % b)
                    S.idma(z2[b][:], None, Z[:, :], IOA(ap=POSI[:, idx, 1:2], axis=0), NSLOT - 1,
                           reads=["Z", "q_POSI"], writes=["q_z2%d" % b], semkey="q_z2%d" % b)
                    P.dma("sp", xo[b][:], X[t * 128:(t + 1) * 128, :], reads=[xk(t)], writes=["q_xo%d" % b], semkey="q_xo%d" % b)

                for idx in range(min(NBUF - 1, ntl)):
                    cload(idx)
                for idx, t in enumerate(tiles):
                    if idx + NBUF - 1 < ntl:
                        cload(idx + NBUF - 1)
                    b = idx % NBUF
                    s_ = 1 if t < 2 else 0
                    k1, k2_, ok = "q_z1%d" % b, "q_z2%d" % b, "q_xo%d" % b
                    P.ts("dve", z1[b][:], z1[b][:], WW[:, idx, 0:1], ALU.mult, [k1, ("q_WW", idx)], [k1])
                    P.stt("dve", z1[b][:], z2[b][:], WW[:, idx, 1:2], z1[b][:], ALU.mult, ALU.add, [k1, k2_, ("q_WW", idx)], [k1])
                    P.tt("pool", z1[b][:], z1[b][:], gates[:, 1, s_, :], ALU.mult, [k1, ("gates", 1, s_)], [k1])
                    P.tt("dve", xo[b][:], xo[b][:], z1[b][:], ALU.add, [ok, k1], [ok])
                    P.dma("sp", X[t * 128:(t + 1) * 128, :], xo[b][:], reads=[ok], writes=[xk(t)], semkey=ok)
                drain(["q_z1%d" % b for b in range(4)] + ["q_z2%d" % b for b in range(4)] + ["q_xo%d" % b for b in range(4)] + ["q_POSI", "q_OFFGU", "q_pst", "q_km",
                       "Hs", "Z"] + [("q_WW", idx) for idx in range(ntl)])

    def final_norm():
        with contextlib.ExitStack() as st:
            gb = P.sb(st, "f_g", [128, D])
            xt = [P.sb(st, "f_x%d" % j, [128, D]) for j in range(2)]
            junk = P.sb(st, "f_junk", [128, D])
            st8 = [P.sb(st, "f_st%d" % j, [128, 4]) for j in range(2)]
            P.dma("sp", gb[:], final_g.partition_broadcast(128) if False else final_g[0:1, :].broadcast_to([128, D]),
                  writes=["f_g"])
            for idx in range(SEQ // 128):
                t = idx + 2
                b = idx % 2
                xkey, skey = "f_x%d" % b, "f_st%d" % b
                P.dma("sp", xt[b][:], X[t * 128:(t + 1) * 128, :], reads=[xk(t)], writes=[xkey], semkey=xkey)
                P.act(junk[:], xt[b][:], AF.Square, [xkey], ["f_junk", skey], accum_out=st8[b][:, 0:1])
                P.act(st8[b][:, 1:2], st8[b][:, 0:1], AF.Sqrt, [skey], [skey], bias=EPS, scale=1.0 / D)
                P.recip(st8[b][:, 2:3], st8[b][:, 1:2], [skey], [skey])
                P.stt("dve", xt[b][:], xt[b][:], st8[b][:, 2:3], gb[:], ALU.mult, ALU.mult, [xkey, skey, "f_g"], [xkey])
                P.dma("sp", out[idx * 128:(idx + 1) * 128, :], xt[b][:], reads=[xkey], writes=[("out", idx)],
                      semkey=xkey)
            for e_ in ("sp", "act", "dve"):
                S.wait_all(e_, ["f_g", "f_x0", "f_x1", "f_junk", "f_st0", "f_st1"])

    ALL = list(range(NT))
    LAT = list(range(2, NT))
    for i in layers:
        kind = i % 3
        ctx_out = i < DEPTH - 1
        adaln(i, kind == 1)
        S.recycle()
        if mixers_on:
            if kind == 0:
                attention(i, i // 3, ctx_out)
            elif kind == 1:
                pool_mixer(i)
            else:
                ssd_mixer(i)
            S.recycle()
        if moe_on:
            if cfg.get("dense_moe"):
                moe(i, ALL if ctx_out else LAT)
            else:
                moe_sparse(i, ALL if ctx_out else LAT)
            S.recycle()
    final_norm()
    for e_ in ("sp", "pe", "act", "dve", "pool"):
        S.wait_all(e_, list(S.wr.keys()))
    P.es.close()
    return P


def host_consts():
    ident = np.eye(128, dtype=np.float32)
    n_freq = HD // 4
    inv = (10000.0 ** (-np.arange(n_freq, dtype=np.float32) / n_freq)).astype(np.float32)
    tok = np.arange(SEQ)
    row = (tok // GRID_W).astype(np.float32)
    col = (tok % GRID_W).astype(np.float32)
    ang = np.concatenate([row[:, None] * inv[None, :], col[:, None] * inv[None, :]], axis=-1).astype(np.float32)
    rope = np.zeros((T, 64), np.float32)
    rope[:CTX, :32] = 1.0
    rope[CTX:, :32] = np.cos(ang)
    rope[CTX:, 32:] = np.sin(ang)
    band = np.zeros((4, 5, 128, 128), np.float32)
    n = 128 * 4
    for wi, w in enumerate(POOL_WINDOWS):
        M = np.zeros((n, n), np.float64)
        for t in range(n):
            lo = max(t - w // 2, 0)
            hi = min(t + w // 2, n)
            M[lo:hi, t] = 1.0 / (hi - lo)
            M[t, t] -= 1.0
        band[wi, 0] = M[0:128, 128:256]
        band[wi, 1] = M[0:128, 0:128]
        band[wi, 2] = M[128:256, 128:256]
        band[wi, 3] = M[384:512, 384:512]
        band[wi, 4] = M[256:384, 128:256]
    tri = np.zeros((4, 128, 128), np.float32)
    s = np.arange(128)[:, None]
    l = np.arange(128)[None, :]
    tri[0] = (s <= l)
    tri[1] = np.where(s <= l, 0.0, -1.0e4)
    tri[2] = (s >= l)
    tri[3] = np.where(s >= l, 0.0, -1.0e4)
    kmoe = np.zeros((128, 256), np.float32)
    kmoe[:, 0:NBLK] = (np.arange(NBLK) * BLK)[None, :]
    kmoe[:, 128:128 + NTH] = (np.arange(NTH) * BLK)[None, :]
    kmoe[:, 200:208] = np.arange(8)[None, :] * 128 + np.arange(128)[:, None]
    return {"k_ident": ident, "k_rope": rope, "k_band": band, "k_tri": tri, "k_moe": kmoe}


_CACHE = {}


def kernel(**inputs):
    cfg = inputs.pop("_cfg", {})
    key = repr(sorted(cfg.items()))
    if key not in _CACHE:
        _CACHE[key] = build(cfg)
    P = _CACHE[key]
    f = lambda a: np.ascontiguousarray(np.asarray(a, dtype=np.float32))
    consts = host_consts()
    shared = {}
    for name in ("norm_mix_g", "norm_ffn_g",
                 "attn_q_norm_g", "attn_k_norm_g", "pool_w", "pool_scale", "ssm_w_in", "ssm_conv_w",
                 "ssm_conv_b", "ssm_norm_g", "ssm_w_out"):
        shared[name] = f(inputs[name])
    wrc = np.concatenate([f(inputs["moe_w_router_group"]), f(inputs["moe_w_router_expert"])], axis=-1)
    for i in range(DEPTH):
        if "w_ada_%d" % i in P.inp:
            shared["w_ada_%d" % i] = f(inputs["w_ada"][i])
            shared["b_ada_%d" % i] = f(inputs["b_ada"][i]).reshape(1, 6 * D)
        if "moe_w_gu_%d" % i in P.inp:
            shared["moe_w_gu_%d" % i] = np.ascontiguousarray(
                f(inputs["moe_w_gate_up"][i]).reshape(NE, 8, 128, 2 * FH).transpose(0, 2, 1, 3)).reshape(NE * 128, 8 * 2 * FH)
            shared["moe_w_dn_%d" % i] = np.ascontiguousarray(
                f(inputs["moe_w_down"][i]).reshape(NE, 4, 128, D).transpose(0, 2, 1, 3)).reshape(NE * 128, 4 * D)
            shared["moe_wr_%d" % i] = np.ascontiguousarray(wrc[i])
    for j in range(2):
        if "attn_w_qkv_%d" % j in P.inp:
            shared["attn_w_qkv_%d" % j] = f(inputs["attn_w_qkv"][j])
            shared["attn_w_o_%d" % j] = f(inputs["attn_w_o"][j])
    shared["final_norm_g"] = f(inputs["final_norm_g"]).reshape(1, D)
    shared["c_ctx"] = f(inputs["c_ctx"]).reshape(1, D)
    for name in ("ssm_dt_bias", "ssm_a_log", "ssm_d"):
        shared[name] = f(inputs[name]).reshape(1, 2 * SSM_H)
    shared.update(consts)
    x = f(inputs["x"])
    ctx = f(inputs["ctx"])
    c = f(inputs["c"])
    in_maps = []
    ncores = cfg.get("cores", 8)
    for core in range(ncores):
        b = core % NB
        m = dict(shared)
        m["x"] = x[b]
        m["ctx"] = ctx[b]
        m["c"] = c[b:b + 1]
        in_maps.append({k: v for k, v in m.items() if k in P.inp})
    if cfg.get("trace"):
        res = run_bass_kernel_spmd(P.nc, in_maps, core_ids=list(range(ncores)), trace=True)
        kernel.exec_ns = res.exec_time_ns
    else:
        res = run_bass_kernel_spmd(P.nc, in_maps, core_ids=list(range(ncores)))
    nb = min(NB, ncores)
    outs = np.stack([np.asarray(res.results[b]["out"], dtype=np.float32) for b in range(nb)], axis=0)
    if cfg.get("dbg"):
        kernel.dbg = [np.asarray(res.results[b]["dbg"]) for b in range(nb)]
    return outs
```

```python
import contextlib
import numpy as np
import concourse.bass as bass
import concourse.mybir as mybir
from concourse.bass_utils import run_bass_kernel_spmd

F32 = mybir.dt.float32
BF16 = mybir.dt.bfloat16
I32 = mybir.dt.int32
ALU = mybir.AluOpType
AF = mybir.ActivationFunctionType
AX = mybir.AxisListType

D = 1024
NB = 4
SEQ = 4096
CTX = 256
T = SEQ + CTX
NT = T // 128
DEPTH = 4
EPS = 1e-6
GRID_W = 64
NH, NKV, HD = 16, 4, 64
POOL_WINDOWS = (2, 4, 8, 16)
SSM_DI, SSM_H, SSM_P, SSM_G, SSM_N = 2048, 32, 64, 4, 128
SSM_CONV_DIM = SSM_DI + 2 * SSM_G * SSM_N
SSM_IN = SSM_DI + SSM_CONV_DIM + 2 * SSM_H
NE, EPG, FH = 32, 8, 512
BIG = 1.0e30
BLK = 256
NSB = BLK // 128
NTH = (2 * T + BLK - 1) // BLK + 1
NBLK = (2 * T + BLK - 1) // BLK + NE
NSLOT = NBLK * BLK

SAME_ENGINE_SYNC = ("act", "dve", "pool")


class Sched:
    def __init__(self, nc):
        self.nc = nc
        self.eng = {"pe": nc.tensor, "act": nc.scalar, "dve": nc.vector,
                    "pool": nc.gpsimd, "sp": nc.sync}
        self.esem = {}
        self.ecnt = {}
        for e in ("pe", "act", "dve", "pool"):
            self.esem[e] = nc.alloc_semaphore("s_" + e)
            self.ecnt[e] = 0
        self.seen = {e: {} for e in self.eng}
        self.wr = {}
        self.rd = {}
        self.dsem = {}
        self.dfree = []
        self.nsem = 0
        self.bregs = {}
        self.n_inst = 0

    def _wait(self, e, toks):
        eng = self.eng[e]
        for name, (sem, val, src) in toks.items():
            if src == e and e not in SAME_ENGINE_SYNC:
                continue
            if self.seen[e].get(name, 0) >= val:
                continue
            eng.wait_ge(sem, val)
            self.seen[e][name] = val

    def _deps(self, e, reads, writes):
        for k in reads:
            self._wait(e, self.wr.get(k, {}))
        for k in writes:
            self._wait(e, self.wr.get(k, {}))
            self._wait(e, self.rd.get(k, {}))

    def _commit(self, name, tok, reads, writes):
        for k in reads:
            self.rd.setdefault(k, {})[name] = tok
        for k in writes:
            self.wr[k] = {name: tok}
            self.rd[k] = {}

    def op(self, e, fn, reads=(), writes=()):
        self._deps(e, reads, writes)
        ins = fn(self.eng[e])
        self.ecnt[e] += 1
        ins.then_inc(self.esem[e], 1)
        self._commit("s_" + e, (self.esem[e], self.ecnt[e], e), reads, writes)
        self.n_inst += 1
        return ins

    def dma(self, q, out, in_, reads=(), writes=(), semkey=None, **kw):
        if semkey is None:
            semkey = tuple(writes) + tuple(reads)
        ent = self._dsem_get(semkey)
        self._deps(q, reads, writes)
        ins = self.eng[q].dma_start(out=out, in_=in_, **kw)
        ent[1] += 16
        ins.then_inc(ent[0], 16)
        self._commit(ent[2], (ent[0], ent[1], "dma"), reads, writes)
        self.n_inst += 1

    def idma(self, out, out_off, in_, in_off, bounds, reads=(), writes=(), semkey=None):
        q = "pool"
        ent = self._dsem_get(semkey)
        self._deps(q, reads, writes)
        if bounds not in self.bregs:
            self.bregs[bounds] = self.eng[q].to_reg(bounds)
        ins = self.eng[q].indirect_dma_start(out=out, out_offset=out_off, in_=in_, in_offset=in_off,
                                             bounds_check=self.bregs[bounds], oob_is_err=False)
        ent[1] += 16
        ins.then_inc(ent[0], 16)
        self._commit(ent[2], (ent[0], ent[1], "dma"), reads, writes)
        self.n_inst += 1

    def recycle(self):
        for key, ent in list(self.dsem.items()):
            for e in self.eng:
                if self.seen[e].get(ent[2], 0) < ent[1]:
                    self.eng[e].wait_ge(ent[0], ent[1])
                    self.seen[e][ent[2]] = ent[1]
            self.dfree.append(ent)
            del self.dsem[key]

    def _dsem_get(self, semkey):
        if semkey not in self.dsem:
            if self.dfree:
                self.dsem[semkey] = self.dfree.pop()
            else:
                nm = "d%d" % self.nsem
                self.nsem += 1
                self.dsem[semkey] = [self.nc.alloc_semaphore(nm), 0, nm]
        return self.dsem[semkey]

    def wait_all(self, e, keys):
        for k in keys:
            self._wait(e, self.wr.get(k, {}))
            self._wait(e, self.rd.get(k, {}))


class Prog:
    def __init__(self, cfg):
        self.cfg = cfg
        self.nc = nc = bass.Bass("TRN2", target_bir_lowering=False)
        self.S = Sched(nc)
        self.es = contextlib.ExitStack()
        self.inp = {}
        self.uid = 0

    def din(self, name, shape, dt=F32):
        t = self.nc.dram_tensor(name, list(shape), dt, kind="ExternalInput").ap()
        self.inp[name] = t
        return t

    def dscratch(self, name, shape, dt=F32):
        return self.nc.dram_tensor(name, list(shape), dt, kind="Internal").ap()

    def sb(self, stack, name, shape, dt=F32):
        self.uid += 1
        return stack.enter_context(self.nc.sbuf_tensor("%s_u%d" % (name, self.uid), list(shape), dt))

    def mm(self, out, lhsT, rhs, start, stop, reads, writes):
        self.S.op("pe", lambda e: e.matmul(out, lhsT=lhsT, rhs=rhs, start=start, stop=stop),
                  reads=reads, writes=writes)

    def tr(self, out, in_, ident, reads, writes):
        self.S.op("pe", lambda e: e.transpose(out, in_, ident), reads=reads, writes=writes)

    def act(self, out, in_, func, reads, writes, bias=None, scale=None, accum_out=None):
        kw = {}
        if bias is not None:
            kw["bias"] = bias
        if scale is not None:
            kw["scale"] = scale
        if accum_out is not None:
            kw["accum_out"] = accum_out
        self.S.op("act", lambda e: e.activation(out=out, in_=in_, func=func, **kw),
                  reads=reads, writes=writes)

    def tt(self, e, out, in0, in1, op, reads, writes):
        self.S.op(e, lambda g: g.tensor_tensor(out=out, in0=in0, in1=in1, op=op),
                  reads=reads, writes=writes)

    def ts(self, e, out, in0, s1, op0, reads, writes, s2=None, op1=None, accum_out=None):
        kw = {}
        if op1 is not None:
            kw["op1"] = op1
        if accum_out is not None:
            kw["accum_out"] = accum_out
        self.S.op(e, lambda g: g.tensor_scalar(out=out, in0=in0, scalar1=s1, scalar2=s2, op0=op0, **kw),
                  reads=reads, writes=writes)

    def stt(self, e, out, in0, scalar, in1, op0, op1, reads, writes):
        self.S.op(e, lambda g: g.scalar_tensor_tensor(out=out, in0=in0, scalar=scalar, in1=in1,
                                                      op0=op0, op1=op1),
                  reads=reads, writes=writes)

    def cp(self, e, out, in_, reads, writes):
        if e == "act":
            self.S.op("act", lambda g: g.copy(out=out, in_=in_), reads=reads, writes=writes)
        else:
            self.S.op(e, lambda g: g.tensor_copy(out=out, in_=in_), reads=reads, writes=writes)

    def red(self, e, out, in_, op, reads, writes, axis=AX.X):
        self.S.op(e, lambda g: g.tensor_reduce(out=out, in_=in_, axis=axis, op=op),
                  reads=reads, writes=writes)

    def recip(self, out, in_, reads, writes):
        self.S.op("dve", lambda g: g.reciprocal(out=out, in_=in_), reads=reads, writes=writes)

    def memset(self, e, ap, val, writes):
        self.S.op(e, lambda g: g.memset(ap, val), writes=writes)

    def dma(self, q, out, in_, reads=(), writes=(), semkey=None, **kw):
        self.S.dma(q, out, in_, reads=reads, writes=writes, semkey=semkey, **kw)


def build(cfg):
    P = Prog(cfg)
    nc, S = P.nc, P.S
    layers = cfg.get("layers", list(range(DEPTH)))
    mixers_on = cfg.get("mixers", True)
    moe_on = cfg.get("moe", True)

    x_in = P.din("x", [SEQ, D])
    ctx_in = P.din("ctx", [CTX, D])
    c_in = P.din("c", [1, D])
    cctx_in = P.din("c_ctx", [1, D])
    w_ada = {i: P.din("w_ada_%d" % i, [D, 6 * D]) for i in layers}
    b_ada = {i: P.din("b_ada_%d" % i, [1, 6 * D]) for i in layers}
    norm_mix_g = P.din("norm_mix_g", [DEPTH, D])
    norm_ffn_g = P.din("norm_ffn_g", [DEPTH, D])
    final_g = P.din("final_norm_g", [1, D])
    attn_w_qkv = {j: P.din("attn_w_qkv_%d" % j, [D, 1536]) for j in range(2) if 3 * j in layers and mixers_on}
    attn_w_o = {j: P.din("attn_w_o_%d" % j, [D, D]) for j in range(2) if 3 * j in layers and mixers_on}
    attn_qg = P.din("attn_q_norm_g", [2, HD])
    attn_kg = P.din("attn_k_norm_g", [2, HD])
    pool_w = P.din("pool_w", [1, 4, 256, 256])
    pool_scale = P.din("pool_scale", [1, D])
    ssm_on = 2 in layers and mixers_on
    ssm_w_in = P.din("ssm_w_in", [1, D, SSM_IN]) if ssm_on else None
    ssm_conv_w = P.din("ssm_conv_w", [1, 4, SSM_CONV_DIM])
    ssm_conv_b = P.din("ssm_conv_b", [1, SSM_CONV_DIM])
    ssm_dt_bias = P.din("ssm_dt_bias", [1, 2 * SSM_H])
    ssm_a_log = P.din("ssm_a_log", [1, 2 * SSM_H])
    ssm_d = P.din("ssm_d", [1, 2 * SSM_H])
    ssm_norm_g = P.din("ssm_norm_g", [1, SSM_DI])
    ssm_w_out = P.din("ssm_w_out", [1, SSM_DI, D]) if ssm_on else None
    moe_l = [i for i in layers if moe_on]
    moe_wr = {i: P.din("moe_wr_%d" % i, [D, 36]) for i in moe_l}
    moe_w_gu = {i: P.din("moe_w_gu_%d" % i, [NE * 128, 8 * 2 * FH]) for i in moe_l}
    moe_w_dn = {i: P.din("moe_w_dn_%d" % i, [NE * 128, 4 * D]) for i in moe_l}
    k_ident = P.din("k_ident", [128, 128])
    k_rope = P.din("k_rope", [T, 64])
    k_band = P.din("k_band", [4, 5, 128, 128])
    k_tri = P.din("k_tri", [4, 128, 128])
    k_moe = P.din("k_moe", [128, 256])
    out = nc.dram_tensor("out", [SEQ, D], F32, kind="ExternalOutput").ap()
    dbg = None
    if cfg.get("dbg"):
        dbg = nc.dram_tensor("dbg", list(cfg["dbg"]), F32, kind="ExternalOutput").ap()

    X = P.dscratch("X", [T, D])

    def xk(t):
        return ("X", t)

    top = P.es
    ident = P.sb(top, "ident", [128, 128])
    identb = P.sb(top, "identb", [128, 128], BF16)
    ones = P.sb(top, "ones", [128, 128])
    cT = P.sb(top, "cT", [128, 8, 2])
    modL = P.sb(top, "modL", [128, 48])
    modC = P.sb(top, "modC", [128, 48])
    vec = P.sb(top, "vec", [128, 2, 2, 2, 8])
    gates = P.sb(top, "gates", [128, 2, 2, D])
    ps = [top.enter_context(nc.psum_tensor("ps%d" % i, [128, 512], F32)) for i in range(8)]

    def pk(i):
        return "ps%d" % i

    P.dma("sp", ident[:], k_ident, writes=["ident"])
    P.cp("dve", identb[:], ident[:], ["ident"], ["identb"])
    P.memset("pool", ones[:], 1.0, ["ones"])
    P.dma("sp", X[0:CTX, :], ctx_in, writes=[xk(0), xk(1)], semkey="xinit")
    P.dma("sp", X[CTX:T, :], x_in, writes=[xk(t) for t in range(2, NT)], semkey="xinit")

    with contextlib.ExitStack() as st:
        craw = P.sb(st, "craw", [128, 8, 2])
        P.dma("sp", craw[:, :, 0], c_in[0].rearrange("(k p) -> p k", p=128), writes=["craw"],
              allow_slow_non_contiguous=True)
        P.dma("sp", craw[:, :, 1], cctx_in[0].rearrange("(k p) -> p k", p=128), writes=["craw"],
              allow_slow_non_contiguous=True)
        P.act(cT[:], craw[:], AF.Silu, ["craw"], ["cT"])
        S.wait_all("sp", ["craw"])
        S.wait_all("act", ["craw"])


    def drain(keys):
        for e_ in ("sp", "pe", "act", "dve", "pool"):
            S.wait_all(e_, keys)

    def adaln(i, need_bc_all):
        with contextlib.ExitStack() as st:
            wblk = [P.sb(st, "wblk%d" % j, [128, 8, 512]) for j in range(2)]
            brow = P.sb(st, "brow", [1, 6 * D])
            bT = P.sb(st, "bT", [128, 48])
            g2 = P.sb(st, "g2", [128, 2, 8])
            cbc = P.sb(st, "cbc", [128, 2, 8, 128])
            for s in range(2):
                P.cp("dve", cbc[:, s], cT[:, :, s:s + 1].broadcast_to([128, 8, 128]), ["cT"], ["cbc"])
            P.dma("sp", brow[:], b_ada[i][0:1, :], writes=["brow"])
            P.dma("sp", bT[:], b_ada[i][0].rearrange("(j p) -> p j", p=128), writes=["bT"],
                  allow_slow_non_contiguous=True)
            P.dma("sp", g2[:, 0, :], norm_mix_g[i].rearrange("(j p) -> p j", p=128), writes=["g2"],
                  allow_slow_non_contiguous=True)
            P.dma("sp", g2[:, 1, :], norm_ffn_g[i].rearrange("(j p) -> p j", p=128), writes=["g2"],
                  allow_slow_non_contiguous=True)
            for n in range(12):
                wb = wblk[n % 2]
                wkey = "wblk%d" % (n % 2)
                P.dma("sp", wb[:], w_ada[i][:, n * 512:(n + 1) * 512].rearrange("(k p) n -> p k n", p=128),
                      writes=[wkey])
                for q in range(4):
                    j = n * 4 + q
                    for k in range(8):
                        P.mm(ps[0][:, 2 * j:2 * j + 2], wb[:, k, q * 128:(q + 1) * 128], cT[:, k, :],
                             k == 0, k == 7, [wkey, "cT"], [pk(0)])
                split = n // 2
                if split in (2, 5):
                    which = 0 if split == 2 else 1
                    half = n % 2
                    for s in range(2):
                        bank = 1 + s
                        for k in range(8):
                            P.mm(ps[bank][:, :], cbc[:, s, k, :], wb[:, k, :], k == 0, False,
                                 [wkey, "cbc"], [pk(bank)])
                        P.mm(ps[bank][:, :], ones[0:1, :], brow[0:1, n * 512:(n + 1) * 512], False, True,
                             ["ones", "brow"], [pk(bank)])
                        P.cp("act", gates[:, which, s, half * 512:(half + 1) * 512], ps[bank][:, :],
                             [pk(bank)], [("gates", which, s)])
            psv = ps[0][:, 0:96].rearrange("p (j s) -> p j s", s=2)
            P.tt("dve", modL[:], psv[:, :, 0], bT[:], ALU.add, [pk(0), "bT"], ["modL"])
            P.tt("dve", modC[:], psv[:, :, 1], bT[:], ALU.add, [pk(0), "bT"], ["modC"])
            for which in range(2):
                for s, m in enumerate((modL, modC)):
                    mk = "modL" if s == 0 else "modC"
                    base = which * 24
                    P.stt("dve", vec[:, which, s, 0, :], m[:, base + 8:base + 16], 1.0, g2[:, which, :],
                          ALU.add, ALU.mult, [mk, "g2"], [("vec", which, s)])
                    P.cp("dve", vec[:, which, s, 1, :], m[:, base:base + 8], [mk], [("vec", which, s)])
            S.wait_all("sp", ["wblk0", "wblk1", "brow", "bT", "g2"])
            S.wait_all("pe", ["wblk0", "wblk1", "brow", "cbc"])
            S.wait_all("dve", ["bT", "g2"])

    def norm_tiles(st, tiles, which, hT, hT_key, col0, hook=None, tag="n", colfn=None):
        NX = 3
        n = len(tiles)
        xt = [P.sb(st, "%s_xt%d" % (tag, j), [128, D]) for j in range(NX)]
        h32 = [P.sb(st, "%s_h32%d" % (tag, j), [128, 8, 128]) for j in range(2)]
        st8 = [P.sb(st, "%s_st%d" % (tag, j), [128, 4]) for j in range(NX)]
        junk = P.sb(st, "%s_junk" % tag, [128, D], BF16)

        def load(idx):
            t = tiles[idx]
            P.dma("sp", xt[idx % NX][:], X[t * 128:(t + 1) * 128, :], reads=[xk(t)],
                  writes=["%s_xt%d" % (tag, idx % NX)], semkey="%s_xt%d" % (tag, idx % NX))

        def prep(idx):
            b = idx % NX
            xkey, skey = "%s_xt%d" % (tag, b), "%s_st%d" % (tag, b)
            P.act(junk[:], xt[b][:], AF.Square, [xkey], ["%s_junk" % tag, skey], accum_out=st8[b][:, 0:1])
            P.act(st8[b][:, 1:2], st8[b][:, 0:1], AF.Sqrt, [skey], [skey], bias=EPS, scale=1.0 / D)
            P.recip(st8[b][:, 2:3], st8[b][:, 1:2], [skey], [skey])
            P.ts("dve", xt[b][:], xt[b][:], st8[b][:, 2:3], ALU.mult, [xkey, skey], [xkey])

        def fin(idx):
            t = tiles[idx]
            b = idx % NX
            hb = idx % 2
            xkey, hkey = "%s_xt%d" % (tag, b), "%s_h32%d" % (tag, hb)
            s = 1 if t < 2 else 0
            for c in range(8):
                bank = 6 + c // 4
                P.tr(ps[bank][:, (c % 4) * 128:(c % 4 + 1) * 128], xt[b][:, c * 128:(c + 1) * 128], ident[:],
                     [xkey, "ident"], [pk(bank)])
            c0 = col0 + idx * 128 if colfn is None else colfn(idx)
            hTk = hT_key if colfn is None else (hT_key, idx % 2)
            if hook is None:
                for c in range(8):
                    bank = 6 + c // 4
                    P.act(hT[:, c, c0:c0 + 128], ps[bank][:, (c % 4) * 128:(c % 4 + 1) * 128], AF.Identity,
                          [pk(bank), ("vec", which, s)], [hTk],
                          bias=vec[:, which, s, 1, c:c + 1], scale=vec[:, which, s, 0, c:c + 1])
            else:
                for c in range(8):
                    bank = 6 + c // 4
                    P.act(h32[hb][:, c, :], ps[bank][:, (c % 4) * 128:(c % 4 + 1) * 128], AF.Identity,
                          [pk(bank), ("vec", which, s)], [hkey],
                          bias=vec[:, which, s, 1, c:c + 1], scale=vec[:, which, s, 0, c:c + 1])
                P.cp("dve", hT[:, :, c0:c0 + 128], h32[hb][:], [hkey], [hTk])
                hook(idx, t, h32[hb], hkey)

        load(0)
        if n > 1:
            load(1)
        prep(0)
        for idx in range(n):
            if idx + 2 < n:
                load(idx + 2)
            if idx + 1 < n:
                prep(idx + 1)
            fin(idx)
        drain(["%s_xt%d" % (tag, j) for j in range(NX)] + ["%s_st%d" % (tag, j) for j in range(NX)]
              + ["%s_h32%d" % (tag, j) for j in range(2)] + ["%s_junk" % tag])

    def moe(i, tiles_all):
        nhalf = 2
        per = len(tiles_all) // nhalf
        for hf in range(nhalf):
            tiles = tiles_all[hf * per:(hf + 1) * per]
            ntk = per * 128
            with contextlib.ExitStack() as st:
                hT = P.sb(st, "m_hT", [128, 8, ntk], BF16)
                Y = P.sb(st, "m_Y", [128, per, D])
                Wt = P.sb(st, "m_Wt", [128, per, NE])
                wr = P.sb(st, "m_wr", [128, 8, 36])
                wgu = [P.sb(st, "m_wgu%d" % j, [128, 8, 2 * FH], BF16) for j in range(2)]
                wdn = [P.sb(st, "m_wdn%d" % j, [128, 4, D], BF16) for j in range(2)]
                sg = [P.sb(st, "m_sg%d" % j, [128, 512], BF16) for j in range(2)]
                aT = [P.sb(st, "m_a%d" % j, [128, 4, 512], BF16) for j in range(2)]
                rt = P.sb(st, "m_rt", [128, 160])

                def loadw(e):
                    b = e % 2
                    P.dma("pool", wgu[b][:], moe_w_gu[i][e * 128:(e + 1) * 128, :].rearrange("p (k n) -> p k n", k=8),
                          writes=["m_wgu%d" % b])
                    P.dma("pool", wdn[b][:], moe_w_dn[i][e * 128:(e + 1) * 128, :].rearrange("p (k n) -> p k n", k=4),
                          writes=["m_wdn%d" % b])

                P.dma("sp", wr[:], moe_wr[i].rearrange("(k p) n -> p k n", p=128), writes=["m_wr"])
                loadw(0)

                def router(idx, t, h32, hkey):
                    lg = ps[5]
                    for k in range(8):
                        P.mm(lg[:, 0:36], h32[:, k, :], wr[:, k, :], k == 0, k == 7, [hkey, "m_wr"], [pk(5)])
                    R = "m_rt"
                    lgs = rt[:, 0:36]
                    P.cp("dve", lgs, lg[:, 0:36], [pk(5)], [R])
                    gmax, ngmax, gsum, gate = rt[:, 36:37], rt[:, 37:38], rt[:, 38:39], rt[:, 39:40]
                    P.red("dve", gmax, rt[:, 0:4], ALU.max, [R], [R])
                    P.ts("dve", ngmax, gmax, -1.0, ALU.mult, [R], [R])
                    P.act(rt[:, 40:44], rt[:, 0:4], AF.Exp, [R], [R], bias=ngmax, scale=1.0, accum_out=gsum)
                    P.recip(gate, gsum, [R], [R])
                    pen = rt[:, 44:48]
                    P.ts("dve", pen, rt[:, 0:4], gmax, ALU.is_ge, [R], [R])
                    P.ts("dve", pen, pen, -1.0, ALU.add, [R], [R], s2=BIG, op1=ALU.mult)
                    le = rt[:, 48:80]
                    P.tt("dve", le.rearrange("p (g j) -> p g j", g=4), rt[:, 4:36].rearrange("p (g j) -> p g j", g=4),
                         pen.unsqueeze(2).broadcast_to([128, 4, 8]), ALU.add, [R], [R])
                    m1, m2 = rt[:, 80:81], rt[:, 81:82]
                    P.red("dve", m1, le, ALU.max, [R], [R])
                    oh1, oh2, le2 = rt[:, 84:116], rt[:, 116:148], rt[:, 4:36]
                    P.ts("dve", oh1, le, m1, ALU.is_ge, [R], [R])
                    P.stt("dve", le2, oh1, -BIG, le, ALU.mult, ALU.add, [R], [R])
                    P.red("dve", m2, le2, ALU.max, [R], [R])
                    P.ts("dve", oh2, le2, m2, ALU.is_ge, [R], [R])
                    dd, ee, p1, p2 = rt[:, 148:149], rt[:, 149:150], rt[:, 150:151], rt[:, 151:152]
                    P.tt("dve", dd, m2, m1, ALU.subtract, [R], [R])
                    P.act(ee, dd, AF.Exp, [R], [R])
                    P.ts("dve", p1, ee, 1.0, ALU.add, [R], [R])
                    P.recip(p1, p1, [R], [R])
                    P.tt("dve", p2, ee, p1, ALU.mult, [R], [R])
                    P.tt("dve", p1, p1, gate, ALU.mult, [R], [R])
                    P.tt("dve", p2, p2, gate, ALU.mult, [R], [R])
                    P.ts("dve", oh1, oh1, p1, ALU.mult, [R], [R])
                    P.stt("dve", Wt[:, idx, :], oh2, p2, oh1, ALU.mult, ALU.add, [R], [("m_Wt", idx)])

                with contextlib.ExitStack() as st2:
                    norm_tiles(st2, tiles, 1, hT, "m_hT", 0, hook=router, tag="mn")
                    S.wait_all("sp", ["mn_xt0", "mn_xt1"])
                    S.wait_all("act", ["mn_xt0", "mn_xt1", "mn_junk", "mn_st0", "mn_st1", "mn_h320", "mn_h321"])
                    S.wait_all("dve", ["mn_xt0", "mn_xt1", "mn_st0", "mn_st1"])
                    S.wait_all("pool", ["mn_h320", "mn_h321"])
                    S.wait_all("pe", ["mn_xt0", "mn_xt1", "mn_h320", "mn_h321"])

                blocks = []
                o = 0
                while o < ntk:
                    n_ = min(512, ntk - o)
                    blocks.append((o, n_))
                    o += n_
                cnt = 0
                dcnt = 0
                for e in range(NE):
                    if e + 1 < NE:
                        loadw(e + 1)
                    b = e % 2
                    gk, dk = "m_wgu%d" % b, "m_wdn%d" % b
                    for (o, n_) in blocks:
                        ab = cnt % 2
                        akey = "m_a%d" % ab
                        for j in range(4):
                            gb, ub = (j % 2), 2 + (j % 2)
                            for k in range(8):
                                P.mm(ps[gb][:, 0:n_], wgu[b][:, k, j * 128:(j + 1) * 128], hT[:, k, o:o + n_],
                                     k == 0, k == 7, [gk, "m_hT"], [pk(gb)])
                            for k in range(8):
                                P.mm(ps[ub][:, 0:n_], wgu[b][:, k, FH + j * 128:FH + (j + 1) * 128],
                                     hT[:, k, o:o + n_], k == 0, k == 7, [gk, "m_hT"], [pk(ub)])
                            sk = "m_sg%d" % (j % 2)
                            P.act(sg[j % 2][:, 0:n_], ps[gb][:, 0:n_], AF.Silu, [pk(gb)], [sk])
                            P.tt("dve", aT[ab][:, j, 0:n_], sg[j % 2][:, 0:n_], ps[ub][:, 0:n_], ALU.mult,
                                 [sk, pk(ub)], [(akey, j)])
                        for tt_ in range(n_ // 128):
                            tidx = o // 128 + tt_
                            for half in range(2):
                                db = 4 + dcnt % 2
                                dcnt += 1
                                for j in range(4):
                                    P.mm(ps[db][:, :], aT[ab][:, j, tt_ * 128:(tt_ + 1) * 128],
                                         wdn[b][:, j, half * 512:(half + 1) * 512], j == 0, j == 3,
                                         [(akey, j), dk], [pk(db)])
                                yk = ("m_Y", tidx, half)
                                ysl = Y[:, tidx, half * 512:(half + 1) * 512]
                                if e == 0:
                                    P.ts("dve", ysl, ps[db][:, :], Wt[:, tidx, e:e + 1], ALU.mult,
                                         [pk(db), ("m_Wt", tidx)], [yk])
                                else:
                                    P.stt("dve", ysl, ps[db][:, :], Wt[:, tidx, e:e + 1], ysl, ALU.mult, ALU.add,
                                          [pk(db), ("m_Wt", tidx), yk], [yk])
                        cnt += 1
                xo = [P.sb(st, "m_xo%d" % j, [128, D]) for j in range(2)]
                for idx, t in enumerate(tiles):
                    b = idx % 2
                    s = 1 if t < 2 else 0
                    ok = "m_xo%d" % b
                    P.dma("sp", xo[b][:], X[t * 128:(t + 1) * 128, :], reads=[xk(t)], writes=[ok], semkey=ok)
                    P.tt("pool", Y[:, idx, :], Y[:, idx, :], gates[:, 1, s, :], ALU.mult,
                         [("m_Y", idx, 0), ("m_Y", idx, 1), ("gates", 1, s)], [("m_Y", idx, 0), ("m_Y", idx, 1)])
                    P.tt("dve", xo[b][:], xo[b][:], Y[:, idx, :], ALU.add,
                         [ok, ("m_Y", idx, 0), ("m_Y", idx, 1)], [ok])
                    P.dma("sp", X[t * 128:(t + 1) * 128, :], xo[b][:], reads=[ok], writes=[xk(t)], semkey=ok)
                keys = ["m_hT", "m_wr", "m_wgu0", "m_wgu1", "m_wdn0", "m_wdn1", "m_sg0", "m_sg1", "m_rt",
                        "m_xo0", "m_xo1"]
                keys += [("m_a%d" % a, j) for a in range(2) for j in range(4)]
                keys += [("m_Y", idx, h) for idx in range(per) for h in range(2)]
                keys += [("m_Wt", idx) for idx in range(per)]
                for e_ in ("sp", "pe", "act", "dve", "pool"):
                    S.wait_all(e_, keys)


    def attention(i, j, ctx_out):
        QD = P.dscratch("QD%d" % i, [64, 16, T], BF16)
        with contextlib.ExitStack() as st:
            KT = P.sb(st, "a_KT", [128, 4, T], BF16)
            Vg = P.sb(st, "a_V", [128, NT, 4, 80], BF16)
            P.memset("dve", Vg[:], 0.0, ["a_V"])
            P.memset("dve", Vg[:, :, :, 64:66], 1.0, ["a_V"])
            with contextlib.ExitStack() as s1:
                hT = P.sb(s1, "a_hT", [128, 8, T], BF16)
                wqkv = P.sb(s1, "a_wqkv", [128, 8, 1536], BF16)
                gqk = P.sb(s1, "a_gqk", [128, 2, 64])
                P.dma("pool", wqkv[:], attn_w_qkv[j].rearrange("(k p) n -> p k n", p=128), writes=["a_wqkv"])
                P.dma("sp", gqk[:, 0, :], attn_qg[j:j + 1, :].broadcast_to([128, 64]), writes=["a_gqk"])
                P.dma("sp", gqk[:, 1, :], attn_kg[j:j + 1, :].broadcast_to([128, 64]), writes=["a_gqk"])
                with contextlib.ExitStack() as s2:
                    norm_tiles(s2, ALL, 0, hT, "a_hT", 0, tag="an")
                    drain(["an_xt0", "an_xt1", "an_junk", "an_st0", "an_st1", "an_h320", "an_h321"])
                qk = [P.sb(s1, "a_qk%d" % b, [128, 20, 64]) for b in range(2)]
                T4 = P.sb(s1, "a_T4", [128, 4 * 512])
                tmp = [T4[:, b * 512:(b + 1) * 512].rearrange("p (h i) -> p h i", i=32) for b in range(4)]
                sq = T4[:, 0:1280].rearrange("p (h d) -> p h d", d=64)
                qr = [P.sb(s1, "a_qr%d" % b, [128, 20, 64], BF16) for b in range(2)]
                ss = [P.sb(s1, "a_ss%d" % b, [128, 20]) for b in range(2)]
                cs = [P.sb(s1, "a_cs%d" % b, [128, 64]) for b in range(2)]
                tab = [P.sb(s1, "a_tab%d" % b, [128, 2, 4, 32]) for b in range(2)]
                QTt_ = P.sb(s1, "a_QTt0", [64, 16, 128], BF16)
                QTt = [QTt_, QTt_]
                gq4 = gqk[:].rearrange("p w (i two) -> p w i two", two=2)
                def qkv_stage(t):
                    b = t % 2
                    qkk, csk = "a_qk%d" % b, "a_cs%d" % b
                    P.dma("sp", cs[b][:], k_rope[t * 128:(t + 1) * 128, :], writes=[csk])
                    for nb in range(3):
                        for k in range(8):
                            P.mm(ps[nb][:, :], hT[:, k, t * 128:(t + 1) * 128], wqkv[:, k, nb * 512:(nb + 1) * 512],
                                 k == 0, k == 7, ["a_hT", "a_wqkv"], [pk(nb)])
                    P.cp("act", qk[b][:, 0:8, :], ps[0][:, :].rearrange("p (h d) -> p h d", d=64), [pk(0)], [qkk])
                    P.cp("act", qk[b][:, 8:16, :], ps[1][:, :].rearrange("p (h d) -> p h d", d=64), [pk(1)], [qkk])
                    P.cp("act", qk[b][:, 16:20, :], ps[2][:, 0:256].rearrange("p (h d) -> p h d", d=64), [pk(2)], [qkk])
                    P.cp("act", Vg[:, t, :, 0:64], ps[2][:, 256:512].rearrange("p (h d) -> p h d", d=64),
                         [pk(2)], [("a_V", t)])

                qkv_stage(0)
                for t in range(NT):
                    b = t % 2
                    qkk, ssk, csk, tabk, qrk, qtk = ("a_qk%d" % b, "a_ss%d" % b, "a_cs%d" % b, "a_tab%d" % b,
                                                     "a_qr%d" % b, "a_QTt0")
                    if t + 1 < NT:
                        qkv_stage(t + 1)
                    if cfg.get("a1_lvl", 9) < 2:
                        continue
                    P.tt("pool", sq, qk[b][:], qk[b][:], ALU.mult, [qkk], ["a_sq", "a_t0", "a_t1", "a_t2"])
                    P.red("dve", ss[b][:], sq, ALU.add, ["a_sq", "a_t0", "a_t1", "a_t2"], [ssk])
                    P.act(ss[b][:], ss[b][:], AF.Sqrt, [ssk], [ssk], bias=EPS, scale=1.0 / HD)
                    P.recip(ss[b][:], ss[b][:], [ssk], [ssk])
                    P.tt("dve", qk[b][:], qk[b][:], ss[b][:].unsqueeze(2).broadcast_to([128, 20, 64]), ALU.mult,
                         [qkk, ssk], [qkk])
                    if cfg.get("a1_lvl", 9) < 3:
                        continue
                    for w in range(2):
                        P.tt("pool", tab[b][:, w, 0, :], cs[b][:, 0:32], gq4[:, w, :, 0], ALU.mult, [csk, "a_gqk"], [tabk])
                        P.tt("pool", tab[b][:, w, 1, :], cs[b][:, 32:64], gq4[:, w, :, 1], ALU.mult, [csk, "a_gqk"], [tabk])
                        P.tt("pool", tab[b][:, w, 2, :], cs[b][:, 32:64], gq4[:, w, :, 0], ALU.mult, [csk, "a_gqk"], [tabk])
                        P.tt("pool", tab[b][:, w, 3, :], cs[b][:, 0:32], gq4[:, w, :, 1], ALU.mult, [csk, "a_gqk"], [tabk])
                    qk4 = qk[b][:].rearrange("p h (i two) -> p h i two", two=2)
                    qr4 = qr[b][:].rearrange("p h (i two) -> p h i two", two=2)
                    for w, (h0, h1) in enumerate(((0, 16), (16, 20))):
                        nh = h1 - h0
                        x0, x1 = qk4[:, h0:h1, :, 0], qk4[:, h0:h1, :, 1]
                        tb = lambda kind: tab[b][:, w, kind, :].unsqueeze(1).broadcast_to([128, nh, 32])
                        P.tt("pool", tmp[0][:, 0:nh, :], x0, tb(0), ALU.mult, [qkk, tabk, "a_sq"], ["a_t0"])
                        P.tt("dve", tmp[1][:, 0:nh, :], x1, tb(1), ALU.mult, [qkk, tabk, "a_sq"], ["a_t1"])
                        P.tt("pool", tmp[2][:, 0:nh, :], x0, tb(2), ALU.mult, [qkk, tabk, "a_sq"], ["a_t2"])
                        P.tt("dve", tmp[3][:, 0:nh, :], x1, tb(3), ALU.mult, [qkk, tabk], ["a_t3"])
                        P.tt("dve", qr4[:, h0:h1, :, 0], tmp[0][:, 0:nh, :], tmp[1][:, 0:nh, :], ALU.subtract,
                             ["a_t0", "a_t1"], [qrk])
                        P.tt("pool", qr4[:, h0:h1, :, 1], tmp[2][:, 0:nh, :], tmp[3][:, 0:nh, :], ALU.add,
                             ["a_t2", "a_t3"], [qrk])
                    if cfg.get("a1_lvl", 9) < 4:
                        continue
                    for hh in range(20):
                        bank = 3 + hh // 8
                        pv = ps[bank][:, :].bitcast(BF16)
                        P.tr(pv[0:64, (hh % 8) * 128:(hh % 8 + 1) * 128], qr[b][:, hh, :], identb[:],
                             [qrk, "identb"], [pk(bank)])
                    if cfg.get("a1_lvl", 9) < 5:
                        continue
                    P.cp("act", QTt[b][:, 0:8, :], ps[3][:, :].bitcast(BF16)[0:64, :].rearrange("p (h t) -> p h t", t=128),
                         [pk(3)], [qtk])
                    P.cp("act", QTt[b][:, 8:16, :], ps[4][:, :].bitcast(BF16)[0:64, :].rearrange("p (h t) -> p h t", t=128),
                         [pk(4)], [qtk])
                    P.cp("act", KT[0:64, :, t * 128:(t + 1) * 128],
                         ps[5][:, :].bitcast(BF16)[0:64, 0:512].rearrange("p (h t) -> p h t", t=128),
                         [pk(5)], [("a_KT", t)])
                    if cfg.get("a1_lvl", 9) < 6:
                        continue
                    P.dma("sp", QD[:, :, t * 128:(t + 1) * 128], QTt[b][:], reads=[qtk], writes=[("QD", t)], semkey=qtk)
                keys = ["a_hT", "a_wqkv", "a_gqk", "a_sq"] + ["a_t%d" % b for b in range(4)]
                for b in range(2):
                    keys += ["a_qk%d" % b, "a_ss%d" % b, "a_cs%d" % b, "a_tab%d" % b, "a_qr%d" % b, "a_QTt0"]
                drain(keys)
            with contextlib.ExitStack() as s1:
                if cfg.get("skip_a2"):
                    return
                wo = P.sb(s1, "a_wo", [64, 16, D], BF16)
                P.dma("pool", wo[:], attn_w_o[j].rearrange("(h d) n -> d h n", d=64), writes=["a_wo"])
                for kv_ in range(4):
                    P.memset("pool", KT[64:128, kv_, :], 0.0, ["a_KTz"])
                QTb = [P.sb(s1, "a_QTb%d" % b, [128, 16, 512], BF16) for b in range(2)]
                for b_ in range(2):
                    P.memset("pool", QTb[b_][64:128, :, :], 0.0, ["a_QTbz"])
                aT = [P.sb(s1, "a_aT%d" % b, [64, 16, 512], BF16) for b in range(2)]
                Pb = [P.sb(s1, "a_P%d" % b, [128, 512], BF16) for b in range(3)]
                rec = P.sb(s1, "a_rec", [65, 512])
                bcs = P.sb(s1, "a_bcs", [64, 512])
                xo = [P.sb(s1, "a_xo%d" % b, [128, D]) for b in range(2)]
                tm = [P.sb(s1, "a_tm%d" % b, [128, D]) for b in range(2)]
                qblocks = []
                if ctx_out:
                    qblocks.append((0, CTX, [0, 1]))
                for qb in range(SEQ // 512):
                    qblocks.append((CTX + qb * 512, 512, list(range(NT))))
                pcnt = 0
                xcnt = 0
                for bi, (qo, nq, ktiles) in enumerate(qblocks):
                    b = bi % 2
                    qbk, atk = "a_QTb%d" % b, "a_aT%d" % b
                    P.dma("sp", QTb[b][0:64, :, 0:nq], QD[:, :, qo:qo + nq],
                          reads=[("QD", t) for t in range(qo // 128, (qo + nq) // 128)], writes=[qbk], semkey=qbk)
                    items = [(h, idx, kt) for h in range(NH) for idx, kt in enumerate(ktiles)]
                    LOOK = 2

                    def emit_S(n):
                        h_, idx_, kt_ = items[n]
                        P.mm(ps[n % 3][:, 0:nq], KT[:, h_ // 4, kt_ * 128:(kt_ + 1) * 128], QTb[b][:, h_, 0:nq], True, True,
                             [("a_KT", kt_), "a_KTz", "a_QTbz", qbk], [pk(n % 3)])

                    def fin1(h_):
                        ob_ = 3 + h_ % 2
                        P.recip(rec[64:65, 0:nq], ps[ob_][64:65, 0:nq], [pk(ob_)], ["a_rec"])

                    def fin2(h_):
                        ob_ = 3 + h_ % 2
                        P.mm(ps[5][0:64, 0:nq], ones[64:65, 0:64], rec[64:65, 0:nq], True, True, ["ones", "a_rec"], [pk(5)])
                        P.cp("dve", bcs[:, 0:nq], ps[5][0:64, 0:nq], [pk(5)], ["a_bcs"])
                        P.tt("dve", aT[b][:, h_, 0:nq], ps[ob_][0:64, 0:nq], bcs[:, 0:nq], ALU.mult,
                             [pk(ob_), "a_bcs"], [(atk, h_)])

                    for n in range(min(LOOK, len(items))):
                        emit_S(n)
                    pend = None
                    for n, (h, idx, kt) in enumerate(items):
                        if n + LOOK < len(items):
                            emit_S(n + LOOK)
                        kv = h // 4
                        ob = 3 + h % 2
                        pb = n % 3
                        P.act(Pb[pb][:, 0:nq], ps[n % 3][:, 0:nq], AF.Exp, [pk(n % 3)], ["a_P%d" % pb], scale=HD ** -0.5)
                        P.mm(ps[ob][0:65, 0:nq], Vg[:, kt, kv, 0:65], Pb[pb][:, 0:nq], idx == 0, idx == len(ktiles) - 1,
                             [("a_V", kt), "a_V", "a_P%d" % pb], [pk(ob)])
                        if pend is not None and (idx == min(3, len(ktiles) - 1)):
                            fin2(pend)
                            pend = None
                        if idx == len(ktiles) - 1:
                            fin1(h)
                            pend = h
                    if pend is not None:
                        fin2(pend)
                    for tt_ in range(nq // 128):
                        t = qo // 128 + tt_
                        s = 1 if t < 2 else 0
                        xb = xcnt % 2
                        xcnt += 1
                        xok, tmk = "a_xo%d" % xb, "a_tm%d" % xb
                        P.dma("sp", xo[xb][:], X[t * 128:(t + 1) * 128, :], reads=[xk(t)], writes=[xok], semkey=xok)
                        for half in range(2):
                            bank = 6 + half
                            for h in range(NH):
                                P.mm(ps[bank][:, :], aT[b][:, h, tt_ * 128:(tt_ + 1) * 128],
                                     wo[:, h, half * 512:(half + 1) * 512], h == 0, h == NH - 1,
                                     [(atk, h), "a_wo"], [pk(bank)])
                            P.tt("dve", tm[xb][:, half * 512:(half + 1) * 512], ps[bank][:, :],
                                 gates[:, 0, s, half * 512:(half + 1) * 512], ALU.mult,
                                 [pk(bank), ("gates", 0, s)], [tmk])
                        P.tt("pool", xo[xb][:], xo[xb][:], tm[xb][:], ALU.add, [xok, tmk], [xok])
                        P.dma("sp", X[t * 128:(t + 1) * 128, :], xo[xb][:], reads=[xok], writes=[xk(t)], semkey=xok)
                if dbg is not None:
                    dt_ = tm[0]
                    P.cp("dve", dt_[:, 0:512], KT[:, 0, 0:512], ["a_KTz"] + [("a_KT", t) for t in range(4)], ["a_dbgt", "a_tm0"])
                    P.cp("dve", dt_[:, 512:1024], QTb[0][:, 0, 0:512], ["a_QTbz", "a_QTb0"], ["a_dbgt", "a_tm0"])
                    P.dma("sp", dbg, dt_[:], reads=["a_dbgt"], writes=["dbg"])
                    drain(["a_dbgt", "a_tm0"])
                keys = ["a_wo", "a_rec", "a_bcs", "a_V", "a_KTz", "a_QTbz"] + ["a_P%d" % b for b in range(3)]
                for b in range(2):
                    keys += ["a_QTb%d" % b, "a_xo%d" % b, "a_tm%d" % b] + [("a_aT%d" % b, h) for h in range(NH)]
                keys += [("a_KT", t) for t in range(NT)] + [("a_V", t) for t in range(NT)]
                drain(keys)


    def pool_mixer(i):
        with contextlib.ExitStack() as st:
            bcm = P.sb(st, "p_bcm", [128, 2, 2, D])
            gmb = P.sb(st, "p_gmb", [128, 2, D])
            psg = P.sb(st, "p_psg", [128, 2, D])
            band = P.sb(st, "p_band", [128, 4, 5, 128])
            wp = P.sb(st, "p_wp", [128, 4, 2, 256])
            P.dma("sp", band[:], k_band.rearrange("w k p n -> p w k n"), writes=["p_band"])
            P.dma("sp", wp[:], pool_w[0].rearrange("g (cc p) n -> p g cc n", p=128), writes=["p_wp"])
            with contextlib.ExitStack() as s1:
                wblk = [P.sb(s1, "p_wblk%d" % b, [128, 8, 512]) for b in range(2)]
                cbc = P.sb(s1, "p_cbc", [128, 2, 8, 128])
                brow = P.sb(s1, "p_brow", [1, 2 * D])
                gb = P.sb(s1, "p_gb", [128, D])
                psb = P.sb(s1, "p_psb", [128, D])
                P.dma("sp", brow[:], b_ada[i][0:1, 0:2 * D], writes=["p_brow"])
                P.dma("sp", gb[:], norm_mix_g[i:i + 1, :].broadcast_to([128, D]), writes=["p_gb"])
                P.dma("sp", psb[:], pool_scale[0:1, :].broadcast_to([128, D]), writes=["p_psb"])
                for s_ in range(2):
                    P.cp("dve", cbc[:, s_], cT[:, :, s_:s_ + 1].broadcast_to([128, 8, 128]), ["cT"], ["p_cbc"])
                for n in range(4):
                    wb, wkey = wblk[n % 2], "p_wblk%d" % (n % 2)
                    P.dma("sp", wb[:], w_ada[i][:, n * 512:(n + 1) * 512].rearrange("(k p) n -> p k n", p=128),
                          writes=[wkey])
                    for s_ in range(2):
                        bank = 1 + s_
                        for k in range(8):
                            P.mm(ps[bank][:, :], cbc[:, s_, k, :], wb[:, k, :], k == 0, False, [wkey, "p_cbc"], [pk(bank)])
                        P.mm(ps[bank][:, :], ones[0:1, :], brow[0:1, n * 512:(n + 1) * 512], False, True,
                             ["ones", "p_brow"], [pk(bank)])
                        P.cp("act", bcm[:, s_, n // 2, (n % 2) * 512:(n % 2 + 1) * 512], ps[bank][:, :],
                             [pk(bank)], ["p_bcm"])
                for s_ in range(2):
                    P.stt("dve", gmb[:, s_, :], bcm[:, s_, 1, :], 1.0, gb[:], ALU.add, ALU.mult, ["p_bcm", "p_gb"], ["p_gmb"])
                    P.tt("pool", psg[:, s_, :], psb[:], gates[:, 0, s_, :], ALU.mult, ["p_psb", ("gates", 0, s_)], ["p_psg"])
                drain(["p_wblk0", "p_wblk1", "p_cbc", "p_brow", "p_gb", "p_psb"])
            xt = [P.sb(st, "p_x%d" % b, [128, D]) for b in range(4)]
            hh = [P.sb(st, "p_h%d" % b, [128, D]) for b in range(4)]
            dT = [P.sb(st, "p_dT%d" % b, [128, 8, 128]) for b in range(2)]
            tm = [P.sb(st, "p_tm%d" % b, [128, D]) for b in range(2)]
            st8 = [P.sb(st, "p_st%d" % b, [128, 4]) for b in range(4)]
            junk = P.sb(st, "p_junk", [128, D], BF16)

            def compute_h(t):
                b = t % 4
                s_ = 1 if t < 2 else 0
                xkey, hkey, skey = "p_x%d" % b, "p_h%d" % b, "p_st%d" % b
                P.dma("sp", xt[b][:], X[t * 128:(t + 1) * 128, :], reads=[xk(t)], writes=[xkey], semkey=xkey)
                P.act(junk[:], xt[b][:], AF.Square, [xkey], ["p_junk", skey], accum_out=st8[b][:, 0:1])
                P.act(st8[b][:, 1:2], st8[b][:, 0:1], AF.Sqrt, [skey], [skey], bias=EPS, scale=1.0 / D)
                P.recip(st8[b][:, 2:3], st8[b][:, 1:2], [skey], [skey])
                P.stt("dve", hh[b][:], xt[b][:], st8[b][:, 2:3], gmb[:, s_, :], ALU.mult, ALU.mult,
                      [xkey, skey, "p_gmb"], [hkey])
                P.tt("pool", hh[b][:], hh[b][:], bcm[:, s_, 0, :], ALU.add, [hkey, "p_bcm"], [hkey])

            done = set()
            for t in range(NT):
                first = t in (0, 2)
                last = t in (1, NT - 1)
                need = [t] + ([] if first else [t - 1]) + ([] if last else [t + 1])
                for tt_ in sorted(need):
                    if tt_ not in done:
                        compute_h(tt_)
                        done.add(tt_)
                s_ = 1 if t < 2 else 0
                db = t % 2
                dkey, tmk = "p_dT%d" % db, "p_tm%d" % db
                for c in range(8):
                    wi = c // 2
                    bank = c // 4
                    col = (c % 4) * 128
                    srcs = []
                    if not first:
                        srcs.append((t - 1, 0))
                    srcs.append((t, 1 if first else (3 if last else 2)))
                    if not last:
                        srcs.append((t + 1, 4))
                    for si, (tt_, kind) in enumerate(srcs):
                        P.mm(ps[bank][:, col:col + 128], hh[tt_ % 4][:, c * 128:(c + 1) * 128], band[:, wi, kind, :],
                             si == 0, si == len(srcs) - 1, ["p_h%d" % (tt_ % 4), "p_band"], [pk(bank)])
                for bank in range(2):
                    P.cp("act", dT[db][:, bank * 4:(bank + 1) * 4, :], ps[bank][:, :].rearrange("p (c t) -> p c t", t=128),
                         [pk(bank)], [dkey])
                for g in range(4):
                    bank = 2 + g // 2
                    for cc in range(2):
                        P.mm(ps[bank][:, (g % 2) * 256:(g % 2 + 1) * 256], dT[db][:, 2 * g + cc, :], wp[:, g, cc, :],
                             cc == 0, cc == 1, [dkey, "p_wp"], [pk(bank)])
                xb = t % 4
                for half in range(2):
                    P.tt("dve", tm[db][:, half * 512:(half + 1) * 512], ps[2 + half][:, :],
                         psg[:, s_, half * 512:(half + 1) * 512], ALU.mult, [pk(2 + half), "p_psg"], [tmk])
                P.tt("pool", tm[db][:], tm[db][:], xt[xb][:], ALU.add, [tmk, "p_x%d" % xb], [tmk])
                P.dma("sp", X[t * 128:(t + 1) * 128, :], tm[db][:], reads=[tmk], writes=[xk(t)], semkey=tmk)
            keys = ["p_bcm", "p_gmb", "p_psg", "p_band", "p_wp", "p_junk", "p_dT0", "p_dT1", "p_tm0", "p_tm1"]
            for b in range(4):
                keys += ["p_x%d" % b, "p_h%d" % b, "p_st%d" % b]
            drain(keys)


    def ssd_mixer(i):
        XT = P.dscratch("s_XT", [T, SSM_DI], BF16)
        BTK = P.dscratch("s_BTK", [T, 512], BF16)
        BF = P.dscratch("s_BF", [4, 128, T], BF16)
        CF = P.dscratch("s_CF", [4, 128, T], BF16)
        Yd = P.dscratch("s_Yd", [2, T, SSM_DI])
        w_in = ssm_w_in[0]
        NU = T + 3

        def xcol(t):
            return t * 128 if t < 2 else 259 + (t - 2) * 128

        ZG = P.dscratch("s_ZG", [T, SSM_DI], BF16)
        with contextlib.ExitStack() as st:
            with contextlib.ExitStack() as sA:
                dtA = P.sb(sA, "s_dtA", [128, NT, 64])
                LA = P.sb(sA, "s_LA", [128, NT, 64])
                sH = contextlib.ExitStack()
                hT = P.sb(sH, "s_hT", [128, 8, T], BF16)
                with contextlib.ExitStack() as s1:
                    wdt = P.sb(s1, "s_wdt", [128, 8, 64])
                    dtb = P.sb(s1, "s_dtb", [128, 64])
                    aB = P.sb(s1, "s_aB", [128, 64])
                    sp_ = P.sb(s1, "s_sp", [128, 4, 64])
                    P.dma("sp", wdt[:], w_in[:, 5120:5184].rearrange("(k p) n -> p k n", p=128), writes=["s_wdt"])
                    P.dma("sp", dtb[:], ssm_dt_bias[0:1, :].broadcast_to([128, 64]), writes=["s_dtb"])
                    P.dma("sp", aB[:], ssm_a_log[0:1, :].broadcast_to([128, 64]), writes=["s_aB"])
                    P.act(aB[:], aB[:], AF.Exp, ["s_aB"], ["s_aB"])
                    P.ts("dve", aB[:], aB[:], -1.0, ALU.mult, ["s_aB"], ["s_aB"])

                    def dthook(idx, t, h32, hkey):
                        for k in range(8):
                            P.mm(ps[5][:, 0:64], h32[:, k, :], wdt[:, k, :], k == 0, k == 7, [hkey, "s_wdt"], [pk(5)])
                        K_ = "s_sp"
                        xr, ab, ee = sp_[:, 0, :], sp_[:, 1, :], sp_[:, 2, :]
                        P.tt("dve", xr, ps[5][:, 0:64], dtb[:], ALU.add, [pk(5), "s_dtb"], [K_])
                        P.ts("dve", sp_[:, 3, :], xr, -1.0, ALU.mult, [K_], [K_])
                        P.tt("dve", ab, xr, sp_[:, 3, :], ALU.max, [K_], [K_])
                        P.act(ee, ab, AF.Exp, [K_], [K_], scale=-1.0)
                        P.act(ee, ee, AF.Ln, [K_], [K_], bias=1.0, scale=1.0)
                        P.stt("dve", dtA[:, t, :], xr, 0.0, ee, ALU.max, ALU.add, [K_], [("s_dtA", t)])
                        P.tt("pool", LA[:, t, :], dtA[:, t, :], aB[:], ALU.mult, [("s_dtA", t), "s_aB"], [("s_LA", t)])

                    with contextlib.ExitStack() as s2:
                        norm_tiles(s2, ALL, 0, hT, "s_hT", 0, hook=dthook, tag="sn")
                        drain(["sn_xt0", "sn_xt1", "sn_junk", "sn_st0", "sn_st1", "sn_h320", "sn_h321"])
                    drain(["s_wdt", "s_dtb", "s_sp"])
                with contextlib.ExitStack() as s1:
                    cwA = P.sb(s1, "s_cwA", [128, 24, 4])
                    cbA = P.sb(s1, "s_cbA", [128, 24])
                    U2 = [P.sb(s1, "s_U%d" % b, [128, T + 8]) for b in range(2)]
                    acc = P.sb(s1, "s_acc", [128, NU])
                    xc = [P.sb(s1, "s_xc%d" % b, [128, NU], BF16) for b in range(2)]
                    wc = [P.sb(s1, "s_wc%d" % b, [128, 8, 128], BF16) for b in range(2)]
                    stg = [P.sb(s1, "s_stg%d" % b, [128, NT, 128], BF16) for b in range(2)]
                    for k in range(4):
                        P.dma("sp", cwA[:, :, k], ssm_conv_w[0, k].rearrange("(c p) -> p c", p=128), writes=["s_cwA"],
                              allow_slow_non_contiguous=True)
                    P.dma("sp", cbA[:], ssm_conv_b[0].rearrange("(c p) -> p c", p=128), writes=["s_cbA"],
                          allow_slow_non_contiguous=True)
                    for b_ in range(2):
                        P.memset("dve" if b_ == 0 else "pool", U2[b_][:], 0.0, ["s_U%d" % b_])
                    blocks = [(0, 256, 2)] + [(256 + b * 512, 512, 261 + b * 512) for b in range(8)]
                    mcnt = [0]

                    def inproj(cc):
                        b = cc % 2
                        wck = "s_wc%d" % b
                        U, uk = U2[b], "s_U%d" % b
                        P.dma("pool", wc[b][:], w_in[:, 2048 + cc * 128:2048 + (cc + 1) * 128].rearrange("(k p) n -> p k n", p=128),
                              writes=[wck])
                        for (t0, n_, uo) in blocks:
                            bank = mcnt[0] % 2
                            mcnt[0] += 1
                            for k in range(8):
                                P.mm(ps[bank][:, 0:n_], wc[b][:, k, :], hT[:, k, t0:t0 + n_], k == 0, k == 7,
                                     [wck, "s_hT"], [pk(bank)])
                            P.cp("act", U[:, uo:uo + n_], ps[bank][:, 0:n_], [pk(bank)], [uk])

                    inproj(0)
                    for cc in range(24):
                        b = cc % 2
                        wck, xck, stk = "s_wc%d" % b, "s_xc%d" % b, "s_stg%d" % b
                        U, uk = U2[b], "s_U%d" % b
                        if cc + 1 < 24:
                            inproj(cc + 1)
                        ce = "dve"
                        P.ts(ce, acc[:], U[:, 0:NU], cwA[:, cc, 0:1], ALU.mult, [uk, "s_cwA"], ["s_acc"])
                        for k in range(1, 4):
                            P.stt(ce, acc[:], U[:, k:k + NU], cwA[:, cc, k:k + 1], acc[:], ALU.mult, ALU.add,
                                  [uk, "s_cwA", "s_acc"], ["s_acc"])
                        P.act(xc[b][:], acc[:], AF.Silu, ["s_acc", "s_cbA"], [xck], bias=cbA[:, cc:cc + 1], scale=1.0)
                        if cc >= 16:
                            g = (cc - 16) % 4
                            dst = BF if cc < 20 else CF
                            dk = "BF" if cc < 20 else "CF"
                            P.dma("sp", dst[g, :, 0:CTX], xc[b][:, 0:CTX], reads=[xck], writes=[(dk, g)], semkey=xck)
                            P.dma("sp", dst[g, :, CTX:T], xc[b][:, 259:259 + SEQ], reads=[xck], writes=[(dk, g)], semkey=xck)
                        if cc < 20:
                            for t in range(NT):
                                bank = 2 + (t // 8) % 4
                                pv = ps[bank][:, :].bitcast(BF16)
                                P.tr(pv[:, (t % 8) * 128:(t % 8 + 1) * 128], xc[b][:, xcol(t):xcol(t) + 128], identb[:],
                                     [xck, "identb"], [pk(bank)])
                                if t % 8 == 7 or t == NT - 1:
                                    t0 = (t // 8) * 8
                                    nt_ = t - t0 + 1
                                    P.cp("act", stg[b][:, t0:t0 + nt_, :],
                                         pv[:, 0:nt_ * 128].rearrange("p (t c) -> p t c", c=128), [pk(bank)], [stk])
                            if cc < 16:
                                dv = XT.rearrange("(t p) c -> p t c", p=128)[:, :, cc * 128:(cc + 1) * 128]
                                wk = ("XT", cc)
                            else:
                                dv = BTK.rearrange("(t p) c -> p t c", p=128)[:, :, (cc - 16) * 128:(cc - 15) * 128]
                                wk = ("BTK", cc - 16)
                            P.dma("sp", dv[:, 0:17, :], stg[b][:, 0:17, :], reads=[stk], writes=[wk], semkey=stk)
                            P.dma("sp", dv[:, 17:NT, :], stg[b][:, 17:NT, :], reads=[stk], writes=[wk], semkey=stk)
                    drain(["s_cwA", "s_cbA", "s_U0", "s_U1", "s_acc", "s_xc0", "s_xc1", "s_wc0", "s_wc1", "s_stg0", "s_stg1"])
                with contextlib.ExitStack() as s1:
                    wz = P.sb(s1, "s_wz", [128, 8, SSM_DI], BF16)
                    szb = [P.sb(s1, "s_szb%d" % b, [128, SSM_DI], BF16) for b in range(2)]
                    P.dma("pool", wz[:], w_in[:, 0:SSM_DI].rearrange("(k p) n -> p k n", p=128), writes=["s_wz"])
                    for t in range(NT):
                        b = t % 2
                        for nb in range(4):
                            bank = (t * 4 + nb) % 8
                            for k in range(8):
                                P.mm(ps[bank][:, :], hT[:, k, t * 128:(t + 1) * 128], wz[:, k, nb * 512:(nb + 1) * 512],
                                     k == 0, k == 7, ["s_hT", "s_wz"], [pk(bank)])
                            P.act(szb[b][:, nb * 512:(nb + 1) * 512], ps[bank][:, :], AF.Silu, [pk(bank)], ["s_szb%d" % b])
                        P.dma("sp", ZG[t * 128:(t + 1) * 128, :], szb[b][:], reads=["s_szb%d" % b], writes=[("ZG", t)],
                              semkey="s_szb%d" % b)
                    drain(["s_wz", "s_szb0", "s_szb1", "s_hT"])
                sH.close()
                with contextlib.ExitStack() as s1:
                    tri = P.sb(s1, "s_tri", [128, 4, 128])
                    state = P.sb(s1, "s_state", [128, 32, 64])
                    stbf = P.sb(s1, "s_stbf", [128, 32, 64], BF16)
                    xk_ = [P.sb(s1, "s_xk%d" % b, [128, 32, 64], BF16) for b in range(2)]
                    btk = [P.sb(s1, "s_btk%d" % b, [128, 512], BF16) for b in range(2)]
                    bfc = [P.sb(s1, "s_bfc%d" % b, [128, 4, 128], BF16) for b in range(2)]
                    cfc = [P.sb(s1, "s_cfc%d" % b, [128, 4, 128], BF16) for b in range(2)]
                    xdt = P.sb(s1, "s_xdt", [128, 32, 64], BF16)
                    xsd = P.sb(s1, "s_xsd", [128, 32, 64], BF16)
                    laB = P.sb(s1, "s_laB", [128, 32, 128])
                    CM = P.sb(s1, "s_CM", [128, 32, 128])
                    sm = P.sb(s1, "s_sm", [128, 6, 32])
                    GTs = [P.sb(s1, "s_GTs%d" % b, [128, 128]) for b in range(2)]
                    Lg = [P.sb(s1, "s_Lg%d" % b, [128, 4, 128]) for b in range(2)]
                    WT = [P.sb(s1, "s_WT%d" % b, [128, 4, 128], BF16) for b in range(2)]
                    yoff = [P.sb(s1, "s_yoff%d" % b, [128, 8, 64]) for b in range(2)]
                    ybuf = [P.sb(s1, "s_ybuf%d" % b, [128, 32, 64]) for b in range(2)]
                    P.dma("sp", tri[:], k_tri.rearrange("w p n -> p w n"), writes=["s_tri"])
                    ccount = 0
                    for d in range(2):
                        order = list(range(NT)) if d == 0 else [1, 0] + list(range(NT - 1, 1, -1))
                        Ut, Mneg = tri[:, 2 * d, :], tri[:, 2 * d + 1, :]
                        P.memset("pool", state[:], 0.0, [("s_state", g_) for g_ in range(4)])
                        for c in order:
                            b = ccount % 2
                            ccount += 1
                            xkk, btkk, bfk, cfk, ybk = "s_xk%d" % b, "s_btk%d" % b, "s_bfc%d" % b, "s_cfc%d" % b, "s_ybuf%d" % b
                            P.dma("sp", xk_[b][:], XT[c * 128:(c + 1) * 128, :].rearrange("p (h d) -> p h d", d=64),
                                  reads=[("XT", q) for q in range(16)], writes=[xkk], semkey=xkk)
                            P.dma("sp", btk[b][:], BTK[c * 128:(c + 1) * 128, :], reads=[("BTK", q) for q in range(4)],
                                  writes=[btkk], semkey=btkk)
                            P.dma("sp", bfc[b][:], BF[:, :, c * 128:(c + 1) * 128].rearrange("g n t -> n g t"),
                                  reads=[("BF", q) for q in range(4)], writes=[bfk], semkey=bfk)
                            P.dma("sp", cfc[b][:], CF[:, :, c * 128:(c + 1) * 128].rearrange("g n t -> n g t"),
                                  reads=[("CF", q) for q in range(4)], writes=[cfk], semkey=cfk)
                            la_c = LA[:, c, d * 32:(d + 1) * 32]
                            dt_c = dtA[:, c, d * 32:(d + 1) * 32]
                            lak, dtk = ("s_LA", c), ("s_dtA", c)
                            SM = "s_sm"
                            csc, tot, ff, fx, ecs, dec = (sm[:, q, :] for q in range(6))
                            P.mm(ps[0][:, 0:32], Ut, la_c, True, True, ["s_tri", lak], [pk(0)])
                            P.mm(ps[0][:, 32:64], ones[:], la_c, True, True, ["ones", lak], [pk(0)])
                            P.cp("dve", sm[:, 0:2, :], ps[0][:, 0:64].rearrange("p (a h) -> p a h", h=32), [pk(0)], [SM])
                            P.tt("dve", ff, tot, csc, ALU.subtract, [SM], [SM])
                            P.act(ff, ff, AF.Exp, [SM], [SM])
                            P.act(ecs, csc, AF.Exp, [SM], [SM])
                            P.act(dec, tot, AF.Exp, [SM], [SM])
                            P.tt("dve", fx, ff, dt_c, ALU.mult, [SM, dtk], [SM])
                            P.tt("pool", xdt[:], xk_[b][:], dt_c.unsqueeze(2).broadcast_to([128, 32, 64]), ALU.mult,
                                 [xkk, dtk], ["s_xdt"])
                            P.tt("pool", xsd[:], xk_[b][:], fx.unsqueeze(2).broadcast_to([128, 32, 64]), ALU.mult,
                                 [xkk, SM], ["s_xsd"])
                            P.cp("dve", laB[:], la_c.unsqueeze(2).broadcast_to([128, 32, 128]), [lak], ["s_laB"])
                            P.cp("act", stbf[:], state[:], [("s_state", g_) for g_ in range(4)], ["s_stbf"])
                            P.tt("dve", CM[:], csc.unsqueeze(2).broadcast_to([128, 32, 128]),
                                 Mneg.unsqueeze(1).broadcast_to([128, 32, 128]), ALU.subtract, [SM, "s_tri"], ["s_CM"])
                            def grp_begin(g):
                                P.mm(ps[1][:, 0:128], bfc[b][:, g, :], cfc[b][:, g, :], True, True, [bfk, cfk], [pk(1)])
                                P.cp("act", GTs[g % 2][:], ps[1][:, 0:128], [pk(1)], ["s_GTs%d" % (g % 2)])
                                P.mm(ps[2][:, :], cfc[b][:, g, :], stbf[:, g * 8:(g + 1) * 8, :].rearrange("p h d -> p (h d)"),
                                     True, True, [cfk, "s_stbf"], [pk(2)])
                                P.cp("act", yoff[g % 2][:], ps[2][:, :].rearrange("p (h d) -> p h d", d=64), [pk(2)],
                                     ["s_yoff%d" % (g % 2)])

                            def quad_cs(n):
                                g, q = n // 2, n % 2
                                if q == 0:
                                    grp_begin(g)
                                cb_ = 3 + n % 2
                                for j in range(4):
                                    P.mm(ps[cb_][:, j * 128:(j + 1) * 128], laB[:, g * 8 + q * 4 + j, :], Ut, True, True,
                                         ["s_laB", "s_tri"], [pk(cb_)])

                            def grp_end(g):
                                yb_ = 5 + g % 2
                                yo, yok = yoff[g % 2], "s_yoff%d" % (g % 2)
                                P.tt("pool", yo[:], yo[:], ecs[:, g * 8:(g + 1) * 8].unsqueeze(2).broadcast_to([128, 8, 64]),
                                     ALU.mult, [yok, SM], [yok])
                                P.tt("dve", ybuf[b][:, g * 8:(g + 1) * 8, :], yo[:],
                                     ps[yb_][:, :].rearrange("p (h d) -> p h d", d=64), ALU.add, [yok, pk(yb_)], [ybk])
                                P.mm(ps[7][:, :], btk[b][:, g * 128:(g + 1) * 128],
                                     xsd[:, g * 8:(g + 1) * 8, :].rearrange("p h d -> p (h d)"), True, True,
                                     [btkk, "s_xsd"], [pk(7)])
                                P.tt("pool", state[:, g * 8:(g + 1) * 8, :], state[:, g * 8:(g + 1) * 8, :],
                                     dec[:, g * 8:(g + 1) * 8].unsqueeze(2).broadcast_to([128, 8, 64]), ALU.mult,
                                     [("s_state", g), SM], [("s_state", g)])
                                P.tt("dve", state[:, g * 8:(g + 1) * 8, :], state[:, g * 8:(g + 1) * 8, :],
                                     ps[7][:, :].rearrange("p (h d) -> p h d", d=64), ALU.add, [("s_state", g), pk(7)],
                                     [("s_state", g)])

                            quad_cs(0)
                            for n in range(8):
                                g, q = n // 2, n % 2
                                h0 = g * 8 + q * 4
                                if n + 1 < 8:
                                    quad_cs(n + 1)
                                cb_ = 3 + n % 2
                                lb = n % 2
                                lgk, wtk = "s_Lg%d" % lb, "s_WT%d" % lb
                                csv = ps[cb_][:, :].rearrange("p (j l) -> p j l", l=128)
                                P.tt("dve", Lg[lb][:], csv, CM[:, h0:h0 + 4, :], ALU.subtract, [pk(cb_), "s_CM"], [lgk])
                                P.act(Lg[lb][:], Lg[lb][:], AF.Exp, [lgk], [lgk])
                                P.tt("dve", WT[lb][:], Lg[lb][:], GTs[g % 2][:].unsqueeze(1).broadcast_to([128, 4, 128]),
                                     ALU.mult, [lgk, "s_GTs%d" % (g % 2)], [wtk])
                                yb_ = 5 + g % 2
                                for j in range(4):
                                    P.mm(ps[yb_][:, (q * 4 + j) * 64:(q * 4 + j + 1) * 64], WT[lb][:, j, :], xdt[:, h0 + j, :],
                                         True, True, [wtk, "s_xdt"], [pk(yb_)])
                                if q == 1:
                                    grp_end(g)
                            P.dma("sp", Yd[d, c * 128:(c + 1) * 128, :], ybuf[b][:].rearrange("p h d -> p (h d)"),
                                  reads=[ybk], writes=[("Yd", d, c)], semkey=ybk)
                    keys = ["s_tri", "s_stbf", "s_xdt", "s_xsd", "s_laB", "s_CM", "s_sm", "s_GTs0", "s_GTs1", "s_yoff0", "s_yoff1"]
                    keys += [("s_state", g_) for g_ in range(4)]
                    for b in range(2):
                        keys += ["s_xk%d" % b, "s_btk%d" % b, "s_bfc%d" % b, "s_cfc%d" % b, "s_Lg%d" % b, "s_WT%d" % b,
                                 "s_ybuf%d" % b]
                    keys += [("s_dtA", t) for t in range(NT)] + [("s_LA", t) for t in range(NT)] + ["s_aB"]
                    drain(keys)
            with contextlib.ExitStack() as s1:
                wo = P.sb(s1, "s_wo", [128, 16, D], BF16)
                ng = P.sb(s1, "s_ng", [128, SSM_DI])
                dsk = P.sb(s1, "s_dsk", [128, 64])
                yf = [P.sb(s1, "s_yf%d" % b, [128, 32, 64]) for b in range(2)]
                yb2 = [P.sb(s1, "s_yb2%d" % b, [128, 32, 64]) for b in range(2)]
                xk3 = [P.sb(s1, "s_xk3%d" % b, [128, 32, 64], BF16) for b in range(2)]
                zg = [P.sb(s1, "s_zg%d" % b, [128, SSM_DI], BF16) for b in range(2)]
                xo = [P.sb(s1, "s_xo%d" % b, [128, D]) for b in range(2)]
                sz_ = [P.sb(s1, "s_sz%d" % b, [128, SSM_DI]) for b in range(2)]
                gbf_ = [P.sb(s1, "s_gbf%d" % b, [128, SSM_DI], BF16) for b in range(2)]
                gT = [P.sb(s1, "s_gT%d" % b, [128, 16, 128], BF16) for b in range(2)]
                g4_ = [P.sb(s1, "s_g4%d" % b, [128, 12]) for b in range(2)]
                junk = P.sb(s1, "s_junk3", [128, 512], BF16)
                tm = [P.sb(s1, "s_tm%d" % b, [128, D]) for b in range(2)]
                P.dma("pool", wo[:], ssm_w_out[0].rearrange("(k p) n -> p k n", p=128), writes=["s_wo"])
                P.dma("sp", ng[:], ssm_norm_g[0:1, :].broadcast_to([128, SSM_DI]), writes=["s_ng"])
                P.dma("sp", dsk[:], ssm_d[0:1, :].broadcast_to([128, 64]), writes=["s_dsk"])
                P.tt("dve", dsk[:, 0:32], dsk[:, 0:32], dsk[:, 32:64], ALU.add, ["s_dsk"], ["s_dsk"])

                def s3load(t):
                    b = t % 2
                    P.dma("sp", yf[b][:], Yd[0, t * 128:(t + 1) * 128, :].rearrange("p (h d) -> p h d", d=64),
                          reads=[("Yd", 0, t)], writes=["s_yf%d" % b], semkey="s_yf%d" % b)
                    P.dma("sp", yb2[b][:], Yd[1, t * 128:(t + 1) * 128, :].rearrange("p (h d) -> p h d", d=64),
                          reads=[("Yd", 1, t)], writes=["s_yb2%d" % b], semkey="s_yb2%d" % b)
                    P.dma("sp", xk3[b][:], XT[t * 128:(t + 1) * 128, :].rearrange("p (h d) -> p h d", d=64),
                          reads=[("XT", q) for q in range(16)], writes=["s_xk3%d" % b], semkey="s_xk3%d" % b)
                    P.dma("sp", zg[b][:], ZG[t * 128:(t + 1) * 128, :], reads=[("ZG", t)], writes=["s_zg%d" % b],
                          semkey="s_zg%d" % b)
                    P.dma("sp", xo[b][:], X[t * 128:(t + 1) * 128, :], reads=[xk(t)], writes=["s_xo%d" % b], semkey="s_xo%d" % b)

                s3load(0)
                for t in range(NT):
                    if t + 1 < NT:
                        s3load(t + 1)
                    b = t % 2
                    s_ = 1 if t < 2 else 0
                    yfk, ybk, xkk, zgk, xok, gtk, tmk = ("s_yf%d" % b, "s_yb2%d" % b, "s_xk3%d" % b, "s_zg%d" % b, "s_xo%d" % b,
                                                         "s_gT%d" % b, "s_tm%d" % b)
                    P.tt("dve", yf[b][:], yf[b][:], yb2[b][:], ALU.add, [yfk, ybk], [yfk])
                    P.tt("pool", yb2[b][:], xk3[b][:], dsk[:, 0:32].unsqueeze(2).broadcast_to([128, 32, 64]), ALU.mult,
                         [xkk, "s_dsk"], [ybk])
                    P.tt("pool", yf[b][:], yf[b][:], yb2[b][:], ALU.add, [yfk, ybk], [yfk])
                    sz, gbf, g4 = sz_[b], gbf_[b], g4_[b]
                    szk, gbk, g4k = "s_sz%d" % b, "s_gbf%d" % b, "s_g4%d" % b
                    yfl = yf[b][:].rearrange("p h d -> p (h d)")
                    P.tt("dve", sz[:], zg[b][:], yfl, ALU.mult, [zgk, yfk], [szk])
                    for q in range(4):
                        P.act(junk[:], sz[:, q * 512:(q + 1) * 512], AF.Square, [szk], ["s_junk3", g4k],
                              accum_out=g4[:, q:q + 1])
                    P.act(g4[:, 4:8], g4[:, 0:4], AF.Sqrt, [g4k], [g4k], bias=EPS, scale=1.0 / 512)
                    P.recip(g4[:, 8:12], g4[:, 4:8], [g4k], [g4k])
                    for q in range(4):
                        P.stt("dve", gbf[:, q * 512:(q + 1) * 512], sz[:, q * 512:(q + 1) * 512], g4[:, 8 + q:9 + q],
                              ng[:, q * 512:(q + 1) * 512], ALU.mult, ALU.mult, [szk, g4k, "s_ng"], [gbk])
                    for k in range(16):
                        bank = (0 if b == 0 else 2) + k // 8
                        pv = ps[bank][:, :].bitcast(BF16)
                        P.tr(pv[:, (k % 8) * 128:(k % 8 + 1) * 128], gbf[:, k * 128:(k + 1) * 128], identb[:],
                             [gbk, "identb"], [pk(bank)])
                    for q in range(2):
                        bank = (0 if b == 0 else 2) + q
                        P.cp("act", gT[b][:, q * 8:(q + 1) * 8, :],
                             ps[bank][:, :].bitcast(BF16).rearrange("p (k t) -> p k t", t=128), [pk(bank)], [gtk])
                    for half in range(2):
                        bank = 4 + 2 * b + half
                        for k in range(16):
                            P.mm(ps[bank][:, :], gT[b][:, k, :], wo[:, k, half * 512:(half + 1) * 512], k == 0, k == 15,
                                 [gtk, "s_wo"], [pk(bank)])
                        P.tt("dve", tm[b][:, half * 512:(half + 1) * 512], ps[bank][:, :],
                             gates[:, 0, s_, half * 512:(half + 1) * 512], ALU.mult, [pk(bank), ("gates", 0, s_)], [tmk])
                    P.tt("pool", xo[b][:], xo[b][:], tm[b][:], ALU.add, [xok, tmk], [xok])
                    P.dma("sp", X[t * 128:(t + 1) * 128, :], xo[b][:], reads=[xok], writes=[xk(t)], semkey=xok)
                keys = ["s_wo", "s_ng", "s_dsk", "s_junk3"]
                for b in range(2):
                    keys += ["s_sz%d" % b, "s_gbf%d" % b, "s_g4%d" % b]
                    keys += ["s_yf%d" % b, "s_yb2%d" % b, "s_xk3%d" % b, "s_zg%d" % b, "s_xo%d" % b, "s_gT%d" % b, "s_tm%d" % b]
                drain(keys)

    def moe_sparse(i, tiles):
        ntl = len(tiles)
        Hs = P.dscratch("ms_Hs%d" % i, [NSLOT, D], BF16)
        Z = P.dscratch("ms_Z%d" % i, [NSLOT, D])
        wgu_rows = moe_w_gu[i]
        wdn_rows = moe_w_dn[i]
        IOA = bass.IndirectOffsetOnAxis
        with contextlib.ExitStack() as st:
            WW = P.sb(st, "q_WW", [128, ntl, 2])
            POSI = P.sb(st, "q_POSI", [128, ntl, 2], I32)
            OFFGU = P.sb(st, "q_OFFGU", [128, NBLK], I32)
            pst = P.sb(st, "q_pst", [128, NE])
            km = P.sb(st, "q_km", [128, 256])
            P.dma("sp", km[:], k_moe, writes=["q_km"])
            with contextlib.ExitStack() as s1:
                HTOK = P.sb(s1, "q_HTOK", [128, ntl, D], BF16)
                OH = P.sb(s1, "q_OH", [128, ntl, 3, NE])
                wr = P.sb(s1, "q_wr", [128, 8, 36])
                rt = P.sb(s1, "q_rt", [128, 160])
                hT2 = P.sb(s1, "q_hT2", [128, 8, 256], BF16)
                P.dma("sp", wr[:], moe_wr[i].rearrange("(k p) n -> p k n", p=128), writes=["q_wr"])

                LG = P.sb(s1, "q_LG", [128, ntl, 36])

                def router(idx, t, h32, hkey):
                    lg = ps[5]
                    for k in range(8):
                        P.mm(lg[:, 0:36], h32[:, k, :], wr[:, k, :], k == 0, k == 7, [hkey, "q_wr"], [pk(5)])
                    P.cp("dve", LG[:, idx, :], lg[:, 0:36], [pk(5)], [("q_LG", idx)])
                    c0 = (idx % 2) * 128
                    pv = ps[3][:, :].bitcast(BF16)
                    for c in range(8):
                        P.tr(pv[:, c * 128:(c + 1) * 128], hT2[:, c, c0:c0 + 128], identb[:],
                             [("q_hT2", idx % 2), "identb"], [pk(3)])
                    P.cp("act", HTOK[:, idx, :], pv[:, :], [pk(3)], [("q_HTOK", idx)])

                with contextlib.ExitStack() as s2:
                    norm_tiles(s2, tiles, 1, hT2, "q_hT2", 0, hook=router, tag="qn", colfn=lambda idx: (idx % 2) * 128)
                    drain(["qn_xt0", "qn_xt1", "qn_junk", "qn_st0", "qn_st1", "qn_h320", "qn_h321"])
                R = "q_rt"
                rb = P.sb(s1, "q_rb", [128, 8, ntl])
                g4 = P.sb(s1, "q_g4", [128, 2, ntl, 4])
                le = P.sb(s1, "q_le", [128, 2, ntl, NE])
                lgk = [("q_LG", idx) for idx in range(ntl)]
                ohk = [("q_OH", idx) for idx in range(ntl)]
                wwk = [("q_WW", idx) for idx in range(ntl)]
                LGg, LGe = LG[:, :, 0:4], LG[:, :, 4:36]
                gmax, gate, m1, m2, dd, p1, p2 = (rb[:, q, :] for q in range(7))
                bc4 = lambda v: v.unsqueeze(2).broadcast_to([128, ntl, 4])
                bc32 = lambda v: v.unsqueeze(2).broadcast_to([128, ntl, NE])
                P.red("dve", gmax, LGg, ALU.max, lgk, [R])
                P.tt("dve", g4[:, 0], LGg, bc4(gmax), ALU.subtract, lgk + [R], [R])
                P.act(g4[:, 0], g4[:, 0], AF.Exp, [R], [R])
                P.red("dve", gate, g4[:, 0], ALU.add, [R], [R])
                P.recip(gate, gate, [R], [R])
                P.tt("dve", g4[:, 1], LGg, bc4(gmax), ALU.is_ge, lgk + [R], [R])
                P.ts("dve", g4[:, 1], g4[:, 1], -1.0, ALU.add, [R], [R], s2=BIG, op1=ALU.mult)
                P.tt("dve", le[:, 0].rearrange("p t (g j) -> p t g j", g=4), LGe.rearrange("p t (g j) -> p t g j", g=4),
                     g4[:, 1].unsqueeze(3).broadcast_to([128, ntl, 4, 8]), ALU.add, lgk + [R], [R])
                P.red("dve", m1, le[:, 0], ALU.max, [R], [R])
                oh1, oh2, oha = OH[:, :, 0, :], OH[:, :, 1, :], OH[:, :, 2, :]
                P.tt("dve", oh1, le[:, 0], bc32(m1), ALU.is_ge, [R], ohk)
                P.stt("dve", le[:, 1], oh1, -BIG, le[:, 0], ALU.mult, ALU.add, [R] + ohk, [R])
                P.red("dve", m2, le[:, 1], ALU.max, [R], [R])
                P.tt("dve", oh2, le[:, 1], bc32(m2), ALU.is_ge, [R], ohk)
                P.tt("pool", oha, oh1, oh2, ALU.add, ohk, ohk)
                P.tt("dve", dd, m2, m1, ALU.subtract, [R], [R])
                P.act(dd, dd, AF.Exp, [R], [R])
                P.ts("dve", p1, dd, 1.0, ALU.add, [R], [R])
                P.recip(p1, p1, [R], [R])
                P.tt("dve", p2, dd, p1, ALU.mult, [R], [R])
                P.tt("dve", WW[:, :, 0], p1, gate, ALU.mult, [R], wwk)
                P.tt("dve", WW[:, :, 1], p2, gate, ALU.mult, [R], wwk)
                for idx in range(ntl):
                    P.mm(ps[4][:, 0:NE], ones[:], OH[:, idx, 2, :], idx == 0, idx == ntl - 1, ["ones", ("q_OH", idx)], [pk(4)])
                ob = P.sb(s1, "q_ob", [128, 8, NE])
                c3 = P.sb(s1, "q_c3", [128, NBLK, NE])
                be = P.sb(s1, "q_be", [128, NBLK])
                O_ = "q_ob"
                cnt, nb_, pend, tmpa = ob[:, 0, :], ob[:, 1, :], ob[:, 2, :], ob[:, 3, :]
                P.cp("dve", cnt, ps[4][:, 0:NE], [pk(4)], [O_])
                P.tt("dve", c3[:, 0:NTH, :],
                     cnt.unsqueeze(1).broadcast_to([128, NTH, NE]), km[:, 128:128 + NTH].unsqueeze(2).broadcast_to([128, NTH, NE]),
                     ALU.is_gt, [O_, "q_km"], ["q_c3"])
                P.red("dve", nb_, c3[:, 0:NTH, :].rearrange("p m e -> p e m"), ALU.add, ["q_c3"], [O_])
                P.ts("dve", nb_, nb_, float(BLK), ALU.mult, [O_], [O_])
                P.cp("dve", pend, nb_, [O_], [O_])
                src, dst = pend, tmpa
                for sft in (1, 2, 4, 8, 16):
                    P.cp("dve", dst[:, 0:sft], src[:, 0:sft], [O_], [O_])
                    P.tt("dve", dst[:, sft:NE], src[:, sft:NE], src[:, 0:NE - sft], ALU.add, [O_], [O_])
                    src, dst = dst, src
                pend_f = src
                P.tt("dve", pst[:], pend_f, nb_, ALU.subtract, [O_], ["q_pst"])
                P.tt("dve", c3[:], pend_f.unsqueeze(1).broadcast_to([128, NBLK, NE]),
                     km[:, 0:NBLK].unsqueeze(2).broadcast_to([128, NBLK, NE]), ALU.is_le, [O_, "q_km"], ["q_c3"])
                P.red("dve", be[:], c3[:], ALU.add, ["q_c3"], ["q_be"])
                P.ts("dve", be[:], be[:], float(NE - 1), ALU.min, ["q_be"], ["q_be"])
                P.ts("dve", be[:], be[:], 128.0, ALU.mult, ["q_be"], ["q_be"])
                P.tt("dve", be[:], be[:], km[:, 200:201].broadcast_to([128, NBLK]), ALU.add, ["q_be", "q_km"], ["q_be"])
                sk_ = c3[:, 0:4, :].rearrange("p a e -> p (a e)")[:, 0:NBLK - 1]
                P.tt("dve", sk_, be[:, 1:NBLK], be[:, 0:NBLK - 1], ALU.is_equal, ["q_be"], ["q_c3"])
                P.stt("dve", be[:, 1:NBLK], sk_, 1.0e6, be[:, 1:NBLK], ALU.mult, ALU.add, ["q_c3", "q_be"], ["q_be"])
                P.cp("dve", OFFGU[:], be[:], ["q_be"], ["q_OFFGU"])
                ustr = P.sb(s1, "q_ustr", [128, 128])
                trl = P.sb(s1, "q_trl", [128, 128])
                P.dma("sp", trl[:], k_tri[0], writes=["q_trl"])
                P.tt("dve", ustr[:], trl[:], ident[:], ALU.subtract, ["q_trl", "ident"], ["q_ustr"])
                rk = P.sb(s1, "q_rk", [128, 3, ntl, NE])
                posf = P.sb(s1, "q_posf", [128, ntl, 2])
                ohk2 = [("q_OH", idx) for idx in range(ntl)]
                for idx in range(ntl):
                    bank, col = idx // 16, (idx % 16) * NE
                    P.mm(ps[bank][:, col:col + NE], ustr[:], OH[:, idx, 2, :], True, True, ["q_ustr", ("q_OH", idx)], [pk(bank)])
                    P.mm(ps[3 + bank][:, col:col + NE], ones[:], OH[:, idx, 2, :], True, True, ["ones", ("q_OH", idx)], [pk(3 + bank)])
                for bank in range((ntl + 15) // 16):
                    n_ = min(16, ntl - bank * 16)
                    P.cp("act", rk[:, 0, bank * 16:bank * 16 + n_, :], ps[bank][:, 0:n_ * NE].rearrange("p (t e) -> p t e", e=NE),
                         [pk(bank)], ["q_rk0"])
                    P.cp("dve", rk[:, 1, bank * 16:bank * 16 + n_, :], ps[3 + bank][:, 0:n_ * NE].rearrange("p (t e) -> p t e", e=NE),
                         [pk(3 + bank)], ["q_rk1"])
                src, dst, sk1, dk1 = 1, 2, "q_rk1", "q_rk2"
                sft = 1
                while sft < ntl:
                    P.cp("dve", rk[:, dst, 0:sft, :], rk[:, src, 0:sft, :], [sk1], [dk1])
                    P.tt("dve", rk[:, dst, sft:ntl, :], rk[:, src, sft:ntl, :], rk[:, src, 0:ntl - sft, :], ALU.add, [sk1], [dk1])
                    src, dst, sk1, dk1 = dst, src, dk1, sk1
                    sft *= 2
                for bank in range((ntl + 15) // 16):
                    n_ = min(16, ntl - bank * 16)
                    P.tt("dve", rk[:, src, bank * 16:bank * 16 + n_, :], rk[:, src, bank * 16:bank * 16 + n_, :],
                         ps[3 + bank][:, 0:n_ * NE].rearrange("p (t e) -> p t e", e=NE), ALU.subtract, [sk1, pk(3 + bank)], [sk1])
                P.tt("dve", rk[:, 0], rk[:, 0], rk[:, src], ALU.add, ["q_rk0", sk1], ["q_rk0"])
                P.tt("dve", rk[:, 0], rk[:, 0], pst[:].unsqueeze(1).broadcast_to([128, ntl, NE]), ALU.add, ["q_rk0", "q_pst"], ["q_rk0"])
                for k2 in range(2):
                    P.tt("dve", rk[:, dst], rk[:, 0], OH[:, :, k2, :], ALU.mult, ["q_rk0", dk1] + ohk2, [dk1])
                    P.red("dve", posf[:, :, k2], rk[:, dst], ALU.add, [dk1], ["q_posf"])
                P.cp("dve", POSI[:], posf[:], ["q_posf"], ["q_POSI"])
                for idx in range(ntl):
                    for k2 in range(2):
                        S.idma(Hs[:, :], IOA(ap=POSI[:, idx, k2:k2 + 1], axis=0), HTOK[:, idx, :], None, NSLOT - 1,
                               reads=[("q_HTOK", idx), "q_POSI"], writes=["Hs"], semkey="q_scat")
                drain(["q_wr", "q_rt", "q_rb", "q_g4", "q_le"] + [("q_LG", idx) for idx in range(ntl)] + ["q_hT2", ("q_hT2", 0), ("q_hT2", 1), "q_ob", "q_c3", "q_be", "q_ustr", "q_trl",
                       "q_rk0", "q_rk1", "q_rk2", "q_posf"] + [("q_HTOK", idx) for idx in range(ntl)] + [("q_OH", idx) for idx in range(ntl)])
            with contextlib.ExitStack() as s1:
                w32g = P.sb(s1, "q_w32g", [128, 8, 2 * FH])
                w32d = P.sb(s1, "q_w32d", [128, 4, D])
                wgu = [P.sb(s1, "q_wgu%d" % b, [128, 8, 2 * FH], BF16) for b in range(2)]
                wdn = [P.sb(s1, "q_wdn%d" % b, [128, 4, D], BF16) for b in range(2)]
                hs = [P.sb(s1, "q_hs%d" % b, [128, NSB, D], BF16) for b in range(2)]
                hTs = P.sb(s1, "q_hTs", [128, 8, BLK], BF16)
                sg = [P.sb(s1, "q_sg%d" % b, [128, BLK], BF16) for b in range(2)]
                aT = P.sb(s1, "q_aT", [128, 4, BLK], BF16)
                zt = [P.sb(s1, "q_zt%d" % b, [128, D]) for b in range(2)]

                def gatherw(j):
                    b = j % 2
                    S.idma(w32g[:].rearrange("p k n -> p (k n)"), None, wgu_rows, IOA(ap=OFFGU[:, j:j + 1], axis=0), NE * 128 - 1,
                           reads=["q_OFFGU"], writes=[("q_w32g", k) for k in range(8)], semkey="q_w32g")
                    S.idma(w32d[:].rearrange("p k n -> p (k n)"), None, wdn_rows, IOA(ap=OFFGU[:, j:j + 1], axis=0), NE * 128 - 1,
                           reads=["q_OFFGU"], writes=[("q_w32d", k) for k in range(4)], semkey="q_w32d")
                    P.dma("sp", hs[b][:], Hs[j * BLK:(j + 1) * BLK, :].rearrange("(s p) d -> p s d", p=128),
                          reads=["Hs"], writes=["q_hs%d" % b], semkey="q_hs%d" % b)

                def castw(j):
                    b = j % 2
                    for k in range(8):
                        eng_ = "act" if k % 2 == 0 else "dve"
                        P.cp(eng_, wgu[b][:, k, :], w32g[:, k, :], [("q_w32g", k)], [("q_wgu%d" % b, k)])
                    for k in range(4):
                        P.cp("act" if k % 2 == 0 else "dve", wdn[b][:, k, :], w32d[:, k, :], [("q_w32d", k)],
                             [("q_wdn%d" % b, k)])

                def slots_T(j):
                    b = j % 2
                    for s_ in range(NSB):
                        bank = 4 + s_
                        pv = ps[bank][:, :].bitcast(BF16)
                        for c in range(8):
                            P.tr(pv[:, c * 128:(c + 1) * 128], hs[b][:, s_, c * 128:(c + 1) * 128], identb[:],
                                 ["q_hs%d" % b, "identb"], [pk(bank)])
                        P.cp("act" if s_ % 2 == 0 else "dve", hTs[:, :, s_ * 128:(s_ + 1) * 128],
                             pv[:, :].rearrange("p (c t) -> p c t", t=128), [pk(bank)], [("q_hTs", s_)])

                gatherw(0)
                castw(0)
                slots_T(0)
                zc = 0
                for j in range(NBLK):
                    b = j % 2
                    if j + 1 < NBLK:
                        gatherw(j + 1)
                    gkeys = [("q_wgu%d" % b, k) for k in range(8)]
                    dkeys = [("q_wdn%d" % b, k) for k in range(4)]
                    hkeys = [("q_hTs", s_) for s_ in range(NSB)]
                    for jj in range(4):
                        gb, ub = (jj % 2), 2 + (jj % 2)
                        for k in range(8):
                            P.mm(ps[gb][:, 0:BLK], wgu[b][:, k, jj * 128:(jj + 1) * 128], hTs[:, k, :], k == 0, k == 7,
                                 [gkeys[k]] + hkeys, [pk(gb)])
                        for k in range(8):
                            P.mm(ps[ub][:, 0:BLK], wgu[b][:, k, FH + jj * 128:FH + (jj + 1) * 128], hTs[:, k, :], k == 0, k == 7,
                                 [gkeys[k]] + hkeys, [pk(ub)])
                        sk = "q_sg%d" % (jj % 2)
                        P.act(sg[jj % 2][:], ps[gb][:, 0:BLK], AF.Silu, [pk(gb)], [sk])
                        P.tt("dve", aT[:, jj, :], sg[jj % 2][:], ps[ub][:, 0:BLK], ALU.mult, [sk, pk(ub)], [("q_aT", jj)])
                    for tt_ in range(NSB):
                        zb = zc % 2
                        zc += 1
                        zk = "q_zt%d" % zb
                        for half in range(2):
                            db = 4 + half
                            for jj in range(4):
                                P.mm(ps[db][:, :], aT[:, jj, tt_ * 128:(tt_ + 1) * 128], wdn[b][:, jj, half * 512:(half + 1) * 512],
                                     jj == 0, jj == 3, [("q_aT", jj), dkeys[jj]], [pk(db)])
                            P.cp("act" if half == 0 else "dve", zt[zb][:, half * 512:(half + 1) * 512], ps[db][:, :],
                                 [pk(db)], [zk])
                        r0 = j * BLK + tt_ * 128
                        P.dma("sp", Z[r0:r0 + 128, :], zt[zb][:], reads=[zk], writes=["Z"], semkey=zk)
                    if j + 1 < NBLK:
                        slots_T(j + 1)
                        castw(j + 1)
                keys = ["q_hTs", "q_sg0", "q_sg1", "q_zt0", "q_zt1", "q_hs0", "q_hs1"]
                keys += [("q_w32g", k) for k in range(8)] + [("q_w32d", k) for k in range(4)]
                keys += [("q_wgu%d" % b, k) for b in range(2) for k in range(8)]
                keys += [("q_wdn%d" % b, k) for b in range(2) for k in range(4)]
                keys += [("q_aT", jj) for jj in range(4)] + [("q_hTs", s_) for s_ in range(NSB)]
                drain(keys)
            with contextlib.ExitStack() as s1:
                NBUF = 4
                z1 = [P.sb(s1, "q_z1%d" % b, [128, D]) for b in range(NBUF)]
                z2 = [P.sb(s1, "q_z2%d" % b, [128, D]) for b in range(NBUF)]
                xo = [P.sb(s1, "q_xo%d" % b, [128, D]) for b in range(NBUF)]

                def cload(idx):
                    b = idx % NBUF
                    t = tiles[idx]
                    S.idma(z1[b][:], None, Z[:, :], IOA(ap=POSI[:, idx, 0:1], axis=0), NSLOT - 1,
                           reads=["Z", "q_POSI"], writes=["q_z1%d" % b], semkey="q_z1%d" % b)
                    S.idma(z2[b][:], None, Z[:, :], IOA(ap=POSI[:, idx, 1:2], axis=0), NSLOT - 1,
                           reads=["Z", "q_POSI"], writes=["q_z2%d" % b], semkey="q_z2%d" % b)
                    P.dma("sp", xo[b][:], X[t * 128:(t + 1) * 128, :], reads=[xk(t)], writes=["q_xo%d" % b], semkey="q_xo%d" % b)

                for idx in range(min(NBUF - 1, ntl)):
                    cload(idx)
                for idx, t in enumerate(tiles):
                    if idx + NBUF - 1 < ntl:
                        cload(idx + NBUF - 1)
                    b = idx % NBUF
                    s_ = 1 if t < 2 else 0
                    k1, k2_, ok = "q_z1%d" % b, "q_z2%d" % b, "q_xo%d" % b
                    P.ts("dve", z1[b][:], z1[b][:], WW[:, idx, 0:1], ALU.mult, [k1, ("q_WW", idx)], [k1])
                    P.stt("dve", z1[b][:], z2[b][:], WW[:, idx, 1:2], z1[b][:], ALU.mult, ALU.add, [k1, k2_, ("q_WW", idx)], [k1])
                    P.tt("pool", z1[b][:], z1[b][:], gates[:, 1, s_, :], ALU.mult, [k1, ("gates", 1, s_)], [k1])
                    P.tt("dve", xo[b][:], xo[b][:], z1[b][:], ALU.add, [ok, k1], [ok])
                    P.dma("sp", X[t * 128:(t + 1) * 128, :], xo[b][:], reads=[ok], writes=[xk(t)], semkey=ok)
                drain(["q_z1%d" % b for b in range(4)] + ["q_z2%d" % b for b in range(4)] + ["q_xo%d" % b for b in range(4)] + ["q_POSI", "q_OFFGU", "q_pst", "q_km",
                       "Hs", "Z"] + [("q_WW", idx) for idx in range(ntl)])

    def final_norm():
        with contextlib.ExitStack() as st:
            gb = P.sb(st, "f_g", [128, D])
            xt = [P.sb(st, "f_x%d" % j, [128, D]) for j in range(2)]
            junk = P.sb(st, "f_junk", [128, D])
            st8 = [P.sb(st, "f_st%d" % j, [128, 4]) for j in range(2)]
            P.dma("sp", gb[:], final_g.partition_broadcast(128) if False else final_g[0:1, :].broadcast_to([128, D]),
                  writes=["f_g"])
            for idx in range(SEQ // 128):
                t = idx + 2
                b = idx % 2
                xkey, skey = "f_x%d" % b, "f_st%d" % b
                P.dma("sp", xt[b][:], X[t * 128:(t + 1) * 128, :], reads=[xk(t)], writes=[xkey], semkey=xkey)
                P.act(junk[:], xt[b][:], AF.Square, [xkey], ["f_junk", skey], accum_out=st8[b][:, 0:1])
                P.act(st8[b][:, 1:2], st8[b][:, 0:1], AF.Sqrt, [skey], [skey], bias=EPS, scale=1.0 / D)
                P.recip(st8[b][:, 2:3], st8[b][:, 1:2], [skey], [skey])
                P.stt("dve", xt[b][:], xt[b][:], st8[b][:, 2:3], gb[:], ALU.mult, ALU.mult, [xkey, skey, "f_g"], [xkey])
                P.dma("sp", out[idx * 128:(idx + 1) * 128, :], xt[b][:], reads=[xkey], writes=[("out", idx)],
                      semkey=xkey)
            for e_ in ("sp", "act", "dve"):
                S.wait_all(e_, ["f_g", "f_x0", "f_x1", "f_junk", "f_st0", "f_st1"])

    ALL = list(range(NT))
    LAT = list(range(2, NT))
    for i in layers:
        kind = i % 3
        ctx_out = i < DEPTH - 1
        adaln(i, kind == 1)
        S.recycle()
        if mixers_on:
            if kind == 0:
                attention(i, i // 3, ctx_out)
            elif kind == 1:
                pool_mixer(i)
            else:
                ssd_mixer(i)
            S.recycle()
        if moe_on:
            if cfg.get("dense_moe"):
                moe(i, ALL if ctx_out else LAT)
            else:
                moe_sparse(i, ALL if ctx_out else LAT)
            S.recycle()
    final_norm()
    for e_ in ("sp", "pe", "act", "dve", "pool"):
        S.wait_all(e_, list(S.wr.keys()))
    P.es.close()
    return P


def host_consts():
    ident = np.eye(128, dtype=np.float32)
    n_freq = HD // 4
    inv = (10000.0 ** (-np.arange(n_freq, dtype=np.float32) / n_freq)).astype(np.float32)
    tok = np.arange(SEQ)
    row = (tok // GRID_W).astype(np.float32)
    col = (tok % GRID_W).astype(np.float32)
    ang = np.concatenate([row[:, None] * inv[None, :], col[:, None] * inv[None, :]], axis=-1).astype(np.float32)
    rope = np.zeros((T, 64), np.float32)
    rope[:CTX, :32] = 1.0
    rope[CTX:, :32] = np.cos(ang)
    rope[CTX:, 32:] = np.sin(ang)
    band = np.zeros((4, 5, 128, 128), np.float32)
    n = 128 * 4
    for wi, w in enumerate(POOL_WINDOWS):
        M = np.zeros((n, n), np.float64)
        for t in range(n):
            lo = max(t - w // 2, 0)
            hi = min(t + w // 2, n)
            M[lo:hi, t] = 1.0 / (hi - lo)
            M[t, t] -= 1.0
        band[wi, 0] = M[0:128, 128:256]
        band[wi, 1] = M[0:128, 0:128]
        band[wi, 2] = M[128:256, 128:256]
        band[wi, 3] = M[384:512, 384:512]
        band[wi, 4] = M[256:384, 128:256]
    tri = np.zeros((4, 128, 128), np.float32)
    s = np.arange(128)[:, None]
    l = np.arange(128)[None, :]
    tri[0] = (s <= l)
    tri[1] = np.where(s <= l, 0.0, -1.0e4)
    tri[2] = (s >= l)
    tri[3] = np.where(s >= l, 0.0, -1.0e4)
    kmoe = np.zeros((128, 256), np.float32)
    kmoe[:, 0:NBLK] = (np.arange(NBLK) * BLK)[None, :]
    kmoe[:, 128:128 + NTH] = (np.arange(NTH) * BLK)[None, :]
    kmoe[:, 200:208] = np.arange(8)[None, :] * 128 + np.arange(128)[:, None]
    return {"k_ident": ident, "k_rope": rope, "k_band": band, "k_tri": tri, "k_moe": kmoe}


_CACHE = {}


def kernel(**inputs):
    cfg = inputs.pop("_cfg", {})
    key = repr(sorted(cfg.items()))
    if key not in _CACHE:
        _CACHE[key] = build(cfg)
    P = _CACHE[key]
    f = lambda a: np.ascontiguousarray(np.asarray(a, dtype=np.float32))
    consts = host_consts()
    shared = {}
    for name in ("norm_mix_g", "norm_ffn_g",
                 "attn_q_norm_g", "attn_k_norm_g", "pool_w", "pool_scale", "ssm_w_in", "ssm_conv_w",
                 "ssm_conv_b", "ssm_norm_g", "ssm_w_out"):
        shared[name] = f(inputs[name])
    wrc = np.concatenate([f(inputs["moe_w_router_group"]), f(inputs["moe_w_router_expert"])], axis=-1)
    for i in range(DEPTH):
        if "w_ada_%d" % i in P.inp:
            shared["w_ada_%d" % i] = f(inputs["w_ada"][i])
            shared["b_ada_%d" % i] = f(inputs["b_ada"][i]).reshape(1, 6 * D)
        if "moe_w_gu_%d" % i in P.inp:
            shared["moe_w_gu_%d" % i] = np.ascontiguousarray(
                f(inputs["moe_w_gate_up"][i]).reshape(NE, 8, 128, 2 * FH).transpose(0, 2, 1, 3)).reshape(NE * 128, 8 * 2 * FH)
            shared["moe_w_dn_%d" % i] = np.ascontiguousarray(
                f(inputs["moe_w_down"][i]).reshape(NE, 4, 128, D).transpose(0, 2, 1, 3)).reshape(NE * 128, 4 * D)
            shared["moe_wr_%d" % i] = np.ascontiguousarray(wrc[i])
    for j in range(2):
        if "attn_w_qkv_%d" % j in P.inp:
            shared["attn_w_qkv_%d" % j] = f(inputs["attn_w_qkv"][j])
            shared["attn_w_o_%d" % j] = f(inputs["attn_w_o"][j])
    shared["final_norm_g"] = f(inputs["final_norm_g"]).reshape(1, D)
    shared["c_ctx"] = f(inputs["c_ctx"]).reshape(1, D)
    for name in ("ssm_dt_bias", "ssm_a_log", "ssm_d"):
        shared[name] = f(inputs[name]).reshape(1, 2 * SSM_H)
    shared.update(consts)
    x = f(inputs["x"])
    ctx = f(inputs["ctx"])
    c = f(inputs["c"])
    in_maps = []
    ncores = cfg.get("cores", 8)
    for core in range(ncores):
        b = core % NB
        m = dict(shared)
        m["x"] = x[b]
        m["ctx"] = ctx[b]
        m["c"] = c[b:b + 1]
        in_maps.append({k: v for k, v in m.items() if k in P.inp})
    if cfg.get("trace"):
        res = run_bass_kernel_spmd(P.nc, in_maps, core_ids=list(range(ncores)), trace=True)
        kernel.exec_ns = res.exec_time_ns
    else:
        res = run_bass_kernel_spmd(P.nc, in_maps, core_ids=list(range(ncores)))
    nb = min(NB, ncores)
    outs = np.stack([np.asarray(res.results[b]["out"], dtype=np.float32) for b in range(nb)], axis=0)
    if cfg.get("dbg"):
        kernel.dbg = [np.asarray(res.results[b]["dbg"]) for b in range(nb)]
    return outs
```

```python
import contextlib
import numpy as np
import concourse.bass as bass
import concourse.mybir as mybir
from concourse.bass_utils import run_bass_kernel_spmd

F32 = mybir.dt.float32
BF16 = mybir.dt.bfloat16
I32 = mybir.dt.int32
ALU = mybir.AluOpType
AF = mybir.ActivationFunctionType
AX = mybir.AxisListType

D = 1024
NB = 4
SEQ = 4096
CTX = 256
T = SEQ + CTX
NT = T // 128
DEPTH = 4
EPS = 1e-6
GRID_W = 64
NH, NKV, HD = 16, 4, 64
POOL_WINDOWS = (2, 4, 8, 16)
SSM_DI, SSM_H, SSM_P, SSM_G, SSM_N = 2048, 32, 64, 4, 128
SSM_CONV_DIM = SSM_DI + 2 * SSM_G * SSM_N
SSM_IN = SSM_DI + SSM_CONV_DIM + 2 * SSM_H
NE, EPG, FH = 32, 8, 512
BIG = 1.0e30
BLK = 256
NSB = BLK // 128
NTH = (2 * T + BLK - 1) // BLK + 1
NBLK = (2 * T + BLK - 1) // BLK + NE
NSLOT = NBLK * BLK

SAME_ENGINE_SYNC = ("act", "dve", "pool")


class Sched:
    def __init__(self, nc):
        self.nc = nc
        self.eng = {"pe": nc.tensor, "act": nc.scalar, "dve": nc.vector,
                    "pool": nc.gpsimd, "sp": nc.sync}
        self.esem = {}
        self.ecnt = {}
        for e in ("pe", "act", "dve", "pool"):
            self.esem[e] = nc.alloc_semaphore("s_" + e)
            self.ecnt[e] = 0
        self.seen = {e: {} for e in self.eng}
        self.wr = {}
        self.rd = {}
        self.dsem = {}
        self.dfree = []
        self.nsem = 0
        self.bregs = {}
        self.n_inst = 0

    def _wait(self, e, toks):
        eng = self.eng[e]
        for name, (sem, val, src) in toks.items():
            if src == e and e not in SAME_ENGINE_SYNC:
                continue
            if self.seen[e].get(name, 0) >= val:
                continue
            eng.wait_ge(sem, val)
            self.seen[e][name] = val

    def _deps(self, e, reads, writes):
        for k in reads:
            self._wait(e, self.wr.get(k, {}))
        for k in writes:
            self._wait(e, self.wr.get(k, {}))
            self._wait(e, self.rd.get(k, {}))

    def _commit(self, name, tok, reads, writes):
        for k in reads:
            self.rd.setdefault(k, {})[name] = tok
        for k in writes:
            self.wr[k] = {name: tok}
            self.rd[k] = {}

    def op(self, e, fn, reads=(), writes=()):
        self._deps(e, reads, writes)
        ins = fn(self.eng[e])
        self.ecnt[e] += 1
        ins.then_inc(self.esem[e], 1)
        self._commit("s_" + e, (self.esem[e], self.ecnt[e], e), reads, writes)
        self.n_inst += 1
        return ins

    def dma(self, q, out, in_, reads=(), writes=(), semkey=None, **kw):
        if semkey is None:
            semkey = tuple(writes) + tuple(reads)
        ent = self._dsem_get(semkey)
        self._deps(q, reads, writes)
        ins = self.eng[q].dma_start(out=out, in_=in_, **kw)
        ent[1] += 16
        ins.then_inc(ent[0], 16)
        self._commit(ent[2], (ent[0], ent[1], "dma"), reads, writes)
        self.n_inst += 1

    def idma(self, out, out_off, in_, in_off, bounds, reads=(), writes=(), semkey=None):
        q = "pool"
        ent = self._dsem_get(semkey)
        self._deps(q, reads, writes)
        if bounds not in self.bregs:
            self.bregs[bounds] = self.eng[q].to_reg(bounds)
        ins = self.eng[q].indirect_dma_start(out=out, out_offset=out_off, in_=in_, in_offset=in_off,
                                             bounds_check=self.bregs[bounds], oob_is_err=False)
        ent[1] += 16
        ins.then_inc(ent[0], 16)
        self._commit(ent[2], (ent[0], ent[1], "dma"), reads, writes)
        self.n_inst += 1

    def recycle(self):
        for key, ent in list(self.dsem.items()):
            for e in self.eng:
                if self.seen[e].get(ent[2], 0) < ent[1]:
                    self.eng[e].wait_ge(ent[0], ent[1])
                    self.seen[e][ent[2]] = ent[1]
            self.dfree.append(ent)
            del self.dsem[key]

    def _dsem_get(self, semkey):
        if semkey not in self.dsem:
            if self.dfree:
                self.dsem[semkey] = self.dfree.pop()
            else:
                nm = "d%d" % self.nsem
                self.nsem += 1
                self.dsem[semkey] = [self.nc.alloc_semaphore(nm), 0, nm]
        return self.dsem[semkey]

    def wait_all(self, e, keys):
        for k in keys:
            self._wait(e, self.wr.get(k, {}))
            self._wait(e, self.rd.get(k, {}))


class Prog:
    def __init__(self, cfg):
        self.cfg = cfg
        self.nc = nc = bass.Bass("TRN2", target_bir_lowering=False)
        self.S = Sched(nc)
        self.es = contextlib.ExitStack()
        self.inp = {}
        self.uid = 0

    def din(self, name, shape, dt=F32):
        t = self.nc.dram_tensor(name, list(shape), dt, kind="ExternalInput").ap()
        self.inp[name] = t
        return t

    def dscratch(self, name, shape, dt=F32):
        return self.nc.dram_tensor(name, list(shape), dt, kind="Internal").ap()

    def sb(self, stack, name, shape, dt=F32):
        self.uid += 1
        return stack.enter_context(self.nc.sbuf_tensor("%s_u%d" % (name, self.uid), list(shape), dt))

    def mm(self, out, lhsT, rhs, start, stop, reads, writes):
        self.S.op("pe", lambda e: e.matmul(out, lhsT=lhsT, rhs=rhs, start=start, stop=stop),
                  reads=reads, writes=writes)

    def tr(self, out, in_, ident, reads, writes):
        self.S.op("pe", lambda e: e.transpose(out, in_, ident), reads=reads, writes=writes)

    def act(self, out, in_, func, reads, writes, bias=None, scale=None, accum_out=None):
        kw = {}
        if bias is not None:
            kw["bias"] = bias
        if scale is not None:
            kw["scale"] = scale
        if accum_out is not None:
            kw["accum_out"] = accum_out
        self.S.op("act", lambda e: e.activation(out=out, in_=in_, func=func, **kw),
                  reads=reads, writes=writes)

    def tt(self, e, out, in0, in1, op, reads, writes):
        self.S.op(e, lambda g: g.tensor_tensor(out=out, in0=in0, in1=in1, op=op),
                  reads=reads, writes=writes)

    def ts(self, e, out, in0, s1, op0, reads, writes, s2=None, op1=None, accum_out=None):
        kw = {}
        if op1 is not None:
            kw["op1"] = op1
        if accum_out is not None:
            kw["accum_out"] = accum_out
        self.S.op(e, lambda g: g.tensor_scalar(out=out, in0=in0, scalar1=s1, scalar2=s2, op0=op0, **kw),
                  reads=reads, writes=writes)

    def stt(self, e, out, in0, scalar, in1, op0, op1, reads, writes):
        self.S.op(e, lambda g: g.scalar_tensor_tensor(out=out, in0=in0, scalar=scalar, in1=in1,
                                                      op0=op0, op1=op1),
                  reads=reads, writes=writes)

    def cp(self, e, out, in_, reads, writes):
        if e == "act":
            self.S.op("act", lambda g: g.copy(out=out, in_=in_), reads=reads, writes=writes)
        else:
            self.S.op(e, lambda g: g.tensor_copy(out=out, in_=in_), reads=reads, writes=writes)

    def red(self, e, out, in_, op, reads, writes, axis=AX.X):
        self.S.op(e, lambda g: g.tensor_reduce(out=out, in_=in_, axis=axis, op=op),
                  reads=reads, writes=writes)

    def recip(self, out, in_, reads, writes):
        self.S.op("dve", lambda g: g.reciprocal(out=out, in_=in_), reads=reads, writes=writes)

    def memset(self, e, ap, val, writes):
        self.S.op(e, lambda g: g.memset(ap, val), writes=writes)

    def dma(self, q, out, in_, reads=(), writes=(), semkey=None, **kw):
        self.S.dma(q, out, in_, reads=reads, writes=writes, semkey=semkey, **kw)


def build(cfg):
    P = Prog(cfg)
    nc, S = P.nc, P.S
    layers = cfg.get("layers", list(range(DEPTH)))
    mixers_on = cfg.get("mixers", True)
    moe_on = cfg.get("moe", True)

    x_in = P.din("x", [SEQ, D])
    ctx_in = P.din("ctx", [CTX, D])
    c_in = P.din("c", [1, D])
    cctx_in = P.din("c_ctx", [1, D])
    w_ada = {i: P.din("w_ada_%d" % i, [D, 6 * D]) for i in layers}
    b_ada = {i: P.din("b_ada_%d" % i, [1, 6 * D]) for i in layers}
    norm_mix_g = P.din("norm_mix_g", [DEPTH, D])
    norm_ffn_g = P.din("norm_ffn_g", [DEPTH, D])
    final_g = P.din("final_norm_g", [1, D])
    attn_w_qkv = {j: P.din("attn_w_qkv_%d" % j, [D, 1536]) for j in range(2) if 3 * j in layers and mixers_on}
    attn_w_o = {j: P.din("attn_w_o_%d" % j, [D, D]) for j in range(2) if 3 * j in layers and mixers_on}
    attn_qg = P.din("attn_q_norm_g", [2, HD])
    attn_kg = P.din("attn_k_norm_g", [2, HD])
    pool_w = P.din("pool_w", [1, 4, 256, 256])
    pool_scale = P.din("pool_scale", [1, D])
    ssm_on = 2 in layers and mixers_on
    ssm_w_in = P.din("ssm_w_in", [1, D, SSM_IN]) if ssm_on else None
    ssm_conv_w = P.din("ssm_conv_w", [1, 4, SSM_CONV_DIM])
    ssm_conv_b = P.din("ssm_conv_b", [1, SSM_CONV_DIM])
    ssm_dt_bias = P.din("ssm_dt_bias", [1, 2 * SSM_H])
    ssm_a_log = P.din("ssm_a_log", [1, 2 * SSM_H])
    ssm_d = P.din("ssm_d", [1, 2 * SSM_H])
    ssm_norm_g = P.din("ssm_norm_g", [1, SSM_DI])
    ssm_w_out = P.din("ssm_w_out", [1, SSM_DI, D]) if ssm_on else None
    moe_l = [i for i in layers if moe_on]
    moe_wr = {i: P.din("moe_wr_%d" % i, [D, 36]) for i in moe_l}
    moe_w_gu = {i: P.din("moe_w_gu_%d" % i, [NE * 128, 8 * 2 * FH]) for i in moe_l}
    moe_w_dn = {i: P.din("moe_w_dn_%d" % i, [NE * 128, 4 * D]) for i in moe_l}
    k_ident = P.din("k_ident", [128, 128])
    k_rope = P.din("k_rope", [T, 64])
    k_band = P.din("k_band", [4, 5, 128, 128])
    k_tri = P.din("k_tri", [4, 128, 128])
    k_moe = P.din("k_moe", [128, 256])
    out = nc.dram_tensor("out", [SEQ, D], F32, kind="ExternalOutput").ap()
    dbg = None
    if cfg.get("dbg"):
        dbg = nc.dram_tensor("dbg", list(cfg["dbg"]), F32, kind="ExternalOutput").ap()

    X = P.dscratch("X", [T, D])

    def xk(t):
        return ("X", t)

    top = P.es
    ident = P.sb(top, "ident", [128, 128])
    identb = P.sb(top, "identb", [128, 128], BF16)
    ones = P.sb(top, "ones", [128, 128])
    cT = P.sb(top, "cT", [128, 8, 2])
    modL = P.sb(top, "modL", [128, 48])
    modC = P.sb(top, "modC", [128, 48])
    vec = P.sb(top, "vec", [128, 2, 2, 2, 8])
    gates = P.sb(top, "gates", [128, 2, 2, D])
    ps = [top.enter_context(nc.psum_tensor("ps%d" % i, [128, 512], F32)) for i in range(8)]

    def pk(i):
        return "ps%d" % i

    P.dma("sp", ident[:], k_ident, writes=["ident"])
    P.cp("dve", identb[:], ident[:], ["ident"], ["identb"])
    P.memset("pool", ones[:], 1.0, ["ones"])
    P.dma("sp", X[0:CTX, :], ctx_in, writes=[xk(0), xk(1)], semkey="xinit")
    P.dma("sp", X[CTX:T, :], x_in, writes=[xk(t) for t in range(2, NT)], semkey="xinit")

    with contextlib.ExitStack() as st:
        craw = P.sb(st, "craw", [128, 8, 2])
        P.dma("sp", craw[:, :, 0], c_in[0].rearrange("(k p) -> p k", p=128), writes=["craw"],
              allow_slow_non_contiguous=True)
        P.dma("sp", craw[:, :, 1], cctx_in[0].rearrange("(k p) -> p k", p=128), writes=["craw"],
              allow_slow_non_contiguous=True)
        P.act(cT[:], craw[:], AF.Silu, ["craw"], ["cT"])
        S.wait_all("sp", ["craw"])
        S.wait_all("act", ["craw"])


    def drain(keys):
        for e_ in ("sp", "pe", "act", "dve", "pool"):
            S.wait_all(e_, keys)

    def adaln(i, need_bc_all):
        with contextlib.ExitStack() as st:
            wblk = [P.sb(st, "wblk%d" % j, [128, 8, 512]) for j in range(2)]
            brow = P.sb(st, "brow", [1, 6 * D])
            bT = P.sb(st, "bT", [128, 48])
            g2 = P.sb(st, "g2", [128, 2, 8])
            cbc = P.sb(st, "cbc", [128, 2, 8, 128])
            for s in range(2):
                P.cp("dve", cbc[:, s], cT[:, :, s:s + 1].broadcast_to([128, 8, 128]), ["cT"], ["cbc"])
            P.dma("sp", brow[:], b_ada[i][0:1, :], writes=["brow"])
            P.dma("sp", bT[:], b_ada[i][0].rearrange("(j p) -> p j", p=128), writes=["bT"],
                  allow_slow_non_contiguous=True)
            P.dma("sp", g2[:, 0, :], norm_mix_g[i].rearrange("(j p) -> p j", p=128), writes=["g2"],
                  allow_slow_non_contiguous=True)
            P.dma("sp", g2[:, 1, :], norm_ffn_g[i].rearrange("(j p) -> p j", p=128), writes=["g2"],
                  allow_slow_non_contiguous=True)
            for n in range(12):
                wb = wblk[n % 2]
                wkey = "wblk%d" % (n % 2)
                P.dma("sp", wb[:], w_ada[i][:, n * 512:(n + 1) * 512].rearrange("(k p) n -> p k n", p=128),
                      writes=[wkey])
                for q in range(4):
                    j = n * 4 + q
                    for k in range(8):
                        P.mm(ps[0][:, 2 * j:2 * j + 2], wb[:, k, q * 128:(q + 1) * 128], cT[:, k, :],
                             k == 0, k == 7, [wkey, "cT"], [pk(0)])
                split = n // 2
                if split in (2, 5):
                    which = 0 if split == 2 else 1
                    half = n % 2
                    for s in range(2):
                        bank = 1 + s
                        for k in range(8):
                            P.mm(ps[bank][:, :], cbc[:, s, k, :], wb[:, k, :], k == 0, False,
                                 [wkey, "cbc"], [pk(bank)])
                        P.mm(ps[bank][:, :], ones[0:1, :], brow[0:1, n * 512:(n + 1) * 512], False, True,
                             ["ones", "brow"], [pk(bank)])
                        P.cp("act", gates[:, which, s, half * 512:(half + 1) * 512], ps[bank][:, :],
                             [pk(bank)], [("gates", which, s)])
            psv = ps[0][:, 0:96].rearrange("p (j s) -> p j s", s=2)
            P.tt("dve", modL[:], psv[:, :, 0], bT[:], ALU.add, [pk(0), "bT"], ["modL"])
            P.tt("dve", modC[:], psv[:, :, 1], bT[:], ALU.add, [pk(0), "bT"], ["modC"])
            for which in range(2):
                for s, m in enumerate((modL, modC)):
                    mk = "modL" if s == 0 else "modC"
                    base = which * 24
                    P.stt("dve", vec[:, which, s, 0, :], m[:, base + 8:base + 16], 1.0, g2[:, which, :],
                          ALU.add, ALU.mult, [mk, "g2"], [("vec", which, s)])
                    P.cp("dve", vec[:, which, s, 1, :], m[:, base:base + 8], [mk], [("vec", which, s)])
            S.wait_all("sp", ["wblk0", "wblk1", "brow", "bT", "g2"])
            S.wait_all("pe", ["wblk0", "wblk1", "brow", "cbc"])
            S.wait_all("dve", ["bT", "g2"])

    def norm_tiles(st, tiles, which, hT, hT_key, col0, hook=None, tag="n", colfn=None):
        NX = 3
        n = len(tiles)
        xt = [P.sb(st, "%s_xt%d" % (tag, j), [128, D]) for j in range(NX)]
        h32 = [P.sb(st, "%s_h32%d" % (tag, j), [128, 8, 128]) for j in range(2)]
        st8 = [P.sb(st, "%s_st%d" % (tag, j), [128, 4]) for j in range(NX)]
        junk = P.sb(st, "%s_junk" % tag, [128, D], BF16)

        def load(idx):
            t = tiles[idx]
            P.dma("sp", xt[idx % NX][:], X[t * 128:(t + 1) * 128, :], reads=[xk(t)],
                  writes=["%s_xt%d" % (tag, idx % NX)], semkey="%s_xt%d" % (tag, idx % NX))

        def prep(idx):
            b = idx % NX
            xkey, skey = "%s_xt%d" % (tag, b), "%s_st%d" % (tag, b)
            P.act(junk[:], xt[b][:], AF.Square, [xkey], ["%s_junk" % tag, skey], accum_out=st8[b][:, 0:1])
            P.act(st8[b][:, 1:2], st8[b][:, 0:1], AF.Sqrt, [skey], [skey], bias=EPS, scale=1.0 / D)
            P.recip(st8[b][:, 2:3], st8[b][:, 1:2], [skey], [skey])
            P.ts("dve", xt[b][:], xt[b][:], st8[b][:, 2:3], ALU.mult, [xkey, skey], [xkey])

        def fin(idx):
            t = tiles[idx]
            b = idx % NX
            hb = idx % 2
            xkey, hkey = "%s_xt%d" % (tag, b), "%s_h32%d" % (tag, hb)
            s = 1 if t < 2 else 0
            for c in range(8):
                bank = 6 + c // 4
                P.tr(ps[bank][:, (c % 4) * 128:(c % 4 + 1) * 128], xt[b][:, c * 128:(c + 1) * 128], ident[:],
                     [xkey, "ident"], [pk(bank)])
            c0 = col0 + idx * 128 if colfn is None else colfn(idx)
            hTk = hT_key if colfn is None else (hT_key, idx % 2)
            if hook is None:
                for c in range(8):
                    bank = 6 + c // 4
                    P.act(hT[:, c, c0:c0 + 128], ps[bank][:, (c % 4) * 128:(c % 4 + 1) * 128], AF.Identity,
                          [pk(bank), ("vec", which, s)], [hTk],
                          bias=vec[:, which, s, 1, c:c + 1], scale=vec[:, which, s, 0, c:c + 1])
            else:
                for c in range(8):
                    bank = 6 + c // 4
                    P.act(h32[hb][:, c, :], ps[bank][:, (c % 4) * 128:(c % 4 + 1) * 128], AF.Identity,
                          [pk(bank), ("vec", which, s)], [hkey],
                          bias=vec[:, which, s, 1, c:c + 1], scale=vec[:, which, s, 0, c:c + 1])
                P.cp("dve", hT[:, :, c0:c0 + 128], h32[hb][:], [hkey], [hTk])
                hook(idx, t, h32[hb], hkey)

        load(0)
        if n > 1:
            load(1)
        prep(0)
        for idx in range(n):
            if idx + 2 < n:
                load(idx + 2)
            if idx + 1 < n:
                prep(idx + 1)
            fin(idx)
        drain(["%s_xt%d" % (tag, j) for j in range(NX)] + ["%s_st%d" % (tag, j) for j in range(NX)]
              + ["%s_h32%d" % (tag, j) for j in range(2)] + ["%s_junk" % tag])

    def moe(i, tiles_all):
        nhalf = 2
        per = len(tiles_all) // nhalf
        for hf in range(nhalf):
            tiles = tiles_all[hf * per:(hf + 1) * per]
            ntk = per * 128
            with contextlib.ExitStack() as st:
                hT = P.sb(st, "m_hT", [128, 8, ntk], BF16)
                Y = P.sb(st, "m_Y", [128, per, D])
                Wt = P.sb(st, "m_Wt", [128, per, NE])
                wr = P.sb(st, "m_wr", [128, 8, 36])
                wgu = [P.sb(st, "m_wgu%d" % j, [128, 8, 2 * FH], BF16) for j in range(2)]
                wdn = [P.sb(st, "m_wdn%d" % j, [128, 4, D], BF16) for j in range(2)]
                sg = [P.sb(st, "m_sg%d" % j, [128, 512], BF16) for j in range(2)]
                aT = [P.sb(st, "m_a%d" % j, [128, 4, 512], BF16) for j in range(2)]
                rt = P.sb(st, "m_rt", [128, 160])

                def loadw(e):
                    b = e % 2
                    P.dma("pool", wgu[b][:], moe_w_gu[i][e * 128:(e + 1) * 128, :].rearrange("p (k n) -> p k n", k=8),
                          writes=["m_wgu%d" % b])
                    P.dma("pool", wdn[b][:], moe_w_dn[i][e * 128:(e + 1) * 128, :].rearrange("p (k n) -> p k n", k=4),
                          writes=["m_wdn%d" % b])

                P.dma("sp", wr[:], moe_wr[i].rearrange("(k p) n -> p k n", p=128), writes=["m_wr"])
                loadw(0)

                def router(idx, t, h32, hkey):
                    lg = ps[5]
                    for k in range(8):
                        P.mm(lg[:, 0:36], h32[:, k, :], wr[:, k, :], k == 0, k == 7, [hkey, "m_wr"], [pk(5)])
                    R = "m_rt"
                    lgs = rt[:, 0:36]
                    P.cp("dve", lgs, lg[:, 0:36], [pk(5)], [R])
                    gmax, ngmax, gsum, gate = rt[:, 36:37], rt[:, 37:38], rt[:, 38:39], rt[:, 39:40]
                    P.red("dve", gmax, rt[:, 0:4], ALU.max, [R], [R])
                    P.ts("dve", ngmax, gmax, -1.0, ALU.mult, [R], [R])
                    P.act(rt[:, 40:44], rt[:, 0:4], AF.Exp, [R], [R], bias=ngmax, scale=1.0, accum_out=gsum)
                    P.recip(gate, gsum, [R], [R])
                    pen = rt[:, 44:48]
                    P.ts("dve", pen, rt[:, 0:4], gmax, ALU.is_ge, [R], [R])
                    P.ts("dve", pen, pen, -1.0, ALU.add, [R], [R], s2=BIG, op1=ALU.mult)
                    le = rt[:, 48:80]
                    P.tt("dve", le.rearrange("p (g j) -> p g j", g=4), rt[:, 4:36].rearrange("p (g j) -> p g j", g=4),
                         pen.unsqueeze(2).broadcast_to([128, 4, 8]), ALU.add, [R], [R])
                    m1, m2 = rt[:, 80:81], rt[:, 81:82]
                    P.red("dve", m1, le, ALU.max, [R], [R])
                    oh1, oh2, le2 = rt[:, 84:116], rt[:, 116:148], rt[:, 4:36]
                    P.ts("dve", oh1, le, m1, ALU.is_ge, [R], [R])
                    P.stt("dve", le2, oh1, -BIG, le, ALU.mult, ALU.add, [R], [R])
                    P.red("dve", m2, le2, ALU.max, [R], [R])
                    P.ts("dve", oh2, le2, m2, ALU.is_ge, [R], [R])
                    dd, ee, p1, p2 = rt[:, 148:149], rt[:, 149:150], rt[:, 150:151], rt[:, 151:152]
                    P.tt("dve", dd, m2, m1, ALU.subtract, [R], [R])
                    P.act(ee, dd, AF.Exp, [R], [R])
                    P.ts("dve", p1, ee, 1.0, ALU.add, [R], [R])
                    P.recip(p1, p1, [R], [R])
                    P.tt("dve", p2, ee, p1, ALU.mult, [R], [R])
                    P.tt("dve", p1, p1, gate, ALU.mult, [R], [R])
                    P.tt("dve", p2, p2, gate, ALU.mult, [R], [R])
                    P.ts("dve", oh1, oh1, p1, ALU.mult, [R], [R])
                    P.stt("dve", Wt[:, idx, :], oh2, p2, oh1, ALU.mult, ALU.add, [R], [("m_Wt", idx)])

                with contextlib.ExitStack() as st2:
                    norm_tiles(st2, tiles, 1, hT, "m_hT", 0, hook=router, tag="mn")
                    S.wait_all("sp", ["mn_xt0", "mn_xt1"])
                    S.wait_all("act", ["mn_xt0", "mn_xt1", "mn_junk", "mn_st0", "mn_st1", "mn_h320", "mn_h321"])
                    S.wait_all("dve", ["mn_xt0", "mn_xt1", "mn_st0", "mn_st1"])
                    S.wait_all("pool", ["mn_h320", "mn_h321"])
                    S.wait_all("pe", ["mn_xt0", "mn_xt1", "mn_h320", "mn_h321"])

                blocks = []
                o = 0
                while o < ntk:
                    n_ = min(512, ntk - o)
                    blocks.append((o, n_))
                    o += n_
                cnt = 0
                dcnt = 0
                for e in range(NE):
                    if e + 1 < NE:
                        loadw(e + 1)
                    b = e % 2
                    gk, dk = "m_wgu%d" % b, "m_wdn%d" % b
                    for (o, n_) in blocks:
                        ab = cnt % 2
                        akey = "m_a%d" % ab
                        for j in range(4):
                            gb, ub = (j % 2), 2 + (j % 2)
                            for k in range(8):
                                P.mm(ps[gb][:, 0:n_], wgu[b][:, k, j * 128:(j + 1) * 128], hT[:, k, o:o + n_],
                                     k == 0, k == 7, [gk, "m_hT"], [pk(gb)])
                            for k in range(8):
                                P.mm(ps[ub][:, 0:n_], wgu[b][:, k, FH + j * 128:FH + (j + 1) * 128],
                                     hT[:, k, o:o + n_], k == 0, k == 7, [gk, "m_hT"], [pk(ub)])
                            sk = "m_sg%d" % (j % 2)
                            P.act(sg[j % 2][:, 0:n_], ps[gb][:, 0:n_], AF.Silu, [pk(gb)], [sk])
                            P.tt("dve", aT[ab][:, j, 0:n_], sg[j % 2][:, 0:n_], ps[ub][:, 0:n_], ALU.mult,
                                 [sk, pk(ub)], [(akey, j)])
                        for tt_ in range(n_ // 128):
                            tidx = o // 128 + tt_
                            for half in range(2):
                                db = 4 + dcnt % 2
                                dcnt += 1
                                for j in range(4):
                                    P.mm(ps[db][:, :], aT[ab][:, j, tt_ * 128:(tt_ + 1) * 128],
                                         wdn[b][:, j, half * 512:(half + 1) * 512], j == 0, j == 3,
                                         [(akey, j), dk], [pk(db)])
                                yk = ("m_Y", tidx, half)
                                ysl = Y[:, tidx, half * 512:(half + 1) * 512]
                                if e == 0:
                                    P.ts("dve", ysl, ps[db][:, :], Wt[:, tidx, e:e + 1], ALU.mult,
                                         [pk(db), ("m_Wt", tidx)], [yk])
                                else:
                                    P.stt("dve", ysl, ps[db][:, :], Wt[:, tidx, e:e + 1], ysl, ALU.mult, ALU.add,
                                          [pk(db), ("m_Wt", tidx), yk], [yk])
                        cnt += 1
                xo = [P.sb(st, "m_xo%d" % j, [128, D]) for j in range(2)]
                for idx, t in enumerate(tiles):
                    b = idx % 2
                    s = 1 if t < 2 else 0
                    ok = "m_xo%d" % b
                    P.dma("sp", xo[b][:], X[t * 128:(t + 1) * 128, :], reads=[xk(t)], writes=[ok], semkey=ok)
                    P.tt("pool", Y[:, idx, :], Y[:, idx, :], gates[:, 1, s, :], ALU.mult,
                         [("m_Y", idx, 0), ("m_Y", idx, 1), ("gates", 1, s)], [("m_Y", idx, 0), ("m_Y", idx, 1)])
                    P.tt("dve", xo[b][:], xo[b][:], Y[:, idx, :], ALU.add,
                         [ok, ("m_Y", idx, 0), ("m_Y", idx, 1)], [ok])
                    P.dma("sp", X[t * 128:(t + 1) * 128, :], xo[b][:], reads=[ok], writes=[xk(t)], semkey=ok)
                keys = ["m_hT", "m_wr", "m_wgu0", "m_wgu1", "m_wdn0", "m_wdn1", "m_sg0", "m_sg1", "m_rt",
                        "m_xo0", "m_xo1"]
                keys += [("m_a%d" % a, j) for a in range(2) for j in range(4)]
                keys += [("m_Y", idx, h) for idx in range(per) for h in range(2)]
                keys += [("m_Wt", idx) for idx in range(per)]
                for e_ in ("sp", "pe", "act", "dve", "pool"):
                    S.wait_all(e_, keys)


    def attention(i, j, ctx_out):
        QD = P.dscratch("QD%d" % i, [64, 16, T], BF16)
        with contextlib.ExitStack() as st:
            KT = P.sb(st, "a_KT", [128, 4, T], BF16)
            Vg = P.sb(st, "a_V", [128, NT, 4, 80], BF16)
            P.memset("dve", Vg[:], 0.0, ["a_V"])
            P.memset("dve", Vg[:, :, :, 64:66], 1.0, ["a_V"])
            with contextlib.ExitStack() as s1:
                hT = P.sb(s1, "a_hT", [128, 8, T], BF16)
                wqkv = P.sb(s1, "a_wqkv", [128, 8, 1536], BF16)
                gqk = P.sb(s1, "a_gqk", [128, 2, 64])
                P.dma("pool", wqkv[:], attn_w_qkv[j].rearrange("(k p) n -> p k n", p=128), writes=["a_wqkv"])
                P.dma("sp", gqk[:, 0, :], attn_qg[j:j + 1, :].broadcast_to([128, 64]), writes=["a_gqk"])
                P.dma("sp", gqk[:, 1, :], attn_kg[j:j + 1, :].broadcast_to([128, 64]), writes=["a_gqk"])
                with contextlib.ExitStack() as s2:
                    norm_tiles(s2, ALL, 0, hT, "a_hT", 0, tag="an")
                    drain(["an_xt0", "an_xt1", "an_junk", "an_st0", "an_st1", "an_h320", "an_h321"])
                qk = [P.sb(s1, "a_qk%d" % b, [128, 20, 64]) for b in range(2)]
                T4 = P.sb(s1, "a_T4", [128, 4 * 512])
                tmp = [T4[:, b * 512:(b + 1) * 512].rearrange("p (h i) -> p h i", i=32) for b in range(4)]
                sq = T4[:, 0:1280].rearrange("p (h d) -> p h d", d=64)
                qr = [P.sb(s1, "a_qr%d" % b, [128, 20, 64], BF16) for b in range(2)]
                ss = [P.sb(s1, "a_ss%d" % b, [128, 20]) for b in range(2)]
                cs = [P.sb(s1, "a_cs%d" % b, [128, 64]) for b in range(2)]
                tab = [P.sb(s1, "a_tab%d" % b, [128, 2, 4, 32]) for b in range(2)]
                QTt_ = P.sb(s1, "a_QTt0", [64, 16, 128], BF16)
                QTt = [QTt_, QTt_]
                gq4 = gqk[:].rearrange("p w (i two) -> p w i two", two=2)
                def qkv_stage(t):
                    b = t % 2
                    qkk, csk = "a_qk%d" % b, "a_cs%d" % b
                    P.dma("sp", cs[b][:], k_rope[t * 128:(t + 1) * 128, :], writes=[csk])
                    for nb in range(3):
                        for k in range(8):
                            P.mm(ps[nb][:, :], hT[:, k, t * 128:(t + 1) * 128], wqkv[:, k, nb * 512:(nb + 1) * 512],
                                 k == 0, k == 7, ["a_hT", "a_wqkv"], [pk(nb)])
                    P.cp("act", qk[b][:, 0:8, :], ps[0][:, :].rearrange("p (h d) -> p h d", d=64), [pk(0)], [qkk])
                    P.cp("act", qk[b][:, 8:16, :], ps[1][:, :].rearrange("p (h d) -> p h d", d=64), [pk(1)], [qkk])
                    P.cp("act", qk[b][:, 16:20, :], ps[2][:, 0:256].rearrange("p (h d) -> p h d", d=64), [pk(2)], [qkk])
                    P.cp("act", Vg[:, t, :, 0:64], ps[2][:, 256:512].rearrange("p (h d) -> p h d", d=64),
                         [pk(2)], [("a_V", t)])

                qkv_stage(0)
                for t in range(NT):
                    b = t % 2
                    qkk, ssk, csk, tabk, qrk, qtk = ("a_qk%d" % b, "a_ss%d" % b, "a_cs%d" % b, "a_tab%d" % b,
                                                     "a_qr%d" % b, "a_QTt0")
                    if t + 1 < NT:
                        qkv_stage(t + 1)
                    if cfg.get("a1_lvl", 9) < 2:
                        continue
                    P.tt("pool", sq, qk[b][:], qk[b][:], ALU.mult, [qkk], ["a_sq", "a_t0", "a_t1", "a_t2"])
                    P.red("dve", ss[b][:], sq, ALU.add, ["a_sq", "a_t0", "a_t1", "a_t2"], [ssk])
                    P.act(ss[b][:], ss[b][:], AF.Sqrt, [ssk], [ssk], bias=EPS, scale=1.0 / HD)
                    P.recip(ss[b][:], ss[b][:], [ssk], [ssk])
                    P.tt("dve", qk[b][:], qk[b][:], ss[b][:].unsqueeze(2).broadcast_to([128, 20, 64]), ALU.mult,
                         [qkk, ssk], [qkk])
                    if cfg.get("a1_lvl", 9) < 3:
                        continue
                    for w in range(2):
                        P.tt("pool", tab[b][:, w, 0, :], cs[b][:, 0:32], gq4[:, w, :, 0], ALU.mult, [csk, "a_gqk"], [tabk])
                        P.tt("pool", tab[b][:, w, 1, :], cs[b][:, 32:64], gq4[:, w, :, 1], ALU.mult, [csk, "a_gqk"], [tabk])
                        P.tt("pool", tab[b][:, w, 2, :], cs[b][:, 32:64], gq4[:, w, :, 0], ALU.mult, [csk, "a_gqk"], [tabk])
                        P.tt("pool", tab[b][:, w, 3, :], cs[b][:, 0:32], gq4[:, w, :, 1], ALU.mult, [csk, "a_gqk"], [tabk])
                    qk4 = qk[b][:].rearrange("p h (i two) -> p h i two", two=2)
                    qr4 = qr[b][:].rearrange("p h (i two) -> p h i two", two=2)
                    for w, (h0, h1) in enumerate(((0, 16), (16, 20))):
                        nh = h1 - h0
                        x0, x1 = qk4[:, h0:h1, :, 0], qk4[:, h0:h1, :, 1]
                        tb = lambda kind: tab[b][:, w, kind, :].unsqueeze(1).broadcast_to([128, nh, 32])
                        P.tt("pool", tmp[0][:, 0:nh, :], x0, tb(0), ALU.mult, [qkk, tabk, "a_sq"], ["a_t0"])
                        P.tt("dve", tmp[1][:, 0:nh, :], x1, tb(1), ALU.mult, [qkk, tabk, "a_sq"], ["a_t1"])
                        P.tt("pool", tmp[2][:, 0:nh, :], x0, tb(2), ALU.mult, [qkk, tabk, "a_sq"], ["a_t2"])
                        P.tt("dve", tmp[3][:, 0:nh, :], x1, tb(3), ALU.mult, [qkk, tabk], ["a_t3"])
                        P.tt("dve", qr4[:, h0:h1, :, 0], tmp[0][:, 0:nh, :], tmp[1][:, 0:nh, :], ALU.subtract,
                             ["a_t0", "a_t1"], [qrk])
                        P.tt("pool", qr4[:, h0:h1, :, 1], tmp[2][:, 0:nh, :], tmp[3][:, 0:nh, :], ALU.add,
                             ["a_t2", "a_t3"], [qrk])
                    if cfg.get("a1_lvl", 9) < 4:
                        continue
                    for hh in range(20):
                        bank = 3 + hh // 8
                        pv = ps[bank][:, :].bitcast(BF16)
                        P.tr(pv[0:64, (hh % 8) * 128:(hh % 8 + 1) * 128], qr[b][:, hh, :], identb[:],
                             [qrk, "identb"], [pk(bank)])
                    if cfg.get("a1_lvl", 9) < 5:
                        continue
                    P.cp("act", QTt[b][:, 0:8, :], ps[3][:, :].bitcast(BF16)[0:64, :].rearrange("p (h t) -> p h t", t=128),
                         [pk(3)], [qtk])
                    P.cp("act", QTt[b][:, 8:16, :], ps[4][:, :].bitcast(BF16)[0:64, :].rearrange("p (h t) -> p h t", t=128),
                         [pk(4)], [qtk])
                    P.cp("act", KT[0:64, :, t * 128:(t + 1) * 128],
                         ps[5][:, :].bitcast(BF16)[0:64, 0:512].rearrange("p (h t) -> p h t", t=128),
                         [pk(5)], [("a_KT", t)])
                    if cfg.get("a1_lvl", 9) < 6:
                        continue
                    P.dma("sp", QD[:, :, t * 128:(t + 1) * 128], QTt[b][:], reads=[qtk], writes=[("QD", t)], semkey=qtk)
                keys = ["a_hT", "a_wqkv", "a_gqk", "a_sq"] + ["a_t%d" % b for b in range(4)]
                for b in range(2):
                    keys += ["a_qk%d" % b, "a_ss%d" % b, "a_cs%d" % b, "a_tab%d" % b, "a_qr%d" % b, "a_QTt0"]
                drain(keys)
            with contextlib.ExitStack() as s1:
                if cfg.get("skip_a2"):
                    return
                wo = P.sb(s1, "a_wo", [64, 16, D], BF16)
                P.dma("pool", wo[:], attn_w_o[j].rearrange("(h d) n -> d h n", d=64), writes=["a_wo"])
                for kv_ in range(4):
                    P.memset("pool", KT[64:128, kv_, :], 0.0, ["a_KTz"])
                QTb = [P.sb(s1, "a_QTb%d" % b, [128, 16, 512], BF16) for b in range(2)]
                for b_ in range(2):
                    P.memset("pool", QTb[b_][64:128, :, :], 0.0, ["a_QTbz"])
                aT = [P.sb(s1, "a_aT%d" % b, [64, 16, 512], BF16) for b in range(2)]
                Pb = [P.sb(s1, "a_P%d" % b, [128, 512], BF16) for b in range(3)]
                rec = P.sb(s1, "a_rec", [65, 512])
                bcs = P.sb(s1, "a_bcs", [64, 512])
                xo = [P.sb(s1, "a_xo%d" % b, [128, D]) for b in range(2)]
                tm = [P.sb(s1, "a_tm%d" % b, [128, D]) for b in range(2)]
                qblocks = []
                if ctx_out:
                    qblocks.append((0, CTX, [0, 1]))
                for qb in range(SEQ // 512):
                    qblocks.append((CTX + qb * 512, 512, list(range(NT))))
                pcnt = 0
                xcnt = 0
                for bi, (qo, nq, ktiles) in enumerate(qblocks):
                    b = bi % 2
                    qbk, atk = "a_QTb%d" % b, "a_aT%d" % b
                    P.dma("sp", QTb[b][0:64, :, 0:nq], QD[:, :, qo:qo + nq],
                          reads=[("QD", t) for t in range(qo // 128, (qo + nq) // 128)], writes=[qbk], semkey=qbk)
                    items = [(h, idx, kt) for h in range(NH) for idx, kt in enumerate(ktiles)]
                    LOOK = 2

                    def emit_S(n):
                        h_, idx_, kt_ = items[n]
                        P.mm(ps[n % 3][:, 0:nq], KT[:, h_ // 4, kt_ * 128:(kt_ + 1) * 128], QTb[b][:, h_, 0:nq], True, True,
                             [("a_KT", kt_), "a_KTz", "a_QTbz", qbk], [pk(n % 3)])

                    def fin1(h_):
                        ob_ = 3 + h_ % 2
                        P.recip(rec[64:65, 0:nq], ps[ob_][64:65, 0:nq], [pk(ob_)], ["a_rec"])

                    def fin2(h_):
                        ob_ = 3 + h_ % 2
                        P.mm(ps[5][0:64, 0:nq], ones[64:65, 0:64], rec[64:65, 0:nq], True, True, ["ones", "a_rec"], [pk(5)])
                        P.cp("dve", bcs[:, 0:nq], ps[5][0:64, 0:nq], [pk(5)], ["a_bcs"])
                        P.tt("dve", aT[b][:, h_, 0:nq], ps[ob_][0:64, 0:nq], bcs[:, 0:nq], ALU.mult,
                             [pk(ob_), "a_bcs"], [(atk, h_)])

                    for n in range(min(LOOK, len(items))):
                        emit_S(n)
                    pend = None
                    for n, (h, idx, kt) in enumerate(items):
                        if n + LOOK < len(items):
                            emit_S(n + LOOK)
                        kv = h // 4
                        ob = 3 + h % 2
                        pb = n % 3
                        P.act(Pb[pb][:, 0:nq], ps[n % 3][:, 0:nq], AF.Exp, [pk(n % 3)], ["a_P%d" % pb], scale=HD ** -0.5)
                        P.mm(ps[ob][0:65, 0:nq], Vg[:, kt, kv, 0:65], Pb[pb][:, 0:nq], idx == 0, idx == len(ktiles) - 1,
                             [("a_V", kt), "a_V", "a_P%d" % pb], [pk(ob)])
                        if pend is not None and (idx == min(3, len(ktiles) - 1)):
                            fin2(pend)
                            pend = None
                        if idx == len(ktiles) - 1:
                            fin1(h)
                            pend = h
                    if pend is not None:
                        fin2(pend)
                    for tt_ in range(nq // 128):
                        t = qo // 128 + tt_
                        s = 1 if t < 2 else 0
                        xb = xcnt % 2
                        xcnt += 1
                        xok, tmk = "a_xo%d" % xb, "a_tm%d" % xb
                        P.dma("sp", xo[xb][:], X[t * 128:(t + 1) * 128, :], reads=[xk(t)], writes=[xok], semkey=xok)
                        for half in range(2):
                            bank = 6 + half
                            for h in range(NH):
                                P.mm(ps[bank][:, :], aT[b][:, h, tt_ * 128:(tt_ + 1) * 128],
                                     wo[:, h, half * 512:(half + 1) * 512], h == 0, h == NH - 1,
                                     [(atk, h), "a_wo"], [pk(bank)])
                            P.tt("dve", tm[xb][:, half * 512:(half + 1) * 512], ps[bank][:, :],
                                 gates[:, 0, s, half * 512:(half + 1) * 512], ALU.mult,
                                 [pk(bank), ("gates", 0, s)], [tmk])
                        P.tt("pool", xo[xb][:], xo[xb][:], tm[xb][:], ALU.add, [xok, tmk], [xok])
                        P.dma("sp", X[t * 128:(t + 1) * 128, :], xo[xb][:], reads=[xok], writes=[xk(t)], semkey=xok)
                if dbg is not None:
                    dt_ = tm[0]
                    P.cp("dve", dt_[:, 0:512], KT[:, 0, 0:512], ["a_KTz"] + [("a_KT", t) for t in range(4)], ["a_dbgt", "a_tm0"])
                    P.cp("dve", dt_[:, 512:1024], QTb[0][:, 0, 0:512], ["a_QTbz", "a_QTb0"], ["a_dbgt", "a_tm0"])
                    P.dma("sp", dbg, dt_[:], reads=["a_dbgt"], writes=["dbg"])
                    drain(["a_dbgt", "a_tm0"])
                keys = ["a_wo", "a_rec", "a_bcs", "a_V", "a_KTz", "a_QTbz"] + ["a_P%d" % b for b in range(3)]
                for b in range(2):
                    keys += ["a_QTb%d" % b, "a_xo%d" % b, "a_tm%d" % b] + [("a_aT%d" % b, h) for h in range(NH)]
                keys += [("a_KT", t) for t in range(NT)] + [("a_V", t) for t in range(NT)]
                drain(keys)


    def pool_mixer(i):
        with contextlib.ExitStack() as st:
            bcm = P.sb(st, "p_bcm", [128, 2, 2, D])
            gmb = P.sb(st, "p_gmb", [128, 2, D])
            psg = P.sb(st, "p_psg", [128, 2, D])
            band = P.sb(st, "p_band", [128, 4, 5, 128])
            wp = P.sb(st, "p_wp", [128, 4, 2, 256])
            P.dma("sp", band[:], k_band.rearrange("w k p n -> p w k n"), writes=["p_band"])
            P.dma("sp", wp[:], pool_w[0].rearrange("g (cc p) n -> p g cc n", p=128), writes=["p_wp"])
            with contextlib.ExitStack() as s1:
                wblk = [P.sb(s1, "p_wblk%d" % b, [128, 8, 512]) for b in range(2)]
                cbc = P.sb(s1, "p_cbc", [128, 2, 8, 128])
                brow = P.sb(s1, "p_brow", [1, 2 * D])
                gb = P.sb(s1, "p_gb", [128, D])
                psb = P.sb(s1, "p_psb", [128, D])
                P.dma("sp", brow[:], b_ada[i][0:1, 0:2 * D], writes=["p_brow"])
                P.dma("sp", gb[:], norm_mix_g[i:i + 1, :].broadcast_to([128, D]), writes=["p_gb"])
                P.dma("sp", psb[:], pool_scale[0:1, :].broadcast_to([128, D]), writes=["p_psb"])
                for s_ in range(2):
                    P.cp("dve", cbc[:, s_], cT[:, :, s_:s_ + 1].broadcast_to([128, 8, 128]), ["cT"], ["p_cbc"])
                for n in range(4):
                    wb, wkey = wblk[n % 2], "p_wblk%d" % (n % 2)
                    P.dma("sp", wb[:], w_ada[i][:, n * 512:(n + 1) * 512].rearrange("(k p) n -> p k n", p=128),
                          writes=[wkey])
                    for s_ in range(2):
                        bank = 1 + s_
                        for k in range(8):
                            P.mm(ps[bank][:, :], cbc[:, s_, k, :], wb[:, k, :], k == 0, False, [wkey, "p_cbc"], [pk(bank)])
                        P.mm(ps[bank][:, :], ones[0:1, :], brow[0:1, n * 512:(n + 1) * 512], False, True,
                             ["ones", "p_brow"], [pk(bank)])
                        P.cp("act", bcm[:, s_, n // 2, (n % 2) * 512:(n % 2 + 1) * 512], ps[bank][:, :],
                             [pk(bank)], ["p_bcm"])
                for s_ in range(2):
                    P.stt("dve", gmb[:, s_, :], bcm[:, s_, 1, :], 1.0, gb[:], ALU.add, ALU.mult, ["p_bcm", "p_gb"], ["p_gmb"])
                    P.tt("pool", psg[:, s_, :], psb[:], gates[:, 0, s_, :], ALU.mult, ["p_psb", ("gates", 0, s_)], ["p_psg"])
                drain(["p_wblk0", "p_wblk1", "p_cbc", "p_brow", "p_gb", "p_psb"])
            xt = [P.sb(st, "p_x%d" % b, [128, D]) for b in range(4)]
            hh = [P.sb(st, "p_h%d" % b, [128, D]) for b in range(4)]
            dT = [P.sb(st, "p_dT%d" % b, [128, 8, 128]) for b in range(2)]
            tm = [P.sb(st, "p_tm%d" % b, [128, D]) for b in range(2)]
            st8 = [P.sb(st, "p_st%d" % b, [128, 4]) for b in range(4)]
            junk = P.sb(st, "p_junk", [128, D], BF16)

            def compute_h(t):
                b = t % 4
                s_ = 1 if t < 2 else 0
                xkey, hkey, skey = "p_x%d" % b, "p_h%d" % b, "p_st%d" % b
                P.dma("sp", xt[b][:], X[t * 128:(t + 1) * 128, :], reads=[xk(t)], writes=[xkey], semkey=xkey)
                P.act(junk[:], xt[b][:], AF.Square, [xkey], ["p_junk", skey], accum_out=st8[b][:, 0:1])
                P.act(st8[b][:, 1:2], st8[b][:, 0:1], AF.Sqrt, [skey], [skey], bias=EPS, scale=1.0 / D)
                P.recip(st8[b][:, 2:3], st8[b][:, 1:2], [skey], [skey])
                P.stt("dve", hh[b][:], xt[b][:], st8[b][:, 2:3], gmb[:, s_, :], ALU.mult, ALU.mult,
                      [xkey, skey, "p_gmb"], [hkey])
                P.tt("pool", hh[b][:], hh[b][:], bcm[:, s_, 0, :], ALU.add, [hkey, "p_bcm"], [hkey])

            done = set()
            for t in range(NT):
                first = t in (0, 2)
                last = t in (1, NT - 1)
                need = [t] + ([] if first else [t - 1]) + ([] if last else [t + 1])
                for tt_ in sorted(need):
                    if tt_ not in done:
                        compute_h(tt_)
                        done.add(tt_)
                s_ = 1 if t < 2 else 0
                db = t % 2
                dkey, tmk = "p_dT%d" % db, "p_tm%d" % db
                for c in range(8):
                    wi = c // 2
                    bank = c // 4
                    col = (c % 4) * 128
                    srcs = []
                    if not first:
                        srcs.append((t - 1, 0))
                    srcs.append((t, 1 if first else (3 if last else 2)))
                    if not last:
                        srcs.append((t + 1, 4))
                    for si, (tt_, kind) in enumerate(srcs):
                        P.mm(ps[bank][:, col:col + 128], hh[tt_ % 4][:, c * 128:(c + 1) * 128], band[:, wi, kind, :],
                             si == 0, si == len(srcs) - 1, ["p_h%d" % (tt_ % 4), "p_band"], [pk(bank)])
                for bank in range(2):
                    P.cp("act", dT[db][:, bank * 4:(bank + 1) * 4, :], ps[bank][:, :].rearrange("p (c t) -> p c t", t=128),
                         [pk(bank)], [dkey])
                for g in range(4):
                    bank = 2 + g // 2
                    for cc in range(2):
                        P.mm(ps[bank][:, (g % 2) * 256:(g % 2 + 1) * 256], dT[db][:, 2 * g + cc, :], wp[:, g, cc, :],
                             cc == 0, cc == 1, [dkey, "p_wp"], [pk(bank)])
                xb = t % 4
                for half in range(2):
                    P.tt("dve", tm[db][:, half * 512:(half + 1) * 512], ps[2 + half][:, :],
                         psg[:, s_, half * 512:(half + 1) * 512], ALU.mult, [pk(2 + half), "p_psg"], [tmk])
                P.tt("pool", tm[db][:], tm[db][:], xt[xb][:], ALU.add, [tmk, "p_x%d" % xb], [tmk])
                P.dma("sp", X[t * 128:(t + 1) * 128, :], tm[db][:], reads=[tmk], writes=[xk(t)], semkey=tmk)
            keys = ["p_bcm", "p_gmb", "p_psg", "p_band", "p_wp", "p_junk", "p_dT0", "p_dT1", "p_tm0", "p_tm1"]
            for b in range(4):
                keys += ["p_x%d" % b, "p_h%d" % b, "p_st%d" % b]
            drain(keys)


    def ssd_mixer(i):
        XT = P.dscratch("s_XT", [T, SSM_DI], BF16)
        BTK = P.dscratch("s_BTK", [T, 512], BF16)
        BF = P.dscratch("s_BF", [4, 128, T], BF16)
        CF = P.dscratch("s_CF", [4, 128, T], BF16)
        Yd = P.dscratch("s_Yd", [2, T, SSM_DI])
        w_in = ssm_w_in[0]
        NU = T + 3

        def xcol(t):
            return t * 128 if t < 2 else 259 + (t - 2) * 128

        ZG = P.dscratch("s_ZG", [T, SSM_DI], BF16)
        with contextlib.ExitStack() as st:
            with contextlib.ExitStack() as sA:
                dtA = P.sb(sA, "s_dtA", [128, NT, 64])
                LA = P.sb(sA, "s_LA", [128, NT, 64])
                sH = contextlib.ExitStack()
                hT = P.sb(sH, "s_hT", [128, 8, T], BF16)
                with contextlib.ExitStack() as s1:
                    wdt = P.sb(s1, "s_wdt", [128, 8, 64])
                    dtb = P.sb(s1, "s_dtb", [128, 64])
                    aB = P.sb(s1, "s_aB", [128, 64])
                    sp_ = P.sb(s1, "s_sp", [128, 4, 64])
                    P.dma("sp", wdt[:], w_in[:, 5120:5184].rearrange("(k p) n -> p k n", p=128), writes=["s_wdt"])
                    P.dma("sp", dtb[:], ssm_dt_bias[0:1, :].broadcast_to([128, 64]), writes=["s_dtb"])
                    P.dma("sp", aB[:], ssm_a_log[0:1, :].broadcast_to([128, 64]), writes=["s_aB"])
                    P.act(aB[:], aB[:], AF.Exp, ["s_aB"], ["s_aB"])
                    P.ts("dve", aB[:], aB[:], -1.0, ALU.mult, ["s_aB"], ["s_aB"])

                    def dthook(idx, t, h32, hkey):
                        for k in range(8):
                            P.mm(ps[5][:, 0:64], h32[:, k, :], wdt[:, k, :], k == 0, k == 7, [hkey, "s_wdt"], [pk(5)])
                        K_ = "s_sp"
                        xr, ab, ee = sp_[:, 0, :], sp_[:, 1, :], sp_[:, 2, :]
                        P.tt("dve", xr, ps[5][:, 0:64], dtb[:], ALU.add, [pk(5), "s_dtb"], [K_])
                        P.ts("dve", sp_[:, 3, :], xr, -1.0, ALU.mult, [K_], [K_])
                        P.tt("dve", ab, xr, sp_[:, 3, :], ALU.max, [K_], [K_])
                        P.act(ee, ab, AF.Exp, [K_], [K_], scale=-1.0)
                        P.act(ee, ee, AF.Ln, [K_], [K_], bias=1.0, scale=1.0)
                        P.stt("dve", dtA[:, t, :], xr, 0.0, ee, ALU.max, ALU.add, [K_], [("s_dtA", t)])
                        P.tt("pool", LA[:, t, :], dtA[:, t, :], aB[:], ALU.mult, [("s_dtA", t), "s_aB"], [("s_LA", t)])

                    with contextlib.ExitStack() as s2:
                        norm_tiles(s2, ALL, 0, hT, "s_hT", 0, hook=dthook, tag="sn")
                        drain(["sn_xt0", "sn_xt1", "sn_junk", "sn_st0", "sn_st1", "sn_h320", "sn_h321"])
                    drain(["s_wdt", "s_dtb", "s_sp"])
                with contextlib.ExitStack() as s1:
                    cwA = P.sb(s1, "s_cwA", [128, 24, 4])
                    cbA = P.sb(s1, "s_cbA", [128, 24])
                    U2 = [P.sb(s1, "s_U%d" % b, [128, T + 8]) for b in range(2)]
                    acc = P.sb(s1, "s_acc", [128, NU])
                    xc = [P.sb(s1, "s_xc%d" % b, [128, NU], BF16) for b in range(2)]
                    wc = [P.sb(s1, "s_wc%d" % b, [128, 8, 128], BF16) for b in range(2)]
                    stg = [P.sb(s1, "s_stg%d" % b, [128, NT, 128], BF16) for b in range(2)]
                    for k in range(4):
                        P.dma("sp", cwA[:, :, k], ssm_conv_w[0, k].rearrange("(c p) -> p c", p=128), writes=["s_cwA"],
                              allow_slow_non_contiguous=True)
                    P.dma("sp", cbA[:], ssm_conv_b[0].rearrange("(c p) -> p c", p=128), writes=["s_cbA"],
                          allow_slow_non_contiguous=True)
                    for b_ in range(2):
                        P.memset("dve" if b_ == 0 else "pool", U2[b_][:], 0.0, ["s_U%d" % b_])
                    blocks = [(0, 256, 2)] + [(256 + b * 512, 512, 261 + b * 512) for b in range(8)]
                    mcnt = [0]

                    def inproj(cc):
                        b = cc % 2
                        wck = "s_wc%d" % b
                        U, uk = U2[b], "s_U%d" % b
                        P.dma("pool", wc[b][:], w_in[:, 2048 + cc * 128:2048 + (cc + 1) * 128].rearrange("(k p) n -> p k n", p=128),
                              writes=[wck])
                        for (t0, n_, uo) in blocks:
                            bank = mcnt[0] % 2
                            mcnt[0] += 1
                            for k in range(8):
                                P.mm(ps[bank][:, 0:n_], wc[b][:, k, :], hT[:, k, t0:t0 + n_], k == 0, k == 7,
                                     [wck, "s_hT"], [pk(bank)])
                            P.cp("act", U[:, uo:uo + n_], ps[bank][:, 0:n_], [pk(bank)], [uk])

                    inproj(0)
                    for cc in range(24):
                        b = cc % 2
                        wck, xck, stk = "s_wc%d" % b, "s_xc%d" % b, "s_stg%d" % b
                        U, uk = U2[b], "s_U%d" % b
                        if cc + 1 < 24:
                            inproj(cc + 1)
                        ce = "dve"
                        P.ts(ce, acc[:], U[:, 0:NU], cwA[:, cc, 0:1], ALU.mult, [uk, "s_cwA"], ["s_acc"])
                        for k in range(1, 4):
                            P.stt(ce, acc[:], U[:, k:k + NU], cwA[:, cc, k:k + 1], acc[:], ALU.mult, ALU.add,
                                  [uk, "s_cwA", "s_acc"], ["s_acc"])
                        P.act(xc[b][:], acc[:], AF.Silu, ["s_acc", "s_cbA"], [xck], bias=cbA[:, cc:cc + 1], scale=1.0)
                        if cc >= 16:
                            g = (cc - 16) % 4
                            dst = BF if cc < 20 else CF
                            dk = "BF" if cc < 20 else "CF"
                            P.dma("sp", dst[g, :, 0:CTX], xc[b][:, 0:CTX], reads=[xck], writes=[(dk, g)], semkey=xck)
                            P.dma("sp", dst[g, :, CTX:T], xc[b][:, 259:259 + SEQ], reads=[xck], writes=[(dk, g)], semkey=xck)
                        if cc < 20:
                            for t in range(NT):
                                bank = 2 + (t // 8) % 4
                                pv = ps[bank][:, :].bitcast(BF16)
                                P.tr(pv[:, (t % 8) * 128:(t % 8 + 1) * 128], xc[b][:, xcol(t):xcol(t) + 128], identb[:],
                                     [xck, "identb"], [pk(bank)])
                                if t % 8 == 7 or t == NT - 1:
                                    t0 = (t // 8) * 8
                                    nt_ = t - t0 + 1
                                    P.cp("act", stg[b][:, t0:t0 + nt_, :],
                                         pv[:, 0:nt_ * 128].rearrange("p (t c) -> p t c", c=128), [pk(bank)], [stk])
                            if cc < 16:
                                dv = XT.rearrange("(t p) c -> p t c", p=128)[:, :, cc * 128:(cc + 1) * 128]
                                wk = ("XT", cc)
                            else:
                                dv = BTK.rearrange("(t p) c -> p t c", p=128)[:, :, (cc - 16) * 128:(cc - 15) * 128]
                                wk = ("BTK", cc - 16)
                            P.dma("sp", dv[:, 0:17, :], stg[b][:, 0:17, :], reads=[stk], writes=[wk], semkey=stk)
                            P.dma("sp", dv[:, 17:NT, :], stg[b][:, 17:NT, :], reads=[stk], writes=[wk], semkey=stk)
                    drain(["s_cwA", "s_cbA", "s_U0", "s_U1", "s_acc", "s_xc0", "s_xc1", "s_wc0", "s_wc1", "s_stg0", "s_stg1"])
                with contextlib.ExitStack() as s1:
                    wz = P.sb(s1, "s_wz", [128, 8, SSM_DI], BF16)
                    szb = [P.sb(s1, "s_szb%d" % b, [128, SSM_DI], BF16) for b in range(2)]
                    P.dma("pool", wz[:], w_in[:, 0:SSM_DI].rearrange("(k p) n -> p k n", p=128), writes=["s_wz"])
                    for t in range(NT):
                        b = t % 2
                        for nb in range(4):
                            bank = (t * 4 + nb) % 8
                            for k in range(8):
                                P.mm(ps[bank][:, :], hT[:, k, t * 128:(t + 1) * 128], wz[:, k, nb * 512:(nb + 1) * 512],
                                     k == 0, k == 7, ["s_hT", "s_wz"], [pk(bank)])
                            P.act(szb[b][:, nb * 512:(nb + 1) * 512], ps[bank][:, :], AF.Silu, [pk(bank)], ["s_szb%d" % b])
                        P.dma("sp", ZG[t * 128:(t + 1) * 128, :], szb[b][:], reads=["s_szb%d" % b], writes=[("ZG", t)],
                              semkey="s_szb%d" % b)
                    drain(["s_wz", "s_szb0", "s_szb1", "s_hT"])
                sH.close()
                with contextlib.ExitStack() as s1:
                    tri = P.sb(s1, "s_tri", [128, 4, 128])
                    state = P.sb(s1, "s_state", [128, 32, 64])
                    stbf = P.sb(s1, "s_stbf", [128, 32, 64], BF16)
                    xk_ = [P.sb(s1, "s_xk%d" % b, [128, 32, 64], BF16) for b in range(2)]
                    btk = [P.sb(s1, "s_btk%d" % b, [128, 512], BF16) for b in range(2)]
                    bfc = [P.sb(s1, "s_bfc%d" % b, [128, 4, 128], BF16) for b in range(2)]
                    cfc = [P.sb(s1, "s_cfc%d" % b, [128, 4, 128], BF16) for b in range(2)]
                    xdt = P.sb(s1, "s_xdt", [128, 32, 64], BF16)
                    xsd = P.sb(s1, "s_xsd", [128, 32, 64], BF16)
                    laB = P.sb(s1, "s_laB", [128, 32, 128])
                    CM = P.sb(s1, "s_CM", [128, 32, 128])
                    sm = P.sb(s1, "s_sm", [128, 6, 32])
                    GTs = [P.sb(s1, "s_GTs%d" % b, [128, 128]) for b in range(2)]
                    Lg = [P.sb(s1, "s_Lg%d" % b, [128, 4, 128]) for b in range(2)]
                    WT = [P.sb(s1, "s_WT%d" % b, [128, 4, 128], BF16) for b in range(2)]
                    yoff = [P.sb(s1, "s_yoff%d" % b, [128, 8, 64]) for b in range(2)]
                    ybuf = [P.sb(s1, "s_ybuf%d" % b, [128, 32, 64]) for b in range(2)]
                    P.dma("sp", tri[:], k_tri.rearrange("w p n -> p w n"), writes=["s_tri"])
                    ccount = 0
                    for d in range(2):
                        order = list(range(NT)) if d == 0 else [1, 0] + list(range(NT - 1, 1, -1))
                        Ut, Mneg = tri[:, 2 * d, :], tri[:, 2 * d + 1, :]
                        P.memset("pool", state[:], 0.0, [("s_state", g_) for g_ in range(4)])
                        for c in order:
                            b = ccount % 2
                            ccount += 1
                            xkk, btkk, bfk, cfk, ybk = "s_xk%d" % b, "s_btk%d" % b, "s_bfc%d" % b, "s_cfc%d" % b, "s_ybuf%d" % b
                            P.dma("sp", xk_[b][:], XT[c * 128:(c + 1) * 128, :].rearrange("p (h d) -> p h d", d=64),
                                  reads=[("XT", q) for q in range(16)], writes=[xkk], semkey=xkk)
                            P.dma("sp", btk[b][:], BTK[c * 128:(c + 1) * 128, :], reads=[("BTK", q) for q in range(4)],
                                  writes=[btkk], semkey=btkk)
                            P.dma("sp", bfc[b][:], BF[:, :, c * 128:(c + 1) * 128].rearrange("g n t -> n g t"),
                                  reads=[("BF", q) for q in range(4)], writes=[bfk], semkey=bfk)
                            P.dma("sp", cfc[b][:], CF[:, :, c * 128:(c + 1) * 128].rearrange("g n t -> n g t"),
                                  reads=[("CF", q) for q in range(4)], writes=[cfk], semkey=cfk)
                            la_c = LA[:, c, d * 32:(d + 1) * 32]
                            dt_c = dtA[:, c, d * 32:(d + 1) * 32]
                            lak, dtk = ("s_LA", c), ("s_dtA", c)
                            SM = "s_sm"
                            csc, tot, ff, fx, ecs, dec = (sm[:, q, :] for q in range(6))
                            P.mm(ps[0][:, 0:32], Ut, la_c, True, True, ["s_tri", lak], [pk(0)])
                            P.mm(ps[0][:, 32:64], ones[:], la_c, True, True, ["ones", lak], [pk(0)])
                            P.cp("dve", sm[:, 0:2, :], ps[0][:, 0:64].rearrange("p (a h) -> p a h", h=32), [pk(0)], [SM])
                            P.tt("dve", ff, tot, csc, ALU.subtract, [SM], [SM])
                            P.act(ff, ff, AF.Exp, [SM], [SM])
                            P.act(ecs, csc, AF.Exp, [SM], [SM])
                            P.act(dec, tot, AF.Exp, [SM], [SM])
                            P.tt("dve", fx, ff, dt_c, ALU.mult, [SM, dtk], [SM])
                            P.tt("pool", xdt[:], xk_[b][:], dt_c.unsqueeze(2).broadcast_to([128, 32, 64]), ALU.mult,
                                 [xkk, dtk], ["s_xdt"])
                            P.tt("pool", xsd[:], xk_[b][:], fx.unsqueeze(2).broadcast_to([128, 32, 64]), ALU.mult,
                                 [xkk, SM], ["s_xsd"])
                            P.cp("dve", laB[:], la_c.unsqueeze(2).broadcast_to([128, 32, 128]), [lak], ["s_laB"])
                            P.cp("act", stbf[:], state[:], [("s_state", g_) for g_ in range(4)], ["s_stbf"])
                            P.tt("dve", CM[:], csc.unsqueeze(2).broadcast_to([128, 32, 128]),
                                 Mneg.unsqueeze(1).broadcast_to([128, 32, 128]), ALU.subtract, [SM, "s_tri"], ["s_CM"])
                            def grp_begin(g):
                                P.mm(ps[1][:, 0:128], bfc[b][:, g, :], cfc[b][:, g, :], True, True, [bfk, cfk], [pk(1)])
                                P.cp("act", GTs[g % 2][:], ps[1][:, 0:128], [pk(1)], ["s_GTs%d" % (g % 2)])
                                P.mm(ps[2][:, :], cfc[b][:, g, :], stbf[:, g * 8:(g + 1) * 8, :].rearrange("p h d -> p (h d)"),
                                     True, True, [cfk, "s_stbf"], [pk(2)])
                                P.cp("act", yoff[g % 2][:], ps[2][:, :].rearrange("p (h d) -> p h d", d=64), [pk(2)],
                                     ["s_yoff%d" % (g % 2)])

                            def quad_cs(n):
                                g, q = n // 2, n % 2
                                if q == 0:
                                    grp_begin(g)
                                cb_ = 3 + n % 2
                                for j in range(4):
                                    P.mm(ps[cb_][:, j * 128:(j + 1) * 128], laB[:, g * 8 + q * 4 + j, :], Ut, True, True,
                                         ["s_laB", "s_tri"], [pk(cb_)])

                            def grp_end(g):
                                yb_ = 5 + g % 2
                                yo, yok = yoff[g % 2], "s_yoff%d" % (g % 2)
                                P.tt("pool", yo[:], yo[:], ecs[:, g * 8:(g + 1) * 8].unsqueeze(2).broadcast_to([128, 8, 64]),
                                     ALU.mult, [yok, SM], [yok])
                                P.tt("dve", ybuf[b][:, g * 8:(g + 1) * 8, :], yo[:],
                                     ps[yb_][:, :].rearrange("p (h d) -> p h d", d=64), ALU.add, [yok, pk(yb_)], [ybk])
                                P.mm(ps[7][:, :], btk[b][:, g * 128:(g + 1) * 128],
                                     xsd[:, g * 8:(g + 1) * 8, :].rearrange("p h d -> p (h d)"), True, True,
                                     [btkk, "s_xsd"], [pk(7)])
                                P.tt("pool", state[:, g * 8:(g + 1) * 8, :], state[:, g * 8:(g + 1) * 8, :],
                                     dec[:, g * 8:(g + 1) * 8].unsqueeze(2).broadcast_to([128, 8, 64]), ALU.mult,
                                     [("s_state", g), SM], [("s_state", g)])
                                P.tt("dve", state[:, g * 8:(g + 1) * 8, :], state[:, g * 8:(g + 1) * 8, :],
                                     ps[7][:, :].rearrange("p (h d) -> p h d", d=64), ALU.add, [("s_state", g), pk(7)],
                                     [("s_state", g)])

                            quad_cs(0)
                            for n in range(8):
                                g, q = n // 2, n % 2
                                h0 = g * 8 + q * 4
                                if n + 1 < 8:
                                    quad_cs(n + 1)
                                cb_ = 3 + n % 2
                                lb = n % 2
                                lgk, wtk = "s_Lg%d" % lb, "s_WT%d" % lb
                                csv = ps[cb_][:, :].rearrange("p (j l) -> p j l", l=128)
                                P.tt("dve", Lg[lb][:], csv, CM[:, h0:h0 + 4, :], ALU.subtract, [pk(cb_), "s_CM"], [lgk])
                                P.act(Lg[lb][:], Lg[lb][:], AF.Exp, [lgk], [lgk])
                                P.tt("dve", WT[lb][:], Lg[lb][:], GTs[g % 2][:].unsqueeze(1).broadcast_to([128, 4, 128]),
                                     ALU.mult, [lgk, "s_GTs%d" % (g % 2)], [wtk])
                                yb_ = 5 + g % 2
                                for j in range(4):
                                    P.mm(ps[yb_][:, (q * 4 + j) * 64:(q * 4 + j + 1) * 64], WT[lb][:, j, :], xdt[:, h0 + j, :],
                                         True, True, [wtk, "s_xdt"], [pk(yb_)])
                                if q == 1:
                                    grp_end(g)
                            P.dma("sp", Yd[d, c * 128:(c + 1) * 128, :], ybuf[b][:].rearrange("p h d -> p (h d)"),
                                  reads=[ybk], writes=[("Yd", d, c)], semkey=ybk)
                    keys = ["s_tri", "s_stbf", "s_xdt", "s_xsd", "s_laB", "s_CM", "s_sm", "s_GTs0", "s_GTs1", "s_yoff0", "s_yoff1"]
                    keys += [("s_state", g_) for g_ in range(4)]
                    for b in range(2):
                        keys += ["s_xk%d" % b, "s_btk%d" % b, "s_bfc%d" % b, "s_cfc%d" % b, "s_Lg%d" % b, "s_WT%d" % b,
                                 "s_ybuf%d" % b]
                    keys += [("s_dtA", t) for t in range(NT)] + [("s_LA", t) for t in range(NT)] + ["s_aB"]
                    drain(keys)
            with contextlib.ExitStack() as s1:
                wo = P.sb(s1, "s_wo", [128, 16, D], BF16)
                ng = P.sb(s1, "s_ng", [128, SSM_DI])
                dsk = P.sb(s1, "s_dsk", [128, 64])
                yf = [P.sb(s1, "s_yf%d" % b, [128, 32, 64]) for b in range(2)]
                yb2 = [P.sb(s1, "s_yb2%d" % b, [128, 32, 64]) for b in range(2)]
                xk3 = [P.sb(s1, "s_xk3%d" % b, [128, 32, 64], BF16) for b in range(2)]
                zg = [P.sb(s1, "s_zg%d" % b, [128, SSM_DI], BF16) for b in range(2)]
                xo = [P.sb(s1, "s_xo%d" % b, [128, D]) for b in range(2)]
                sz_ = [P.sb(s1, "s_sz%d" % b, [128, SSM_DI]) for b in range(2)]
                gbf_ = [P.sb(s1, "s_gbf%d" % b, [128, SSM_DI], BF16) for b in range(2)]
                gT = [P.sb(s1, "s_gT%d" % b, [128, 16, 128], BF16) for b in range(2)]
                g4_ = [P.sb(s1, "s_g4%d" % b, [128, 12]) for b in range(2)]
                junk = P.sb(s1, "s_junk3", [128, 512], BF16)
                tm = [P.sb(s1, "s_tm%d" % b, [128, D]) for b in range(2)]
                P.dma("pool", wo[:], ssm_w_out[0].rearrange("(k p) n -> p k n", p=128), writes=["s_wo"])
                P.dma("sp", ng[:], ssm_norm_g[0:1, :].broadcast_to([128, SSM_DI]), writes=["s_ng"])
                P.dma("sp", dsk[:], ssm_d[0:1, :].broadcast_to([128, 64]), writes=["s_dsk"])
                P.tt("dve", dsk[:, 0:32], dsk[:, 0:32], dsk[:, 32:64], ALU.add, ["s_dsk"], ["s_dsk"])

                def s3load(t):
                    b = t % 2
                    P.dma("sp", yf[b][:], Yd[0, t * 128:(t + 1) * 128, :].rearrange("p (h d) -> p h d", d=64),
                          reads=[("Yd", 0, t)], writes=["s_yf%d" % b], semkey="s_yf%d" % b)
                    P.dma("sp", yb2[b][:], Yd[1, t * 128:(t + 1) * 128, :].rearrange("p (h d) -> p h d", d=64),
                          reads=[("Yd", 1, t)], writes=["s_yb2%d" % b], semkey="s_yb2%d" % b)
                    P.dma("sp", xk3[b][:], XT[t * 128:(t + 1) * 128, :].rearrange("p (h d) -> p h d", d=64),
                          reads=[("XT", q) for q in range(16)], writes=["s_xk3%d" % b], semkey="s_xk3%d" % b)
                    P.dma("sp", zg[b][:], ZG[t * 128:(t + 1) * 128, :], reads=[("ZG", t)], writes=["s_zg%d" % b],
                          semkey="s_zg%d" % b)
                    P.dma("sp", xo[b][:], X[t * 128:(t + 1) * 128, :], reads=[xk(t)], writes=["s_xo%d" % b], semkey="s_xo%d" % b)

                s3load(0)
                for t in range(NT):
                    if t + 1 < NT:
                        s3load(t + 1)
                    b = t % 2
                    s_ = 1 if t < 2 else 0
                    yfk, ybk, xkk, zgk, xok, gtk, tmk = ("s_yf%d" % b, "s_yb2%d" % b, "s_xk3%d" % b, "s_zg%d" % b, "s_xo%d" % b,
                                                         "s_gT%d" % b, "s_tm%d" % b)
                    P.tt("dve", yf[b][:], yf[b][:], yb2[b][:], ALU.add, [yfk, ybk], [yfk])
                    P.tt("pool", yb2[b][:], xk3[b][:], dsk[:, 0:32].unsqueeze(2).broadcast_to([128, 32, 64]), ALU.mult,
                         [xkk, "s_dsk"], [ybk])
                    P.tt("pool", yf[b][:], yf[b][:], yb2[b][:], ALU.add, [yfk, ybk], [yfk])
                    sz, gbf, g4 = sz_[b], gbf_[b], g4_[b]
                    szk, gbk, g4k = "s_sz%d" % b, "s_gbf%d" % b, "s_g4%d" % b
                    yfl = yf[b][:].rearrange("p h d -> p (h d)")
                    P.tt("dve", sz[:], zg[b][:], yfl, ALU.mult, [zgk, yfk], [szk])
                    for q in range(4):
                        P.act(junk[:], sz[:, q * 512:(q + 1) * 512], AF.Square, [szk], ["s_junk3", g4k],
                              accum_out=g4[:, q:q + 1])
                    P.act(g4[:, 4:8], g4[:, 0:4], AF.Sqrt, [g4k], [g4k], bias=EPS, scale=1.0 / 512)
                    P.recip(g4[:, 8:12], g4[:, 4:8], [g4k], [g4k])
                    for q in range(4):
                        P.stt("dve", gbf[:, q * 512:(q + 1) * 512], sz[:, q * 512:(q + 1) * 512], g4[:, 8 + q:9 + q],
                              ng[:, q * 512:(q + 1) * 512], ALU.mult, ALU.mult, [szk, g4k, "s_ng"], [gbk])
                    for k in range(16):
                        bank = (0 if b == 0 else 2) + k // 8
                        pv = ps[bank][:, :].bitcast(BF16)
                        P.tr(pv[:, (k % 8) * 128:(k % 8 + 1) * 128], gbf[:, k * 128:(k + 1) * 128], identb[:],
                             [gbk, "identb"], [pk(bank)])
                    for q in range(2):
                        bank = (0 if b == 0 else 2) + q
                        P.cp("act", gT[b][:, q * 8:(q + 1) * 8, :],
                             ps[bank][:, :].bitcast(BF16).rearrange("p (k t) -> p k t", t=128), [pk(bank)], [gtk])
                    for half in range(2):
                        bank = 4 + 2 * b + half
                        for k in range(16):
                            P.mm(ps[bank][:, :], gT[b][:, k, :], wo[:, k, half * 512:(half + 1) * 512], k == 0, k == 15,
                                 [gtk, "s_wo"], [pk(bank)])
                        P.tt("dve", tm[b][:, half * 512:(half + 1) * 512], ps[bank][:, :],
                             gates[:, 0, s_, half * 512:(half + 1) * 512], ALU.mult, [pk(bank), ("gates", 0, s_)], [tmk])
                    P.tt("pool", xo[b][:], xo[b][:], tm[b][:], ALU.add, [xok, tmk], [xok])
                    P.dma("sp", X[t * 128:(t + 1) * 128, :], xo[b][:], reads=[xok], writes=[xk(t)], semkey=xok)
                keys = ["s_wo", "s_ng", "s_dsk", "s_junk3"]
                for b in range(2):
                    keys += ["s_sz%d" % b, "s_gbf%d" % b, "s_g4%d" % b]
                    keys += ["s_yf%d" % b, "s_yb2%d" % b, "s_xk3%d" % b, "s_zg%d" % b, "s_xo%d" % b, "s_gT%d" % b, "s_tm%d" % b]
                drain(keys)

    def moe_sparse(i, tiles):
        ntl = len(tiles)
        Hs = P.dscratch("ms_Hs%d" % i, [NSLOT, D], BF16)
        Z = P.dscratch("ms_Z%d" % i, [NSLOT, D])
        wgu_rows = moe_w_gu[i]
        wdn_rows = moe_w_dn[i]
        IOA = bass.IndirectOffsetOnAxis
        with contextlib.ExitStack() as st:
            WW = P.sb(st, "q_WW", [128, ntl, 2])
            POSI = P.sb(st, "q_POSI", [128, ntl, 2], I32)
            OFFGU = P.sb(st, "q_OFFGU", [128, NBLK], I32)
            pst = P.sb(st, "q_pst", [128, NE])
            km = P.sb(st, "q_km", [128, 256])
            P.dma("sp", km[:], k_moe, writes=["q_km"])
            with contextlib.ExitStack() as s1:
                HTOK = P.sb(s1, "q_HTOK", [128, ntl, D], BF16)
                OH = P.sb(s1, "q_OH", [128, ntl, 3, NE])
                wr = P.sb(s1, "q_wr", [128, 8, 36])
                rt = P.sb(s1, "q_rt", [128, 160])
                hT2 = P.sb(s1, "q_hT2", [128, 8, 256], BF16)
                P.dma("sp", wr[:], moe_wr[i].rearrange("(k p) n -> p k n", p=128), writes=["q_wr"])

                LG = P.sb(s1, "q_LG", [128, ntl, 36])

                def router(idx, t, h32, hkey):
                    lg = ps[5]
                    for k in range(8):
                        P.mm(lg[:, 0:36], h32[:, k, :], wr[:, k, :], k == 0, k == 7, [hkey, "q_wr"], [pk(5)])
                    P.cp("dve", LG[:, idx, :], lg[:, 0:36], [pk(5)], [("q_LG", idx)])
                    c0 = (idx % 2) * 128
                    pv = ps[3][:, :].bitcast(BF16)
                    for c in range(8):
                        P.tr(pv[:, c * 128:(c + 1) * 128], hT2[:, c, c0:c0 + 128], identb[:],
                             [("q_hT2", idx % 2), "identb"], [pk(3)])
                    P.cp("act", HTOK[:, idx, :], pv[:, :], [pk(3)], [("q_HTOK", idx)])

                with contextlib.ExitStack() as s2:
                    norm_tiles(s2, tiles, 1, hT2, "q_hT2", 0, hook=router, tag="qn", colfn=lambda idx: (idx % 2) * 128)
                    drain(["qn_xt0", "qn_xt1", "qn_junk", "qn_st0", "qn_st1", "qn_h320", "qn_h321"])
                R = "q_rt"
                rb = P.sb(s1, "q_rb", [128, 8, ntl])
                g4 = P.sb(s1, "q_g4", [128, 2, ntl, 4])
                le = P.sb(s1, "q_le", [128, 2, ntl, NE])
                lgk = [("q_LG", idx) for idx in range(ntl)]
                ohk = [("q_OH", idx) for idx in range(ntl)]
                wwk = [("q_WW", idx) for idx in range(ntl)]
                LGg, LGe = LG[:, :, 0:4], LG[:, :, 4:36]
                gmax, gate, m1, m2, dd, p1, p2 = (rb[:, q, :] for q in range(7))
                bc4 = lambda v: v.unsqueeze(2).broadcast_to([128, ntl, 4])
                bc32 = lambda v: v.unsqueeze(2).broadcast_to([128, ntl, NE])
                P.red("dve", gmax, LGg, ALU.max, lgk, [R])
                P.tt("dve", g4[:, 0], LGg, bc4(gmax), ALU.subtract, lgk + [R], [R])
                P.act(g4[:, 0], g4[:, 0], AF.Exp, [R], [R])
                P.red("dve", gate, g4[:, 0], ALU.add, [R], [R])
                P.recip(gate, gate, [R], [R])
                P.tt("dve", g4[:, 1], LGg, bc4(gmax), ALU.is_ge, lgk + [R], [R])
                P.ts("dve", g4[:, 1], g4[:, 1], -1.0, ALU.add, [R], [R], s2=BIG, op1=ALU.mult)
                P.tt("dve", le[:, 0].rearrange("p t (g j) -> p t g j", g=4), LGe.rearrange("p t (g j) -> p t g j", g=4),
                     g4[:, 1].unsqueeze(3).broadcast_to([128, ntl, 4, 8]), ALU.add, lgk + [R], [R])
                P.red("dve", m1, le[:, 0], ALU.max, [R], [R])
                oh1, oh2, oha = OH[:, :, 0, :], OH[:, :, 1, :], OH[:, :, 2, :]
                P.tt("dve", oh1, le[:, 0], bc32(m1), ALU.is_ge, [R], ohk)
                P.stt("dve", le[:, 1], oh1, -BIG, le[:, 0], ALU.mult, ALU.add, [R] + ohk, [R])
                P.red("dve", m2, le[:, 1], ALU.max, [R], [R])
                P.tt("dve", oh2, le[:, 1], bc32(m2), ALU.is_ge, [R], ohk)
                P.tt("pool", oha, oh1, oh2, ALU.add, ohk, ohk)
                P.tt("dve", dd, m2, m1, ALU.subtract, [R], [R])
                P.act(dd, dd, AF.Exp, [R], [R])
                P.ts("dve", p1, dd, 1.0, ALU.add, [R], [R])
                P.recip(p1, p1, [R], [R])
                P.tt("dve", p2, dd, p1, ALU.mult, [R], [R])
                P.tt("dve", WW[:, :, 0], p1, gate, ALU.mult, [R], wwk)
                P.tt("dve", WW[:, :, 1], p2, gate, ALU.mult, [R], wwk)
                for idx in range(ntl):
                    P.mm(ps[4][:, 0:NE], ones[:], OH[:, idx, 2, :], idx == 0, idx == ntl - 1, ["ones", ("q_OH", idx)], [pk(4)])
                ob = P.sb(s1, "q_ob", [128, 8, NE])
                c3 = P.sb(s1, "q_c3", [128, NBLK, NE])
                be = P.sb(s1, "q_be", [128, NBLK])
                O_ = "q_ob"
                cnt, nb_, pend, tmpa = ob[:, 0, :], ob[:, 1, :], ob[:, 2, :], ob[:, 3, :]
                P.cp("dve", cnt, ps[4][:, 0:NE], [pk(4)], [O_])
                P.tt("dve", c3[:, 0:NTH, :],
                     cnt.unsqueeze(1).broadcast_to([128, NTH, NE]), km[:, 128:128 + NTH].unsqueeze(2).broadcast_to([128, NTH, NE]),
                     ALU.is_gt, [O_, "q_km"], ["q_c3"])
                P.red("dve", nb_, c3[:, 0:NTH, :].rearrange("p m e -> p e m"), ALU.add, ["q_c3"], [O_])
                P.ts("dve", nb_, nb_, float(BLK), ALU.mult, [O_], [O_])
                P.cp("dve", pend, nb_, [O_], [O_])
                src, dst = pend, tmpa
                for sft in (1, 2, 4, 8, 16):
                    P.cp("dve", dst[:, 0:sft], src[:, 0:sft], [O_], [O_])
                    P.tt("dve", dst[:, sft:NE], src[:, sft:NE], src[:, 0:NE - sft], ALU.add, [O_], [O_])
                    src, dst = dst, src
                pend_f = src
                P.tt("dve", pst[:], pend_f, nb_, ALU.subtract, [O_], ["q_pst"])
                P.tt("dve", c3[:], pend_f.unsqueeze(1).broadcast_to([128, NBLK, NE]),
                     km[:, 0:NBLK].unsqueeze(2).broadcast_to([128, NBLK, NE]), ALU.is_le, [O_, "q_km"], ["q_c3"])
                P.red("dve", be[:], c3[:], ALU.add, ["q_c3"], ["q_be"])
                P.ts("dve", be[:], be[:], float(NE - 1), ALU.min, ["q_be"], ["q_be"])
                P.ts("dve", be[:], be[:], 128.0, ALU.mult, ["q_be"], ["q_be"])
                P.tt("dve", be[:], be[:], km[:, 200:201].broadcast_to([128, NBLK]), ALU.add, ["q_be", "q_km"], ["q_be"])
                sk_ = c3[:, 0:4, :].rearrange("p a e -> p (a e)")[:, 0:NBLK - 1]
                P.tt("dve", sk_, be[:, 1:NBLK], be[:, 0:NBLK - 1], ALU.is_equal, ["q_be"], ["q_c3"])
                P.stt("dve", be[:, 1:NBLK], sk_, 1.0e6, be[:, 1:NBLK], ALU.mult, ALU.add, ["q_c3", "q_be"], ["q_be"])
                P.cp("dve", OFFGU[:], be[:], ["q_be"], ["q_OFFGU"])
                ustr = P.sb(s1, "q_ustr", [128, 128])
                trl = P.sb(s1, "q_trl", [128, 128])
                P.dma("sp", trl[:], k_tri[0], writes=["q_trl"])
                P.tt("dve", ustr[:], trl[:], ident[:], ALU.subtract, ["q_trl", "ident"], ["q_ustr"])
                rk = P.sb(s1, "q_rk", [128, 3, ntl, NE])
                posf = P.sb(s1, "q_posf", [128, ntl, 2])
                ohk2 = [("q_OH", idx) for idx in range(ntl)]
                for idx in range(ntl):
                    bank, col = idx // 16, (idx % 16) * NE
                    P.mm(ps[bank][:, col:col + NE], ustr[:], OH[:, idx, 2, :], True, True, ["q_ustr", ("q_OH", idx)], [pk(bank)])
                    P.mm(ps[3 + bank][:, col:col + NE], ones[:], OH[:, idx, 2, :], True, True, ["ones", ("q_OH", idx)], [pk(3 + bank)])
                for bank in range((ntl + 15) // 16):
                    n_ = min(16, ntl - bank * 16)
                    P.cp("act", rk[:, 0, bank * 16:bank * 16 + n_, :], ps[bank][:, 0:n_ * NE].rearrange("p (t e) -> p t e", e=NE),
                         [pk(bank)], ["q_rk0"])
                    P.cp("dve", rk[:, 1, bank * 16:bank * 16 + n_, :], ps[3 + bank][:, 0:n_ * NE].rearrange("p (t e) -> p t e", e=NE),
                         [pk(3 + bank)], ["q_rk1"])
                src, dst, sk1, dk1 = 1, 2, "q_rk1", "q_rk2"
                sft = 1
                while sft < ntl:
                    P.cp("dve", rk[:, dst, 0:sft, :], rk[:, src, 0:sft, :], [sk1], [dk1])
                    P.tt("dve", rk[:, dst, sft:ntl, :], rk[:, src, sft:ntl, :], rk[:, src, 0:ntl - sft, :], ALU.add, [sk1], [dk1])
                    src, dst, sk1, dk1 = dst, src, dk1, sk1
                    sft *= 2
                for bank in range((ntl + 15) // 16):
                    n_ = min(16, ntl - bank * 16)
                    P.tt("dve", rk[:, src, bank * 16:bank * 16 + n_, :], rk[:, src, bank * 16:bank * 16 + n_, :],
                         ps[3 + bank][:, 0:n_ * NE].rearrange("p (t e) -> p t e", e=NE), ALU.subtract, [sk1, pk(3 + bank)], [sk1])
                P.tt("dve", rk[:, 0], rk[:, 0], rk[:, src], ALU.add, ["q_rk0", sk1], ["q_rk0"])
                P.tt("dve", rk[:, 0], rk[:, 0], pst[:].unsqueeze(1).broadcast_to([128, ntl, NE]), ALU.add, ["q_rk0", "q_pst"], ["q_rk0"])
                for k2 in range(2):
                    P.tt("dve", rk[:, dst], rk[:, 0], OH[:, :, k2, :], ALU.mult, ["q_rk0", dk1] + ohk2, [dk1])
                    P.red("dve", posf[:, :, k2], rk[:, dst], ALU.add, [dk1], ["q_posf"])
                P.cp("dve", POSI[:], posf[:], ["q_posf"], ["q_POSI"])
                for idx in range(ntl):
                    for k2 in range(2):
                        S.idma(Hs[:, :], IOA(ap=POSI[:, idx, k2:k2 + 1], axis=0), HTOK[:, idx, :], None, NSLOT - 1,
                               reads=[("q_HTOK", idx), "q_POSI"], writes=["Hs"], semkey="q_scat")
                drain(["q_wr", "q_rt", "q_rb", "q_g4", "q_le"] + [("q_LG", idx) for idx in range(ntl)] + ["q_hT2", ("q_hT2", 0), ("q_hT2", 1), "q_ob", "q_c3", "q_be", "q_ustr", "q_trl",
                       "q_rk0", "q_rk1", "q_rk2", "q_posf"] + [("q_HTOK", idx) for idx in range(ntl)] + [("q_OH", idx) for idx in range(ntl)])
            with contextlib.ExitStack() as s1:
                w32g = P.sb(s1, "q_w32g", [128, 8, 2 * FH])
                w32d = P.sb(s1, "q_w32d", [128, 4, D])
                wgu = [P.sb(s1, "q_wgu%d" % b, [128, 8, 2 * FH], BF16) for b in range(2)]
                wdn = [P.sb(s1, "q_wdn%d" % b, [128, 4, D], BF16) for b in range(2)]
                hs = [P.sb(s1, "q_hs%d" % b, [128, NSB, D], BF16) for b in range(2)]
                hTs = P.sb(s1, "q_hTs", [128, 8, BLK], BF16)
                sg = [P.sb(s1, "q_sg%d" % b, [128, BLK], BF16) for b in range(2)]
                aT = P.sb(s1, "q_aT", [128, 4, BLK], BF16)
                zt = [P.sb(s1, "q_zt%d" % b, [128, D]) for b in range(2)]

                def gatherw(j):
                    b = j % 2
                    S.idma(w32g[:].rearrange("p k n -> p (k n)"), None, wgu_rows, IOA(ap=OFFGU[:, j:j + 1], axis=0), NE * 128 - 1,
                           reads=["q_OFFGU"], writes=[("q_w32g", k) for k in range(8)], semkey="q_w32g")
                    S.idma(w32d[:].rearrange("p k n -> p (k n)"), None, wdn_rows, IOA(ap=OFFGU[:, j:j + 1], axis=0), NE * 128 - 1,
                           reads=["q_OFFGU"], writes=[("q_w32d", k) for k in range(4)], semkey="q_w32d")
                    P.dma("sp", hs[b][:], Hs[j * BLK:(j + 1) * BLK, :].rearrange("(s p) d -> p s d", p=128),
                          reads=["Hs"], writes=["q_hs%d" % b], semkey="q_hs%d" % b)

                def castw(j):
                    b = j % 2
                    for k in range(8):
                        eng_ = "act" if k % 2 == 0 else "dve"
                        P.cp(eng_, wgu[b][:, k, :], w32g[:, k, :], [("q_w32g", k)], [("q_wgu%d" % b, k)])
                    for k in range(4):
                        P.cp("act" if k % 2 == 0 else "dve", wdn[b][:, k, :], w32d[:, k, :], [("q_w32d", k)],
                             [("q_wdn%d" % b, k)])

                def slots_T(j):
                    b = j % 2
                    for s_ in range(NSB):
                        bank = 4 + s_
                        pv = ps[bank][:, :].bitcast(BF16)
                        for c in range(8):
                            P.tr(pv[:, c * 128:(c + 1) * 128], hs[b][:, s_, c * 128:(c + 1) * 128], identb[:],
                                 ["q_hs%d" % b, "identb"], [pk(bank)])
                        P.cp("act" if s_ % 2 == 0 else "dve", hTs[:, :, s_ * 128:(s_ + 1) * 128],
                             pv[:, :].rearrange("p (c t) -> p c t", t=128), [pk(bank)], [("q_hTs", s_)])

                gatherw(0)
                castw(0)
                slots_T(0)
                zc = 0
                for j in range(NBLK):
                    b = j % 2
                    if j + 1 < NBLK:
                        gatherw(j + 1)
                    gkeys = [("q_wgu%d" % b, k) for k in range(8)]
                    dkeys = [("q_wdn%d" % b, k) for k in range(4)]
                    hkeys = [("q_hTs", s_) for s_ in range(NSB)]
                    for jj in range(4):
                        gb, ub = (jj % 2), 2 + (jj % 2)
                        for k in range(8):
                            P.mm(ps[gb][:, 0:BLK], wgu[b][:, k, jj * 128:(jj + 1) * 128], hTs[:, k, :], k == 0, k == 7,
                                 [gkeys[k]] + hkeys, [pk(gb)])
                        for k in range(8):
                            P.mm(ps[ub][:, 0:BLK], wgu[b][:, k, FH + jj * 128:FH + (jj + 1) * 128], hTs[:, k, :], k == 0, k == 7,
                                 [gkeys[k]] + hkeys, [pk(ub)])
                        sk = "q_sg%d" % (jj % 2)
                        P.act(sg[jj % 2][:], ps[gb][:, 0:BLK], AF.Silu, [pk(gb)], [sk])
                        P.tt("dve", aT[:, jj, :], sg[jj % 2][:], ps[ub][:, 0:BLK], ALU.mult, [sk, pk(ub)], [("q_aT", jj)])
                    for tt_ in range(NSB):
                        zb = zc % 2
                        zc += 1
                        zk = "q_zt%d" % zb
                        for half in range(2):
                            db = 4 + half
                            for jj in range(4):
                                P.mm(ps[db][:, :], aT[:, jj, tt_ * 128:(tt_ + 1) * 128], wdn[b][:, jj, half * 512:(half + 1) * 512],
                                     jj == 0, jj == 3, [("q_aT", jj), dkeys[jj]], [pk(db)])
                            P.cp("act" if half == 0 else "dve", zt[zb][:, half * 512:(half + 1) * 512], ps[db][:, :],
                                 [pk(db)], [zk])
                        r0 = j * BLK + tt_ * 128
                        P.dma("sp", Z[r0:r0 + 128, :], zt[zb][:], reads=[zk], writes=["Z"], semkey=zk)
                    if j + 1 < NBLK:
                        slots_T(j + 1)
                        castw(j + 1)
                keys = ["q_hTs", "q_sg0", "q_sg1", "q_zt0", "q_zt1", "q_hs0", "q_hs1"]
                keys += [("q_w32g", k) for k in range(8)] + [("q_w32d", k) for k in range(4)]
                keys += [("q_wgu%d" % b, k) for b in range(2) for k in range(8)]
                keys += [("q_wdn%d" % b, k) for b in range(2) for k in range(4)]
                keys += [("q_aT", jj) for jj in range(4)] + [("q_hTs", s_) for s_ in range(NSB)]
                drain(keys)
            with contextlib.ExitStack() as s1:
                NBUF = 4
                fuse_final = cfg.get("fuse_final", True) and i == DEPTH - 1 and i == layers[-1]
                if fuse_final:
                    gfin = P.sb(s1, "q_gfin", [128, D])
                    fjunk = P.sb(s1, "q_fjunk", [128, D], BF16)
                    fst = [P.sb(s1, "q_fst%d" % b, [128, 4]) for b in range(NBUF)]
                    P.dma("sp", gfin[:], final_g[0:1, :].broadcast_to([128, D]), writes=["q_gfin"])
                    fused_final[0] = True
                z1 = [P.sb(s1, "q_z1%d" % b, [128, D]) for b in range(NBUF)]
                z2 = [P.sb(s1, "q_z2%d" % b, [128, D]) for b in range(NBUF)]
                xo = [P.sb(s1, "q_xo%d" % b, [128, D]) for b in range(NBUF)]

                def cload(idx):
                    b = idx % NBUF
                    t = tiles[idx]
                    S.idma(z1[b][:], None, Z[:, :], IOA(ap=POSI[:, idx, 0:1], axis=0), NSLOT - 1,
                           reads=["Z", "q_POSI"], writes=["q_z1%d" % b], semkey="q_z1%d" % b)
                    S.idma(z2[b][:], None, Z[:, :], IOA(ap=POSI[:, idx, 1:2], axis=0), NSLOT - 1,
                           reads=["Z", "q_POSI"], writes=["q_z2%d" % b], semkey="q_z2%d" % b)
                    P.dma("sp", xo[b][:], X[t * 128:(t + 1) * 128, :], reads=[xk(t)], writes=["q_xo%d" % b], semkey="q_xo%d" % b)

                for idx in range(min(NBUF - 1, ntl)):
                    cload(idx)
                for idx, t in enumerate(tiles):
                    if idx + NBUF - 1 < ntl:
                        cload(idx + NBUF - 1)
                    b = idx % NBUF
                    s_ = 1 if t < 2 else 0
                    k1, k2_, ok = "q_z1%d" % b, "q_z2%d" % b, "q_xo%d" % b
                    P.ts("dve", z1[b][:], z1[b][:], WW[:, idx, 0:1], ALU.mult, [k1, ("q_WW", idx)], [k1])
                    P.stt("dve", z1[b][:], z2[b][:], WW[:, idx, 1:2], z1[b][:], ALU.mult, ALU.add, [k1, k2_, ("q_WW", idx)], [k1])
                    P.tt("pool", z1[b][:], z1[b][:], gates[:, 1, s_, :], ALU.mult, [k1, ("gates", 1, s_)], [k1])
                    P.tt("dve", xo[b][:], xo[b][:], z1[b][:], ALU.add, [ok, k1], [ok])
                    if fuse_final and t >= 2:
                        fk = "q_fst%d" % b
                        P.act(fjunk[:], xo[b][:], AF.Square, [ok], ["q_fjunk", fk], accum_out=fst[b][:, 0:1])
                        P.act(fst[b][:, 1:2], fst[b][:, 0:1], AF.Sqrt, [fk], [fk], bias=EPS, scale=1.0 / D)
                        P.recip(fst[b][:, 2:3], fst[b][:, 1:2], [fk], [fk])
                        P.stt("dve", xo[b][:], xo[b][:], fst[b][:, 2:3], gfin[:], ALU.mult, ALU.mult, [ok, fk, "q_gfin"], [ok])
                        P.dma("sp", out[(t - 2) * 128:(t - 1) * 128, :], xo[b][:], reads=[ok], writes=[("out", t - 2)], semkey=ok)
                    else:
                        P.dma("sp", X[t * 128:(t + 1) * 128, :], xo[b][:], reads=[ok], writes=[xk(t)], semkey=ok)
                drain(["q_gfin", "q_fjunk"] + ["q_fst%d" % b for b in range(4)])
                drain(["q_z1%d" % b for b in range(4)] + ["q_z2%d" % b for b in range(4)] + ["q_xo%d" % b for b in range(4)] + ["q_POSI", "q_OFFGU", "q_pst", "q_km",
                       "Hs", "Z"] + [("q_WW", idx) for idx in range(ntl)])

    def final_norm():
        with contextlib.ExitStack() as st:
            gb = P.sb(st, "f_g", [128, D])
            xt = [P.sb(st, "f_x%d" % j, [128, D]) for j in range(2)]
            junk = P.sb(st, "f_junk", [128, D])
            st8 = [P.sb(st, "f_st%d" % j, [128, 4]) for j in range(2)]
            P.dma("sp", gb[:], final_g.partition_broadcast(128) if False else final_g[0:1, :].broadcast_to([128, D]),
                  writes=["f_g"])
            for idx in range(SEQ // 128):
                t = idx + 2
                b = idx % 2
                xkey, skey = "f_x%d" % b, "f_st%d" % b
                P.dma("sp", xt[b][:], X[t * 128:(t + 1) * 128, :], reads=[xk(t)], writes=[xkey], semkey=xkey)
                P.act(junk[:], xt[b][:], AF.Square, [xkey], ["f_junk", skey], accum_out=st8[b][:, 0:1])
                P.act(st8[b][:, 1:2], st8[b][:, 0:1], AF.Sqrt, [skey], [skey], bias=EPS, scale=1.0 / D)
                P.recip(st8[b][:, 2:3], st8[b][:, 1:2], [skey], [skey])
                P.stt("dve", xt[b][:], xt[b][:], st8[b][:, 2:3], gb[:], ALU.mult, ALU.mult, [xkey, skey, "f_g"], [xkey])
                P.dma("sp", out[idx * 128:(idx + 1) * 128, :], xt[b][:], reads=[xkey], writes=[("out", idx)],
                      semkey=xkey)
            for e_ in ("sp", "act", "dve"):
                S.wait_all(e_, ["f_g", "f_x0", "f_x1", "f_junk", "f_st0", "f_st1"])

    fused_final = [False]
    ALL = list(range(NT))
    LAT = list(range(2, NT))
    for i in layers:
        kind = i % 3
        ctx_out = i < DEPTH - 1
        adaln(i, kind == 1)
        S.recycle()
        if mixers_on:
            if kind == 0:
                attention(i, i // 3, ctx_out)
            elif kind == 1:
                pool_mixer(i)
            else:
                ssd_mixer(i)
            S.recycle()
        if moe_on:
            if cfg.get("dense_moe"):
                moe(i, ALL if ctx_out else LAT)
            else:
                moe_sparse(i, ALL if ctx_out else LAT)
            S.recycle()
    if not fused_final[0]:
        final_norm()
    for e_ in ("sp", "pe", "act", "dve", "pool"):
        S.wait_all(e_, list(S.wr.keys()))
    P.es.close()
    return P


def host_consts():
    ident = np.eye(128, dtype=np.float32)
    n_freq = HD // 4
    inv = (10000.0 ** (-np.arange(n_freq, dtype=np.float32) / n_freq)).astype(np.float32)
    tok = np.arange(SEQ)
    row = (tok // GRID_W).astype(np.float32)
    col = (tok % GRID_W).astype(np.float32)
    ang = np.concatenate([row[:, None] * inv[None, :], col[:, None] * inv[None, :]], axis=-1).astype(np.float32)
    rope = np.zeros((T, 64), np.float32)
    rope[:CTX, :32] = 1.0
    rope[CTX:, :32] = np.cos(ang)
    rope[CTX:, 32:] = np.sin(ang)
    band = np.zeros((4, 5, 128, 128), np.float32)
    n = 128 * 4
    for wi, w in enumerate(POOL_WINDOWS):
        M = np.zeros((n, n), np.float64)
        for t in range(n):
            lo = max(t - w // 2, 0)
            hi = min(t + w // 2, n)
            M[lo:hi, t] = 1.0 / (hi - lo)
            M[t, t] -= 1.0
        band[wi, 0] = M[0:128, 128:256]
        band[wi, 1] = M[0:128, 0:128]
        band[wi, 2] = M[128:256, 128:256]
        band[wi, 3] = M[384:512, 384:512]
        band[wi, 4] = M[256:384, 128:256]
    tri = np.zeros((4, 128, 128), np.float32)
    s = np.arange(128)[:, None]
    l = np.arange(128)[None, :]
    tri[0] = (s <= l)
    tri[1] = np.where(s <= l, 0.0, -1.0e4)
    tri[2] = (s >= l)
    tri[3] = np.where(s >= l, 0.0, -1.0e4)
    kmoe = np.zeros((128, 256), np.float32)
    kmoe[:, 0:NBLK] = (np.arange(NBLK) * BLK)[None, :]
    kmoe[:, 128:128 + NTH] = (np.arange(NTH) * BLK)[None, :]
    kmoe[:, 200:208] = np.arange(8)[None, :] * 128 + np.arange(128)[:, None]
    return {"k_ident": ident, "k_rope": rope, "k_band": band, "k_tri": tri, "k_moe": kmoe}


_CACHE = {}


def kernel(**inputs):
    cfg = inputs.pop("_cfg", {})
    key = repr(sorted(cfg.items()))
    if key not in _CACHE:
        _CACHE[key] = build(cfg)
    P = _CACHE[key]
    f = lambda a: np.ascontiguousarray(np.asarray(a, dtype=np.float32))
    consts = host_consts()
    shared = {}
    for name in ("norm_mix_g", "norm_ffn_g",
                 "attn_q_norm_g", "attn_k_norm_g", "pool_w", "pool_scale", "ssm_w_in", "ssm_conv_w",
                 "ssm_conv_b", "ssm_norm_g", "ssm_w_out"):
        shared[name] = f(inputs[name])
    wrc = np.concatenate([f(inputs["moe_w_router_group"]), f(inputs["moe_w_router_expert"])], axis=-1)
    for i in range(DEPTH):
        if "w_ada_%d" % i in P.inp:
            shared["w_ada_%d" % i] = f(inputs["w_ada"][i])
            shared["b_ada_%d" % i] = f(inputs["b_ada"][i]).reshape(1, 6 * D)
        if "moe_w_gu_%d" % i in P.inp:
            shared["moe_w_gu_%d" % i] = np.ascontiguousarray(
                f(inputs["moe_w_gate_up"][i]).reshape(NE, 8, 128, 2 * FH).transpose(0, 2, 1, 3)).reshape(NE * 128, 8 * 2 * FH)
            shared["moe_w_dn_%d" % i] = np.ascontiguousarray(
                f(inputs["moe_w_down"][i]).reshape(NE, 4, 128, D).transpose(0, 2, 1, 3)).reshape(NE * 128, 4 * D)
            shared["moe_wr_%d" % i] = np.ascontiguousarray(wrc[i])
    for j in range(2):
        if "attn_w_qkv_%d" % j in P.inp:
            shared["attn_w_qkv_%d" % j] = f(inputs["attn_w_qkv"][j])
            shared["attn_w_o_%d" % j] = f(inputs["attn_w_o"][j])
    shared["final_norm_g"] = f(inputs["final_norm_g"]).reshape(1, D)
    shared["c_ctx"] = f(inputs["c_ctx"]).reshape(1, D)
    for name in ("ssm_dt_bias", "ssm_a_log", "ssm_d"):
        shared[name] = f(inputs[name]).reshape(1, 2 * SSM_H)
    shared.update(consts)
    x = f(inputs["x"])
    ctx = f(inputs["ctx"])
    c = f(inputs["c"])
    in_maps = []
    ncores = cfg.get("cores", 8)
    for core in range(ncores):
        b = core % NB
        m = dict(shared)
        m["x"] = x[b]
        m["ctx"] = ctx[b]
        m["c"] = c[b:b + 1]
        in_maps.append({k: v for k, v in m.items() if k in P.inp})
    if cfg.get("trace"):
        res = run_bass_kernel_spmd(P.nc, in_maps, core_ids=list(range(ncores)), trace=True)
        kernel.exec_ns = res.exec_time_ns
    else:
        res = run_bass_kernel_spmd(P.nc, in_maps, core_ids=list(range(ncores)))
    nb = min(NB, ncores)
    outs = np.stack([np.asarray(res.results[b]["out"], dtype=np.float32) for b in range(nb)], axis=0)
    if cfg.get("dbg"):
        kernel.dbg = [np.asarray(res.results[b]["dbg"]) for b in range(nb)]
    return outs
```
